# Optimizing a Trainium2 kernel written in Bass

```python
import math
import jax, jax.numpy as jnp
from jax import lax
import numpy as np

D_MODEL = 1024
BATCH = 8
SEQ = 2048
DEPTH = 1

MIX_WIDTH = D_MODEL
MLA_HEADS = 8
MLA_NOPE_DIM = 64
MLA_ROPE_DIM = 32
MLA_QK_DIM = MLA_NOPE_DIM + MLA_ROPE_DIM
MLA_V_DIM = 64
MLA_Q_RANK = 256
MLA_KV_RANK = 128
MLA_WIDTH = MLA_HEADS * MLA_V_DIM
CONV_CH = MIX_WIDTH - MLA_WIDTH
CONV_WIDTH = 31
CONV_PAD = (CONV_WIDTH - 1) // 2
ROPE_BASE = 10000.0
MEM_LEN = 256
MEM_HEADS = 4
MEM_HEAD_DIM = D_MODEL // MEM_HEADS
N_EXPERTS = 16
EXPERT_FF = 2048
CAPACITY_FACTOR = 2
Q_BLOCK = 128
NORM_EPS = 1e-5
DEEPNORM_ALPHA = (2.0 * DEPTH) ** 0.25
DEEPNORM_BETA = (8.0 * DEPTH) ** -0.25
SPLIT_Q = MLA_Q_RANK
SPLIT_KV = SPLIT_Q + MLA_KV_RANK
SPLIT_KR = SPLIT_KV + MLA_ROPE_DIM
IN_PROJ_WIDTH = SPLIT_KR + 2 * CONV_CH

kernel_name = "hybrid_mla_conformer_ecmoe_encoder"


def layer_norm(x, g, b):
    xf = x.astype(jnp.float32)
    mu = jnp.mean(xf, axis=-1, keepdims=True)
    var = jnp.mean(jnp.square(xf - mu), axis=-1, keepdims=True)
    y = (xf - mu) * lax.rsqrt(var + NORM_EPS) * g.astype(jnp.float32) + b.astype(jnp.float32)
    return y.astype(x.dtype)


def rms_norm(x, g):
    xf = x.astype(jnp.float32)
    y = xf * lax.rsqrt(jnp.mean(jnp.square(xf), axis=-1, keepdims=True) + NORM_EPS) * g.astype(jnp.float32)
    return y.astype(x.dtype)


def rotary(x, cos, sin):
    x1, x2 = jnp.split(x, 2, axis=-1)
    return jnp.concatenate([x1 * cos - x2 * sin, x2 * cos + x1 * sin], axis=-1)


def blocked_attention(q, k, v, scale):
    b, s, h, dq = q.shape
    nb = s // Q_BLOCK
    qb = q.reshape(b, nb, Q_BLOCK, h, dq).transpose(1, 0, 2, 3, 4)

    def one_block(qblk):
        sc = jnp.einsum('bqhd,bkhd->bhqk', qblk, k).astype(jnp.float32) * scale
        p = jax.nn.softmax(sc, axis=-1).astype(v.dtype)
        return jnp.einsum('bhqk,bkhd->bqhd', p, v)

    o = lax.map(one_block, qb)
    return o.transpose(1, 0, 2, 3, 4).reshape(b, s, h * v.shape[-1])


def parallel_mixer(x, positions, w_in, q_norm_g, w_uq, kv_norm_g, w_uk, w_uv,
                   conv_w, conv_b, conv_ln_g, conv_ln_b, w_o):
    b, s, _ = x.shape
    hcat = x @ w_in
    c_q = hcat[..., :SPLIT_Q]
    c_kv = hcat[..., SPLIT_Q:SPLIT_KV]
    k_rope = hcat[..., SPLIT_KV:SPLIT_KR]
    conv_in = hcat[..., SPLIT_KR:]

    q = (rms_norm(c_q, q_norm_g) @ w_uq).reshape(b, s, MLA_HEADS, MLA_QK_DIM)
    q_nope, q_rope = q[..., :MLA_NOPE_DIM], q[..., MLA_NOPE_DIM:]
    ckv = rms_norm(c_kv, kv_norm_g)
    k_nope = (ckv @ w_uk).reshape(b, s, MLA_HEADS, MLA_NOPE_DIM)
    v = (ckv @ w_uv).reshape(b, s, MLA_HEADS, MLA_V_DIM)
    inv_freq = ROPE_BASE ** (-jnp.arange(0, MLA_ROPE_DIM, 2, dtype=jnp.float32) / MLA_ROPE_DIM)
    ang = positions.astype(jnp.float32)[..., None] * inv_freq
    cos = jnp.cos(ang).astype(x.dtype)
    sin = jnp.sin(ang).astype(x.dtype)
    q_rope = rotary(q_rope, cos[:, :, None, :], sin[:, :, None, :])
    k_rope = rotary(k_rope, cos, sin)
    q_full = jnp.concatenate([q_nope, q_rope], axis=-1)
    k_full = jnp.concatenate(
        [k_nope, jnp.broadcast_to(k_rope[:, :, None, :], (b, s, MLA_HEADS, MLA_ROPE_DIM))], axis=-1)
    attn_out = blocked_attention(q_full, k_full, v, 1.0 / math.sqrt(MLA_QK_DIM))

    a, g = conv_in[..., :CONV_CH], conv_in[..., CONV_CH:]
    u = a * jax.nn.sigmoid(g)
    u = lax.conv_general_dilated(
        u, conv_w[:, None, :], window_strides=(1,), padding=[(CONV_PAD, CONV_PAD)],
        dimension_numbers=('NWC', 'WIO', 'NWC'), feature_group_count=CONV_CH) + conv_b
    u = jax.nn.silu(layer_norm(u, conv_ln_g, conv_ln_b))

    return jnp.concatenate([attn_out, u], axis=-1) @ w_o


def memory_cross_attention(x, mem, w_q, w_k, w_v, w_o):
    b, s, _ = x.shape
    m = mem.shape[1]
    q = (x @ w_q).reshape(b, s, MEM_HEADS, MEM_HEAD_DIM)
    k = (mem @ w_k).reshape(b, m, MEM_HEADS, MEM_HEAD_DIM)
    v = (mem @ w_v).reshape(b, m, MEM_HEADS, MEM_HEAD_DIM)
    sc = jnp.einsum('bshd,bmhd->bhsm', q, k).astype(jnp.float32) / math.sqrt(MEM_HEAD_DIM)
    p = jax.nn.softmax(sc, axis=-1).astype(v.dtype)
    o = jnp.einsum('bhsm,bmhd->bshd', p, v).reshape(b, s, D_MODEL)
    return o @ w_o


def expert_choice_moe(x, w_router, w_gate, w_up, w_down):
    b, s, d = x.shape
    cap = CAPACITY_FACTOR * s // N_EXPERTS
    affinity = jax.nn.softmax((x @ w_router).astype(jnp.float32), axis=-1)
    gate, idx = lax.top_k(affinity.transpose(0, 2, 1), cap)
    bidx = jnp.arange(b)[:, None, None]
    xs = x[bidx, idx]
    hid = jax.nn.silu(jnp.einsum('becd,edf->becf', xs, w_gate)) * jnp.einsum('becd,edf->becf', xs, w_up)
    y = jnp.einsum('becf,efd->becd', hid, w_down).astype(jnp.float32) * gate[..., None]
    out = jnp.zeros((b, s, d), jnp.float32).at[bidx, idx].add(y)
    return out.astype(x.dtype)


def setup_inputs(seed: int = 0) -> dict:
    key = jax.random.key(seed)
    ks = jax.random.split(key, 32)
    f32 = jnp.float32

    def nrm(k, shape, fan_in, scale=1.0):
        return jax.random.normal(k, shape, f32) * (scale * fan_in ** -0.5)

    def gain(k, shape):
        return 1.0 + 0.02 * jax.random.normal(k, shape, f32)

    def bias(k, shape):
        return 0.02 * jax.random.normal(k, shape, f32)

    L = DEPTH
    beta = DEEPNORM_BETA
    x = jax.random.normal(ks[0], (BATCH, SEQ, D_MODEL), f32)
    mem = jax.random.normal(ks[1], (BATCH, MEM_LEN, D_MODEL), f32)
    offs = jax.random.randint(ks[2], (BATCH, 1), 0, 4096)
    positions = (jnp.arange(SEQ, dtype=jnp.int32)[None, :] + offs).astype(jnp.int32)
    return {
        "x": x,
        "mem": mem,
        "positions": positions,
        "w_in": nrm(ks[3], (L, D_MODEL, IN_PROJ_WIDTH), D_MODEL),
        "q_norm_g": gain(ks[4], (L, MLA_Q_RANK)),
        "w_uq": nrm(ks[5], (L, MLA_Q_RANK, MLA_HEADS * MLA_QK_DIM), MLA_Q_RANK),
        "kv_norm_g": gain(ks[6], (L, MLA_KV_RANK)),
        "w_uk": nrm(ks[7], (L, MLA_KV_RANK, MLA_HEADS * MLA_NOPE_DIM), MLA_KV_RANK),
        "w_uv": nrm(ks[8], (L, MLA_KV_RANK, MLA_HEADS * MLA_V_DIM), MLA_KV_RANK, beta),
        "conv_w": nrm(ks[9], (L, CONV_WIDTH, CONV_CH), CONV_WIDTH),
        "conv_b": bias(ks[10], (L, CONV_CH)),
        "conv_ln_g": gain(ks[11], (L, CONV_CH)),
        "conv_ln_b": bias(ks[12], (L, CONV_CH)),
        "w_o": nrm(ks[13], (L, MIX_WIDTH, D_MODEL), MIX_WIDTH, beta),
        "ln1_g": gain(ks[14], (L, D_MODEL)),
        "ln1_b": bias(ks[15], (L, D_MODEL)),
        "xa_w_q": nrm(ks[16], (L, D_MODEL, D_MODEL), D_MODEL),
        "xa_w_k": nrm(ks[17], (L, D_MODEL, D_MODEL), D_MODEL),
        "xa_w_v": nrm(ks[18], (L, D_MODEL, D_MODEL), D_MODEL, beta),
        "xa_w_o": nrm(ks[19], (L, D_MODEL, D_MODEL), D_MODEL, beta),
        "ln2_g": gain(ks[20], (L, D_MODEL)),
        "ln2_b": bias(ks[21], (L, D_MODEL)),
        "w_router": nrm(ks[22], (L, D_MODEL, N_EXPERTS), D_MODEL),
        "w_gate": nrm(ks[23], (L, N_EXPERTS, D_MODEL, EXPERT_FF), D_MODEL),
        "w_up": nrm(ks[24], (L, N_EXPERTS, D_MODEL, EXPERT_FF), D_MODEL),
        "w_down": nrm(ks[25], (L, N_EXPERTS, EXPERT_FF, D_MODEL), EXPERT_FF, beta),
        "ln3_g": gain(ks[26], (L, D_MODEL)),
        "ln3_b": bias(ks[27], (L, D_MODEL)),
    }


def reference(x, mem, positions, w_in, q_norm_g, w_uq, kv_norm_g, w_uk, w_uv,
              conv_w, conv_b, conv_ln_g, conv_ln_b, w_o, ln1_g, ln1_b,
              xa_w_q, xa_w_k, xa_w_v, xa_w_o, ln2_g, ln2_b,
              w_router, w_gate, w_up, w_down, ln3_g, ln3_b):
    h = x
    for l in range(DEPTH):
        mix = parallel_mixer(h, positions, w_in[l], q_norm_g[l], w_uq[l], kv_norm_g[l], w_uk[l], w_uv[l],
                             conv_w[l], conv_b[l], conv_ln_g[l], conv_ln_b[l], w_o[l])
        h = layer_norm(DEEPNORM_ALPHA * h + mix, ln1_g[l], ln1_b[l])
        xa = memory_cross_attention(h, mem, xa_w_q[l], xa_w_k[l], xa_w_v[l], xa_w_o[l])
        h = layer_norm(DEEPNORM_ALPHA * h + xa, ln2_g[l], ln2_b[l])
        ff = expert_choice_moe(h, w_router[l], w_gate[l], w_up[l], w_down[l])
        h = layer_norm(DEEPNORM_ALPHA * h + ff, ln3_g[l], ln3_b[l])
    return h
```

```python
import math
import os
from contextlib import ExitStack

import numpy as np
import concourse.bass as bass
import concourse.mybir as mybir
from concourse.bass_utils import run_bass_kernel_spmd

F32 = mybir.dt.float32
BF16 = mybir.dt.bfloat16
I32 = mybir.dt.int32
U32 = mybir.dt.uint32
ALU = mybir.AluOpType
AF = mybir.ActivationFunctionType

S_ = 2048
D = 1024
NT = 16
EPS = 1e-5
ALPHA = 2.0 ** 0.25
NE = 16
CAP = 256
FF = 2048


class Buf:
    __slots__ = ("name", "w", "r", "sem", "cnt")

    def __init__(self, name):
        self.name = name
        self.w = None
        self.r = {}
        self.sem = None
        self.cnt = 0


class Sched:
    ENG = ["pe", "act", "dve", "pool", "sp"]

    def __init__(self, nc, es):
        self.nc = nc
        self.es = es
        self.streams = {e: [] for e in self.ENG}
        self.cnt = {e: 0 for e in self.ENG}
        self.esem = {e: es.enter_context(nc.semaphore("c_" + e)) for e in self.ENG}
        self.waited = {e: {} for e in self.ENG}
        self.nsem = len(self.ENG)

    def new_sem(self, name):
        self.nsem += 1
        return self.es.enter_context(self.nc.semaphore("d%d_%s" % (self.nsem, name)))

    def _waits(self, eng, reads, writes):
        deps = {}

        def add(s, v):
            if deps.get(s, 0) < v:
                deps[s] = v
        for b in reads:
            if b.w is not None:
                add(*b.w)
        for b in writes:
            if b.w is not None:
                add(*b.w)
            for s, v in b.r.items():
                add(s, v)
        own = self.esem[eng]
        wd = self.waited[eng]
        for s, v in deps.items():
            if s == own and eng == "pe":
                continue
            if wd.get(s, 0) < v:
                wd[s] = v
                self.streams[eng].append(("wait", s, v))

    def _commit(self, ev, reads, writes):
        s, v = ev
        for b in reads:
            if b.r.get(s, 0) < v:
                b.r[s] = v
        for b in writes:
            b.w = ev
            b.r = {}

    def op(self, eng, fn, reads=(), writes=()):
        self._waits(eng, reads, writes)
        self.cnt[eng] += 1
        s = self.esem[eng]
        self.streams[eng].append(("op", fn, s, 1))
        self._commit((s, self.cnt[eng]), reads, writes)

    def pe(self, fns, reads=(), writes=()):
        self._waits("pe", reads, writes)
        self.cnt["pe"] += 1
        s = self.esem["pe"]
        for f in fns[:-1]:
            self.streams["pe"].append(("op", f, None, 0))
        self.streams["pe"].append(("op", fns[-1], s, 1))
        self._commit((s, self.cnt["pe"]), reads, writes)

    def dma(self, eng, fn, dst, reads=(), writes=None):
        if writes is None:
            writes = (dst,)
        self._waits(eng, reads, writes)
        if dst.sem is None:
            dst.sem = self.new_sem(dst.name)
        dst.cnt += 16
        self.streams[eng].append(("op", fn, dst.sem, 16))
        self._commit((dst.sem, dst.cnt), reads, writes)

    def wait_bufs(self, eng, bufs):
        self._waits(eng, bufs, bufs)

    def inherit(self, new, olds):
        for o in olds:
            if o.w is not None:
                s, v = o.w
                if new.r.get(s, 0) < v:
                    new.r[s] = v
            for s, v in o.r.items():
                if new.r.get(s, 0) < v:
                    new.r[s] = v

    def emit(self):
        nc = self.nc
        streams = self.streams

        def run(name, eng):
            for item in streams[name]:
                if item[0] == "wait":
                    eng.wait_ge(item[1], item[2])
                else:
                    ins = item[1](eng)
                    if item[2] is not None:
                        ins.then_inc(item[2], item[3])

        with nc.Block() as block:
            @block.tensor
            def _(e):
                run("pe", e)

            @block.scalar
            def _(e):
                run("act", e)

            @block.vector
            def _(e):
                run("dve", e)

            @block.gpsimd
            def _(e):
                run("pool", e)

            @block.sync
            def _(e):
                run("sp", e)


def build_program(stop_after=None, dumps=()):
    nc = bass.Bass("TRN2", target_bir_lowering=False)

    def din(name, shape, dt=F32):
        return nc.dram_tensor(name, list(shape), dt, kind="ExternalInput").ap()

    x = din("x", [S_, D])
    mem = din("mem", [256, D])
    pos = din("positions", [1, S_], I32)
    w_in = din("w_in", [D, 1440])
    q_norm_g = din("q_norm_g", [256])
    w_uq = din("w_uq", [256, 768])
    kv_norm_g = din("kv_norm_g", [128])
    w_uk = din("w_uk", [128, 512])
    w_uv = din("w_uv", [128, 512])
    conv_w = din("conv_w", [31, 512])
    conv_b = din("conv_b", [512])
    conv_ln_g = din("conv_ln_g", [512])
    conv_ln_b = din("conv_ln_b", [512])
    w_o = din("w_o", [D, D])
    ln_g = [din("ln%d_g" % i, [1, D]) for i in (1, 2, 3)]
    ln_b = [din("ln%d_b" % i, [1, D]) for i in (1, 2, 3)]
    xa_w = {k: din("xa_w_" + k, [D, D]) for k in "qkvo"}
    w_router = din("w_router", [D, NE])
    w_gate = din("w_gate", [NE, D, FF])
    w_up = din("w_up", [NE, D, FF])
    w_down = din("w_down", [NE, FF, D])
    c_ident = din("c_ident", [128, 128])
    c_iota = din("c_iota", [128, 128])
    c_tokid = din("c_tokid", [128, NT])
    c_esel = din("c_esel", [16, 16 * 128])
    c_rope = din("c_rope", [128, 2])
    out = nc.dram_tensor("out", [S_, D], F32, kind="ExternalOutput").ap()
    dump_d = {}
    for (nm, shape, dt) in dumps:
        dump_d[nm] = nc.dram_tensor("dbg_" + nm, list(shape), dt, kind="ExternalOutput").ap()

    es = ExitStack()
    with es:
        S = Sched(nc, es)
        cnt = [0]

        def T(shape, dt, off, name=None):
            cnt[0] += 1
            return nc.alloc_sbuf_tensor_at("%s_%d" % (name or "t", cnt[0]), list(shape), dt, offset=off)

        b_dbg = Buf("dbg")

        def dump(nm, ap, bufs):
            if nm in dump_d:
                S.dma("sp", lambda e: e.dma_start(out=dump_d[nm], in_=ap), b_dbg, reads=bufs, writes=[b_dbg])

        R0 = 16640
        R1 = R0 + 66048
        R2 = R1 + 32768
        R3 = R2 + 26624
        R4 = R3 + 24576
        R5 = R4 + 8192
        R6 = R5 + 16384
        R7 = R6 + 8192
        R8 = R7 + 8192
        R9 = R8 + 8192
        ident = T([128, 128], F32, R9); b_ident = Buf("ident")
        identb = T([128, 128], BF16, R9 + 512); b_identb = Buf("identb")
        onesb = T([128, 128], BF16, R9 + 768); b_onesb = Buf("onesb")
        onesf = T([128, 128], F32, R9 + 1024); b_onesf = Buf("onesf")
        ropec = T([128, 2], F32, R9 + 1536); b_ropec = Buf("ropec")
        gsm = T([128, 16], F32, R9 + 1600); b_gsm = Buf("gsm")
        cwT = T([128, 4, 32], F32, R9 + 1664); b_cwT = Buf("cwT")
        iota = T([128, 128], F32, R9 + 2176); b_iota = Buf("iota")
        tokid = T([128, NT], F32, R9 + 2688); b_tokid = Buf("tokid")
        stat = T([128, 16], F32, R9 + 2752); b_stat = Buf("stat")
        cst = T([128, 8], F32, R9 + 2816); b_cst = Buf('cst')
        CEND = R9 + 2848
        assert CEND <= 229376, CEND

        PP = [nc.alloc_psum_tensor("pp%d" % i, [128, 1024], F32) for i in range(4)]
        PB = [Buf("bank%d" % i) for i in range(8)]

        def bank(k):
            return PP[k // 2][:, (k % 2) * 512:(k % 2) * 512 + 512]

        S.dma("sp", lambda e: e.dma_start(out=ident[:], in_=c_ident), b_ident)
        S.dma("sp", lambda e: e.dma_start(out=iota[:], in_=c_iota), b_iota)
        S.dma("sp", lambda e: e.dma_start(out=tokid[:], in_=c_tokid), b_tokid)
        S.dma("sp", lambda e: e.dma_start(out=ropec[:], in_=c_rope), b_ropec)
        S.op("dve", lambda e: e.tensor_copy(out=identb[:], in_=ident[:]), reads=[b_ident], writes=[b_identb])
        S.op("dve", lambda e: e.memset(onesb[:], 1.0), writes=[b_onesb])
        S.op("dve", lambda e: e.memset(onesf[:], 1.0), writes=[b_onesf])
        S.op("dve", lambda e: e.memset(cst[:, 0:1], 256.0 * EPS), writes=[b_cst])
        S.op("dve", lambda e: e.memset(cst[:, 1:2], 128.0 * EPS), writes=[b_cst])
        S.op("dve", lambda e: e.memset(cst[:, 2:3], EPS), writes=[b_cst])
        S.op("dve", lambda e: e.memset(cst[:, 3:4], 0.0), writes=[b_cst])
        with nc.allow_non_contiguous_dma(reason="tiny per-channel parameter vectors"):
            S.dma("sp", lambda e: e.dma_start(out=gsm[:, 0:2], in_=q_norm_g.rearrange("(c p) -> p c", p=128), allow_slow_non_contiguous=True), b_gsm)
            S.dma("sp", lambda e: e.dma_start(out=gsm[:, 2:3], in_=kv_norm_g.rearrange("(c p) -> p c", p=128), allow_slow_non_contiguous=True), b_gsm)
            S.dma("sp", lambda e: e.dma_start(out=gsm[:, 4:8], in_=conv_b.rearrange("(c p) -> p c", p=128), allow_slow_non_contiguous=True), b_gsm)
            S.dma("sp", lambda e: e.dma_start(out=gsm[:, 8:12], in_=conv_ln_g.rearrange("(c p) -> p c", p=128), allow_slow_non_contiguous=True), b_gsm)
            S.dma("sp", lambda e: e.dma_start(out=gsm[:, 12:16], in_=conv_ln_b.rearrange("(c p) -> p c", p=128), allow_slow_non_contiguous=True), b_gsm)
        S.op("dve", lambda e: e.tensor_scalar(out=gsm[:, 0:2], in0=gsm[:, 0:2], scalar1=16.0, scalar2=None, op0=ALU.mult),
             reads=[b_gsm], writes=[b_gsm])
        S.op("dve", lambda e: e.tensor_scalar(out=gsm[:, 2:3], in0=gsm[:, 2:3], scalar1=math.sqrt(128.0), scalar2=None, op0=ALU.mult),
             reads=[b_gsm], writes=[b_gsm])

        resid = T([128, NT, D], F32, R0); b_res = [Buf("res%d" % j) for j in range(NT)]
        u = T([128, 4, 2078], F32, R0); b_u = [Buf("u%d" % c) for c in range(4)]
        acc = T([128, 4, 2048], F32, R0 + 33248); b_acc = [Buf("acc%d" % c) for c in range(4)]
        xstage = [T([128, D], F32, R0 + 33248 + i * 4096) for i in range(2)]; b_xst = [Buf("xst%d" % i) for i in range(2)]
        actT = T([128, 8, S_], BF16, R1); b_actT = [Buf("actT%d" % t) for t in range(4)]
        kT = actT; b_kT = Buf("kT")
        w_in_sb = T([128, 8, 1440], BF16, R2); b_w_in = Buf("w_in")
        wkr = T([128, 8, 2, 96], BF16, R2 + 23040); b_wkr = Buf("wkr")
        attnT = T([128, 4, S_], BF16, R2); b_attnT = Buf("attnT")
        qT = [T([128, 512], BF16, R2 + 16384 + i * 1024) for i in range(2)]; b_qT = [Buf("qT%d" % i) for i in range(2)]
        NPT = 4
        pTt = [T([128, 512], BF16, R2 + 18432 + i * 1024) for i in range(NPT)]; b_pT = [Buf("pT%d" % i) for i in range(NPT)]
        Vaug = T([128, NT, 4, 192], BF16, R3); b_V = Buf("V")
        cqnT = T([128, 2, S_], BF16, R4); b_cqn = Buf("cqn")
        convT = T([128, 4, S_], BF16, R5); b_convT = Buf("convT")
        kr = T([128, S_], BF16, R5); b_kr = Buf("kr")
        ckvnT = T([128, S_], BF16, R5 + 4096); b_ckvn = Buf("ckvn")
        COS = T([128, S_], BF16, R6); SIN = T([128, S_], BF16, R6 + 4096); b_cs = Buf("cossin")
        scr = [T([128, 512], F32, R7 + i * 2048) for i in range(4)]; b_scr = [Buf("scr%d" % i) for i in range(4)]
        wuq = T([128, 2, 768], BF16, R8); b_wuq = Buf("wuq")
        wqsw = T([128, 2, 8, 96], BF16, R8 + 3072); b_wqsw = Buf("wqsw")
        wuk = T([128, 512], BF16, R8 + 6144); b_wuk = Buf("wuk")
        wuv = T([128, 512], BF16, R8 + 7168); b_wuv = Buf("wuv")

        w_in_v = w_in.rearrange("(c p) f -> p c f", p=128)
        for c0 in range(0, 8, 4):
            S.dma("pool", lambda e, c0=c0: e.dma_start(out=w_in_sb[:, c0:c0 + 4, :], in_=w_in_v[:, c0:c0 + 4, :]), b_w_in)
        S.op("pool", lambda e: e.memset(wkr[:], 0.0), writes=[b_wkr])
        S.op("pool", lambda e: e.memset(wqsw[:], 0.0), writes=[b_wqsw])
        with nc.allow_non_contiguous_dma(reason="rope column permutation"):
            S.dma("pool", lambda e: e.dma_start(out=wkr[:, :, 0, 64:96], in_=w_in_v[:, :, 384:416], allow_slow_non_contiguous=True), b_wkr)
            S.dma("pool", lambda e: e.dma_start(out=wkr[:, :, 1, 64:80], in_=w_in_v[:, :, 400:416], allow_slow_non_contiguous=True), b_wkr)
            S.dma("pool", lambda e: e.dma_start(out=wkr[:, :, 1, 80:96], in_=w_in_v[:, :, 384:400], allow_slow_non_contiguous=True), b_wkr)
            w_uq_v = w_uq.rearrange("(c p) (h f) -> p c h f", p=128, f=96)
            for c in range(2):
                S.dma("pool", lambda e, c=c: e.dma_start(out=wqsw[:, c, :, 64:80], in_=w_uq_v[:, c, :, 80:96], allow_slow_non_contiguous=True), b_wqsw)
                S.dma("pool", lambda e, c=c: e.dma_start(out=wqsw[:, c, :, 80:96], in_=w_uq_v[:, c, :, 64:80], allow_slow_non_contiguous=True), b_wqsw)
        S.dma("pool", lambda e: e.dma_start(out=wuq[:], in_=w_uq.rearrange("(c p) f -> p c f", p=128)), b_wuq)
        S.dma("pool", lambda e: e.dma_start(out=wuk[:], in_=w_uk), b_wuk)
        S.dma("pool", lambda e: e.dma_start(out=wuv[:], in_=w_uv), b_wuv)

        cw_st = scr[0]
        S.dma("sp", lambda e: e.dma_start(out=cw_st[0:31, 0:512], in_=conv_w), b_scr[0])
        S.pe([lambda e, cc=cc: e.transpose(bank(0)[:, cc * 32:cc * 32 + 31], cw_st[0:31, cc * 128:(cc + 1) * 128], ident[0:31, 0:31])
              for cc in range(4)], reads=[b_scr[0], b_ident], writes=[PB[0]])
        S.op("dve", lambda e: e.tensor_copy(out=cwT[:, :, 0:31], in_=bank(0)[:, 0:128].rearrange("p (c k) -> p c k", k=32)[:, :, 0:31]),
             reads=[PB[0]], writes=[b_cwT])

        posi = T([128, S_], I32, R0)
        posf = T([128, S_], F32, R0 + 8192)
        angb = T([128, S_], F32, R0 + 16384)
        b_pos = Buf("pos")
        S.dma("sp", lambda e: e.dma_start(out=posi[:], in_=pos.broadcast_to([128, S_])), b_pos)
        rp = slice(64, 96)
        S.op("dve", lambda e: e.tensor_copy(out=posf[rp, :], in_=posi[rp, :]), reads=[b_pos], writes=[b_pos])
        kint = T([128, S_], I32, R0 + 24576)
        sinf = T([128, S_], F32, R0 + 24576)

        def trig(phase, dst_fn):
            S.op("dve", lambda e: e.tensor_scalar(out=angb[rp, :], in0=posf[rp, :], scalar1=ropec[rp, 0:1], scalar2=phase,
                                                  op0=ALU.mult, op1=ALU.add), reads=[b_pos, b_ropec], writes=[b_pos])
            S.op("dve", lambda e: e.tensor_copy(out=kint[rp, :], in_=angb[rp, :]), reads=[b_pos], writes=[b_pos])
            S.op("dve", lambda e: e.tensor_copy(out=posi[rp, :].bitcast(F32), in_=kint[rp, :]), reads=[b_pos], writes=[b_pos])
            S.op("dve", lambda e: e.tensor_tensor(out=angb[rp, :], in0=angb[rp, :], in1=posi[rp, :].bitcast(F32), op=ALU.subtract),
                 reads=[b_pos], writes=[b_pos])
            S.op("dve", lambda e: e.scalar_tensor_tensor(out=angb[rp, :], in0=angb[rp, :], scalar=0.5, in1=angb[rp, :],
                                                         op0=ALU.is_gt, op1=ALU.subtract), reads=[b_pos], writes=[b_pos])
            dst_fn()

        def fin_sin():
            S.op("act", lambda e: e.activation(out=sinf[rp, :], in_=angb[rp, :], func=AF.Sin, scale=-2.0 * math.pi), reads=[b_pos], writes=[b_pos])
            S.op("dve", lambda e: e.tensor_scalar(out=SIN[rp, :], in0=sinf[rp, :], scalar1=ropec[rp, 1:2], scalar2=None, op0=ALU.mult),
                 reads=[b_pos, b_ropec], writes=[b_cs])

        def fin_cos():
            S.op("act", lambda e: e.activation(out=COS[rp, :], in_=angb[rp, :], func=AF.Sin, scale=-2.0 * math.pi), reads=[b_pos], writes=[b_cs])
        trig(0.0, fin_sin)
        trig(0.25, fin_cos)
        for c in range(4):
            S.inherit(b_u[c], [b_pos])

        xv = x.rearrange("(j p) d -> j p d", p=128)
        for j in range(NT):
            st = j % 2
            S.dma("sp", lambda e, j=j, st=st: e.dma_start(out=xstage[st][:], in_=xv[j]), b_xst[st])
            pb = [PB[(j % 2) * 2], PB[(j % 2) * 2 + 1]]
            pt = PP[j % 2]
            S.pe([lambda e, c=c, st=st, pt=pt: e.transpose(pt[:, c * 128:(c + 1) * 128], xstage[st][:, c * 128:(c + 1) * 128], ident[:])
                  for c in range(8)], reads=[b_xst[st], b_ident], writes=pb)
            eng = "act" if j % 2 == 0 else "dve"
            if eng == "act":
                S.op("act", lambda e, j=j, pt=pt: e.activation(out=actT[:, :, j * 128:(j + 1) * 128],
                                                             in_=pt[:].rearrange("p (c t) -> p c t", t=128), func=AF.Copy),
                     reads=pb, writes=[b_actT[j // 4]])
            else:
                S.op("dve", lambda e, j=j, pt=pt: e.tensor_copy(out=actT[:, :, j * 128:(j + 1) * 128],
                                                              in_=pt[:].rearrange("p (c t) -> p c t", t=128)),
                     reads=pb, writes=[b_actT[j // 4]])
        dump("xT", actT[:, :, :], b_actT)

        S.op("pool", lambda e: e.memset(u[:, :, 0:15], 0.0), writes=b_u)
        S.op("pool", lambda e: e.memset(u[:, :, 2063:2078], 0.0), writes=b_u)
        for c in range(4):
            S.inherit(b_acc[c], b_xst)

        def inproj_group(bk, lhs_fn, tb, M):
            cols = slice(tb * 512, (tb + 1) * 512)
            S.pe([lambda e, c=c: e.matmul(bank(bk)[0:M, :], lhsT=lhs_fn(c), rhs=actT[:, c, cols], start=(c == 0), stop=(c == 7))
                  for c in range(8)], reads=[b_w_in, b_wkr, b_actT[tb]], writes=[PB[bk]])

        def inproj_tb(tb):
            cols = slice(tb * 512, (tb + 1) * 512)
            for c2 in range(2):
                inproj_group(c2, lambda c, c2=c2: w_in_sb[:, c, c2 * 128:(c2 + 1) * 128], tb, 128)
                S.op("act", lambda e, c2=c2: e.activation(out=scr[c2][:].bitcast(BF16)[:, 0:512], in_=bank(c2), func=AF.Square),
                     reads=[PB[c2]], writes=[b_scr[c2]])
            S.pe([lambda e, c2=c2: e.matmul(bank(2), lhsT=onesb[:], rhs=scr[c2][:].bitcast(BF16)[:, 0:512], start=(c2 == 0), stop=(c2 == 1))
                  for c2 in range(2)], reads=[b_onesb, b_scr[0], b_scr[1]], writes=[PB[2]])
            S.op("act", lambda e: e.activation(out=scr[2][:], in_=bank(2), func=AF.Sqrt, bias=cst[:, 0:1], scale=1.0),
                 reads=[PB[2], b_cst], writes=[b_scr[2]])
            S.op("dve", lambda e: e.reciprocal(out=scr[2][:], in_=scr[2][:]), reads=[b_scr[2]], writes=[b_scr[2]])
            for c2 in range(2):
                S.op("dve", lambda e, c2=c2: e.scalar_tensor_tensor(out=cqnT[:, c2, cols], in0=bank(c2), scalar=gsm[:, c2:c2 + 1], in1=scr[2][:],
                                                                   op0=ALU.mult, op1=ALU.mult),
                     reads=[PB[c2], b_gsm, b_scr[2]], writes=[b_cqn])
            inproj_group(3, lambda c: w_in_sb[:, c, 256:384], tb, 128)
            S.op("act", lambda e: e.activation(out=scr[3][:].bitcast(BF16)[:, 0:512], in_=bank(3), func=AF.Square),
                 reads=[PB[3]], writes=[b_scr[3]])
            S.pe([lambda e: e.matmul(bank(2), lhsT=onesb[:], rhs=scr[3][:].bitcast(BF16)[:, 0:512], start=True, stop=True)],
                 reads=[b_onesb, b_scr[3]], writes=[PB[2]])
            S.op("act", lambda e: e.activation(out=scr[2][:], in_=bank(2), func=AF.Sqrt, bias=cst[:, 1:2], scale=1.0),
                 reads=[PB[2], b_cst], writes=[b_scr[2]])
            S.op("dve", lambda e: e.reciprocal(out=scr[2][:], in_=scr[2][:]), reads=[b_scr[2]], writes=[b_scr[2]])
            S.op("dve", lambda e: e.scalar_tensor_tensor(out=ckvnT[:, cols], in0=bank(3), scalar=gsm[:, 2:3], in1=scr[2][:],
                                                         op0=ALU.mult, op1=ALU.mult),
                 reads=[PB[3], b_gsm, b_scr[2]], writes=[b_ckvn])
            inproj_group(4, lambda c: wkr[:, c, 0, :], tb, 96)
            inproj_group(5, lambda c: wkr[:, c, 1, :], tb, 96)
            S.op("dve", lambda e: e.tensor_tensor(out=scr[0][rp, :], in0=bank(4)[rp, :], in1=COS[rp, cols], op=ALU.mult),
                 reads=[PB[4], b_cs], writes=[b_scr[0]])
            S.op("dve", lambda e: e.tensor_tensor(out=scr[1][rp, :], in0=bank(5)[rp, :], in1=SIN[rp, cols], op=ALU.mult),
                 reads=[PB[5], b_cs], writes=[b_scr[1]])
            S.op("dve", lambda e: e.tensor_tensor(out=kr[rp, cols], in0=scr[0][rp, :], in1=scr[1][rp, :], op=ALU.add),
                 reads=[b_scr[0], b_scr[1]], writes=[b_kr])
            for cc in range(4):
                inproj_group(6, lambda c, cc=cc: w_in_sb[:, c, 416 + cc * 128:416 + (cc + 1) * 128], tb, 128)
                inproj_group(7, lambda c, cc=cc: w_in_sb[:, c, 928 + cc * 128:928 + (cc + 1) * 128], tb, 128)
                S.op("act", lambda e: e.activation(out=scr[3][:], in_=bank(7), func=AF.Sigmoid), reads=[PB[7]], writes=[b_scr[3]])
                S.op("dve", lambda e, cc=cc: e.tensor_tensor(out=u[:, cc, 15 + tb * 512:15 + (tb + 1) * 512], in0=bank(6), in1=scr[3][:], op=ALU.mult),
                     reads=[PB[6], b_scr[3]], writes=[b_u[cc]])
        for tb in range(4):
            inproj_tb(tb)
        dump("cqnT", cqnT[:, :, :], [b_cqn])
        dump("ckvnT", ckvnT[:, :], [b_ckvn])
        dump("kr", kr[64:96, :], [b_kr])
        dump("u", u[:, :, 15:2063], b_u)
        if stop_after == "inproj":
            S.wait_bufs("sp", [b_dbg])
            S.emit()
            return nc


        def mm(o, l, r, st=True, sp=True):
            return lambda e: e.matmul(o, lhsT=l, rhs=r, start=st, stop=sp)

        evc = [0]

        def evac(out_ap, in_ap, reads, writes):
            evc[0] += 1
            if evc[0] % 2 == 0:
                S.op("act", lambda e: e.activation(out=out_ap, in_=in_ap, func=AF.Copy), reads=reads, writes=writes)
            else:
                S.op("dve", lambda e: e.tensor_copy(out=out_ap, in_=in_ap), reads=reads, writes=writes)

        S.inherit(b_kT, b_actT)

        def kup(h, tb):
            cols = slice(tb * 512, (tb + 1) * 512)
            bk = (h * 4 + tb) % 4
            S.pe([mm(bank(bk)[0:64, :], wuk[:, h * 64:(h + 1) * 64], ckvnT[:, cols])], reads=[b_wuk, b_ckvn], writes=[PB[bk]])
            evac(kT[0:64, h, cols], bank(bk)[0:64, :], [PB[bk]], [b_kT])
        for h in range(8):
            for tb in range(4):
                kup(h, tb)

        def krb(tb):
            cols = slice(tb * 512, (tb + 1) * 512)
            S.op("pool", lambda e: e.tensor_copy(out=kT[64:96, :, cols], in_=kr[64:96, cols].unsqueeze(1).broadcast_to([32, 8, 512])),
                 reads=[b_kr], writes=[b_kT])
        for tb in range(4):
            krb(tb)
        S.op("pool", lambda e: e.memset(Vaug[:, :, :, 64:128], 0.0), writes=[b_V])
        S.op("pool", lambda e: e.memset(Vaug[:, :, :, 64:65], 1.0), writes=[b_V])

        def vup(j):
            bk = 4 + (j % 4)
            S.pe([mm(bank(bk), ckvnT[:, j * 128:(j + 1) * 128], wuv[:])], reads=[b_wuv, b_ckvn], writes=[PB[bk]])
            pv = bank(bk).rearrange("p (q t d) -> p q t d", t=2, d=64)
            S.op("act", lambda e: e.activation(out=Vaug[:, j, :, 0:64], in_=pv[:, :, 0, :], func=AF.Copy), reads=[PB[bk]], writes=[b_V])
            S.op("dve", lambda e: e.tensor_copy(out=Vaug[:, j, :, 128:192], in_=pv[:, :, 1, :]), reads=[PB[bk]], writes=[b_V])
        for j in range(NT):
            vup(j)
        dump("kT", kT[0:96, :, :], [b_kT])
        dump("V", Vaug[:, :, :, :], [b_V])

        def conv_chunk(cc, eng):
            S.op(eng, lambda e: e.tensor_scalar(out=acc[:, cc, :], in0=u[:, cc, 0:2048], scalar1=cwT[:, cc, 0:1], scalar2=gsm[:, 4 + cc:5 + cc],
                                                op0=ALU.mult, op1=ALU.add), reads=[b_u[cc], b_cwT, b_gsm], writes=[b_acc[cc]])
            for k in range(1, 31):
                S.op(eng, lambda e, k=k: e.scalar_tensor_tensor(out=acc[:, cc, :], in0=u[:, cc, k:k + 2048], scalar=cwT[:, cc, k:k + 1],
                                                               in1=acc[:, cc, :], op0=ALU.mult, op1=ALU.add),
                     reads=[b_u[cc], b_acc[cc], b_cwT], writes=[b_acc[cc]])
        for cc in range(4):
            conv_chunk(cc, "dve")
        dump("acc", acc[:, :, :], b_acc)

        S.inherit(b_attnT, [b_w_in, b_wkr])
        for i in range(2):
            S.inherit(b_qT[i], [b_w_in, b_wkr])
        for i in range(NPT):
            S.inherit(b_pT[i], [b_w_in, b_wkr])
        SCALE = 1.0 / math.sqrt(96.0)

        def qup(s):
            qb, h = divmod(s, 8)
            cols = slice(qb * 512, (qb + 1) * 512)
            qt = qT[s % 2]
            bq = b_qT[s % 2]
            S.pe([mm(bank(5)[0:96, :], wuq[:, c, h * 96:(h + 1) * 96], cqnT[:, c, cols], c == 0, c == 1) for c in range(2)],
                 reads=[b_wuq, b_cqn], writes=[PB[5]])
            S.pe([mm(bank(6)[0:96, :], wqsw[:, c, h, :], cqnT[:, c, cols], c == 0, c == 1) for c in range(2)],
                 reads=[b_wqsw, b_cqn], writes=[PB[6]])
            S.op("dve", lambda e: e.tensor_copy(out=qt[0:64, :], in_=bank(5)[0:64, :]), reads=[PB[5]], writes=[bq])
            S.op("dve", lambda e: e.tensor_tensor(out=scr[0][rp, :], in0=bank(5)[rp, :], in1=COS[rp, cols], op=ALU.mult),
                 reads=[PB[5], b_cs], writes=[b_scr[0]])
            S.op("dve", lambda e: e.tensor_tensor(out=scr[1][rp, :], in0=bank(6)[rp, :], in1=SIN[rp, cols], op=ALU.mult),
                 reads=[PB[6], b_cs], writes=[b_scr[1]])
            S.op("dve", lambda e: e.tensor_tensor(out=qt[rp, :], in0=scr[0][rp, :], in1=scr[1][rp, :], op=ALU.add),
                 reads=[b_scr[0], b_scr[1]], writes=[bq])

        pctr = [0]

        def attn_step(s):
            qb, h = divmod(s, 8)
            pair, odd = divmod(h, 2)
            cols = slice(qb * 512, (qb + 1) * 512)
            qt = qT[s % 2]
            bq = b_qT[s % 2]
            ob = 3 + (s % 2)
            M = 128 if odd else 65

            def lhsV(kt):
                return Vaug[:, kt, pair, 64:192] if odd else Vaug[:, kt, pair, 0:65]

            def Smm(kt):
                bk = kt % 3
                S.pe([mm(bank(bk), kT[0:96, h, kt * 128:(kt + 1) * 128], qt[0:96, :])], reads=[b_kT, bq], writes=[PB[bk]])

            def EXPV(kt):
                pi = pctr[0] % NPT
                pctr[0] += 1
                p = pTt[pi]
                S.op("act", lambda e: e.activation(out=p[:], in_=bank(kt % 3), func=AF.Exp, scale=SCALE), reads=[PB[kt % 3]], writes=[b_pT[pi]])
                if kt + 2 < 16:
                    Smm(kt + 2)
                S.pe([mm(bank(ob)[0:M, :], lhsV(kt), p[:], kt == 0, kt == 15)], reads=[b_V, b_pT[pi]], writes=[PB[ob]])
            Smm(0)
            Smm(1)
            for kt in range(16):
                EXPV(kt)
            if odd:
                dr, orow = slice(0, 1), slice(64, 128)
                lo = onesf[0:1, 0:128]
                bo = bank(7)
            else:
                dr, orow = slice(64, 65), slice(0, 64)
                lo = onesf[64:65, 0:64]
                bo = bank(7)[0:64, :]
            S.op("dve", lambda e: e.reciprocal(out=scr[2][dr, :], in_=bank(ob)[dr, :]), reads=[PB[ob]], writes=[b_scr[2]])
            S.pe([mm(bo, lo, scr[2][dr, :])], reads=[b_onesf, b_scr[2]], writes=[PB[7]])
            S.op("dve", lambda e: e.tensor_copy(out=scr[3][orow, :], in_=bank(ob)[orow, :]), reads=[PB[ob]], writes=[b_scr[3]])
            S.op("dve", lambda e: e.tensor_tensor(out=attnT[orow, pair, cols], in0=scr[3][orow, :], in1=bank(7)[orow, :], op=ALU.mult),
                 reads=[b_scr[3], PB[7]], writes=[b_attnT])

        NSTEP = 32
        qup(0)
        for s in range(NSTEP):
            if s + 1 < NSTEP:
                qup(s + 1)
            attn_step(s)
        dump("attnT", attnT[:, :, :], [b_attnT])

        S.inherit(b_convT, [b_kr, b_ckvn])

        def conv_ln(tb):
            cols = slice(tb * 512, (tb + 1) * 512)
            S.pe([mm(bank(0), onesf[:], acc[:, cc, cols], cc == 0, cc == 3) for cc in range(4)], reads=[b_onesf] + b_acc, writes=[PB[0]])
            for cc in range(4):
                S.op("act", lambda e, cc=cc: e.activation(out=scr[3][:], in_=acc[:, cc, cols], func=AF.Square), reads=[b_acc[cc]], writes=[b_scr[3]])
                S.pe([mm(bank(1), onesf[:], scr[3][:], cc == 0, cc == 3)], reads=[b_onesf, b_scr[3]], writes=[PB[1]])
            S.op("dve", lambda e: e.tensor_scalar(out=scr[0][:], in0=bank(0), scalar1=1.0 / 512.0, scalar2=None, op0=ALU.mult),
                 reads=[PB[0]], writes=[b_scr[0]])
            S.op("dve", lambda e: e.tensor_tensor(out=scr[1][:], in0=scr[0][:], in1=scr[0][:], op=ALU.mult), reads=[b_scr[0]], writes=[b_scr[1]])
            S.op("dve", lambda e: e.scalar_tensor_tensor(out=scr[1][:], in0=bank(1), scalar=1.0 / 512.0, in1=scr[1][:], op0=ALU.mult, op1=ALU.subtract),
                 reads=[PB[1], b_scr[1]], writes=[b_scr[1]])
            S.op("act", lambda e: e.activation(out=scr[1][:], in_=scr[1][:], func=AF.Sqrt, bias=cst[:, 2:3], scale=1.0), reads=[b_scr[1], b_cst], writes=[b_scr[1]])
            S.op("dve", lambda e: e.reciprocal(out=scr[1][:], in_=scr[1][:]), reads=[b_scr[1]], writes=[b_scr[1]])
            for cc in range(4):
                S.op("dve", lambda e, cc=cc: e.tensor_tensor(out=scr[2][:], in0=acc[:, cc, cols], in1=scr[0][:], op=ALU.subtract),
                     reads=[b_acc[cc], b_scr[0]], writes=[b_scr[2]])
                S.op("dve", lambda e: e.tensor_tensor(out=scr[2][:], in0=scr[2][:], in1=scr[1][:], op=ALU.mult), reads=[b_scr[2], b_scr[1]], writes=[b_scr[2]])
                S.op("act", lambda e, cc=cc: e.activation(out=convT[:, cc, cols], in_=scr[2][:], func=AF.Silu, scale=gsm[:, 8 + cc:9 + cc], bias=gsm[:, 12 + cc:13 + cc]),
                     reads=[b_scr[2], b_gsm], writes=[b_convT])
        for tb in range(4):
            conv_ln(tb)
        dump("convT", convT[:, :, :], [b_convT])

        lnp = T([128, 2, D], F32, R6); b_lnp = Buf("lnp")
        S.inherit(b_lnp, [b_cs])
        ysb = T([128, D], F32, R7); junk = T([128, D], F32, R7 + 4096)
        b_ysb = Buf("ysb"); b_junk = Buf("junk")
        S.inherit(b_ysb, b_scr); S.inherit(b_junk, b_scr)
        b_st = [Buf("st0"), Buf("st1")]

        def load_lnp(i):
            S.dma("sp", lambda e: e.dma_start(out=lnp[:, 0, :], in_=ln_g[i].broadcast_to([128, D])), b_lnp)
            S.dma("sp", lambda e: e.dma_start(out=lnp[:, 1, :], in_=ln_b[i].broadcast_to([128, D])), b_lnp)

        def ln_tile(j, xin_ap, xin_bufs, pk, tpk, extra=None):
            so = (j % 2) * 8
            bs = b_st[j % 2]
            st_ = lambda a: stat[:, so + a:so + a + 1]
            pbs = [PB[2 * pk], PB[2 * pk + 1]]
            S.op("dve", lambda e: e.scalar_tensor_tensor(out=ysb[:], in0=xin_ap, scalar=ALPHA, in1=PP[pk][:], op0=ALU.mult, op1=ALU.add),
                 reads=pbs + xin_bufs, writes=[b_ysb])
            S.op("dve", lambda e: e.memset(stat[:, so:so + 2], 0.0), writes=[bs])
            S.op("act", lambda e: e.activation(out=junk[:], in_=ysb[:], func=AF.Copy, accum_out=st_(0)), reads=[b_ysb], writes=[b_junk, bs])
            S.op("act", lambda e: e.activation(out=junk[:], in_=ysb[:], func=AF.Square, accum_out=st_(1)), reads=[b_ysb], writes=[b_junk, bs])
            S.op("dve", lambda e: e.tensor_scalar(out=st_(2), in0=st_(0), scalar1=1.0 / D, scalar2=None, op0=ALU.mult), reads=[bs], writes=[bs])
            S.op("dve", lambda e: e.tensor_tensor(out=st_(3), in0=st_(2), in1=st_(2), op=ALU.mult), reads=[bs], writes=[bs])
            S.op("dve", lambda e: e.scalar_tensor_tensor(out=st_(3), in0=st_(1), scalar=1.0 / D, in1=st_(3), op0=ALU.mult, op1=ALU.subtract),
                 reads=[bs], writes=[bs])
            S.op("act", lambda e: e.activation(out=st_(4), in_=st_(3), func=AF.Sqrt, bias=cst[:, 2:3], scale=1.0), reads=[bs, b_cst], writes=[bs])
            S.op("dve", lambda e: e.reciprocal(out=st_(4), in_=st_(4)), reads=[bs], writes=[bs])
            S.op("dve", lambda e: e.scalar_tensor_tensor(out=st_(5), in0=st_(2), scalar=-1.0, in1=st_(4), op0=ALU.mult, op1=ALU.mult), reads=[bs], writes=[bs])
            S.op("act", lambda e: e.activation(out=ysb[:], in_=ysb[:], func=AF.Identity, scale=st_(4), bias=st_(5)), reads=[b_ysb, bs], writes=[b_ysb])
            S.op("pool", lambda e: e.tensor_tensor(out=ysb[:], in0=ysb[:], in1=lnp[:, 0, :], op=ALU.mult), reads=[b_ysb, b_lnp], writes=[b_ysb])
            S.op("pool", lambda e: e.tensor_tensor(out=resid[:, j, :], in0=ysb[:], in1=lnp[:, 1, :], op=ALU.add), reads=[b_ysb, b_lnp], writes=[b_res[j]])
            if extra is not None:
                extra(j)
            tpb = [PB[2 * tpk], PB[2 * tpk + 1]]
            S.pe([lambda e, c=c: e.transpose(PP[tpk][:, c * 128:(c + 1) * 128], resid[:, j, c * 128:(c + 1) * 128], ident[:]) for c in range(8)],
                 reads=[b_res[j], b_ident], writes=tpb)
            evac(actT[:, :, j * 128:(j + 1) * 128], PP[tpk][:].rearrange("p (c t) -> p c t", t=128), tpb, [b_actT[j // 4]])

        wo_sb = T([128, 8, D], BF16, R3); b_wo = Buf("wo")
        S.inherit(b_wo, [b_V])
        w_o_v = w_o.rearrange("(c p) f -> p c f", p=128)
        for c0 in range(0, 8, 4):
            S.dma("pool", lambda e, c0=c0: e.dma_start(out=wo_sb[:, c0:c0 + 4, :], in_=w_o_v[:, c0:c0 + 4, :]), b_wo)
        load_lnp(0)
        xst2 = [T([128, D], F32, R4 + i * 4096) for i in range(2)]; b_xst2 = [Buf("xs2_%d" % i) for i in range(2)]
        for i in range(2):
            S.inherit(b_xst2[i], [b_cqn])
        for j in range(NT):
            S.inherit(b_res[j], b_u + b_acc + b_xst + [b_pos])
        for t in range(4):
            S.inherit(b_actT[t], [b_kT])

        def wo_tile(j):
            st = j % 2
            S.dma("sp", lambda e: e.dma_start(out=xst2[st][:], in_=xv[j]), b_xst2[st])
            tsl = slice(j * 128, (j + 1) * 128)
            for half in range(2):
                hs = slice(half * 512, (half + 1) * 512)
                fns = [mm(bank(half), attnT[:, pr, tsl], wo_sb[:, pr, hs], pr == 0, False) for pr in range(4)]
                fns += [mm(bank(half), convT[:, cc, tsl], wo_sb[:, 4 + cc, hs], False, cc == 3) for cc in range(4)]
                S.pe(fns, reads=[b_attnT, b_convT, b_wo], writes=[PB[half]])
            ln_tile(j, xst2[st][:], [b_xst2[st]], 0, 1)
        for j in range(NT):
            wo_tile(j)
        dump("h1", resid[:, :, :], b_res)
        if stop_after == "mixer":
            S.wait_bufs("sp", [b_dbg])
            S.emit()
            return nc

        phaseA = [b_attnT, b_convT, b_wo, b_cqn, b_kr, b_ckvn, b_w_in, b_wkr, b_V, b_wuq, b_wqsw, b_wuk, b_wuv] + b_qT + b_pT + b_xst2
        B0 = R2
        xw = {}
        bxw = {}
        for i, k in enumerate("qokv"):
            xw[k] = T([128, 8, D], BF16, B0 + i * 16384)
            bxw[k] = Buf("xw" + k)
            S.inherit(bxw[k], phaseA)
        xqT = [T([128, 2, 512], BF16, B0 + 65536 + i * 2048) for i in range(2)]; b_xq = [Buf("xq%d" % i) for i in range(2)]
        xvv = T([128, 2, D], BF16, B0 + 69632); b_xv = Buf("xv")
        pT2 = [T([128, 512], BF16, B0 + 73728 + i * 1024) for i in range(2)]; b_p2 = [Buf("p2_%d" % i) for i in range(2)]
        oT = T([128, 8, 512], BF16, R8); b_oT = Buf("oT")
        TAIL = CEND + 32
        memT = T([128, 8, 256], BF16, TAIL); b_memT = Buf("memT")
        xkT = T([128, 8, 256], BF16, TAIL + 4096); b_xk = Buf("xkT")
        assert TAIL + 8192 <= 229376
        for bb in b_xq + [b_xv] + b_p2 + [b_oT]:
            S.inherit(bb, phaseA)
        for k in "kvqo":
            wv_ = xa_w[k].rearrange("(c p) f -> p c f", p=128)
            for c0 in range(0, 8, 4):
                S.dma("pool", lambda e, k=k, c0=c0, wv_=wv_: e.dma_start(out=xw[k][:, c0:c0 + 4, :], in_=wv_[:, c0:c0 + 4, :]), bxw[k])
        load_lnp(1)
        memv = mem.rearrange("(j p) d -> j p d", p=128)

        def mem_tile(mt):
            S.dma("sp", lambda e: e.dma_start(out=ysb[:], in_=memv[mt]), b_ysb)
            S.pe([lambda e, c=c: e.transpose(PP[1][:, c * 128:(c + 1) * 128], ysb[:, c * 128:(c + 1) * 128], ident[:]) for c in range(8)],
                 reads=[b_ysb, b_ident], writes=[PB[2], PB[3]])
            evac(memT[:, :, mt * 128:(mt + 1) * 128], PP[1][:].rearrange("p (c t) -> p c t", t=128), [PB[2], PB[3]], [b_memT])
        for mt in range(2):
            mem_tile(mt)

        def xk_fc(fc):
            bk = fc % 2
            S.pe([mm(bank(bk)[:, 0:256], xw["k"][:, c, fc * 128:(fc + 1) * 128], memT[:, c, :], c == 0, c == 7) for c in range(8)],
                 reads=[bxw["k"], b_memT], writes=[PB[bk]])
            evac(xkT[:, fc, :], bank(bk)[:, 0:256], [PB[bk]], [b_xk])
        for fc in range(8):
            xk_fc(fc)

        def xv_mh(mt, half):
            bk = 2 + (mt * 2 + half) % 2
            hs = slice(half * 512, (half + 1) * 512)
            S.pe([mm(bank(bk), memT[:, c, mt * 128:(mt + 1) * 128], xw["v"][:, c, hs], c == 0, c == 7) for c in range(8)],
                 reads=[bxw["v"], b_memT], writes=[PB[bk]])
            evac(xvv[:, mt, hs], bank(bk), [PB[bk]], [b_xv])
        for mt in range(2):
            for half in range(2):
                xv_mh(mt, half)

        h2tm = T([128, NT, D], BF16, B0 + 32768); b_h2tm = Buf("h2tm")
        S.inherit(b_h2tm, [bxw["k"], bxw["v"]])

        def xa_head(tb, hh):
            cols = slice(tb * 512, (tb + 1) * 512)
            xq = xqT[hh % 2]
            bq = b_xq[hh % 2]
            for i in range(2):
                fc = 2 * hh + i
                S.pe([mm(bank(i), xw["q"][:, c, fc * 128:(fc + 1) * 128], actT[:, c, cols], c == 0, c == 7) for c in range(8)],
                     reads=[bxw["q"], b_actT[tb]], writes=[PB[i]])
                evac(xq[:, i, :], bank(i), [PB[i]], [bq])
            for mt in range(2):
                S.pe([mm(bank(2 + mt), xkT[:, 2 * hh + i, mt * 128:(mt + 1) * 128], xq[:, i, :], i == 0, i == 1) for i in range(2)],
                     reads=[b_xk, bq], writes=[PB[2 + mt]])
                S.op("act", lambda e, mt=mt: e.activation(out=pT2[mt][:], in_=bank(2 + mt), func=AF.Exp, scale=1.0 / 16.0),
                     reads=[PB[2 + mt]], writes=[b_p2[mt]])
            S.pe([mm(bank(4), onesb[:], pT2[mt][:], mt == 0, mt == 1) for mt in range(2)], reads=[b_onesb] + b_p2, writes=[PB[4]])
            S.op("dve", lambda e: e.reciprocal(out=junk[:, 0:512], in_=bank(4)), reads=[PB[4]], writes=[b_junk])
            for dc in range(2):
                S.pe([mm(bank(5 + dc), xvv[:, mt, hh * 256 + dc * 128:hh * 256 + (dc + 1) * 128], pT2[mt][:], mt == 0, mt == 1) for mt in range(2)],
                     reads=[b_xv] + b_p2, writes=[PB[5 + dc]])
                S.op("dve", lambda e, dc=dc: e.tensor_tensor(out=oT[:, hh * 2 + dc, :], in0=bank(5 + dc), in1=junk[:, 0:512], op=ALU.mult),
                     reads=[PB[5 + dc], b_junk], writes=[b_oT])

        def cast_h2(j):
            S.op("pool", lambda e: e.tensor_copy(out=h2tm[:, j, :], in_=resid[:, j, :]), reads=[b_res[j]], writes=[b_h2tm])

        def xa_out(tb, jj):
            j = tb * 4 + jj
            tsl = slice(jj * 128, (jj + 1) * 128)
            for half in range(2):
                hs = slice(half * 512, (half + 1) * 512)
                S.pe([mm(bank(6 + half), oT[:, fc, tsl], xw["o"][:, fc, hs], fc == 0, fc == 7) for fc in range(8)],
                     reads=[b_oT, bxw["o"]], writes=[PB[6 + half]])
            ln_tile(j, resid[:, j, :], [b_res[j]], 3, 0, extra=cast_h2)
        for tb in range(4):
            for hh in range(4):
                xa_head(tb, hh)
            for jj in range(4):
                xa_out(tb, jj)
        dump("h2", resid[:, :, :], b_res)
        if stop_after == "xattn":
            S.wait_bufs("sp", [b_dbg])
            S.emit()
            return nc

        load_lnp(2)
        phaseB = [bxw["q"], bxw["o"], b_xv, b_oT, b_memT, b_xk] + b_xq + b_p2
        wr_sb = T([128, 8, NE], BF16, TAIL); b_wr = Buf("wr")
        S.inherit(b_wr, [b_memT])
        with_nc = w_router.rearrange("(c p) e -> p c e", p=128)
        S.dma("pool", lambda e: e.dma_start(out=wr_sb[:], in_=with_nc), b_wr)
        esel = T([16, 16 * 128], F32, TAIL + 256); b_esel = Buf("esel")
        S.inherit(b_esel, [b_memT, b_xk])
        S.dma("sp", lambda e: e.dma_start(out=esel[:], in_=c_esel), b_esel)
        aff = T([16, S_], F32, B0); affw = [T([16, S_], F32, B0 + 8192), T([16, S_], F32, B0 + 16384)]
        b_aff = Buf("aff"); b_affw = [Buf("affw0"), Buf("affw1")]
        gate = T([16, CAP], F32, B0 + 24576); idxu = T([16, CAP], U32, B0 + 25600); idxf = T([16, CAP], F32, B0 + 26624)
        b_gate = Buf("gate"); b_idxu = Buf("idxu"); b_idxf = Buf("idxf")
        idxT = T([128, 2, NE], F32, B0 + 27648); gateT = T([128, 2, NE], F32, B0 + 27776); b_igT = Buf("igT")
        idxs = T([128, 32], F32, B0 + 27904); b_idxs = Buf("idxs")
        for bb in [b_aff, b_gate, b_idxu, b_idxf, b_igT, b_idxs] + b_affw:
            S.inherit(bb, phaseB)
        for tb in range(4):
            def router_tb(tb=tb):
                cols = slice(tb * 512, (tb + 1) * 512)
                S.pe([mm(bank(tb)[0:16, :], wr_sb[:, c, :], actT[:, c, cols], c == 0, c == 7) for c in range(8)],
                     reads=[b_wr, b_actT[tb]], writes=[PB[tb]])
                S.op("act", lambda e: e.activation(out=affw[0][:, cols], in_=bank(tb)[0:16, :], func=AF.Exp), reads=[PB[tb]], writes=[b_affw[0]])
                S.pe([mm(bank(4 + tb)[0:16, :], onesf[0:16, 0:16], affw[0][:, cols])], reads=[b_onesf, b_affw[0]], writes=[PB[4 + tb]])
                S.op("dve", lambda e: e.reciprocal(out=affw[1][:, cols], in_=bank(4 + tb)[0:16, :]), reads=[PB[4 + tb]], writes=[b_affw[1]])
                S.op("dve", lambda e: e.tensor_tensor(out=aff[:, cols], in0=affw[0][:, cols], in1=affw[1][:, cols], op=ALU.mult),
                     reads=b_affw, writes=[b_aff])
            router_tb()
        dump("aff", aff[:, :], [b_aff])
        for r in range(CAP // 8):
            def topk_round(r=r):
                src = aff if r == 0 else affw[(r - 1) % 2]
                bsrc = b_aff if r == 0 else b_affw[(r - 1) % 2]
                dst = affw[r % 2]
                sl = slice(r * 8, r * 8 + 8)
                S.op("dve", lambda e: e.max(out=gate[:, sl], in_=src[:]), reads=[bsrc], writes=[b_gate])
                S.op("dve", lambda e: e.max_index(out=idxu[:, sl], in_max=gate[:, sl], in_values=src[:]), reads=[bsrc, b_gate], writes=[b_idxu])
                if r + 1 < CAP // 8:
                    S.op("dve", lambda e: e.match_replace(out=dst[:], in_to_replace=gate[:, sl], in_values=src[:], imm_value=-1.0),
                         reads=[bsrc, b_gate], writes=[b_affw[r % 2]])
            topk_round()
        S.op("dve", lambda e: e.tensor_copy(out=idxf[:], in_=idxu[:]), reads=[b_idxu], writes=[b_idxf])
        dump("gate", gate[:, :], [b_gate])
        dump("idxf", idxf[:, :], [b_idxf])
        S.pe([lambda e, ch=ch: e.transpose(bank(0)[:, ch * 16:(ch + 1) * 16], idxf[0:16, ch * 128:(ch + 1) * 128], ident[0:16, 0:16]) for ch in range(2)]
             + [lambda e, ch=ch: e.transpose(bank(0)[:, 32 + ch * 16:32 + (ch + 1) * 16], gate[0:16, ch * 128:(ch + 1) * 128], ident[0:16, 0:16]) for ch in range(2)],
             reads=[b_idxf, b_gate, b_ident], writes=[PB[0]])
        S.op("dve", lambda e: e.tensor_copy(out=idxT[:].rearrange("p a b -> p (a b)"), in_=bank(0)[:, 0:32]), reads=[PB[0]], writes=[b_igT])
        S.op("dve", lambda e: e.tensor_copy(out=gateT[:].rearrange("p a b -> p (a b)"), in_=bank(0)[:, 32:64]), reads=[PB[0]], writes=[b_igT])

        for j in range(NT):
            S.op("pool", lambda e, j=j: e.tensor_scalar(out=resid[:, j, :], in0=resid[:, j, :], scalar1=ALPHA, scalar2=None, op0=ALU.mult),
                 reads=[b_res[j]], writes=[b_res[j]])

        C1 = R1
        SelT = [T([128, NT, CAP], BF16, C1 + i * 8192) for i in range(2)]; b_sel = [Buf("sel%d" % i) for i in range(2)]
        GRP = 4
        ysc = T([128, GRP * 2, D], BF16, C1 + 16384); b_ysc = Buf("ysc")
        for bb in b_sel + [b_ysc]:
            S.inherit(bb, b_actT)
        xsT = [T([128, 8, CAP], BF16, B0 + 65536 + i * 4096) for i in range(2)]; b_xs = [Buf("xs%d" % i) for i in range(2)]
        hidT = [T([128, 2, CAP], BF16, B0 + 73728 + i * 1024) for i in range(2)]; b_hid = [Buf("hid%d" % i) for i in range(2)]
        for bb in b_xs + b_hid:
            S.inherit(bb, phaseB)
        selsc = [T([128, GRP * 2, 128], BF16, R8 + i * 2048) for i in range(2)]; b_selsc = [Buf("selsc%d" % i) for i in range(2)]
        silu_t = T([128, 512], F32, R8 + 4096); b_silu = Buf("silu")
        idxs = [T([128, 8], F32, R8 + 6144 + i * 32) for i in range(2)]; b_idxs2 = [Buf("idxs%d" % i) for i in range(2)]
        for bb in b_selsc + [b_silu] + b_idxs2:
            S.inherit(bb, [b_oT])
        ringG = [T([128, 8, 256], BF16, B0 + i * 12288) for i in range(2)]
        ringU = [T([128, 8, 256], BF16, B0 + i * 12288 + 4096) for i in range(2)]
        ringD = [T([128, 2, D], BF16, B0 + i * 12288 + 8192) for i in range(2)]
        b_ring = [Buf("ring%d" % i) for i in range(2)]
        for bb in b_ring:
            S.inherit(bb, [b_aff] + b_affw + phaseB)

        def load_unit(n):
            e, fb = divmod(n, 8)
            sl = n % 2
            fs = slice(fb * 256, (fb + 1) * 256)
            S.dma("pool", lambda e_: e_.dma_start(out=ringG[sl][:], in_=w_gate[e].rearrange("(c p) f -> p c f", p=128)[:, :, fs]), b_ring[sl])
            S.dma("pool", lambda e_: e_.dma_start(out=ringU[sl][:], in_=w_up[e].rearrange("(c p) f -> p c f", p=128)[:, :, fs]), b_ring[sl])
            S.dma("pool", lambda e_: e_.dma_start(out=ringD[sl][:], in_=w_down[e][fb * 256:(fb + 1) * 256, :].rearrange("(c p) d -> p c d", p=128)), b_ring[sl])

        def gather_expert(e):
            st = SelT[e % 2]
            bs_ = b_sel[e % 2]
            xs = xsT[e % 2]
            S.pe([mm(bank(3)[:, 0:CAP], esel[0:16, e * 128:(e + 1) * 128], idxf[0:16, :])], reads=[b_esel, b_idxf], writes=[PB[3]])
            S.op("dve", lambda e_: e_.tensor_tensor(out=st[:], in0=bank(3)[:, 0:CAP].unsqueeze(1).broadcast_to([128, NT, CAP]),
                                                    in1=tokid[:, :].unsqueeze(2).broadcast_to([128, NT, CAP]), op=ALU.is_equal),
                 reads=[PB[3], b_tokid], writes=[bs_])
            for ps_ in range(2):
                for dcl in range(4):
                    dc = ps_ * 4 + dcl
                    bk = dcl // 2
                    co = (dcl % 2) * 256
                    S.pe([mm(bank(bk)[:, co:co + 256], h2tm[:, j, dc * 128:(dc + 1) * 128], st[:, j, :], j == 0, j == NT - 1) for j in range(NT)],
                         reads=[b_h2tm, bs_], writes=[PB[bk]])
                evac(xs[:, ps_ * 4:(ps_ + 1) * 4, :], PP[0][:].rearrange("p (d c) -> p d c", c=CAP), [PB[0], PB[1]], [b_xs[e % 2]])

        def ffn_unit(n):
            e, fb = divmod(n, 8)
            sl = n % 2
            xs = xsT[e % 2]
            hd = hidT[n % 2]
            for (bk, W) in ((2, ringG[sl]), (3, ringU[sl])):
                fns = []
                for fl in range(2):
                    fns += [mm(bank(bk)[:, fl * 256:(fl + 1) * 256], W[:, dc, fl * 128:(fl + 1) * 128], xs[:, dc, :], dc == 0, dc == 7) for dc in range(8)]
                S.pe(fns, reads=[b_ring[sl], b_xs[e % 2]], writes=[PB[bk]])
            S.op("act", lambda e_: e_.activation(out=silu_t[:], in_=bank(2), func=AF.Silu), reads=[PB[2]], writes=[b_silu])
            S.op("dve", lambda e_: e_.tensor_tensor(out=hd[:].rearrange("p a b -> p (a b)"), in0=silu_t[:], in1=bank(3), op=ALU.mult),
                 reads=[b_silu, PB[3]], writes=[b_hid[n % 2]])
            for cc2 in range(2):
                for half in range(2):
                    bk = 4 + cc2 * 2 + half
                    S.pe([mm(bank(bk), hd[:, fl, cc2 * 128:(cc2 + 1) * 128], ringD[sl][:, fl, half * 512:(half + 1) * 512],
                             fb == 0 and fl == 0, fb == 7 and fl == 1) for fl in range(2)],
                         reads=[b_hid[n % 2], b_ring[sl]], writes=[PB[bk]])

        def finish_expert(e):
            for cc2 in range(2):
                S.op("act", lambda e_, cc2=cc2: e_.activation(out=ysc[:, (e % GRP) * 2 + cc2, :], in_=PP[2 + cc2][:], func=AF.Copy,
                                                              scale=gateT[:, cc2, e:e + 1]),
                     reads=[PB[4 + 2 * cc2], PB[5 + 2 * cc2], b_igT], writes=[b_ysc])

        def scatter_group(g):
            def sc_tile(j):
                ix = idxs[j % 2]
                bi = b_idxs2[j % 2]
                sc = selsc[j % 2]
                bsc = b_selsc[j % 2]
                S.op("dve", lambda e_: e_.tensor_scalar(out=ix[:, 0:8].rearrange("p (e c) -> p e c", c=2),
                                                        in0=idxT[:, :, g * GRP:(g + 1) * GRP].rearrange("p c e -> p e c"),
                                                        scalar1=-128.0 * j, scalar2=None, op0=ALU.add), reads=[b_igT], writes=[bi])
                S.op("dve", lambda e_: e_.tensor_tensor(out=sc[:], in0=ix[:, 0:8].unsqueeze(2).broadcast_to([128, 8, 128]),
                                                        in1=iota[:, :].unsqueeze(1).broadcast_to([128, 8, 128]), op=ALU.is_equal),
                     reads=[bi, b_iota], writes=[bsc])
                for half in range(2):
                    S.pe([mm(bank(half), sc[:, k, :], ysc[:, k, half * 512:(half + 1) * 512], k == 0, k == 7) for k in range(8)],
                         reads=[bsc, b_ysc], writes=[PB[half]])
                S.op("dve", lambda e_: e_.tensor_tensor(out=resid[:, j, :], in0=resid[:, j, :], in1=PP[0][:], op=ALU.add),
                     reads=[b_res[j], PB[0], PB[1]], writes=[b_res[j]])
            for j in range(NT):
                sc_tile(j)

        NEXP = NE
        load_unit(0)
        load_unit(1)
        for e in range(NEXP):
            gather_expert(e)
            for fb in range(8):
                n = e * 8 + fb
                ffn_unit(n)
                if n + 2 < NEXP * 8:
                    load_unit(n + 2)
            finish_expert(e)
            if e % GRP == GRP - 1:
                scatter_group(e // GRP)

        outv = out.rearrange("(j p) d -> j p d", p=128)
        b_out = Buf("out")

        def ln3_tile(j):
            so = (j % 2) * 8
            bs = b_st[j % 2]
            st_ = lambda a: stat[:, so + a:so + a + 1]
            S.op("dve", lambda e: e.tensor_copy(out=ysb[:], in_=resid[:, j, :]), reads=[b_res[j]], writes=[b_ysb])
            S.op("dve", lambda e: e.memset(stat[:, so:so + 2], 0.0), writes=[bs])
            S.op("act", lambda e: e.activation(out=junk[:], in_=ysb[:], func=AF.Copy, accum_out=st_(0)), reads=[b_ysb], writes=[b_junk, bs])
            S.op("act", lambda e: e.activation(out=junk[:], in_=ysb[:], func=AF.Square, accum_out=st_(1)), reads=[b_ysb], writes=[b_junk, bs])
            S.op("dve", lambda e: e.tensor_scalar(out=st_(2), in0=st_(0), scalar1=1.0 / D, scalar2=None, op0=ALU.mult), reads=[bs], writes=[bs])
            S.op("dve", lambda e: e.tensor_tensor(out=st_(3), in0=st_(2), in1=st_(2), op=ALU.mult), reads=[bs], writes=[bs])
            S.op("dve", lambda e: e.scalar_tensor_tensor(out=st_(3), in0=st_(1), scalar=1.0 / D, in1=st_(3), op0=ALU.mult, op1=ALU.subtract),
                 reads=[bs], writes=[bs])
            S.op("act", lambda e: e.activation(out=st_(4), in_=st_(3), func=AF.Sqrt, bias=cst[:, 2:3], scale=1.0), reads=[bs, b_cst], writes=[bs])
            S.op("dve", lambda e: e.reciprocal(out=st_(4), in_=st_(4)), reads=[bs], writes=[bs])
            S.op("dve", lambda e: e.scalar_tensor_tensor(out=st_(5), in0=st_(2), scalar=-1.0, in1=st_(4), op0=ALU.mult, op1=ALU.mult), reads=[bs], writes=[bs])
            S.op("act", lambda e: e.activation(out=ysb[:], in_=ysb[:], func=AF.Identity, scale=st_(4), bias=st_(5)), reads=[b_ysb, bs], writes=[b_ysb])
            S.op("pool", lambda e: e.tensor_tensor(out=ysb[:], in0=ysb[:], in1=lnp[:, 0, :], op=ALU.mult), reads=[b_ysb, b_lnp], writes=[b_ysb])
            S.op("pool", lambda e: e.tensor_tensor(out=resid[:, j, :], in0=ysb[:], in1=lnp[:, 1, :], op=ALU.add), reads=[b_ysb, b_lnp], writes=[b_res[j]])
            S.dma("sp", lambda e: e.dma_start(out=outv[j], in_=resid[:, j, :]), b_out, reads=[b_res[j]], writes=[b_out])
        for j in range(NT):
            ln3_tile(j)
        S.wait_bufs("sp", [b_out, b_dbg])
        S.emit()
    return nc


def make_consts():
    c = {}
    c["c_ident"] = np.eye(128, dtype=np.float32)
    c["c_iota"] = np.tile(np.arange(128, dtype=np.float32)[None, :], (128, 1))
    c["c_tokid"] = (np.arange(NT, dtype=np.float32)[None, :] * 128 + np.arange(128, dtype=np.float32)[:, None]).astype(np.float32)
    es = np.zeros((16, 16, 128), np.float32)
    for e in range(16):
        es[e, e, :] = 1.0
    c["c_esel"] = es.reshape(16, 16 * 128)
    rope = np.zeros((128, 2), np.float32)
    inv_freq = (10000.0 ** (-np.arange(0, 32, 2, dtype=np.float32) / 32.0)).astype(np.float32)
    for p in range(64, 96):
        rope[p, 0] = np.float32(np.float64(inv_freq[(p - 64) % 16]) / (2.0 * np.pi))
        rope[p, 1] = -1.0 if p < 80 else 1.0
    c["c_rope"] = rope
    return c


def make_in_maps(inputs, n_cores=8):
    consts = make_consts()
    maps = []
    f = lambda a: np.ascontiguousarray(np.asarray(a))
    for b in range(n_cores):
        m = dict(consts)
        m["x"] = f(inputs["x"][b])
        m["mem"] = f(inputs["mem"][b])
        m["positions"] = f(np.asarray(inputs["positions"])[b:b + 1]).astype(np.int32)
        for k in ["w_in", "q_norm_g", "w_uq", "kv_norm_g", "w_uk", "w_uv", "conv_w", "conv_b", "conv_ln_g", "conv_ln_b",
                  "w_o", "xa_w_q", "xa_w_k", "xa_w_v", "xa_w_o", "w_router", "w_gate", "w_up", "w_down"]:
            m[k] = f(np.asarray(inputs[k])[0])
        for i in (1, 2, 3):
            m["ln%d_g" % i] = f(np.asarray(inputs["ln%d_g" % i])[0:1])
            m["ln%d_b" % i] = f(np.asarray(inputs["ln%d_b" % i])[0:1])
        maps.append(m)
    return maps


def kernel(**inputs):
    nc = build_program()
    maps = make_in_maps(inputs, 8)
    res = run_bass_kernel_spmd(nc, maps, core_ids=list(range(8)))
    return np.stack([r["out"] for r in res.results], axis=0).astype(np.float32)
```

```python
import math
import os
from contextlib import ExitStack

import numpy as np
import concourse.bass as bass
import concourse.mybir as mybir
from concourse.bass_utils import run_bass_kernel_spmd

F32 = mybir.dt.float32
BF16 = mybir.dt.bfloat16
I32 = mybir.dt.int32
U32 = mybir.dt.uint32
ALU = mybir.AluOpType
AF = mybir.ActivationFunctionType

S_ = 2048
D = 1024
NT = 16
EPS = 1e-5
ALPHA = 2.0 ** 0.25
NE = 16
CAP = 256
FF = 2048


class Buf:
    __slots__ = ("name", "w", "r", "sem", "cnt")

    def __init__(self, name):
        self.name = name
        self.w = None
        self.r = {}
        self.sem = None
        self.cnt = 0


class Sched:
    ENG = ["pe", "act", "dve", "pool", "sp"]

    def __init__(self, nc, es):
        self.nc = nc
        self.es = es
        self.streams = {e: [] for e in self.ENG}
        self.cnt = {e: 0 for e in self.ENG}
        self.esem = {e: es.enter_context(nc.semaphore("c_" + e)) for e in self.ENG}
        self.waited = {e: {} for e in self.ENG}
        self.nsem = len(self.ENG)

    def new_sem(self, name):
        self.nsem += 1
        return self.es.enter_context(self.nc.semaphore("d%d_%s" % (self.nsem, name)))

    def _waits(self, eng, reads, writes):
        deps = {}

        def add(s, v):
            if deps.get(s, 0) < v:
                deps[s] = v
        for b in reads:
            if b.w is not None:
                add(*b.w)
        for b in writes:
            if b.w is not None:
                add(*b.w)
            for s, v in b.r.items():
                add(s, v)
        own = self.esem[eng]
        wd = self.waited[eng]
        for s, v in deps.items():
            if s == own and eng == "pe":
                continue
            if wd.get(s, 0) < v:
                wd[s] = v
                self.streams[eng].append(("wait", s, v))

    def _commit(self, ev, reads, writes):
        s, v = ev
        for b in reads:
            if b.r.get(s, 0) < v:
                b.r[s] = v
        for b in writes:
            b.w = ev
            b.r = {}

    def op(self, eng, fn, reads=(), writes=()):
        self._waits(eng, reads, writes)
        self.cnt[eng] += 1
        s = self.esem[eng]
        self.streams[eng].append(("op", fn, s, 1))
        self._commit((s, self.cnt[eng]), reads, writes)

    def pe(self, fns, reads=(), writes=()):
        self._waits("pe", reads, writes)
        self.cnt["pe"] += 1
        s = self.esem["pe"]
        for f in fns[:-1]:
            self.streams["pe"].append(("op", f, None, 0))
        self.streams["pe"].append(("op", fns[-1], s, 1))
        self._commit((s, self.cnt["pe"]), reads, writes)

    def dma(self, eng, fn, dst, reads=(), writes=None):
        if writes is None:
            writes = (dst,)
        self._waits(eng, reads, writes)
        if dst.sem is None:
            dst.sem = self.new_sem(dst.name)
        dst.cnt += 16
        self.streams[eng].append(("op", fn, dst.sem, 16))
        self._commit((dst.sem, dst.cnt), reads, writes)

    def wait_bufs(self, eng, bufs):
        self._waits(eng, bufs, bufs)

    def inherit(self, new, olds):
        for o in olds:
            if o.w is not None:
                s, v = o.w
                if new.r.get(s, 0) < v:
                    new.r[s] = v
            for s, v in o.r.items():
                if new.r.get(s, 0) < v:
                    new.r[s] = v

    def emit(self):
        nc = self.nc
        streams = self.streams

        def run(name, eng):
            for item in streams[name]:
                if item[0] == "wait":
                    eng.wait_ge(item[1], item[2])
                else:
                    ins = item[1](eng)
                    if item[2] is not None:
                        ins.then_inc(item[2], item[3])

        with nc.Block() as block:
            @block.tensor
            def _(e):
                run("pe", e)

            @block.scalar
            def _(e):
                run("act", e)

            @block.vector
            def _(e):
                run("dve", e)

            @block.gpsimd
            def _(e):
                run("pool", e)

            @block.sync
            def _(e):
                run("sp", e)


def build_program(stop_after=None, dumps=()):
    nc = bass.Bass("TRN2", target_bir_lowering=False)

    def din(name, shape, dt=F32):
        return nc.dram_tensor(name, list(shape), dt, kind="ExternalInput").ap()

    x = din("x", [S_, D])
    mem = din("mem", [256, D])
    pos = din("positions", [1, S_], I32)
    w_in = din("w_in", [D, 1440])
    q_norm_g = din("q_norm_g", [256])
    w_uq = din("w_uq", [256, 768])
    kv_norm_g = din("kv_norm_g", [128])
    w_uk = din("w_uk", [128, 512])
    w_uv = din("w_uv", [128, 512])
    conv_w = din("conv_w", [31, 512])
    conv_b = din("conv_b", [512])
    conv_ln_g = din("conv_ln_g", [512])
    conv_ln_b = din("conv_ln_b", [512])
    w_o = din("w_o", [D, D])
    ln_g = [din("ln%d_g" % i, [1, D]) for i in (1, 2, 3)]
    ln_b = [din("ln%d_b" % i, [1, D]) for i in (1, 2, 3)]
    xa_w = {k: din("xa_w_" + k, [D, D]) for k in "qkvo"}
    w_router = din("w_router", [D, NE])
    w_gate = din("w_gate", [NE, D, FF])
    w_up = din("w_up", [NE, D, FF])
    w_down = din("w_down", [NE, FF, D])
    c_ident = din("c_ident", [128, 128])
    c_iota = din("c_iota", [128, 128])
    c_tokid = din("c_tokid", [128, NT])
    c_esel = din("c_esel", [16, 16 * 128])
    c_rope = din("c_rope", [128, 2])
    out = nc.dram_tensor("out", [S_, D], F32, kind="ExternalOutput").ap()
    dump_d = {}
    for (nm, shape, dt) in dumps:
        dump_d[nm] = nc.dram_tensor("dbg_" + nm, list(shape), dt, kind="ExternalOutput").ap()

    es = ExitStack()
    with es:
        S = Sched(nc, es)
        cnt = [0]

        def T(shape, dt, off, name=None):
            cnt[0] += 1
            return nc.alloc_sbuf_tensor_at("%s_%d" % (name or "t", cnt[0]), list(shape), dt, offset=off)

        b_dbg = Buf("dbg")

        def dump(nm, ap, bufs):
            if nm in dump_d:
                S.dma("sp", lambda e: e.dma_start(out=dump_d[nm], in_=ap), b_dbg, reads=bufs, writes=[b_dbg])

        R0 = 16640
        R1 = R0 + 66048
        R2 = R1 + 32768
        R3 = R2 + 26624
        R4 = R3 + 24576
        R5 = R4 + 8192
        R6 = R5 + 16384
        R7 = R6 + 8192
        R8 = R7 + 8192
        R9 = R8 + 8192
        ident = T([128, 128], F32, R9); b_ident = Buf("ident")
        identb = T([128, 128], BF16, R9 + 512); b_identb = Buf("identb")
        onesb = T([128, 128], BF16, R9 + 768); b_onesb = Buf("onesb")
        onesf = T([128, 128], F32, R9 + 1024); b_onesf = Buf("onesf")
        ropec = T([128, 2], F32, R9 + 1536); b_ropec = Buf("ropec")
        gsm = T([128, 16], F32, R9 + 1600); b_gsm = Buf("gsm")
        cwT = T([128, 4, 32], F32, R9 + 1664); b_cwT = Buf("cwT")
        iota = T([128, 128], F32, R9 + 2176); b_iota = Buf("iota")
        tokid = T([128, NT], F32, R9 + 2688); b_tokid = Buf("tokid")
        stat = T([128, 16], F32, R9 + 2752); b_stat = Buf("stat")
        cst = T([128, 8], F32, R9 + 2816); b_cst = Buf('cst')
        CEND = R9 + 2848
        assert CEND <= 229376, CEND

        PP = [nc.alloc_psum_tensor("pp%d" % i, [128, 1024], F32) for i in range(4)]
        PB = [Buf("bank%d" % i) for i in range(8)]

        def bank(k):
            return PP[k // 2][:, (k % 2) * 512:(k % 2) * 512 + 512]

        S.dma("sp", lambda e: e.dma_start(out=ident[:], in_=c_ident), b_ident)
        S.dma("sp", lambda e: e.dma_start(out=iota[:], in_=c_iota), b_iota)
        S.dma("sp", lambda e: e.dma_start(out=tokid[:], in_=c_tokid), b_tokid)
        S.dma("sp", lambda e: e.dma_start(out=ropec[:], in_=c_rope), b_ropec)
        S.op("dve", lambda e: e.tensor_copy(out=identb[:], in_=ident[:]), reads=[b_ident], writes=[b_identb])
        S.op("dve", lambda e: e.memset(onesb[:], 1.0), writes=[b_onesb])
        S.op("dve", lambda e: e.memset(onesf[:], 1.0), writes=[b_onesf])
        S.op("dve", lambda e: e.memset(cst[:, 0:1], 256.0 * EPS), writes=[b_cst])
        S.op("dve", lambda e: e.memset(cst[:, 1:2], 128.0 * EPS), writes=[b_cst])
        S.op("dve", lambda e: e.memset(cst[:, 2:3], EPS), writes=[b_cst])
        S.op("dve", lambda e: e.memset(cst[:, 3:4], 0.0), writes=[b_cst])
        with nc.allow_non_contiguous_dma(reason="tiny per-channel parameter vectors"):
            S.dma("sp", lambda e: e.dma_start(out=gsm[:, 0:2], in_=q_norm_g.rearrange("(c p) -> p c", p=128), allow_slow_non_contiguous=True), b_gsm)
            S.dma("sp", lambda e: e.dma_start(out=gsm[:, 2:3], in_=kv_norm_g.rearrange("(c p) -> p c", p=128), allow_slow_non_contiguous=True), b_gsm)
            S.dma("sp", lambda e: e.dma_start(out=gsm[:, 4:8], in_=conv_b.rearrange("(c p) -> p c", p=128), allow_slow_non_contiguous=True), b_gsm)
            S.dma("sp", lambda e: e.dma_start(out=gsm[:, 8:12], in_=conv_ln_g.rearrange("(c p) -> p c", p=128), allow_slow_non_contiguous=True), b_gsm)
            S.dma("sp", lambda e: e.dma_start(out=gsm[:, 12:16], in_=conv_ln_b.rearrange("(c p) -> p c", p=128), allow_slow_non_contiguous=True), b_gsm)
        S.op("dve", lambda e: e.tensor_scalar(out=gsm[:, 0:2], in0=gsm[:, 0:2], scalar1=16.0, scalar2=None, op0=ALU.mult),
             reads=[b_gsm], writes=[b_gsm])
        S.op("dve", lambda e: e.tensor_scalar(out=gsm[:, 2:3], in0=gsm[:, 2:3], scalar1=math.sqrt(128.0), scalar2=None, op0=ALU.mult),
             reads=[b_gsm], writes=[b_gsm])

        resid = T([128, NT, D], F32, R0); b_res = [Buf("res%d" % j) for j in range(NT)]
        u = T([128, 4, 2078], F32, R0); b_u = [Buf("u%d" % c) for c in range(4)]
        acc = T([128, 4, 2048], F32, R0 + 33248); b_acc = [Buf("acc%d" % c) for c in range(4)]
        xstage = [T([128, D], F32, R0 + 33248 + i * 4096) for i in range(2)]; b_xst = [Buf("xst%d" % i) for i in range(2)]
        actT = T([128, 8, S_], BF16, R1); b_actT = [Buf("actT%d" % t) for t in range(4)]
        kT = actT; b_kT = Buf("kT")
        w_in_sb = T([128, 8, 1440], BF16, R2); b_w_in = Buf("w_in")
        wkr = T([128, 8, 2, 96], BF16, R2 + 23040); b_wkr = Buf("wkr")
        attnT = T([128, 4, S_], BF16, R2); b_attnT = Buf("attnT")
        qT = [T([128, 512], BF16, R2 + 16384 + i * 1024) for i in range(2)]; b_qT = [Buf("qT%d" % i) for i in range(2)]
        NPT = 4
        pTt = [T([128, 512], BF16, R2 + 18432 + i * 1024) for i in range(NPT)]; b_pT = [Buf("pT%d" % i) for i in range(NPT)]
        Vaug = T([128, NT, 4, 192], BF16, R3); b_V = Buf("V")
        cqnT = T([128, 2, S_], BF16, R4); b_cqn = Buf("cqn")
        convT = T([128, 4, S_], BF16, R5); b_convT = Buf("convT")
        kr = T([128, S_], BF16, R5); b_kr = Buf("kr")
        ckvnT = T([128, S_], BF16, R5 + 4096); b_ckvn = Buf("ckvn")
        COS = T([128, S_], BF16, R6); SIN = T([128, S_], BF16, R6 + 4096); b_cs = Buf("cossin")
        scr = [T([128, 512], F32, R7 + i * 2048) for i in range(4)]; b_scr = [Buf("scr%d" % i) for i in range(4)]
        wuq = T([128, 2, 768], BF16, R8); b_wuq = Buf("wuq")
        wqsw = T([128, 2, 8, 96], BF16, R8 + 3072); b_wqsw = Buf("wqsw")
        wuk = T([128, 512], BF16, R8 + 6144); b_wuk = Buf("wuk")
        wuv = T([128, 512], BF16, R8 + 7168); b_wuv = Buf("wuv")

        w_in_v = w_in.rearrange("(c p) f -> p c f", p=128)
        for c0 in range(0, 8, 4):
            S.dma("pool", lambda e, c0=c0: e.dma_start(out=w_in_sb[:, c0:c0 + 4, :], in_=w_in_v[:, c0:c0 + 4, :]), b_w_in)
        S.op("pool", lambda e: e.memset(wkr[:], 0.0), writes=[b_wkr])
        S.op("pool", lambda e: e.memset(wqsw[:], 0.0), writes=[b_wqsw])
        with nc.allow_non_contiguous_dma(reason="rope column permutation"):
            S.dma("pool", lambda e: e.dma_start(out=wkr[:, :, 0, 64:96], in_=w_in_v[:, :, 384:416], allow_slow_non_contiguous=True), b_wkr)
            S.dma("pool", lambda e: e.dma_start(out=wkr[:, :, 1, 64:80], in_=w_in_v[:, :, 400:416], allow_slow_non_contiguous=True), b_wkr)
            S.dma("pool", lambda e: e.dma_start(out=wkr[:, :, 1, 80:96], in_=w_in_v[:, :, 384:400], allow_slow_non_contiguous=True), b_wkr)
            w_uq_v = w_uq.rearrange("(c p) (h f) -> p c h f", p=128, f=96)
            for c in range(2):
                S.dma("pool", lambda e, c=c: e.dma_start(out=wqsw[:, c, :, 64:80], in_=w_uq_v[:, c, :, 80:96], allow_slow_non_contiguous=True), b_wqsw)
                S.dma("pool", lambda e, c=c: e.dma_start(out=wqsw[:, c, :, 80:96], in_=w_uq_v[:, c, :, 64:80], allow_slow_non_contiguous=True), b_wqsw)
        S.dma("pool", lambda e: e.dma_start(out=wuq[:], in_=w_uq.rearrange("(c p) f -> p c f", p=128)), b_wuq)
        S.dma("pool", lambda e: e.dma_start(out=wuk[:], in_=w_uk), b_wuk)
        S.dma("pool", lambda e: e.dma_start(out=wuv[:], in_=w_uv), b_wuv)

        cw_st = scr[0]
        S.dma("sp", lambda e: e.dma_start(out=cw_st[0:31, 0:512], in_=conv_w), b_scr[0])
        S.pe([lambda e, cc=cc: e.transpose(bank(0)[:, cc * 32:cc * 32 + 31], cw_st[0:31, cc * 128:(cc + 1) * 128], ident[0:31, 0:31])
              for cc in range(4)], reads=[b_scr[0], b_ident], writes=[PB[0]])
        S.op("dve", lambda e: e.tensor_copy(out=cwT[:, :, 0:31], in_=bank(0)[:, 0:128].rearrange("p (c k) -> p c k", k=32)[:, :, 0:31]),
             reads=[PB[0]], writes=[b_cwT])

        posi = T([128, S_], I32, R0)
        posf = T([128, S_], F32, R0 + 8192)
        angb = T([128, S_], F32, R0 + 16384)
        b_pos = Buf("pos")
        S.dma("sp", lambda e: e.dma_start(out=posi[:], in_=pos.broadcast_to([128, S_])), b_pos)
        rp = slice(64, 96)
        S.op("dve", lambda e: e.tensor_copy(out=posf[rp, :], in_=posi[rp, :]), reads=[b_pos], writes=[b_pos])
        kint = T([128, S_], I32, R0 + 24576)
        sinf = T([128, S_], F32, R0 + 24576)

        def trig(phase, dst_fn):
            S.op("dve", lambda e: e.tensor_scalar(out=angb[rp, :], in0=posf[rp, :], scalar1=ropec[rp, 0:1], scalar2=phase,
                                                  op0=ALU.mult, op1=ALU.add), reads=[b_pos, b_ropec], writes=[b_pos])
            S.op("dve", lambda e: e.tensor_copy(out=kint[rp, :], in_=angb[rp, :]), reads=[b_pos], writes=[b_pos])
            S.op("dve", lambda e: e.tensor_copy(out=posi[rp, :].bitcast(F32), in_=kint[rp, :]), reads=[b_pos], writes=[b_pos])
            S.op("dve", lambda e: e.tensor_tensor(out=angb[rp, :], in0=angb[rp, :], in1=posi[rp, :].bitcast(F32), op=ALU.subtract),
                 reads=[b_pos], writes=[b_pos])
            S.op("dve", lambda e: e.scalar_tensor_tensor(out=angb[rp, :], in0=angb[rp, :], scalar=0.5, in1=angb[rp, :],
                                                         op0=ALU.is_gt, op1=ALU.subtract), reads=[b_pos], writes=[b_pos])
            dst_fn()

        def fin_sin():
            S.op("act", lambda e: e.activation(out=sinf[rp, :], in_=angb[rp, :], func=AF.Sin, scale=-2.0 * math.pi), reads=[b_pos], writes=[b_pos])
            S.op("dve", lambda e: e.tensor_scalar(out=SIN[rp, :], in0=sinf[rp, :], scalar1=ropec[rp, 1:2], scalar2=None, op0=ALU.mult),
                 reads=[b_pos, b_ropec], writes=[b_cs])

        def fin_cos():
            S.op("act", lambda e: e.activation(out=COS[rp, :], in_=angb[rp, :], func=AF.Sin, scale=-2.0 * math.pi), reads=[b_pos], writes=[b_cs])
        trig(0.0, fin_sin)
        trig(0.25, fin_cos)
        for c in range(4):
            S.inherit(b_u[c], [b_pos])

        xv = x.rearrange("(j p) d -> j p d", p=128)
        for j in range(NT):
            st = j % 2
            S.dma("sp", lambda e, j=j, st=st: e.dma_start(out=xstage[st][:], in_=xv[j]), b_xst[st])
            pb = [PB[(j % 2) * 2], PB[(j % 2) * 2 + 1]]
            pt = PP[j % 2]
            S.pe([lambda e, c=c, st=st, pt=pt: e.transpose(pt[:, c * 128:(c + 1) * 128], xstage[st][:, c * 128:(c + 1) * 128], ident[:])
                  for c in range(8)], reads=[b_xst[st], b_ident], writes=pb)
            eng = "act" if j % 2 == 0 else "dve"
            if eng == "act":
                S.op("act", lambda e, j=j, pt=pt: e.activation(out=actT[:, :, j * 128:(j + 1) * 128],
                                                             in_=pt[:].rearrange("p (c t) -> p c t", t=128), func=AF.Copy),
                     reads=pb, writes=[b_actT[j // 4]])
            else:
                S.op("dve", lambda e, j=j, pt=pt: e.tensor_copy(out=actT[:, :, j * 128:(j + 1) * 128],
                                                              in_=pt[:].rearrange("p (c t) -> p c t", t=128)),
                     reads=pb, writes=[b_actT[j // 4]])
        dump("xT", actT[:, :, :], b_actT)

        S.op("pool", lambda e: e.memset(u[:, :, 0:15], 0.0), writes=b_u)
        S.op("pool", lambda e: e.memset(u[:, :, 2063:2078], 0.0), writes=b_u)
        for c in range(4):
            S.inherit(b_acc[c], b_xst)

        def inproj_group(bk, lhs_fn, tb, M):
            cols = slice(tb * 512, (tb + 1) * 512)
            S.pe([lambda e, c=c: e.matmul(bank(bk)[0:M, :], lhsT=lhs_fn(c), rhs=actT[:, c, cols], start=(c == 0), stop=(c == 7))
                  for c in range(8)], reads=[b_w_in, b_wkr, b_actT[tb]], writes=[PB[bk]])

        def inproj_tb(tb):
            cols = slice(tb * 512, (tb + 1) * 512)
            for c2 in range(2):
                inproj_group(c2, lambda c, c2=c2: w_in_sb[:, c, c2 * 128:(c2 + 1) * 128], tb, 128)
                S.op("act", lambda e, c2=c2: e.activation(out=scr[c2][:].bitcast(BF16)[:, 0:512], in_=bank(c2), func=AF.Square),
                     reads=[PB[c2]], writes=[b_scr[c2]])
            S.pe([lambda e, c2=c2: e.matmul(bank(2), lhsT=onesb[:], rhs=scr[c2][:].bitcast(BF16)[:, 0:512], start=(c2 == 0), stop=(c2 == 1))
                  for c2 in range(2)], reads=[b_onesb, b_scr[0], b_scr[1]], writes=[PB[2]])
            S.op("act", lambda e: e.activation(out=scr[2][:], in_=bank(2), func=AF.Sqrt, bias=cst[:, 0:1], scale=1.0),
                 reads=[PB[2], b_cst], writes=[b_scr[2]])
            S.op("dve", lambda e: e.reciprocal(out=scr[2][:], in_=scr[2][:]), reads=[b_scr[2]], writes=[b_scr[2]])
            for c2 in range(2):
                S.op("dve", lambda e, c2=c2: e.scalar_tensor_tensor(out=cqnT[:, c2, cols], in0=bank(c2), scalar=gsm[:, c2:c2 + 1], in1=scr[2][:],
                                                                   op0=ALU.mult, op1=ALU.mult),
                     reads=[PB[c2], b_gsm, b_scr[2]], writes=[b_cqn])
            inproj_group(3, lambda c: w_in_sb[:, c, 256:384], tb, 128)
            S.op("act", lambda e: e.activation(out=scr[3][:].bitcast(BF16)[:, 0:512], in_=bank(3), func=AF.Square),
                 reads=[PB[3]], writes=[b_scr[3]])
            S.pe([lambda e: e.matmul(bank(2), lhsT=onesb[:], rhs=scr[3][:].bitcast(BF16)[:, 0:512], start=True, stop=True)],
                 reads=[b_onesb, b_scr[3]], writes=[PB[2]])
            S.op("act", lambda e: e.activation(out=scr[2][:], in_=bank(2), func=AF.Sqrt, bias=cst[:, 1:2], scale=1.0),
                 reads=[PB[2], b_cst], writes=[b_scr[2]])
            S.op("dve", lambda e: e.reciprocal(out=scr[2][:], in_=scr[2][:]), reads=[b_scr[2]], writes=[b_scr[2]])
            S.op("dve", lambda e: e.scalar_tensor_tensor(out=ckvnT[:, cols], in0=bank(3), scalar=gsm[:, 2:3], in1=scr[2][:],
                                                         op0=ALU.mult, op1=ALU.mult),
                 reads=[PB[3], b_gsm, b_scr[2]], writes=[b_ckvn])
            inproj_group(4, lambda c: wkr[:, c, 0, :], tb, 96)
            inproj_group(5, lambda c: wkr[:, c, 1, :], tb, 96)
            S.op("dve", lambda e: e.tensor_tensor(out=scr[0][rp, :], in0=bank(4)[rp, :], in1=COS[rp, cols], op=ALU.mult),
                 reads=[PB[4], b_cs], writes=[b_scr[0]])
            S.op("dve", lambda e: e.tensor_tensor(out=scr[1][rp, :], in0=bank(5)[rp, :], in1=SIN[rp, cols], op=ALU.mult),
                 reads=[PB[5], b_cs], writes=[b_scr[1]])
            S.op("dve", lambda e: e.tensor_tensor(out=kr[rp, cols], in0=scr[0][rp, :], in1=scr[1][rp, :], op=ALU.add),
                 reads=[b_scr[0], b_scr[1]], writes=[b_kr])
            for cc in range(4):
                inproj_group(6, lambda c, cc=cc: w_in_sb[:, c, 416 + cc * 128:416 + (cc + 1) * 128], tb, 128)
                inproj_group(7, lambda c, cc=cc: w_in_sb[:, c, 928 + cc * 128:928 + (cc + 1) * 128], tb, 128)
                S.op("act", lambda e: e.activation(out=scr[3][:], in_=bank(7), func=AF.Sigmoid), reads=[PB[7]], writes=[b_scr[3]])
                S.op("dve", lambda e, cc=cc: e.tensor_tensor(out=u[:, cc, 15 + tb * 512:15 + (tb + 1) * 512], in0=bank(6), in1=scr[3][:], op=ALU.mult),
                     reads=[PB[6], b_scr[3]], writes=[b_u[cc]])
        for tb in range(4):
            inproj_tb(tb)
        dump("cqnT", cqnT[:, :, :], [b_cqn])
        dump("ckvnT", ckvnT[:, :], [b_ckvn])
        dump("kr", kr[64:96, :], [b_kr])
        dump("u", u[:, :, 15:2063], b_u)
        if stop_after == "inproj":
            S.wait_bufs("sp", [b_dbg])
            S.emit()
            return nc


        def mm(o, l, r, st=True, sp=True):
            return lambda e: e.matmul(o, lhsT=l, rhs=r, start=st, stop=sp)

        evc = [0]

        def evac(out_ap, in_ap, reads, writes):
            evc[0] += 1
            if evc[0] % 2 == 0:
                S.op("act", lambda e: e.activation(out=out_ap, in_=in_ap, func=AF.Copy), reads=reads, writes=writes)
            else:
                S.op("dve", lambda e: e.tensor_copy(out=out_ap, in_=in_ap), reads=reads, writes=writes)

        S.inherit(b_kT, b_actT)

        def kup(h, tb):
            cols = slice(tb * 512, (tb + 1) * 512)
            bk = (h * 4 + tb) % 4
            S.pe([mm(bank(bk)[0:64, :], wuk[:, h * 64:(h + 1) * 64], ckvnT[:, cols])], reads=[b_wuk, b_ckvn], writes=[PB[bk]])
            evac(kT[0:64, h, cols], bank(bk)[0:64, :], [PB[bk]], [b_kT])
        for h in range(8):
            for tb in range(4):
                kup(h, tb)

        def krb(tb):
            cols = slice(tb * 512, (tb + 1) * 512)
            S.op("pool", lambda e: e.tensor_copy(out=kT[64:96, :, cols], in_=kr[64:96, cols].unsqueeze(1).broadcast_to([32, 8, 512])),
                 reads=[b_kr], writes=[b_kT])
        for tb in range(4):
            krb(tb)
        S.op("pool", lambda e: e.memset(Vaug[:, :, :, 64:128], 0.0), writes=[b_V])
        S.op("pool", lambda e: e.memset(Vaug[:, :, :, 64:65], 1.0), writes=[b_V])

        def vup(j):
            bk = 4 + (j % 4)
            S.pe([mm(bank(bk), ckvnT[:, j * 128:(j + 1) * 128], wuv[:])], reads=[b_wuv, b_ckvn], writes=[PB[bk]])
            pv = bank(bk).rearrange("p (q t d) -> p q t d", t=2, d=64)
            S.op("act", lambda e: e.activation(out=Vaug[:, j, :, 0:64], in_=pv[:, :, 0, :], func=AF.Copy), reads=[PB[bk]], writes=[b_V])
            S.op("dve", lambda e: e.tensor_copy(out=Vaug[:, j, :, 128:192], in_=pv[:, :, 1, :]), reads=[PB[bk]], writes=[b_V])
        for j in range(NT):
            vup(j)
        dump("kT", kT[0:96, :, :], [b_kT])
        dump("V", Vaug[:, :, :, :], [b_V])

        def conv_chunk(cc, eng):
            S.op(eng, lambda e: e.tensor_scalar(out=acc[:, cc, :], in0=u[:, cc, 0:2048], scalar1=cwT[:, cc, 0:1], scalar2=gsm[:, 4 + cc:5 + cc],
                                                op0=ALU.mult, op1=ALU.add), reads=[b_u[cc], b_cwT, b_gsm], writes=[b_acc[cc]])
            for k in range(1, 31):
                S.op(eng, lambda e, k=k: e.scalar_tensor_tensor(out=acc[:, cc, :], in0=u[:, cc, k:k + 2048], scalar=cwT[:, cc, k:k + 1],
                                                               in1=acc[:, cc, :], op0=ALU.mult, op1=ALU.add),
                     reads=[b_u[cc], b_acc[cc], b_cwT], writes=[b_acc[cc]])
        for cc in range(4):
            conv_chunk(cc, "dve")
        dump("acc", acc[:, :, :], b_acc)

        S.inherit(b_attnT, [b_w_in, b_wkr])
        for i in range(2):
            S.inherit(b_qT[i], [b_w_in, b_wkr])
        for i in range(NPT):
            S.inherit(b_pT[i], [b_w_in, b_wkr])
        SCALE = 1.0 / math.sqrt(96.0)

        def qup(s):
            qb, h = divmod(s, 8)
            cols = slice(qb * 512, (qb + 1) * 512)
            qt = qT[s % 2]
            bq = b_qT[s % 2]
            S.pe([mm(bank(5)[0:96, :], wuq[:, c, h * 96:(h + 1) * 96], cqnT[:, c, cols], c == 0, c == 1) for c in range(2)],
                 reads=[b_wuq, b_cqn], writes=[PB[5]])
            S.pe([mm(bank(6)[0:96, :], wqsw[:, c, h, :], cqnT[:, c, cols], c == 0, c == 1) for c in range(2)],
                 reads=[b_wqsw, b_cqn], writes=[PB[6]])
            S.op("dve", lambda e: e.tensor_copy(out=qt[0:64, :], in_=bank(5)[0:64, :]), reads=[PB[5]], writes=[bq])
            S.op("dve", lambda e: e.tensor_tensor(out=scr[0][rp, :], in0=bank(5)[rp, :], in1=COS[rp, cols], op=ALU.mult),
                 reads=[PB[5], b_cs], writes=[b_scr[0]])
            S.op("dve", lambda e: e.tensor_tensor(out=scr[1][rp, :], in0=bank(6)[rp, :], in1=SIN[rp, cols], op=ALU.mult),
                 reads=[PB[6], b_cs], writes=[b_scr[1]])
            S.op("dve", lambda e: e.tensor_tensor(out=qt[rp, :], in0=scr[0][rp, :], in1=scr[1][rp, :], op=ALU.add),
                 reads=[b_scr[0], b_scr[1]], writes=[bq])

        pctr = [0]

        def attn_step(s):
            qb, h = divmod(s, 8)
            pair, odd = divmod(h, 2)
            cols = slice(qb * 512, (qb + 1) * 512)
            qt = qT[s % 2]
            bq = b_qT[s % 2]
            ob = 3 + (s % 2)
            M = 128 if odd else 65

            def lhsV(kt):
                return Vaug[:, kt, pair, 64:192] if odd else Vaug[:, kt, pair, 0:65]

            def Smm(kt):
                bk = kt % 3
                S.pe([mm(bank(bk), kT[0:96, h, kt * 128:(kt + 1) * 128], qt[0:96, :])], reads=[b_kT, bq], writes=[PB[bk]])

            def EXPV(kt):
                pi = pctr[0] % NPT
                pctr[0] += 1
                p = pTt[pi]
                S.op("act", lambda e: e.activation(out=p[:], in_=bank(kt % 3), func=AF.Exp, scale=SCALE), reads=[PB[kt % 3]], writes=[b_pT[pi]])
                if kt + 2 < 16:
                    Smm(kt + 2)
                S.pe([mm(bank(ob)[0:M, :], lhsV(kt), p[:], kt == 0, kt == 15)], reads=[b_V, b_pT[pi]], writes=[PB[ob]])
            Smm(0)
            Smm(1)
            for kt in range(16):
                EXPV(kt)
            if odd:
                dr, orow = slice(0, 1), slice(64, 128)
                lo = onesf[0:1, 0:128]
                bo = bank(7)
            else:
                dr, orow = slice(64, 65), slice(0, 64)
                lo = onesf[64:65, 0:64]
                bo = bank(7)[0:64, :]
            S.op("dve", lambda e: e.reciprocal(out=scr[2][dr, :], in_=bank(ob)[dr, :]), reads=[PB[ob]], writes=[b_scr[2]])
            S.pe([mm(bo, lo, scr[2][dr, :])], reads=[b_onesf, b_scr[2]], writes=[PB[7]])
            S.op("dve", lambda e: e.tensor_copy(out=scr[3][orow, :], in_=bank(ob)[orow, :]), reads=[PB[ob]], writes=[b_scr[3]])
            S.op("dve", lambda e: e.tensor_tensor(out=attnT[orow, pair, cols], in0=scr[3][orow, :], in1=bank(7)[orow, :], op=ALU.mult),
                 reads=[b_scr[3], PB[7]], writes=[b_attnT])

        NSTEP = 32
        qup(0)
        for s in range(NSTEP):
            if s + 1 < NSTEP:
                qup(s + 1)
            attn_step(s)
        dump("attnT", attnT[:, :, :], [b_attnT])

        S.inherit(b_convT, [b_kr, b_ckvn])

        def conv_ln(tb):
            cols = slice(tb * 512, (tb + 1) * 512)
            S.pe([mm(bank(0), onesf[:], acc[:, cc, cols], cc == 0, cc == 3) for cc in range(4)], reads=[b_onesf] + b_acc, writes=[PB[0]])
            for cc in range(4):
                S.op("act", lambda e, cc=cc: e.activation(out=scr[3][:], in_=acc[:, cc, cols], func=AF.Square), reads=[b_acc[cc]], writes=[b_scr[3]])
                S.pe([mm(bank(1), onesf[:], scr[3][:], cc == 0, cc == 3)], reads=[b_onesf, b_scr[3]], writes=[PB[1]])
            S.op("dve", lambda e: e.tensor_scalar(out=scr[0][:], in0=bank(0), scalar1=1.0 / 512.0, scalar2=None, op0=ALU.mult),
                 reads=[PB[0]], writes=[b_scr[0]])
            S.op("dve", lambda e: e.tensor_tensor(out=scr[1][:], in0=scr[0][:], in1=scr[0][:], op=ALU.mult), reads=[b_scr[0]], writes=[b_scr[1]])
            S.op("dve", lambda e: e.scalar_tensor_tensor(out=scr[1][:], in0=bank(1), scalar=1.0 / 512.0, in1=scr[1][:], op0=ALU.mult, op1=ALU.subtract),
                 reads=[PB[1], b_scr[1]], writes=[b_scr[1]])
            S.op("act", lambda e: e.activation(out=scr[1][:], in_=scr[1][:], func=AF.Sqrt, bias=cst[:, 2:3], scale=1.0), reads=[b_scr[1], b_cst], writes=[b_scr[1]])
            S.op("dve", lambda e: e.reciprocal(out=scr[1][:], in_=scr[1][:]), reads=[b_scr[1]], writes=[b_scr[1]])
            for cc in range(4):
                S.op("dve", lambda e, cc=cc: e.tensor_tensor(out=scr[2][:], in0=acc[:, cc, cols], in1=scr[0][:], op=ALU.subtract),
                     reads=[b_acc[cc], b_scr[0]], writes=[b_scr[2]])
                S.op("dve", lambda e: e.tensor_tensor(out=scr[2][:], in0=scr[2][:], in1=scr[1][:], op=ALU.mult), reads=[b_scr[2], b_scr[1]], writes=[b_scr[2]])
                S.op("act", lambda e, cc=cc: e.activation(out=convT[:, cc, cols], in_=scr[2][:], func=AF.Silu, scale=gsm[:, 8 + cc:9 + cc], bias=gsm[:, 12 + cc:13 + cc]),
                     reads=[b_scr[2], b_gsm], writes=[b_convT])
        for tb in range(4):
            conv_ln(tb)
        dump("convT", convT[:, :, :], [b_convT])

        lnp = T([128, 2, D], F32, R6); b_lnp = Buf("lnp")
        S.inherit(b_lnp, [b_cs])
        ysb = T([128, D], F32, R7); junk = T([128, D], F32, R7 + 4096)
        b_ysb = Buf("ysb"); b_junk = Buf("junk")
        S.inherit(b_ysb, b_scr); S.inherit(b_junk, b_scr)
        b_st = [Buf("st0"), Buf("st1")]

        def load_lnp(i):
            S.dma("sp", lambda e: e.dma_start(out=lnp[:, 0, :], in_=ln_g[i].broadcast_to([128, D])), b_lnp)
            S.dma("sp", lambda e: e.dma_start(out=lnp[:, 1, :], in_=ln_b[i].broadcast_to([128, D])), b_lnp)

        def ln_tile(j, xin_ap, xin_bufs, pk, tpk, extra=None):
            so = (j % 2) * 8
            bs = b_st[j % 2]
            st_ = lambda a: stat[:, so + a:so + a + 1]
            pbs = [PB[2 * pk], PB[2 * pk + 1]]
            S.op("dve", lambda e: e.scalar_tensor_tensor(out=ysb[:], in0=xin_ap, scalar=ALPHA, in1=PP[pk][:], op0=ALU.mult, op1=ALU.add),
                 reads=pbs + xin_bufs, writes=[b_ysb])
            S.op("dve", lambda e: e.memset(stat[:, so:so + 2], 0.0), writes=[bs])
            S.op("act", lambda e: e.activation(out=junk[:], in_=ysb[:], func=AF.Copy, accum_out=st_(0)), reads=[b_ysb], writes=[b_junk, bs])
            S.op("act", lambda e: e.activation(out=junk[:], in_=ysb[:], func=AF.Square, accum_out=st_(1)), reads=[b_ysb], writes=[b_junk, bs])
            S.op("dve", lambda e: e.tensor_scalar(out=st_(2), in0=st_(0), scalar1=1.0 / D, scalar2=None, op0=ALU.mult), reads=[bs], writes=[bs])
            S.op("dve", lambda e: e.tensor_tensor(out=st_(3), in0=st_(2), in1=st_(2), op=ALU.mult), reads=[bs], writes=[bs])
            S.op("dve", lambda e: e.scalar_tensor_tensor(out=st_(3), in0=st_(1), scalar=1.0 / D, in1=st_(3), op0=ALU.mult, op1=ALU.subtract),
                 reads=[bs], writes=[bs])
            S.op("act", lambda e: e.activation(out=st_(4), in_=st_(3), func=AF.Sqrt, bias=cst[:, 2:3], scale=1.0), reads=[bs, b_cst], writes=[bs])
            S.op("dve", lambda e: e.reciprocal(out=st_(4), in_=st_(4)), reads=[bs], writes=[bs])
            S.op("dve", lambda e: e.scalar_tensor_tensor(out=st_(5), in0=st_(2), scalar=-1.0, in1=st_(4), op0=ALU.mult, op1=ALU.mult), reads=[bs], writes=[bs])
            S.op("act", lambda e: e.activation(out=ysb[:], in_=ysb[:], func=AF.Identity, scale=st_(4), bias=st_(5)), reads=[b_ysb, bs], writes=[b_ysb])
            S.op("pool", lambda e: e.tensor_tensor(out=ysb[:], in0=ysb[:], in1=lnp[:, 0, :], op=ALU.mult), reads=[b_ysb, b_lnp], writes=[b_ysb])
            S.op("pool", lambda e: e.tensor_tensor(out=resid[:, j, :], in0=ysb[:], in1=lnp[:, 1, :], op=ALU.add), reads=[b_ysb, b_lnp], writes=[b_res[j]])
            if extra is not None:
                extra(j)
            tpb = [PB[2 * tpk], PB[2 * tpk + 1]]
            S.pe([lambda e, c=c: e.transpose(PP[tpk][:, c * 128:(c + 1) * 128], resid[:, j, c * 128:(c + 1) * 128], ident[:]) for c in range(8)],
                 reads=[b_res[j], b_ident], writes=tpb)
            evac(actT[:, :, j * 128:(j + 1) * 128], PP[tpk][:].rearrange("p (c t) -> p c t", t=128), tpb, [b_actT[j // 4]])

        wo_sb = T([128, 8, D], BF16, R3); b_wo = Buf("wo")
        S.inherit(b_wo, [b_V])
        w_o_v = w_o.rearrange("(c p) f -> p c f", p=128)
        for c0 in range(0, 8, 4):
            S.dma("pool", lambda e, c0=c0: e.dma_start(out=wo_sb[:, c0:c0 + 4, :], in_=w_o_v[:, c0:c0 + 4, :]), b_wo)
        load_lnp(0)
        xst2 = [T([128, D], F32, R4 + i * 4096) for i in range(2)]; b_xst2 = [Buf("xs2_%d" % i) for i in range(2)]
        for i in range(2):
            S.inherit(b_xst2[i], [b_cqn])
        for j in range(NT):
            S.inherit(b_res[j], b_u + b_acc + b_xst + [b_pos])
        for t in range(4):
            S.inherit(b_actT[t], [b_kT])

        def wo_tile(j):
            st = j % 2
            S.dma("sp", lambda e: e.dma_start(out=xst2[st][:], in_=xv[j]), b_xst2[st])
            tsl = slice(j * 128, (j + 1) * 128)
            for half in range(2):
                hs = slice(half * 512, (half + 1) * 512)
                fns = [mm(bank(half), attnT[:, pr, tsl], wo_sb[:, pr, hs], pr == 0, False) for pr in range(4)]
                fns += [mm(bank(half), convT[:, cc, tsl], wo_sb[:, 4 + cc, hs], False, cc == 3) for cc in range(4)]
                S.pe(fns, reads=[b_attnT, b_convT, b_wo], writes=[PB[half]])
            ln_tile(j, xst2[st][:], [b_xst2[st]], 0, 1)
        for j in range(NT):
            wo_tile(j)
        dump("h1", resid[:, :, :], b_res)
        if stop_after == "mixer":
            S.wait_bufs("sp", [b_dbg])
            S.emit()
            return nc

        phaseA = [b_attnT, b_convT, b_wo, b_cqn, b_kr, b_ckvn, b_w_in, b_wkr, b_V, b_wuq, b_wqsw, b_wuk, b_wuv] + b_qT + b_pT + b_xst2
        B0 = R2
        xw = {}
        bxw = {}
        for i, k in enumerate("qokv"):
            xw[k] = T([128, 8, D], BF16, B0 + i * 16384)
            bxw[k] = Buf("xw" + k)
            S.inherit(bxw[k], phaseA)
        xqT = [T([128, 2, 512], BF16, B0 + 65536 + i * 2048) for i in range(2)]; b_xq = [Buf("xq%d" % i) for i in range(2)]
        xvv = T([128, 2, D], BF16, B0 + 69632); b_xv = Buf("xv")
        pT2 = [T([128, 512], BF16, B0 + 73728 + i * 1024) for i in range(2)]; b_p2 = [Buf("p2_%d" % i) for i in range(2)]
        oT = T([128, 8, 512], BF16, R8); b_oT = Buf("oT")
        TAIL = CEND + 32
        memT = T([128, 8, 256], BF16, TAIL); b_memT = Buf("memT")
        xkT = T([128, 8, 256], BF16, TAIL + 4096); b_xk = Buf("xkT")
        assert TAIL + 8192 <= 229376
        for bb in b_xq + [b_xv] + b_p2 + [b_oT]:
            S.inherit(bb, phaseA)
        for k in "kvqo":
            wv_ = xa_w[k].rearrange("(c p) f -> p c f", p=128)
            for c0 in range(0, 8, 4):
                S.dma("pool", lambda e, k=k, c0=c0, wv_=wv_: e.dma_start(out=xw[k][:, c0:c0 + 4, :], in_=wv_[:, c0:c0 + 4, :]), bxw[k])
        load_lnp(1)
        memv = mem.rearrange("(j p) d -> j p d", p=128)

        def mem_tile(mt):
            S.dma("sp", lambda e: e.dma_start(out=ysb[:], in_=memv[mt]), b_ysb)
            S.pe([lambda e, c=c: e.transpose(PP[1][:, c * 128:(c + 1) * 128], ysb[:, c * 128:(c + 1) * 128], ident[:]) for c in range(8)],
                 reads=[b_ysb, b_ident], writes=[PB[2], PB[3]])
            evac(memT[:, :, mt * 128:(mt + 1) * 128], PP[1][:].rearrange("p (c t) -> p c t", t=128), [PB[2], PB[3]], [b_memT])
        for mt in range(2):
            mem_tile(mt)

        def xk_fc(fc):
            bk = fc % 2
            S.pe([mm(bank(bk)[:, 0:256], xw["k"][:, c, fc * 128:(fc + 1) * 128], memT[:, c, :], c == 0, c == 7) for c in range(8)],
                 reads=[bxw["k"], b_memT], writes=[PB[bk]])
            evac(xkT[:, fc, :], bank(bk)[:, 0:256], [PB[bk]], [b_xk])
        for fc in range(8):
            xk_fc(fc)

        def xv_mh(mt, half):
            bk = 2 + (mt * 2 + half) % 2
            hs = slice(half * 512, (half + 1) * 512)
            S.pe([mm(bank(bk), memT[:, c, mt * 128:(mt + 1) * 128], xw["v"][:, c, hs], c == 0, c == 7) for c in range(8)],
                 reads=[bxw["v"], b_memT], writes=[PB[bk]])
            evac(xvv[:, mt, hs], bank(bk), [PB[bk]], [b_xv])
        for mt in range(2):
            for half in range(2):
                xv_mh(mt, half)

        h2tm = T([128, NT, D], BF16, B0 + 32768); b_h2tm = Buf("h2tm")
        S.inherit(b_h2tm, [bxw["k"], bxw["v"]])

        def xa_head(tb, hh):
            cols = slice(tb * 512, (tb + 1) * 512)
            xq = xqT[hh % 2]
            bq = b_xq[hh % 2]
            for i in range(2):
                fc = 2 * hh + i
                S.pe([mm(bank(i), xw["q"][:, c, fc * 128:(fc + 1) * 128], actT[:, c, cols], c == 0, c == 7) for c in range(8)],
                     reads=[bxw["q"], b_actT[tb]], writes=[PB[i]])
                evac(xq[:, i, :], bank(i), [PB[i]], [bq])
            for mt in range(2):
                S.pe([mm(bank(2 + mt), xkT[:, 2 * hh + i, mt * 128:(mt + 1) * 128], xq[:, i, :], i == 0, i == 1) for i in range(2)],
                     reads=[b_xk, bq], writes=[PB[2 + mt]])
                S.op("act", lambda e, mt=mt: e.activation(out=pT2[mt][:], in_=bank(2 + mt), func=AF.Exp, scale=1.0 / 16.0),
                     reads=[PB[2 + mt]], writes=[b_p2[mt]])
            S.pe([mm(bank(4), onesb[:], pT2[mt][:], mt == 0, mt == 1) for mt in range(2)], reads=[b_onesb] + b_p2, writes=[PB[4]])
            S.op("dve", lambda e: e.reciprocal(out=junk[:, 0:512], in_=bank(4)), reads=[PB[4]], writes=[b_junk])
            for dc in range(2):
                S.pe([mm(bank(5 + dc), xvv[:, mt, hh * 256 + dc * 128:hh * 256 + (dc + 1) * 128], pT2[mt][:], mt == 0, mt == 1) for mt in range(2)],
                     reads=[b_xv] + b_p2, writes=[PB[5 + dc]])
                S.op("dve", lambda e, dc=dc: e.tensor_tensor(out=oT[:, hh * 2 + dc, :], in0=bank(5 + dc), in1=junk[:, 0:512], op=ALU.mult),
                     reads=[PB[5 + dc], b_junk], writes=[b_oT])

        def cast_h2(j):
            S.op("pool", lambda e: e.tensor_copy(out=h2tm[:, j, :], in_=resid[:, j, :]), reads=[b_res[j]], writes=[b_h2tm])

        def xa_out(tb, jj):
            j = tb * 4 + jj
            tsl = slice(jj * 128, (jj + 1) * 128)
            for half in range(2):
                hs = slice(half * 512, (half + 1) * 512)
                S.pe([mm(bank(6 + half), oT[:, fc, tsl], xw["o"][:, fc, hs], fc == 0, fc == 7) for fc in range(8)],
                     reads=[b_oT, bxw["o"]], writes=[PB[6 + half]])
            ln_tile(j, resid[:, j, :], [b_res[j]], 3, 0, extra=cast_h2)
        for tb in range(4):
            for hh in range(4):
                xa_head(tb, hh)
            for jj in range(4):
                xa_out(tb, jj)
        dump("h2", resid[:, :, :], b_res)
        if stop_after == "xattn":
            S.wait_bufs("sp", [b_dbg])
            S.emit()
            return nc

        phaseB = [bxw["q"], bxw["o"], b_xv, b_oT, b_memT, b_xk] + b_xq + b_p2
        wr_sb = T([128, 8, NE], BF16, TAIL); b_wr = Buf("wr")
        S.inherit(b_wr, [b_memT])
        with_nc = w_router.rearrange("(c p) e -> p c e", p=128)
        S.dma("pool", lambda e: e.dma_start(out=wr_sb[:], in_=with_nc), b_wr)
        esel = T([16, 16 * 128], F32, TAIL + 256); b_esel = Buf("esel")
        S.inherit(b_esel, [b_memT, b_xk])
        S.dma("sp", lambda e: e.dma_start(out=esel[:], in_=c_esel), b_esel)
        aff = T([16, S_], F32, B0); affw = [T([16, S_], F32, B0 + 8192), T([16, S_], F32, B0 + 16384)]
        b_aff = Buf("aff"); b_affw = [Buf("affw0"), Buf("affw1")]
        gate = T([16, CAP], F32, B0 + 24576); idxu = T([16, CAP], U32, B0 + 25600); idxf = T([16, CAP], F32, B0 + 26624)
        b_gate = Buf("gate"); b_idxu = Buf("idxu"); b_idxf = Buf("idxf")
        idxT = T([128, 2, NE], F32, B0 + 27648); gateT = T([128, 2, NE], F32, B0 + 27776); b_igT = Buf("igT")
        idxs = T([128, 32], F32, B0 + 27904); b_idxs = Buf("idxs")
        for bb in [b_aff, b_gate, b_idxu, b_idxf, b_igT, b_idxs] + b_affw:
            S.inherit(bb, phaseB)
        for tb in range(4):
            def router_tb(tb=tb):
                cols = slice(tb * 512, (tb + 1) * 512)
                S.pe([mm(bank(tb)[0:16, :], wr_sb[:, c, :], actT[:, c, cols], c == 0, c == 7) for c in range(8)],
                     reads=[b_wr, b_actT[tb]], writes=[PB[tb]])
                S.op("act", lambda e: e.activation(out=affw[0][:, cols], in_=bank(tb)[0:16, :], func=AF.Exp), reads=[PB[tb]], writes=[b_affw[0]])
                S.pe([mm(bank(4 + tb)[0:16, :], onesf[0:16, 0:16], affw[0][:, cols])], reads=[b_onesf, b_affw[0]], writes=[PB[4 + tb]])
                S.op("dve", lambda e: e.reciprocal(out=affw[1][:, cols], in_=bank(4 + tb)[0:16, :]), reads=[PB[4 + tb]], writes=[b_affw[1]])
                S.op("dve", lambda e: e.tensor_tensor(out=aff[:, cols], in0=affw[0][:, cols], in1=affw[1][:, cols], op=ALU.mult),
                     reads=b_affw, writes=[b_aff])
            router_tb()
        dump("aff", aff[:, :], [b_aff])
        for r in range(CAP // 8):
            def topk_round(r=r):
                src = aff if r == 0 else affw[(r - 1) % 2]
                bsrc = b_aff if r == 0 else b_affw[(r - 1) % 2]
                dst = affw[r % 2]
                sl = slice(r * 8, r * 8 + 8)
                S.op("dve", lambda e: e.max(out=gate[:, sl], in_=src[:]), reads=[bsrc], writes=[b_gate])
                S.op("dve", lambda e: e.max_index(out=idxu[:, sl], in_max=gate[:, sl], in_values=src[:]), reads=[bsrc, b_gate], writes=[b_idxu])
                if r + 1 < CAP // 8:
                    S.op("dve", lambda e: e.match_replace(out=dst[:], in_to_replace=gate[:, sl], in_values=src[:], imm_value=-1.0),
                         reads=[bsrc, b_gate], writes=[b_affw[r % 2]])
            topk_round()
        S.op("dve", lambda e: e.tensor_copy(out=idxf[:], in_=idxu[:]), reads=[b_idxu], writes=[b_idxf])
        dump("gate", gate[:, :], [b_gate])
        dump("idxf", idxf[:, :], [b_idxf])
        S.pe([lambda e, ch=ch: e.transpose(bank(0)[:, ch * 16:(ch + 1) * 16], idxf[0:16, ch * 128:(ch + 1) * 128], ident[0:16, 0:16]) for ch in range(2)]
             + [lambda e, ch=ch: e.transpose(bank(0)[:, 32 + ch * 16:32 + (ch + 1) * 16], gate[0:16, ch * 128:(ch + 1) * 128], ident[0:16, 0:16]) for ch in range(2)],
             reads=[b_idxf, b_gate, b_ident], writes=[PB[0]])
        S.op("dve", lambda e: e.tensor_copy(out=idxT[:].rearrange("p a b -> p (a b)"), in_=bank(0)[:, 0:32]), reads=[PB[0]], writes=[b_igT])
        S.op("dve", lambda e: e.tensor_copy(out=gateT[:].rearrange("p a b -> p (a b)"), in_=bank(0)[:, 32:64]), reads=[PB[0]], writes=[b_igT])

        for j in range(NT):
            S.op("pool", lambda e, j=j: e.tensor_scalar(out=resid[:, j, :], in0=resid[:, j, :], scalar1=ALPHA, scalar2=None, op0=ALU.mult),
                 reads=[b_res[j]], writes=[b_res[j]])

        C1 = R1
        SelT = [T([128, NT, CAP], BF16, C1 + i * 8192) for i in range(2)]; b_sel = [Buf("sel%d" % i) for i in range(2)]
        GRP = 4
        ysc = T([128, GRP * 2, D], BF16, C1 + 16384); b_ysc = Buf("ysc")
        for bb in b_sel + [b_ysc]:
            S.inherit(bb, b_actT)
        xsT = [T([128, 8, CAP], BF16, B0 + 65536 + i * 4096) for i in range(2)]; b_xs = [Buf("xs%d" % i) for i in range(2)]
        hidT = [T([128, 2, CAP], BF16, B0 + 73728 + i * 1024) for i in range(2)]; b_hid = [Buf("hid%d" % i) for i in range(2)]
        for bb in b_xs + b_hid:
            S.inherit(bb, phaseB)
        selsc = [T([128, GRP * 2, 128], BF16, R8 + i * 2048) for i in range(2)]; b_selsc = [Buf("selsc%d" % i) for i in range(2)]
        silu_t = T([128, 512], F32, R8 + 4096); b_silu = Buf("silu")
        idxs = [T([128, 8], F32, R8 + 6144 + i * 32) for i in range(2)]; b_idxs2 = [Buf("idxs%d" % i) for i in range(2)]
        for bb in b_selsc + [b_silu] + b_idxs2:
            S.inherit(bb, [b_oT])
        NRING = 3
        ring_off = [B0, B0 + 12288, R6]
        ringG = [T([128, 8, 256], BF16, ring_off[i]) for i in range(NRING)]
        ringU = [T([128, 8, 256], BF16, ring_off[i] + 4096) for i in range(NRING)]
        ringD = [T([128, 2, D], BF16, ring_off[i] + 8192) for i in range(NRING)]
        b_ring = [Buf("ring%d" % i) for i in range(NRING)]
        for bb in b_ring[0:2]:
            S.inherit(bb, [b_aff] + b_affw + phaseB)
        S.inherit(b_ring[2], [b_lnp, b_ysb, b_junk])

        def load_unit(n):
            e, fb = divmod(n, 8)
            sl = n % NRING
            fs = slice(fb * 256, (fb + 1) * 256)
            S.dma("pool", lambda e_: e_.dma_start(out=ringG[sl][:], in_=w_gate[e].rearrange("(c p) f -> p c f", p=128)[:, :, fs]), b_ring[sl])
            S.dma("pool", lambda e_: e_.dma_start(out=ringU[sl][:], in_=w_up[e].rearrange("(c p) f -> p c f", p=128)[:, :, fs]), b_ring[sl])
            S.dma("pool", lambda e_: e_.dma_start(out=ringD[sl][:], in_=w_down[e][fb * 256:(fb + 1) * 256, :].rearrange("(c p) d -> p c d", p=128)), b_ring[sl])

        def gather_expert(e):
            st = SelT[e % 2]
            bs_ = b_sel[e % 2]
            xs = xsT[e % 2]
            S.pe([mm(bank(3)[:, 0:CAP], esel[0:16, e * 128:(e + 1) * 128], idxf[0:16, :])], reads=[b_esel, b_idxf], writes=[PB[3]])
            S.op("dve", lambda e_: e_.tensor_tensor(out=st[:], in0=bank(3)[:, 0:CAP].unsqueeze(1).broadcast_to([128, NT, CAP]),
                                                    in1=tokid[:, :].unsqueeze(2).broadcast_to([128, NT, CAP]), op=ALU.is_equal),
                 reads=[PB[3], b_tokid], writes=[bs_])
            for ps_ in range(2):
                for dcl in range(4):
                    dc = ps_ * 4 + dcl
                    bk = dcl // 2
                    co = (dcl % 2) * 256
                    S.pe([mm(bank(bk)[:, co:co + 256], h2tm[:, j, dc * 128:(dc + 1) * 128], st[:, j, :], j == 0, j == NT - 1) for j in range(NT)],
                         reads=[b_h2tm, bs_], writes=[PB[bk]])
                evac(xs[:, ps_ * 4:(ps_ + 1) * 4, :], PP[0][:].rearrange("p (d c) -> p d c", c=CAP), [PB[0], PB[1]], [b_xs[e % 2]])

        def ffn_unit(n):
            e, fb = divmod(n, 8)
            sl = n % NRING
            xs = xsT[e % 2]
            hd = hidT[n % 2]
            for (bk, W) in ((2, ringG[sl]), (3, ringU[sl])):
                fns = []
                for fl in range(2):
                    fns += [mm(bank(bk)[:, fl * 256:(fl + 1) * 256], W[:, dc, fl * 128:(fl + 1) * 128], xs[:, dc, :], dc == 0, dc == 7) for dc in range(8)]
                S.pe(fns, reads=[b_ring[sl], b_xs[e % 2]], writes=[PB[bk]])
            S.op("act", lambda e_: e_.activation(out=silu_t[:], in_=bank(2), func=AF.Silu), reads=[PB[2]], writes=[b_silu])
            S.op("dve", lambda e_: e_.tensor_tensor(out=hd[:].rearrange("p a b -> p (a b)"), in0=silu_t[:], in1=bank(3), op=ALU.mult),
                 reads=[b_silu, PB[3]], writes=[b_hid[n % 2]])
            for cc2 in range(2):
                for half in range(2):
                    bk = 4 + cc2 * 2 + half
                    S.pe([mm(bank(bk), hd[:, fl, cc2 * 128:(cc2 + 1) * 128], ringD[sl][:, fl, half * 512:(half + 1) * 512],
                             fb == 0 and fl == 0, fb == 7 and fl == 1) for fl in range(2)],
                         reads=[b_hid[n % 2], b_ring[sl]], writes=[PB[bk]])

        def finish_expert(e):
            for cc2 in range(2):
                S.op("act", lambda e_, cc2=cc2: e_.activation(out=ysc[:, (e % GRP) * 2 + cc2, :], in_=PP[2 + cc2][:], func=AF.Copy,
                                                              scale=gateT[:, cc2, e:e + 1]),
                     reads=[PB[4 + 2 * cc2], PB[5 + 2 * cc2], b_igT], writes=[b_ysc])

        def scatter_group(g):
            def sc_tile(j):
                ix = idxs[j % 2]
                bi = b_idxs2[j % 2]
                sc = selsc[j % 2]
                bsc = b_selsc[j % 2]
                S.op("dve", lambda e_: e_.tensor_scalar(out=ix[:, 0:8].rearrange("p (e c) -> p e c", c=2),
                                                        in0=idxT[:, :, g * GRP:(g + 1) * GRP].rearrange("p c e -> p e c"),
                                                        scalar1=-128.0 * j, scalar2=None, op0=ALU.add), reads=[b_igT], writes=[bi])
                S.op("dve", lambda e_: e_.tensor_tensor(out=sc[:], in0=ix[:, 0:8].unsqueeze(2).broadcast_to([128, 8, 128]),
                                                        in1=iota[:, :].unsqueeze(1).broadcast_to([128, 8, 128]), op=ALU.is_equal),
                     reads=[bi, b_iota], writes=[bsc])
                for half in range(2):
                    S.pe([mm(bank(half), sc[:, k, :], ysc[:, k, half * 512:(half + 1) * 512], k == 0, k == 7) for k in range(8)],
                         reads=[bsc, b_ysc], writes=[PB[half]])
                S.op("dve", lambda e_: e_.tensor_tensor(out=resid[:, j, :], in0=resid[:, j, :], in1=PP[0][:], op=ALU.add),
                     reads=[b_res[j], PB[0], PB[1]], writes=[b_res[j]])
            for j in range(NT):
                sc_tile(j)

        NEXP = NE
        for n0 in range(NRING):
            load_unit(n0)
        for e in range(NEXP):
            gather_expert(e)
            for fb in range(8):
                n = e * 8 + fb
                ffn_unit(n)
                if n + NRING < NEXP * 8:
                    load_unit(n + NRING)
            finish_expert(e)
            if e % GRP == GRP - 1:
                scatter_group(e // GRP)

        for bb in (b_lnp, b_ysb, b_junk):
            S.inherit(bb, [b_ring[2]])
        load_lnp(2)
        outv = out.rearrange("(j p) d -> j p d", p=128)
        b_out = Buf("out")

        def ln3_tile(j):
            so = (j % 2) * 8
            bs = b_st[j % 2]
            st_ = lambda a: stat[:, so + a:so + a + 1]
            S.op("dve", lambda e: e.tensor_copy(out=ysb[:], in_=resid[:, j, :]), reads=[b_res[j]], writes=[b_ysb])
            S.op("dve", lambda e: e.memset(stat[:, so:so + 2], 0.0), writes=[bs])
            S.op("act", lambda e: e.activation(out=junk[:], in_=ysb[:], func=AF.Copy, accum_out=st_(0)), reads=[b_ysb], writes=[b_junk, bs])
            S.op("act", lambda e: e.activation(out=junk[:], in_=ysb[:], func=AF.Square, accum_out=st_(1)), reads=[b_ysb], writes=[b_junk, bs])
            S.op("dve", lambda e: e.tensor_scalar(out=st_(2), in0=st_(0), scalar1=1.0 / D, scalar2=None, op0=ALU.mult), reads=[bs], writes=[bs])
            S.op("dve", lambda e: e.tensor_tensor(out=st_(3), in0=st_(2), in1=st_(2), op=ALU.mult), reads=[bs], writes=[bs])
            S.op("dve", lambda e: e.scalar_tensor_tensor(out=st_(3), in0=st_(1), scalar=1.0 / D, in1=st_(3), op0=ALU.mult, op1=ALU.subtract),
                 reads=[bs], writes=[bs])
            S.op("act", lambda e: e.activation(out=st_(4), in_=st_(3), func=AF.Sqrt, bias=cst[:, 2:3], scale=1.0), reads=[bs, b_cst], writes=[bs])
            S.op("dve", lambda e: e.reciprocal(out=st_(4), in_=st_(4)), reads=[bs], writes=[bs])
            S.op("dve", lambda e: e.scalar_tensor_tensor(out=st_(5), in0=st_(2), scalar=-1.0, in1=st_(4), op0=ALU.mult, op1=ALU.mult), reads=[bs], writes=[bs])
            S.op("act", lambda e: e.activation(out=ysb[:], in_=ysb[:], func=AF.Identity, scale=st_(4), bias=st_(5)), reads=[b_ysb, bs], writes=[b_ysb])
            S.op("pool", lambda e: e.tensor_tensor(out=ysb[:], in0=ysb[:], in1=lnp[:, 0, :], op=ALU.mult), reads=[b_ysb, b_lnp], writes=[b_ysb])
            S.op("pool", lambda e: e.tensor_tensor(out=resid[:, j, :], in0=ysb[:], in1=lnp[:, 1, :], op=ALU.add), reads=[b_ysb, b_lnp], writes=[b_res[j]])
            S.dma("sp", lambda e: e.dma_start(out=outv[j], in_=resid[:, j, :]), b_out, reads=[b_res[j]], writes=[b_out])
        for j in range(NT):
            ln3_tile(j)
        S.wait_bufs("sp", [b_out, b_dbg])
        S.emit()
    return nc


def make_consts():
    c = {}
    c["c_ident"] = np.eye(128, dtype=np.float32)
    c["c_iota"] = np.tile(np.arange(128, dtype=np.float32)[None, :], (128, 1))
    c["c_tokid"] = (np.arange(NT, dtype=np.float32)[None, :] * 128 + np.arange(128, dtype=np.float32)[:, None]).astype(np.float32)
    es = np.zeros((16, 16, 128), np.float32)
    for e in range(16):
        es[e, e, :] = 1.0
    c["c_esel"] = es.reshape(16, 16 * 128)
    rope = np.zeros((128, 2), np.float32)
    inv_freq = (10000.0 ** (-np.arange(0, 32, 2, dtype=np.float32) / 32.0)).astype(np.float32)
    for p in range(64, 96):
        rope[p, 0] = np.float32(np.float64(inv_freq[(p - 64) % 16]) / (2.0 * np.pi))
        rope[p, 1] = -1.0 if p < 80 else 1.0
    c["c_rope"] = rope
    return c


def make_in_maps(inputs, n_cores=8):
    consts = make_consts()
    maps = []
    f = lambda a: np.ascontiguousarray(np.asarray(a))
    for b in range(n_cores):
        m = dict(consts)
        m["x"] = f(inputs["x"][b])
        m["mem"] = f(inputs["mem"][b])
        m["positions"] = f(np.asarray(inputs["positions"])[b:b + 1]).astype(np.int32)
        for k in ["w_in", "q_norm_g", "w_uq", "kv_norm_g", "w_uk", "w_uv", "conv_w", "conv_b", "conv_ln_g", "conv_ln_b",
                  "w_o", "xa_w_q", "xa_w_k", "xa_w_v", "xa_w_o", "w_router", "w_gate", "w_up", "w_down"]:
            m[k] = f(np.asarray(inputs[k])[0])
        for k in ["ln1_g", "ln1_b", "ln2_g", "ln2_b", "ln3_g", "ln3_b"]:
            m[k] = f(np.asarray(inputs[k])[0:1])
        maps.append(m)
    return maps


def kernel(**inputs):
    nc = build_program()
    maps = make_in_maps(inputs, 8)
    res = run_bass_kernel_spmd(nc, maps, core_ids=list(range(8)))
    return np.stack([r["out"] for r in res.results], axis=0).astype(np.float32)
```

```python
import math
import os
from contextlib import ExitStack

import numpy as np
import concourse.bass as bass
import concourse.mybir as mybir
from concourse.bass_utils import run_bass_kernel_spmd

F32 = mybir.dt.float32
BF16 = mybir.dt.bfloat16
I32 = mybir.dt.int32
U32 = mybir.dt.uint32
ALU = mybir.AluOpType
AF = mybir.ActivationFunctionType

S_ = 2048
D = 1024
NT = 16
EPS = 1e-5
ALPHA = 2.0 ** 0.25
NE = 16
CAP = 256
FF = 2048


class Buf:
    __slots__ = ("name", "w", "r", "sem", "cnt")

    def __init__(self, name):
        self.name = name
        self.w = None
        self.r = {}
        self.sem = None
        self.cnt = 0


class Sched:
    ENG = ["pe", "act", "dve", "pool", "sp"]

    def __init__(self, nc, es):
        self.nc = nc
        self.es = es
        self.streams = {e: [] for e in self.ENG}
        self.cnt = {e: 0 for e in self.ENG}
        self.esem = {e: es.enter_context(nc.semaphore("c_" + e)) for e in self.ENG}
        self.waited = {e: {} for e in self.ENG}
        self.nsem = len(self.ENG)

    def new_sem(self, name):
        self.nsem += 1
        return self.es.enter_context(self.nc.semaphore("d%d_%s" % (self.nsem, name)))

    def _waits(self, eng, reads, writes):
        deps = {}

        def add(s, v):
            if deps.get(s, 0) < v:
                deps[s] = v
        for b in reads:
            if b.w is not None:
                add(*b.w)
        for b in writes:
            if b.w is not None:
                add(*b.w)
            for s, v in b.r.items():
                add(s, v)
        own = self.esem[eng]
        wd = self.waited[eng]
        for s, v in deps.items():
            if s == own and eng == "pe":
                continue
            if wd.get(s, 0) < v:
                wd[s] = v
                self.streams[eng].append(("wait", s, v))

    def _commit(self, ev, reads, writes):
        s, v = ev
        for b in reads:
            if b.r.get(s, 0) < v:
                b.r[s] = v
        for b in writes:
            b.w = ev
            b.r = {}

    def op(self, eng, fn, reads=(), writes=()):
        self._waits(eng, reads, writes)
        self.cnt[eng] += 1
        s = self.esem[eng]
        self.streams[eng].append(("op", fn, s, 1))
        self._commit((s, self.cnt[eng]), reads, writes)

    def pe(self, fns, reads=(), writes=()):
        self._waits("pe", reads, writes)
        self.cnt["pe"] += 1
        s = self.esem["pe"]
        for f in fns[:-1]:
            self.streams["pe"].append(("op", f, None, 0))
        self.streams["pe"].append(("op", fns[-1], s, 1))
        self._commit((s, self.cnt["pe"]), reads, writes)

    def dma(self, eng, fn, dst, reads=(), writes=None):
        if writes is None:
            writes = (dst,)
        self._waits(eng, reads, writes)
        if dst.sem is None:
            dst.sem = self.new_sem(dst.name)
        dst.cnt += 16
        self.streams[eng].append(("op", fn, dst.sem, 16))
        self._commit((dst.sem, dst.cnt), reads, writes)

    def wait_bufs(self, eng, bufs):
        self._waits(eng, bufs, bufs)

    def inherit(self, new, olds):
        for o in olds:
            if o.w is not None:
                s, v = o.w
                if new.r.get(s, 0) < v:
                    new.r[s] = v
            for s, v in o.r.items():
                if new.r.get(s, 0) < v:
                    new.r[s] = v

    def emit(self):
        nc = self.nc
        streams = self.streams

        def run(name, eng):
            for item in streams[name]:
                if item[0] == "wait":
                    eng.wait_ge(item[1], item[2])
                else:
                    ins = item[1](eng)
                    if item[2] is not None:
                        ins.then_inc(item[2], item[3])

        with nc.Block() as block:
            @block.tensor
            def _(e):
                run("pe", e)

            @block.scalar
            def _(e):
                run("act", e)

            @block.vector
            def _(e):
                run("dve", e)

            @block.gpsimd
            def _(e):
                run("pool", e)

            @block.sync
            def _(e):
                run("sp", e)


def build_program(stop_after=None, dumps=()):
    nc = bass.Bass("TRN2", target_bir_lowering=False)

    def din(name, shape, dt=F32):
        return nc.dram_tensor(name, list(shape), dt, kind="ExternalInput").ap()

    x = din("x", [S_, D])
    mem = din("mem", [256, D])
    pos = din("positions", [1, S_], I32)
    w_in = din("w_in", [D, 1440])
    q_norm_g = din("q_norm_g", [256])
    w_uq = din("w_uq", [256, 768])
    kv_norm_g = din("kv_norm_g", [128])
    w_uk = din("w_uk", [128, 512])
    w_uv = din("w_uv", [128, 512])
    conv_w = din("conv_w", [31, 512])
    conv_b = din("conv_b", [512])
    conv_ln_g = din("conv_ln_g", [512])
    conv_ln_b = din("conv_ln_b", [512])
    w_o = din("w_o", [D, D])
    ln_g = [din("ln%d_g" % i, [1, D]) for i in (1, 2, 3)]
    ln_b = [din("ln%d_b" % i, [1, D]) for i in (1, 2, 3)]
    xa_w = {k: din("xa_w_" + k, [D, D]) for k in "qkvo"}
    w_router = din("w_router", [D, NE])
    w_gate = din("w_gate", [NE, D, FF])
    w_up = din("w_up", [NE, D, FF])
    w_down = din("w_down", [NE, FF, D])
    c_ident = din("c_ident", [128, 128])
    c_iota = din("c_iota", [128, 128])
    c_tokid = din("c_tokid", [128, NT])
    c_esel = din("c_esel", [16, 16 * 128])
    c_rope = din("c_rope", [128, 2])
    out = nc.dram_tensor("out", [S_, D], F32, kind="ExternalOutput").ap()
    dump_d = {}
    for (nm, shape, dt) in dumps:
        dump_d[nm] = nc.dram_tensor("dbg_" + nm, list(shape), dt, kind="ExternalOutput").ap()

    es = ExitStack()
    with es:
        S = Sched(nc, es)
        cnt = [0]

        def T(shape, dt, off, name=None):
            cnt[0] += 1
            return nc.alloc_sbuf_tensor_at("%s_%d" % (name or "t", cnt[0]), list(shape), dt, offset=off)

        b_dbg = Buf("dbg")

        def dump(nm, ap, bufs):
            if nm in dump_d:
                S.dma("sp", lambda e: e.dma_start(out=dump_d[nm], in_=ap), b_dbg, reads=bufs, writes=[b_dbg])

        R0 = 16640
        R1 = R0 + 66048
        R2 = R1 + 32768
        R3 = R2 + 26624
        R4 = R3 + 24576
        R5 = R4 + 8192
        R6 = R5 + 16384
        R7 = R6 + 8192
        R8 = R7 + 8192
        R9 = R8 + 8192
        ident = T([128, 128], F32, R9); b_ident = Buf("ident")
        identb = T([128, 128], BF16, R9 + 512); b_identb = Buf("identb")
        onesb = T([128, 128], BF16, R9 + 768); b_onesb = Buf("onesb")
        onesf = T([128, 128], F32, R9 + 1024); b_onesf = Buf("onesf")
        ropec = T([128, 2], F32, R9 + 1536); b_ropec = Buf("ropec")
        gsm = T([128, 16], F32, R9 + 1600); b_gsm = Buf("gsm")
        cwT = T([128, 4, 32], F32, R9 + 1664); b_cwT = Buf("cwT")
        iota = T([128, 128], F32, R9 + 2176); b_iota = Buf("iota")
        tokid = T([128, NT], F32, R9 + 2688); b_tokid = Buf("tokid")
        stat = T([128, 16], F32, R9 + 2752); b_stat = Buf("stat")
        cst = T([128, 8], F32, R9 + 2816); b_cst = Buf('cst')
        CEND = R9 + 2848
        assert CEND <= 229376, CEND

        PP = [nc.alloc_psum_tensor("pp%d" % i, [128, 1024], F32) for i in range(4)]
        PB = [Buf("bank%d" % i) for i in range(8)]

        def bank(k):
            return PP[k // 2][:, (k % 2) * 512:(k % 2) * 512 + 512]

        S.dma("sp", lambda e: e.dma_start(out=ident[:], in_=c_ident), b_ident)
        S.dma("sp", lambda e: e.dma_start(out=iota[:], in_=c_iota), b_iota)
        S.dma("sp", lambda e: e.dma_start(out=tokid[:], in_=c_tokid), b_tokid)
        S.dma("sp", lambda e: e.dma_start(out=ropec[:], in_=c_rope), b_ropec)
        S.op("dve", lambda e: e.tensor_copy(out=identb[:], in_=ident[:]), reads=[b_ident], writes=[b_identb])
        S.op("dve", lambda e: e.memset(onesb[:], 1.0), writes=[b_onesb])
        S.op("dve", lambda e: e.memset(onesf[:], 1.0), writes=[b_onesf])
        S.op("dve", lambda e: e.memset(cst[:, 0:1], 256.0 * EPS), writes=[b_cst])
        S.op("dve", lambda e: e.memset(cst[:, 1:2], 128.0 * EPS), writes=[b_cst])
        S.op("dve", lambda e: e.memset(cst[:, 2:3], EPS), writes=[b_cst])
        S.op("dve", lambda e: e.memset(cst[:, 3:4], 0.0), writes=[b_cst])
        with nc.allow_non_contiguous_dma(reason="tiny per-channel parameter vectors"):
            S.dma("sp", lambda e: e.dma_start(out=gsm[:, 0:2], in_=q_norm_g.rearrange("(c p) -> p c", p=128), allow_slow_non_contiguous=True), b_gsm)
            S.dma("sp", lambda e: e.dma_start(out=gsm[:, 2:3], in_=kv_norm_g.rearrange("(c p) -> p c", p=128), allow_slow_non_contiguous=True), b_gsm)
            S.dma("sp", lambda e: e.dma_start(out=gsm[:, 4:8], in_=conv_b.rearrange("(c p) -> p c", p=128), allow_slow_non_contiguous=True), b_gsm)
            S.dma("sp", lambda e: e.dma_start(out=gsm[:, 8:12], in_=conv_ln_g.rearrange("(c p) -> p c", p=128), allow_slow_non_contiguous=True), b_gsm)
            S.dma("sp", lambda e: e.dma_start(out=gsm[:, 12:16], in_=conv_ln_b.rearrange("(c p) -> p c", p=128), allow_slow_non_contiguous=True), b_gsm)
        S.op("dve", lambda e: e.tensor_scalar(out=gsm[:, 0:2], in0=gsm[:, 0:2], scalar1=16.0, scalar2=None, op0=ALU.mult),
             reads=[b_gsm], writes=[b_gsm])
        S.op("dve", lambda e: e.tensor_scalar(out=gsm[:, 2:3], in0=gsm[:, 2:3], scalar1=math.sqrt(128.0), scalar2=None, op0=ALU.mult),
             reads=[b_gsm], writes=[b_gsm])

        resid = T([128, NT, D], F32, R0); b_res = [Buf("res%d" % j) for j in range(NT)]
        u = T([128, 4, 2078], F32, R0); b_u = [Buf("u%d" % c) for c in range(4)]
        acc = T([128, 4, 2048], F32, R0 + 33248); b_acc = [Buf("acc%d" % c) for c in range(4)]
        xstage = [T([128, D], F32, R0 + 33248 + i * 4096) for i in range(2)]; b_xst = [Buf("xst%d" % i) for i in range(2)]
        actT = T([128, 8, S_], BF16, R1); b_actT = [Buf("actT%d" % t) for t in range(4)]
        kT = actT; b_kT = Buf("kT")
        w_in_sb = T([128, 8, 1440], BF16, R2); b_w_in = Buf("w_in")
        wkr = T([128, 8, 2, 96], BF16, R2 + 23040); b_wkr = Buf("wkr")
        attnT = T([128, 4, S_], BF16, R2); b_attnT = Buf("attnT")
        qT = [T([128, 512], BF16, R2 + 16384 + i * 1024) for i in range(2)]; b_qT = [Buf("qT%d" % i) for i in range(2)]
        NPT = 4
        pTt = [T([128, 512], BF16, R2 + 18432 + i * 1024) for i in range(NPT)]; b_pT = [Buf("pT%d" % i) for i in range(NPT)]
        Vaug = T([128, NT, 4, 192], BF16, R3); b_V = Buf("V")
        cqnT = T([128, 2, S_], BF16, R4); b_cqn = Buf("cqn")
        convT = T([128, 4, S_], BF16, R5); b_convT = Buf("convT")
        kr = T([128, S_], BF16, R5); b_kr = Buf("kr")
        ckvnT = T([128, S_], BF16, R5 + 4096); b_ckvn = Buf("ckvn")
        COS = T([128, S_], BF16, R6); SIN = T([128, S_], BF16, R6 + 4096); b_cs = Buf("cossin")
        scr = [T([128, 512], F32, R7 + i * 2048) for i in range(4)]; b_scr = [Buf("scr%d" % i) for i in range(4)]
        wuq = T([128, 2, 768], BF16, R8); b_wuq = Buf("wuq")
        wqsw = T([128, 2, 8, 96], BF16, R8 + 3072); b_wqsw = Buf("wqsw")
        wuk = T([128, 512], BF16, R8 + 6144); b_wuk = Buf("wuk")
        wuv = T([128, 512], BF16, R8 + 7168); b_wuv = Buf("wuv")

        w_in_v = w_in.rearrange("(c p) f -> p c f", p=128)
        for c0 in range(0, 8, 4):
            S.dma("pool", lambda e, c0=c0: e.dma_start(out=w_in_sb[:, c0:c0 + 4, :], in_=w_in_v[:, c0:c0 + 4, :]), b_w_in)
        S.op("pool", lambda e: e.memset(wkr[:], 0.0), writes=[b_wkr])
        S.op("pool", lambda e: e.memset(wqsw[:], 0.0), writes=[b_wqsw])
        with nc.allow_non_contiguous_dma(reason="rope column permutation"):
            S.dma("pool", lambda e: e.dma_start(out=wkr[:, :, 0, 64:96], in_=w_in_v[:, :, 384:416], allow_slow_non_contiguous=True), b_wkr)
            S.dma("pool", lambda e: e.dma_start(out=wkr[:, :, 1, 64:80], in_=w_in_v[:, :, 400:416], allow_slow_non_contiguous=True), b_wkr)
            S.dma("pool", lambda e: e.dma_start(out=wkr[:, :, 1, 80:96], in_=w_in_v[:, :, 384:400], allow_slow_non_contiguous=True), b_wkr)
            w_uq_v = w_uq.rearrange("(c p) (h f) -> p c h f", p=128, f=96)
            for c in range(2):
                S.dma("pool", lambda e, c=c: e.dma_start(out=wqsw[:, c, :, 64:80], in_=w_uq_v[:, c, :, 80:96], allow_slow_non_contiguous=True), b_wqsw)
                S.dma("pool", lambda e, c=c: e.dma_start(out=wqsw[:, c, :, 80:96], in_=w_uq_v[:, c, :, 64:80], allow_slow_non_contiguous=True), b_wqsw)
        S.dma("pool", lambda e: e.dma_start(out=wuq[:], in_=w_uq.rearrange("(c p) f -> p c f", p=128)), b_wuq)
        S.dma("pool", lambda e: e.dma_start(out=wuk[:], in_=w_uk), b_wuk)
        S.dma("pool", lambda e: e.dma_start(out=wuv[:], in_=w_uv), b_wuv)

        cw_st = scr[0]
        S.dma("sp", lambda e: e.dma_start(out=cw_st[0:31, 0:512], in_=conv_w), b_scr[0])
        S.pe([lambda e, cc=cc: e.transpose(bank(0)[:, cc * 32:cc * 32 + 31], cw_st[0:31, cc * 128:(cc + 1) * 128], ident[0:31, 0:31])
              for cc in range(4)], reads=[b_scr[0], b_ident], writes=[PB[0]])
        S.op("dve", lambda e: e.tensor_copy(out=cwT[:, :, 0:31], in_=bank(0)[:, 0:128].rearrange("p (c k) -> p c k", k=32)[:, :, 0:31]),
             reads=[PB[0]], writes=[b_cwT])

        posi = T([128, S_], I32, R0)
        posf = T([128, S_], F32, R0 + 8192)
        angb = T([128, S_], F32, R0 + 16384)
        b_pos = Buf("pos")
        S.dma("sp", lambda e: e.dma_start(out=posi[:], in_=pos.broadcast_to([128, S_])), b_pos)
        rp = slice(64, 96)
        S.op("dve", lambda e: e.tensor_copy(out=posf[rp, :], in_=posi[rp, :]), reads=[b_pos], writes=[b_pos])
        kint = T([128, S_], I32, R0 + 24576)
        sinf = T([128, S_], F32, R0 + 24576)

        def trig(phase, dst_fn):
            S.op("dve", lambda e: e.tensor_scalar(out=angb[rp, :], in0=posf[rp, :], scalar1=ropec[rp, 0:1], scalar2=phase,
                                                  op0=ALU.mult, op1=ALU.add), reads=[b_pos, b_ropec], writes=[b_pos])
            S.op("dve", lambda e: e.tensor_copy(out=kint[rp, :], in_=angb[rp, :]), reads=[b_pos], writes=[b_pos])
            S.op("dve", lambda e: e.tensor_copy(out=posi[rp, :].bitcast(F32), in_=kint[rp, :]), reads=[b_pos], writes=[b_pos])
            S.op("dve", lambda e: e.tensor_tensor(out=angb[rp, :], in0=angb[rp, :], in1=posi[rp, :].bitcast(F32), op=ALU.subtract),
                 reads=[b_pos], writes=[b_pos])
            S.op("dve", lambda e: e.scalar_tensor_tensor(out=angb[rp, :], in0=angb[rp, :], scalar=0.5, in1=angb[rp, :],
                                                         op0=ALU.is_gt, op1=ALU.subtract), reads=[b_pos], writes=[b_pos])
            dst_fn()

        def fin_sin():
            S.op("act", lambda e: e.activation(out=sinf[rp, :], in_=angb[rp, :], func=AF.Sin, scale=-2.0 * math.pi), reads=[b_pos], writes=[b_pos])
            S.op("dve", lambda e: e.tensor_scalar(out=SIN[rp, :], in0=sinf[rp, :], scalar1=ropec[rp, 1:2], scalar2=None, op0=ALU.mult),
                 reads=[b_pos, b_ropec], writes=[b_cs])

        def fin_cos():
            S.op("act", lambda e: e.activation(out=COS[rp, :], in_=angb[rp, :], func=AF.Sin, scale=-2.0 * math.pi), reads=[b_pos], writes=[b_cs])
        trig(0.0, fin_sin)
        trig(0.25, fin_cos)
        for c in range(4):
            S.inherit(b_u[c], [b_pos])

        xv = x.rearrange("(j p) d -> j p d", p=128)
        for j in range(NT):
            st = j % 2
            S.dma("sp", lambda e, j=j, st=st: e.dma_start(out=xstage[st][:], in_=xv[j]), b_xst[st])
            pb = [PB[(j % 2) * 2], PB[(j % 2) * 2 + 1]]
            pt = PP[j % 2]
            S.pe([lambda e, c=c, st=st, pt=pt: e.transpose(pt[:, c * 128:(c + 1) * 128], xstage[st][:, c * 128:(c + 1) * 128], ident[:])
                  for c in range(8)], reads=[b_xst[st], b_ident], writes=pb)
            eng = "act" if j % 2 == 0 else "dve"
            if eng == "act":
                S.op("act", lambda e, j=j, pt=pt: e.activation(out=actT[:, :, j * 128:(j + 1) * 128],
                                                             in_=pt[:].rearrange("p (c t) -> p c t", t=128), func=AF.Copy),
                     reads=pb, writes=[b_actT[j // 4]])
            else:
                S.op("dve", lambda e, j=j, pt=pt: e.tensor_copy(out=actT[:, :, j * 128:(j + 1) * 128],
                                                              in_=pt[:].rearrange("p (c t) -> p c t", t=128)),
                     reads=pb, writes=[b_actT[j // 4]])
        dump("xT", actT[:, :, :], b_actT)

        S.op("pool", lambda e: e.memset(u[:, :, 0:15], 0.0), writes=b_u)
        S.op("pool", lambda e: e.memset(u[:, :, 2063:2078], 0.0), writes=b_u)
        for c in range(4):
            S.inherit(b_acc[c], b_xst)

        def inproj_group(bk, lhs_fn, tb, M):
            cols = slice(tb * 512, (tb + 1) * 512)
            S.pe([lambda e, c=c: e.matmul(bank(bk)[0:M, :], lhsT=lhs_fn(c), rhs=actT[:, c, cols], start=(c == 0), stop=(c == 7))
                  for c in range(8)], reads=[b_w_in, b_wkr, b_actT[tb]], writes=[PB[bk]])

        def inproj_tb(tb):
            cols = slice(tb * 512, (tb + 1) * 512)
            for c2 in range(2):
                inproj_group(c2, lambda c, c2=c2: w_in_sb[:, c, c2 * 128:(c2 + 1) * 128], tb, 128)
                S.op("act", lambda e, c2=c2: e.activation(out=scr[c2][:].bitcast(BF16)[:, 0:512], in_=bank(c2), func=AF.Square),
                     reads=[PB[c2]], writes=[b_scr[c2]])
            S.pe([lambda e, c2=c2: e.matmul(bank(2), lhsT=onesb[:], rhs=scr[c2][:].bitcast(BF16)[:, 0:512], start=(c2 == 0), stop=(c2 == 1))
                  for c2 in range(2)], reads=[b_onesb, b_scr[0], b_scr[1]], writes=[PB[2]])
            S.op("act", lambda e: e.activation(out=scr[2][:], in_=bank(2), func=AF.Sqrt, bias=cst[:, 0:1], scale=1.0),
                 reads=[PB[2], b_cst], writes=[b_scr[2]])
            S.op("dve", lambda e: e.reciprocal(out=scr[2][:], in_=scr[2][:]), reads=[b_scr[2]], writes=[b_scr[2]])
            for c2 in range(2):
                S.op("dve", lambda e, c2=c2: e.scalar_tensor_tensor(out=cqnT[:, c2, cols], in0=bank(c2), scalar=gsm[:, c2:c2 + 1], in1=scr[2][:],
                                                                   op0=ALU.mult, op1=ALU.mult),
                     reads=[PB[c2], b_gsm, b_scr[2]], writes=[b_cqn])
            inproj_group(3, lambda c: w_in_sb[:, c, 256:384], tb, 128)
            S.op("act", lambda e: e.activation(out=scr[3][:].bitcast(BF16)[:, 0:512], in_=bank(3), func=AF.Square),
                 reads=[PB[3]], writes=[b_scr[3]])
            S.pe([lambda e: e.matmul(bank(2), lhsT=onesb[:], rhs=scr[3][:].bitcast(BF16)[:, 0:512], start=True, stop=True)],
                 reads=[b_onesb, b_scr[3]], writes=[PB[2]])
            S.op("act", lambda e: e.activation(out=scr[2][:], in_=bank(2), func=AF.Sqrt, bias=cst[:, 1:2], scale=1.0),
                 reads=[PB[2], b_cst], writes=[b_scr[2]])
            S.op("dve", lambda e: e.reciprocal(out=scr[2][:], in_=scr[2][:]), reads=[b_scr[2]], writes=[b_scr[2]])
            S.op("dve", lambda e: e.scalar_tensor_tensor(out=ckvnT[:, cols], in0=bank(3), scalar=gsm[:, 2:3], in1=scr[2][:],
                                                         op0=ALU.mult, op1=ALU.mult),
                 reads=[PB[3], b_gsm, b_scr[2]], writes=[b_ckvn])
            inproj_group(4, lambda c: wkr[:, c, 0, :], tb, 96)
            inproj_group(5, lambda c: wkr[:, c, 1, :], tb, 96)
            S.op("dve", lambda e: e.tensor_tensor(out=scr[0][rp, :], in0=bank(4)[rp, :], in1=COS[rp, cols], op=ALU.mult),
                 reads=[PB[4], b_cs], writes=[b_scr[0]])
            S.op("dve", lambda e: e.tensor_tensor(out=scr[1][rp, :], in0=bank(5)[rp, :], in1=SIN[rp, cols], op=ALU.mult),
                 reads=[PB[5], b_cs], writes=[b_scr[1]])
            S.op("dve", lambda e: e.tensor_tensor(out=kr[rp, cols], in0=scr[0][rp, :], in1=scr[1][rp, :], op=ALU.add),
                 reads=[b_scr[0], b_scr[1]], writes=[b_kr])
            for cc in range(4):
                inproj_group(6, lambda c, cc=cc: w_in_sb[:, c, 416 + cc * 128:416 + (cc + 1) * 128], tb, 128)
                inproj_group(7, lambda c, cc=cc: w_in_sb[:, c, 928 + cc * 128:928 + (cc + 1) * 128], tb, 128)
                S.op("act", lambda e: e.activation(out=scr[3][:], in_=bank(7), func=AF.Sigmoid), reads=[PB[7]], writes=[b_scr[3]])
                S.op("dve", lambda e, cc=cc: e.tensor_tensor(out=u[:, cc, 15 + tb * 512:15 + (tb + 1) * 512], in0=bank(6), in1=scr[3][:], op=ALU.mult),
                     reads=[PB[6], b_scr[3]], writes=[b_u[cc]])
        for tb in range(4):
            inproj_tb(tb)
        dump("cqnT", cqnT[:, :, :], [b_cqn])
        dump("ckvnT", ckvnT[:, :], [b_ckvn])
        dump("kr", kr[64:96, :], [b_kr])
        dump("u", u[:, :, 15:2063], b_u)
        if stop_after == "inproj":
            S.wait_bufs("sp", [b_dbg])
            S.emit()
            return nc


        def mm(o, l, r, st=True, sp=True):
            return lambda e: e.matmul(o, lhsT=l, rhs=r, start=st, stop=sp)

        evc = [0]

        def evac(out_ap, in_ap, reads, writes):
            evc[0] += 1
            if evc[0] % 2 == 0:
                S.op("act", lambda e: e.activation(out=out_ap, in_=in_ap, func=AF.Copy), reads=reads, writes=writes)
            else:
                S.op("dve", lambda e: e.tensor_copy(out=out_ap, in_=in_ap), reads=reads, writes=writes)

        S.inherit(b_kT, b_actT)

        def kup(h, tb):
            cols = slice(tb * 512, (tb + 1) * 512)
            bk = (h * 4 + tb) % 4
            S.pe([mm(bank(bk)[0:64, :], wuk[:, h * 64:(h + 1) * 64], ckvnT[:, cols])], reads=[b_wuk, b_ckvn], writes=[PB[bk]])
            evac(kT[0:64, h, cols], bank(bk)[0:64, :], [PB[bk]], [b_kT])
        for h in range(8):
            for tb in range(4):
                kup(h, tb)

        def krb(tb):
            cols = slice(tb * 512, (tb + 1) * 512)
            S.op("pool", lambda e: e.tensor_copy(out=kT[64:96, :, cols], in_=kr[64:96, cols].unsqueeze(1).broadcast_to([32, 8, 512])),
                 reads=[b_kr], writes=[b_kT])
        for tb in range(4):
            krb(tb)
        S.op("pool", lambda e: e.memset(Vaug[:, :, :, 64:128], 0.0), writes=[b_V])
        S.op("pool", lambda e: e.memset(Vaug[:, :, :, 64:65], 1.0), writes=[b_V])

        def vup(j):
            bk = 4 + (j % 4)
            S.pe([mm(bank(bk), ckvnT[:, j * 128:(j + 1) * 128], wuv[:])], reads=[b_wuv, b_ckvn], writes=[PB[bk]])
            pv = bank(bk).rearrange("p (q t d) -> p q t d", t=2, d=64)
            S.op("act", lambda e: e.activation(out=Vaug[:, j, :, 0:64], in_=pv[:, :, 0, :], func=AF.Copy), reads=[PB[bk]], writes=[b_V])
            S.op("dve", lambda e: e.tensor_copy(out=Vaug[:, j, :, 128:192], in_=pv[:, :, 1, :]), reads=[PB[bk]], writes=[b_V])
        for j in range(NT):
            vup(j)
        dump("kT", kT[0:96, :, :], [b_kT])
        dump("V", Vaug[:, :, :, :], [b_V])

        conv_todo = []

        def conv_chunk(cc, eng):
            conv_todo.append(lambda: S.op(eng, lambda e: e.tensor_scalar(out=acc[:, cc, :], in0=u[:, cc, 0:2048], scalar1=cwT[:, cc, 0:1],
                                                                         scalar2=gsm[:, 4 + cc:5 + cc], op0=ALU.mult, op1=ALU.add),
                                          reads=[b_u[cc], b_cwT, b_gsm], writes=[b_acc[cc]]))
            for k in range(1, 31):
                conv_todo.append(lambda k=k: S.op(eng, lambda e: e.scalar_tensor_tensor(out=acc[:, cc, :], in0=u[:, cc, k:k + 2048],
                                                                                        scalar=cwT[:, cc, k:k + 1], in1=acc[:, cc, :],
                                                                                        op0=ALU.mult, op1=ALU.add),
                                                  reads=[b_u[cc], b_acc[cc], b_cwT], writes=[b_acc[cc]]))
        for cc in range(4):
            conv_chunk(cc, "dve")

        S.inherit(b_attnT, [b_w_in, b_wkr])
        for i in range(2):
            S.inherit(b_qT[i], [b_w_in, b_wkr])
        for i in range(NPT):
            S.inherit(b_pT[i], [b_w_in, b_wkr])
        SCALE = 1.0 / math.sqrt(96.0)

        def qup(s):
            qb, h = divmod(s, 8)
            cols = slice(qb * 512, (qb + 1) * 512)
            qt = qT[s % 2]
            bq = b_qT[s % 2]
            S.pe([mm(bank(5)[0:96, :], wuq[:, c, h * 96:(h + 1) * 96], cqnT[:, c, cols], c == 0, c == 1) for c in range(2)],
                 reads=[b_wuq, b_cqn], writes=[PB[5]])
            S.pe([mm(bank(6)[0:96, :], wqsw[:, c, h, :], cqnT[:, c, cols], c == 0, c == 1) for c in range(2)],
                 reads=[b_wqsw, b_cqn], writes=[PB[6]])
            S.op("dve", lambda e: e.tensor_copy(out=qt[0:64, :], in_=bank(5)[0:64, :]), reads=[PB[5]], writes=[bq])
            S.op("dve", lambda e: e.tensor_tensor(out=scr[0][rp, :], in0=bank(5)[rp, :], in1=COS[rp, cols], op=ALU.mult),
                 reads=[PB[5], b_cs], writes=[b_scr[0]])
            S.op("dve", lambda e: e.tensor_tensor(out=scr[1][rp, :], in0=bank(6)[rp, :], in1=SIN[rp, cols], op=ALU.mult),
                 reads=[PB[6], b_cs], writes=[b_scr[1]])
            S.op("dve", lambda e: e.tensor_tensor(out=qt[rp, :], in0=scr[0][rp, :], in1=scr[1][rp, :], op=ALU.add),
                 reads=[b_scr[0], b_scr[1]], writes=[bq])

        pctr = [0]

        def attn_step(s):
            qb, h = divmod(s, 8)
            pair, odd = divmod(h, 2)
            cols = slice(qb * 512, (qb + 1) * 512)
            qt = qT[s % 2]
            bq = b_qT[s % 2]
            ob = 3 + (s % 2)
            M = 128 if odd else 65

            def lhsV(kt):
                return Vaug[:, kt, pair, 64:192] if odd else Vaug[:, kt, pair, 0:65]

            def Smm(kt):
                bk = kt % 3
                S.pe([mm(bank(bk), kT[0:96, h, kt * 128:(kt + 1) * 128], qt[0:96, :])], reads=[b_kT, bq], writes=[PB[bk]])

            def EXPV(kt):
                pi = pctr[0] % NPT
                pctr[0] += 1
                p = pTt[pi]
                S.op("act", lambda e: e.activation(out=p[:], in_=bank(kt % 3), func=AF.Exp, scale=SCALE), reads=[PB[kt % 3]], writes=[b_pT[pi]])
                if kt + 2 < 16:
                    Smm(kt + 2)
                S.pe([mm(bank(ob)[0:M, :], lhsV(kt), p[:], kt == 0, kt == 15)], reads=[b_V, b_pT[pi]], writes=[PB[ob]])
            Smm(0)
            Smm(1)
            for kt in range(16):
                EXPV(kt)
            if odd:
                dr, orow = slice(0, 1), slice(64, 128)
                lo = onesf[0:1, 0:128]
                bo = bank(7)
            else:
                dr, orow = slice(64, 65), slice(0, 64)
                lo = onesf[64:65, 0:64]
                bo = bank(7)[0:64, :]
            S.op("dve", lambda e: e.reciprocal(out=scr[2][dr, :], in_=bank(ob)[dr, :]), reads=[PB[ob]], writes=[b_scr[2]])
            S.pe([mm(bo, lo, scr[2][dr, :])], reads=[b_onesf, b_scr[2]], writes=[PB[7]])
            S.op("dve", lambda e: e.tensor_copy(out=scr[3][orow, :], in_=bank(ob)[orow, :]), reads=[PB[ob]], writes=[b_scr[3]])
            S.op("dve", lambda e: e.tensor_tensor(out=attnT[orow, pair, cols], in0=scr[3][orow, :], in1=bank(7)[orow, :], op=ALU.mult),
                 reads=[b_scr[3], PB[7]], writes=[b_attnT])

        NSTEP = 32
        qup(0)
        for s in range(NSTEP):
            if s + 1 < NSTEP:
                qup(s + 1)
            attn_step(s)
            for _ in range(4):
                if conv_todo:
                    conv_todo.pop(0)()
        while conv_todo:
            conv_todo.pop(0)()
        dump("attnT", attnT[:, :, :], [b_attnT])

        S.inherit(b_convT, [b_kr, b_ckvn])

        def conv_ln(tb):
            cols = slice(tb * 512, (tb + 1) * 512)
            S.pe([mm(bank(0), onesf[:], acc[:, cc, cols], cc == 0, cc == 3) for cc in range(4)], reads=[b_onesf] + b_acc, writes=[PB[0]])
            for cc in range(4):
                S.op("act", lambda e, cc=cc: e.activation(out=scr[3][:], in_=acc[:, cc, cols], func=AF.Square), reads=[b_acc[cc]], writes=[b_scr[3]])
                S.pe([mm(bank(1), onesf[:], scr[3][:], cc == 0, cc == 3)], reads=[b_onesf, b_scr[3]], writes=[PB[1]])
            S.op("dve", lambda e: e.tensor_scalar(out=scr[0][:], in0=bank(0), scalar1=1.0 / 512.0, scalar2=None, op0=ALU.mult),
                 reads=[PB[0]], writes=[b_scr[0]])
            S.op("dve", lambda e: e.tensor_tensor(out=scr[1][:], in0=scr[0][:], in1=scr[0][:], op=ALU.mult), reads=[b_scr[0]], writes=[b_scr[1]])
            S.op("dve", lambda e: e.scalar_tensor_tensor(out=scr[1][:], in0=bank(1), scalar=1.0 / 512.0, in1=scr[1][:], op0=ALU.mult, op1=ALU.subtract),
                 reads=[PB[1], b_scr[1]], writes=[b_scr[1]])
            S.op("act", lambda e: e.activation(out=scr[1][:], in_=scr[1][:], func=AF.Sqrt, bias=cst[:, 2:3], scale=1.0), reads=[b_scr[1], b_cst], writes=[b_scr[1]])
            S.op("dve", lambda e: e.reciprocal(out=scr[1][:], in_=scr[1][:]), reads=[b_scr[1]], writes=[b_scr[1]])
            for cc in range(4):
                S.op("dve", lambda e, cc=cc: e.tensor_tensor(out=scr[2][:], in0=acc[:, cc, cols], in1=scr[0][:], op=ALU.subtract),
                     reads=[b_acc[cc], b_scr[0]], writes=[b_scr[2]])
                S.op("dve", lambda e: e.tensor_tensor(out=scr[2][:], in0=scr[2][:], in1=scr[1][:], op=ALU.mult), reads=[b_scr[2], b_scr[1]], writes=[b_scr[2]])
                S.op("act", lambda e, cc=cc: e.activation(out=convT[:, cc, cols], in_=scr[2][:], func=AF.Silu, scale=gsm[:, 8 + cc:9 + cc], bias=gsm[:, 12 + cc:13 + cc]),
                     reads=[b_scr[2], b_gsm], writes=[b_convT])
        for tb in range(4):
            conv_ln(tb)
        dump("convT", convT[:, :, :], [b_convT])

        lnp = T([128, 2, D], F32, R6); b_lnp = Buf("lnp")
        S.inherit(b_lnp, [b_cs])
        ysb = T([128, D], F32, R7); junk = T([128, D], F32, R7 + 4096)
        b_ysb = Buf("ysb"); b_junk = Buf("junk")
        S.inherit(b_ysb, b_scr); S.inherit(b_junk, b_scr)
        b_st = [Buf("st0"), Buf("st1")]

        def load_lnp(i):
            S.dma("sp", lambda e: e.dma_start(out=lnp[:, 0, :], in_=ln_g[i].broadcast_to([128, D])), b_lnp)
            S.dma("sp", lambda e: e.dma_start(out=lnp[:, 1, :], in_=ln_b[i].broadcast_to([128, D])), b_lnp)

        def ln_tile(j, xin_ap, xin_bufs, pk, tpk, extra=None):
            so = (j % 2) * 8
            bs = b_st[j % 2]
            st_ = lambda a: stat[:, so + a:so + a + 1]
            pbs = [PB[2 * pk], PB[2 * pk + 1]]
            S.op("dve", lambda e: e.scalar_tensor_tensor(out=ysb[:], in0=xin_ap, scalar=ALPHA, in1=PP[pk][:], op0=ALU.mult, op1=ALU.add),
                 reads=pbs + xin_bufs, writes=[b_ysb])
            S.op("dve", lambda e: e.memset(stat[:, so:so + 2], 0.0), writes=[bs])
            S.op("act", lambda e: e.activation(out=junk[:], in_=ysb[:], func=AF.Copy, accum_out=st_(0)), reads=[b_ysb], writes=[b_junk, bs])
            S.op("act", lambda e: e.activation(out=junk[:], in_=ysb[:], func=AF.Square, accum_out=st_(1)), reads=[b_ysb], writes=[b_junk, bs])
            S.op("dve", lambda e: e.tensor_scalar(out=st_(2), in0=st_(0), scalar1=1.0 / D, scalar2=None, op0=ALU.mult), reads=[bs], writes=[bs])
            S.op("dve", lambda e: e.tensor_tensor(out=st_(3), in0=st_(2), in1=st_(2), op=ALU.mult), reads=[bs], writes=[bs])
            S.op("dve", lambda e: e.scalar_tensor_tensor(out=st_(3), in0=st_(1), scalar=1.0 / D, in1=st_(3), op0=ALU.mult, op1=ALU.subtract),
                 reads=[bs], writes=[bs])
            S.op("act", lambda e: e.activation(out=st_(4), in_=st_(3), func=AF.Sqrt, bias=cst[:, 2:3], scale=1.0), reads=[bs, b_cst], writes=[bs])
            S.op("dve", lambda e: e.reciprocal(out=st_(4), in_=st_(4)), reads=[bs], writes=[bs])
            S.op("dve", lambda e: e.scalar_tensor_tensor(out=st_(5), in0=st_(2), scalar=-1.0, in1=st_(4), op0=ALU.mult, op1=ALU.mult), reads=[bs], writes=[bs])
            S.op("act", lambda e: e.activation(out=ysb[:], in_=ysb[:], func=AF.Identity, scale=st_(4), bias=st_(5)), reads=[b_ysb, bs], writes=[b_ysb])
            S.op("pool", lambda e: e.tensor_tensor(out=ysb[:], in0=ysb[:], in1=lnp[:, 0, :], op=ALU.mult), reads=[b_ysb, b_lnp], writes=[b_ysb])
            S.op("pool", lambda e: e.tensor_tensor(out=resid[:, j, :], in0=ysb[:], in1=lnp[:, 1, :], op=ALU.add), reads=[b_ysb, b_lnp], writes=[b_res[j]])
            if extra is not None:
                extra(j)
            tpb = [PB[2 * tpk], PB[2 * tpk + 1]]
            S.pe([lambda e, c=c: e.transpose(PP[tpk][:, c * 128:(c + 1) * 128], resid[:, j, c * 128:(c + 1) * 128], ident[:]) for c in range(8)],
                 reads=[b_res[j], b_ident], writes=tpb)
            evac(actT[:, :, j * 128:(j + 1) * 128], PP[tpk][:].rearrange("p (c t) -> p c t", t=128), tpb, [b_actT[j // 4]])

        wo_sb = T([128, 8, D], BF16, R3); b_wo = Buf("wo")
        S.inherit(b_wo, [b_V])
        w_o_v = w_o.rearrange("(c p) f -> p c f", p=128)
        for c0 in range(0, 8, 4):
            S.dma("pool", lambda e, c0=c0: e.dma_start(out=wo_sb[:, c0:c0 + 4, :], in_=w_o_v[:, c0:c0 + 4, :]), b_wo)
        load_lnp(0)
        xst2 = [T([128, D], F32, R4 + i * 4096) for i in range(2)]; b_xst2 = [Buf("xs2_%d" % i) for i in range(2)]
        for i in range(2):
            S.inherit(b_xst2[i], [b_cqn])
        for j in range(NT):
            S.inherit(b_res[j], b_u + b_acc + b_xst + [b_pos])
        for t in range(4):
            S.inherit(b_actT[t], [b_kT])

        def wo_tile(j):
            st = j % 2
            S.dma("sp", lambda e: e.dma_start(out=xst2[st][:], in_=xv[j]), b_xst2[st])
            tsl = slice(j * 128, (j + 1) * 128)
            for half in range(2):
                hs = slice(half * 512, (half + 1) * 512)
                fns = [mm(bank(half), attnT[:, pr, tsl], wo_sb[:, pr, hs], pr == 0, False) for pr in range(4)]
                fns += [mm(bank(half), convT[:, cc, tsl], wo_sb[:, 4 + cc, hs], False, cc == 3) for cc in range(4)]
                S.pe(fns, reads=[b_attnT, b_convT, b_wo], writes=[PB[half]])
            ln_tile(j, xst2[st][:], [b_xst2[st]], 0, 1)
        for j in range(NT):
            wo_tile(j)
        dump("h1", resid[:, :, :], b_res)
        if stop_after == "mixer":
            S.wait_bufs("sp", [b_dbg])
            S.emit()
            return nc

        phaseA = [b_attnT, b_convT, b_wo, b_cqn, b_kr, b_ckvn, b_w_in, b_wkr, b_V, b_wuq, b_wqsw, b_wuk, b_wuv] + b_qT + b_pT + b_xst2
        B0 = R2
        xw = {}
        bxw = {}
        for i, k in enumerate("qokv"):
            xw[k] = T([128, 8, D], BF16, B0 + i * 16384)
            bxw[k] = Buf("xw" + k)
            S.inherit(bxw[k], phaseA)
        xqT = [T([128, 2, 512], BF16, B0 + 65536 + i * 2048) for i in range(2)]; b_xq = [Buf("xq%d" % i) for i in range(2)]
        xvv = T([128, 2, D], BF16, B0 + 69632); b_xv = Buf("xv")
        pT2 = [T([128, 512], BF16, B0 + 73728 + i * 1024) for i in range(2)]; b_p2 = [Buf("p2_%d" % i) for i in range(2)]
        oT = T([128, 8, 512], BF16, R8); b_oT = Buf("oT")
        TAIL = CEND + 32
        memT = T([128, 8, 256], BF16, TAIL); b_memT = Buf("memT")
        xkT = T([128, 8, 256], BF16, TAIL + 4096); b_xk = Buf("xkT")
        assert TAIL + 8192 <= 229376
        for bb in b_xq + [b_xv] + b_p2 + [b_oT]:
            S.inherit(bb, phaseA)
        for k in "kvqo":
            wv_ = xa_w[k].rearrange("(c p) f -> p c f", p=128)
            for c0 in range(0, 8, 4):
                S.dma("pool", lambda e, k=k, c0=c0, wv_=wv_: e.dma_start(out=xw[k][:, c0:c0 + 4, :], in_=wv_[:, c0:c0 + 4, :]), bxw[k])
        load_lnp(1)
        memv = mem.rearrange("(j p) d -> j p d", p=128)

        def mem_tile(mt):
            S.dma("sp", lambda e: e.dma_start(out=ysb[:], in_=memv[mt]), b_ysb)
            S.pe([lambda e, c=c: e.transpose(PP[1][:, c * 128:(c + 1) * 128], ysb[:, c * 128:(c + 1) * 128], ident[:]) for c in range(8)],
                 reads=[b_ysb, b_ident], writes=[PB[2], PB[3]])
            evac(memT[:, :, mt * 128:(mt + 1) * 128], PP[1][:].rearrange("p (c t) -> p c t", t=128), [PB[2], PB[3]], [b_memT])
        for mt in range(2):
            mem_tile(mt)

        def xk_fc(fc):
            bk = fc % 2
            S.pe([mm(bank(bk)[:, 0:256], xw["k"][:, c, fc * 128:(fc + 1) * 128], memT[:, c, :], c == 0, c == 7) for c in range(8)],
                 reads=[bxw["k"], b_memT], writes=[PB[bk]])
            evac(xkT[:, fc, :], bank(bk)[:, 0:256], [PB[bk]], [b_xk])
        for fc in range(8):
            xk_fc(fc)

        def xv_mh(mt, half):
            bk = 2 + (mt * 2 + half) % 2
            hs = slice(half * 512, (half + 1) * 512)
            S.pe([mm(bank(bk), memT[:, c, mt * 128:(mt + 1) * 128], xw["v"][:, c, hs], c == 0, c == 7) for c in range(8)],
                 reads=[bxw["v"], b_memT], writes=[PB[bk]])
            evac(xvv[:, mt, hs], bank(bk), [PB[bk]], [b_xv])
        for mt in range(2):
            for half in range(2):
                xv_mh(mt, half)

        h2tm = T([128, NT, D], BF16, B0 + 32768); b_h2tm = Buf("h2tm")
        S.inherit(b_h2tm, [bxw["k"], bxw["v"]])

        def xa_head(tb, hh):
            cols = slice(tb * 512, (tb + 1) * 512)
            xq = xqT[hh % 2]
            bq = b_xq[hh % 2]
            for i in range(2):
                fc = 2 * hh + i
                S.pe([mm(bank(i), xw["q"][:, c, fc * 128:(fc + 1) * 128], actT[:, c, cols], c == 0, c == 7) for c in range(8)],
                     reads=[bxw["q"], b_actT[tb]], writes=[PB[i]])
                evac(xq[:, i, :], bank(i), [PB[i]], [bq])
            for mt in range(2):
                S.pe([mm(bank(2 + mt), xkT[:, 2 * hh + i, mt * 128:(mt + 1) * 128], xq[:, i, :], i == 0, i == 1) for i in range(2)],
                     reads=[b_xk, bq], writes=[PB[2 + mt]])
                S.op("act", lambda e, mt=mt: e.activation(out=pT2[mt][:], in_=bank(2 + mt), func=AF.Exp, scale=1.0 / 16.0),
                     reads=[PB[2 + mt]], writes=[b_p2[mt]])
            S.pe([mm(bank(4), onesb[:], pT2[mt][:], mt == 0, mt == 1) for mt in range(2)], reads=[b_onesb] + b_p2, writes=[PB[4]])
            S.op("dve", lambda e: e.reciprocal(out=junk[:, 0:512], in_=bank(4)), reads=[PB[4]], writes=[b_junk])
            for dc in range(2):
                S.pe([mm(bank(5 + dc), xvv[:, mt, hh * 256 + dc * 128:hh * 256 + (dc + 1) * 128], pT2[mt][:], mt == 0, mt == 1) for mt in range(2)],
                     reads=[b_xv] + b_p2, writes=[PB[5 + dc]])
                S.op("dve", lambda e, dc=dc: e.tensor_tensor(out=oT[:, hh * 2 + dc, :], in0=bank(5 + dc), in1=junk[:, 0:512], op=ALU.mult),
                     reads=[PB[5 + dc], b_junk], writes=[b_oT])

        def cast_h2(j):
            S.op("pool", lambda e: e.tensor_copy(out=h2tm[:, j, :], in_=resid[:, j, :]), reads=[b_res[j]], writes=[b_h2tm])

        def xa_out(tb, jj):
            j = tb * 4 + jj
            tsl = slice(jj * 128, (jj + 1) * 128)
            for half in range(2):
                hs = slice(half * 512, (half + 1) * 512)
                S.pe([mm(bank(6 + half), oT[:, fc, tsl], xw["o"][:, fc, hs], fc == 0, fc == 7) for fc in range(8)],
                     reads=[b_oT, bxw["o"]], writes=[PB[6 + half]])
            ln_tile(j, resid[:, j, :], [b_res[j]], 3, 0, extra=cast_h2)
        for tb in range(4):
            for hh in range(4):
                xa_head(tb, hh)
            for jj in range(4):
                xa_out(tb, jj)
        dump("h2", resid[:, :, :], b_res)
        if stop_after == "xattn":
            S.wait_bufs("sp", [b_dbg])
            S.emit()
            return nc

        phaseB = [bxw["q"], bxw["o"], b_xv, b_oT, b_memT, b_xk] + b_xq + b_p2
        wr_sb = T([128, 8, NE], BF16, TAIL); b_wr = Buf("wr")
        S.inherit(b_wr, [b_memT])
        with_nc = w_router.rearrange("(c p) e -> p c e", p=128)
        S.dma("pool", lambda e: e.dma_start(out=wr_sb[:], in_=with_nc), b_wr)
        esel = T([16, 16 * 128], F32, TAIL + 256); b_esel = Buf("esel")
        S.inherit(b_esel, [b_memT, b_xk])
        S.dma("sp", lambda e: e.dma_start(out=esel[:], in_=c_esel), b_esel)
        aff = T([16, S_], F32, B0); affw = [T([16, S_], F32, B0 + 8192), T([16, S_], F32, B0 + 16384)]
        b_aff = Buf("aff"); b_affw = [Buf("affw0"), Buf("affw1")]
        gate = T([16, CAP], F32, B0 + 24576); idxu = T([16, CAP], U32, B0 + 25600); idxf = T([16, CAP], F32, B0 + 26624)
        b_gate = Buf("gate"); b_idxu = Buf("idxu"); b_idxf = Buf("idxf")
        idxT = T([128, 2, NE], F32, B0 + 27648); gateT = T([128, 2, NE], F32, B0 + 27776); b_igT = Buf("igT")
        idxs = T([128, 32], F32, B0 + 27904); b_idxs = Buf("idxs")
        for bb in [b_aff, b_gate, b_idxu, b_idxf, b_igT, b_idxs] + b_affw:
            S.inherit(bb, phaseB)
        for tb in range(4):
            def router_tb(tb=tb):
                cols = slice(tb * 512, (tb + 1) * 512)
                S.pe([mm(bank(tb)[0:16, :], wr_sb[:, c, :], actT[:, c, cols], c == 0, c == 7) for c in range(8)],
                     reads=[b_wr, b_actT[tb]], writes=[PB[tb]])
                S.op("act", lambda e: e.activation(out=affw[0][:, cols], in_=bank(tb)[0:16, :], func=AF.Exp), reads=[PB[tb]], writes=[b_affw[0]])
                S.pe([mm(bank(4 + tb)[0:16, :], onesf[0:16, 0:16], affw[0][:, cols])], reads=[b_onesf, b_affw[0]], writes=[PB[4 + tb]])
                S.op("dve", lambda e: e.reciprocal(out=affw[1][:, cols], in_=bank(4 + tb)[0:16, :]), reads=[PB[4 + tb]], writes=[b_affw[1]])
                S.op("dve", lambda e: e.tensor_tensor(out=aff[:, cols], in0=affw[0][:, cols], in1=affw[1][:, cols], op=ALU.mult),
                     reads=b_affw, writes=[b_aff])
            router_tb()
        dump("aff", aff[:, :], [b_aff])
        for r in range(CAP // 8):
            def topk_round(r=r):
                src = aff if r == 0 else affw[(r - 1) % 2]
                bsrc = b_aff if r == 0 else b_affw[(r - 1) % 2]
                dst = affw[r % 2]
                sl = slice(r * 8, r * 8 + 8)
                S.op("dve", lambda e: e.max(out=gate[:, sl], in_=src[:]), reads=[bsrc], writes=[b_gate])
                S.op("dve", lambda e: e.max_index(out=idxu[:, sl], in_max=gate[:, sl], in_values=src[:]), reads=[bsrc, b_gate], writes=[b_idxu])
                if r + 1 < CAP // 8:
                    S.op("dve", lambda e: e.match_replace(out=dst[:], in_to_replace=gate[:, sl], in_values=src[:], imm_value=-1.0),
                         reads=[bsrc, b_gate], writes=[b_affw[r % 2]])
            topk_round()
        S.op("dve", lambda e: e.tensor_copy(out=idxf[:], in_=idxu[:]), reads=[b_idxu], writes=[b_idxf])
        dump("gate", gate[:, :], [b_gate])
        dump("idxf", idxf[:, :], [b_idxf])
        S.pe([lambda e, ch=ch: e.transpose(bank(0)[:, ch * 16:(ch + 1) * 16], idxf[0:16, ch * 128:(ch + 1) * 128], ident[0:16, 0:16]) for ch in range(2)]
             + [lambda e, ch=ch: e.transpose(bank(0)[:, 32 + ch * 16:32 + (ch + 1) * 16], gate[0:16, ch * 128:(ch + 1) * 128], ident[0:16, 0:16]) for ch in range(2)],
             reads=[b_idxf, b_gate, b_ident], writes=[PB[0]])
        S.op("dve", lambda e: e.tensor_copy(out=idxT[:].rearrange("p a b -> p (a b)"), in_=bank(0)[:, 0:32]), reads=[PB[0]], writes=[b_igT])
        S.op("dve", lambda e: e.tensor_copy(out=gateT[:].rearrange("p a b -> p (a b)"), in_=bank(0)[:, 32:64]), reads=[PB[0]], writes=[b_igT])

        for j in range(NT):
            S.op("pool", lambda e, j=j: e.tensor_scalar(out=resid[:, j, :], in0=resid[:, j, :], scalar1=ALPHA, scalar2=None, op0=ALU.mult),
                 reads=[b_res[j]], writes=[b_res[j]])

        C1 = R1
        SelT = [T([128, NT, CAP], BF16, C1 + i * 8192) for i in range(2)]; b_sel = [Buf("sel%d" % i) for i in range(2)]
        GRP = 4
        ysc = T([128, GRP * 2, D], BF16, C1 + 16384); b_ysc = Buf("ysc")
        for bb in b_sel + [b_ysc]:
            S.inherit(bb, b_actT)
        xsT = [T([128, 8, CAP], BF16, B0 + 65536 + i * 4096) for i in range(2)]; b_xs = [Buf("xs%d" % i) for i in range(2)]
        hidT = [T([128, 2, CAP], BF16, B0 + 73728 + i * 1024) for i in range(2)]; b_hid = [Buf("hid%d" % i) for i in range(2)]
        for bb in b_xs + b_hid:
            S.inherit(bb, phaseB)
        selsc = [T([128, GRP * 2, 128], BF16, R8 + i * 2048) for i in range(2)]; b_selsc = [Buf("selsc%d" % i) for i in range(2)]
        silu_t = T([128, 512], F32, R8 + 4096); b_silu = Buf("silu")
        idxs = [T([128, 8], F32, R8 + 6144 + i * 32) for i in range(2)]; b_idxs2 = [Buf("idxs%d" % i) for i in range(2)]
        for bb in b_selsc + [b_silu] + b_idxs2:
            S.inherit(bb, [b_oT])
        NRING = 3
        ring_off = [B0, B0 + 12288, R6]
        ringG = [T([128, 8, 256], BF16, ring_off[i]) for i in range(NRING)]
        ringU = [T([128, 8, 256], BF16, ring_off[i] + 4096) for i in range(NRING)]
        ringD = [T([128, 2, D], BF16, ring_off[i] + 8192) for i in range(NRING)]
        b_ring = [Buf("ring%d" % i) for i in range(NRING)]
        for bb in b_ring[0:2]:
            S.inherit(bb, [b_aff] + b_affw + phaseB)
        S.inherit(b_ring[2], [b_lnp, b_ysb, b_junk])

        def load_unit(n):
            e, fb = divmod(n, 8)
            sl = n % NRING
            fs = slice(fb * 256, (fb + 1) * 256)
            S.dma("pool", lambda e_: e_.dma_start(out=ringG[sl][:], in_=w_gate[e].rearrange("(c p) f -> p c f", p=128)[:, :, fs]), b_ring[sl])
            S.dma("pool", lambda e_: e_.dma_start(out=ringU[sl][:], in_=w_up[e].rearrange("(c p) f -> p c f", p=128)[:, :, fs]), b_ring[sl])
            S.dma("pool", lambda e_: e_.dma_start(out=ringD[sl][:], in_=w_down[e][fb * 256:(fb + 1) * 256, :].rearrange("(c p) d -> p c d", p=128)), b_ring[sl])

        def gather_expert(e):
            st = SelT[e % 2]
            bs_ = b_sel[e % 2]
            xs = xsT[e % 2]
            S.pe([mm(bank(3)[:, 0:CAP], esel[0:16, e * 128:(e + 1) * 128], idxf[0:16, :])], reads=[b_esel, b_idxf], writes=[PB[3]])
            S.op("dve", lambda e_: e_.tensor_tensor(out=st[:], in0=bank(3)[:, 0:CAP].unsqueeze(1).broadcast_to([128, NT, CAP]),
                                                    in1=tokid[:, :].unsqueeze(2).broadcast_to([128, NT, CAP]), op=ALU.is_equal),
                 reads=[PB[3], b_tokid], writes=[bs_])
            for ps_ in range(2):
                for dcl in range(4):
                    dc = ps_ * 4 + dcl
                    bk = dcl // 2
                    co = (dcl % 2) * 256
                    S.pe([mm(bank(bk)[:, co:co + 256], h2tm[:, j, dc * 128:(dc + 1) * 128], st[:, j, :], j == 0, j == NT - 1) for j in range(NT)],
                         reads=[b_h2tm, bs_], writes=[PB[bk]])
                evac(xs[:, ps_ * 4:(ps_ + 1) * 4, :], PP[0][:].rearrange("p (d c) -> p d c", c=CAP), [PB[0], PB[1]], [b_xs[e % 2]])

        def ffn_unit(n):
            e, fb = divmod(n, 8)
            sl = n % NRING
            xs = xsT[e % 2]
            hd = hidT[n % 2]
            for (bk, W) in ((2, ringG[sl]), (3, ringU[sl])):
                fns = []
                for fl in range(2):
                    fns += [mm(bank(bk)[:, fl * 256:(fl + 1) * 256], W[:, dc, fl * 128:(fl + 1) * 128], xs[:, dc, :], dc == 0, dc == 7) for dc in range(8)]
                S.pe(fns, reads=[b_ring[sl], b_xs[e % 2]], writes=[PB[bk]])
            S.op("act", lambda e_: e_.activation(out=silu_t[:], in_=bank(2), func=AF.Silu), reads=[PB[2]], writes=[b_silu])
            S.op("dve", lambda e_: e_.tensor_tensor(out=hd[:].rearrange("p a b -> p (a b)"), in0=silu_t[:], in1=bank(3), op=ALU.mult),
                 reads=[b_silu, PB[3]], writes=[b_hid[n % 2]])
            for cc2 in range(2):
                for half in range(2):
                    bk = 4 + cc2 * 2 + half
                    S.pe([mm(bank(bk), hd[:, fl, cc2 * 128:(cc2 + 1) * 128], ringD[sl][:, fl, half * 512:(half + 1) * 512],
                             fb == 0 and fl == 0, fb == 7 and fl == 1) for fl in range(2)],
                         reads=[b_hid[n % 2], b_ring[sl]], writes=[PB[bk]])

        def finish_expert(e):
            for cc2 in range(2):
                S.op("act", lambda e_, cc2=cc2: e_.activation(out=ysc[:, (e % GRP) * 2 + cc2, :], in_=PP[2 + cc2][:], func=AF.Copy,
                                                              scale=gateT[:, cc2, e:e + 1]),
                     reads=[PB[4 + 2 * cc2], PB[5 + 2 * cc2], b_igT], writes=[b_ysc])

        def scatter_group(g):
            def sc_tile(j):
                ix = idxs[j % 2]
                bi = b_idxs2[j % 2]
                sc = selsc[j % 2]
                bsc = b_selsc[j % 2]
                S.op("dve", lambda e_: e_.tensor_scalar(out=ix[:, 0:8].rearrange("p (e c) -> p e c", c=2),
                                                        in0=idxT[:, :, g * GRP:(g + 1) * GRP].rearrange("p c e -> p e c"),
                                                        scalar1=-128.0 * j, scalar2=None, op0=ALU.add), reads=[b_igT], writes=[bi])
                S.op("dve", lambda e_: e_.tensor_tensor(out=sc[:], in0=ix[:, 0:8].unsqueeze(2).broadcast_to([128, 8, 128]),
                                                        in1=iota[:, :].unsqueeze(1).broadcast_to([128, 8, 128]), op=ALU.is_equal),
                     reads=[bi, b_iota], writes=[bsc])
                for half in range(2):
                    S.pe([mm(bank(half), sc[:, k, :], ysc[:, k, half * 512:(half + 1) * 512], k == 0, k == 7) for k in range(8)],
                         reads=[bsc, b_ysc], writes=[PB[half]])
                S.op("dve", lambda e_: e_.tensor_tensor(out=resid[:, j, :], in0=resid[:, j, :], in1=PP[0][:], op=ALU.add),
                     reads=[b_res[j], PB[0], PB[1]], writes=[b_res[j]])
            for j in range(NT):
                sc_tile(j)

        NEXP = NE
        for n0 in range(NRING):
            load_unit(n0)
        for e in range(NEXP):
            gather_expert(e)
            for fb in range(8):
                n = e * 8 + fb
                ffn_unit(n)
                if n + NRING < NEXP * 8:
                    load_unit(n + NRING)
            finish_expert(e)
            if e % GRP == GRP - 1:
                scatter_group(e // GRP)

        for bb in (b_lnp, b_ysb, b_junk):
            S.inherit(bb, [b_ring[2]])
        load_lnp(2)
        outv = out.rearrange("(j p) d -> j p d", p=128)
        b_out = Buf("out")

        def ln3_tile(j):
            so = (j % 2) * 8
            bs = b_st[j % 2]
            st_ = lambda a: stat[:, so + a:so + a + 1]
            S.op("dve", lambda e: e.tensor_copy(out=ysb[:], in_=resid[:, j, :]), reads=[b_res[j]], writes=[b_ysb])
            S.op("dve", lambda e: e.memset(stat[:, so:so + 2], 0.0), writes=[bs])
            S.op("act", lambda e: e.activation(out=junk[:], in_=ysb[:], func=AF.Copy, accum_out=st_(0)), reads=[b_ysb], writes=[b_junk, bs])
            S.op("act", lambda e: e.activation(out=junk[:], in_=ysb[:], func=AF.Square, accum_out=st_(1)), reads=[b_ysb], writes=[b_junk, bs])
            S.op("dve", lambda e: e.tensor_scalar(out=st_(2), in0=st_(0), scalar1=1.0 / D, scalar2=None, op0=ALU.mult), reads=[bs], writes=[bs])
            S.op("dve", lambda e: e.tensor_tensor(out=st_(3), in0=st_(2), in1=st_(2), op=ALU.mult), reads=[bs], writes=[bs])
            S.op("dve", lambda e: e.scalar_tensor_tensor(out=st_(3), in0=st_(1), scalar=1.0 / D, in1=st_(3), op0=ALU.mult, op1=ALU.subtract),
                 reads=[bs], writes=[bs])
            S.op("act", lambda e: e.activation(out=st_(4), in_=st_(3), func=AF.Sqrt, bias=cst[:, 2:3], scale=1.0), reads=[bs, b_cst], writes=[bs])
            S.op("dve", lambda e: e.reciprocal(out=st_(4), in_=st_(4)), reads=[bs], writes=[bs])
            S.op("dve", lambda e: e.scalar_tensor_tensor(out=st_(5), in0=st_(2), scalar=-1.0, in1=st_(4), op0=ALU.mult, op1=ALU.mult), reads=[bs], writes=[bs])
            S.op("act", lambda e: e.activation(out=ysb[:], in_=ysb[:], func=AF.Identity, scale=st_(4), bias=st_(5)), reads=[b_ysb, bs], writes=[b_ysb])
            S.op("pool", lambda e: e.tensor_tensor(out=ysb[:], in0=ysb[:], in1=lnp[:, 0, :], op=ALU.mult), reads=[b_ysb, b_lnp], writes=[b_ysb])
            S.op("pool", lambda e: e.tensor_tensor(out=resid[:, j, :], in0=ysb[:], in1=lnp[:, 1, :], op=ALU.add), reads=[b_ysb, b_lnp], writes=[b_res[j]])
            S.dma("sp", lambda e: e.dma_start(out=outv[j], in_=resid[:, j, :]), b_out, reads=[b_res[j]], writes=[b_out])
        for j in range(NT):
            ln3_tile(j)
        S.wait_bufs("sp", [b_out, b_dbg])
        S.emit()
    return nc


def make_consts():
    c = {}
    c["c_ident"] = np.eye(128, dtype=np.float32)
    c["c_iota"] = np.tile(np.arange(128, dtype=np.float32)[None, :], (128, 1))
    c["c_tokid"] = (np.arange(NT, dtype=np.float32)[None, :] * 128 + np.arange(128, dtype=np.float32)[:, None]).astype(np.float32)
    es = np.zeros((16, 16, 128), np.float32)
    for e in range(16):
        es[e, e, :] = 1.0
    c["c_esel"] = es.reshape(16, 16 * 128)
    rope = np.zeros((128, 2), np.float32)
    inv_freq = (10000.0 ** (-np.arange(0, 32, 2, dtype=np.float32) / 32.0)).astype(np.float32)
    for p in range(64, 96):
        rope[p, 0] = np.float32(np.float64(inv_freq[(p - 64) % 16]) / (2.0 * np.pi))
        rope[p, 1] = -1.0 if p < 80 else 1.0
    c["c_rope"] = rope
    return c


def make_in_maps(inputs, n_cores=8):
    consts = make_consts()
    maps = []
    f = lambda a: np.ascontiguousarray(np.asarray(a))
    for b in range(n_cores):
        m = dict(consts)
        m["x"] = f(inputs["x"][b])
        m["mem"] = f(inputs["mem"][b])
        m["positions"] = f(np.asarray(inputs["positions"])[b:b + 1]).astype(np.int32)
        for k in ["w_in", "q_norm_g", "w_uq", "kv_norm_g", "w_uk", "w_uv", "conv_w", "conv_b", "conv_ln_g", "conv_ln_b",
                  "w_o", "xa_w_q", "xa_w_k", "xa_w_v", "xa_w_o", "w_router", "w_gate", "w_up", "w_down"]:
            m[k] = f(np.asarray(inputs[k])[0])
        for k in ["ln1_g", "ln1_b", "ln2_g", "ln2_b", "ln3_g", "ln3_b"]:
            m[k] = f(np.asarray(inputs[k])[0:1])
        maps.append(m)
    return maps


def kernel(**inputs):
    nc = build_program()
    maps = make_in_maps(inputs, 8)
    res = run_bass_kernel_spmd(nc, maps, core_ids=list(range(8)))
    return np.stack([r["out"] for r in res.results], axis=0).astype(np.float32)
```

```python
import math
import os
from contextlib import ExitStack

import numpy as np
import concourse.bass as bass
import concourse.mybir as mybir
from concourse.bass_utils import run_bass_kernel_spmd

F32 = mybir.dt.float32
BF16 = mybir.dt.bfloat16
I32 = mybir.dt.int32
U32 = mybir.dt.uint32
ALU = mybir.AluOpType
AF = mybir.ActivationFunctionType

S_ = 2048
D = 1024
NT = 16
EPS = 1e-5
ALPHA = 2.0 ** 0.25
NE = 16
CAP = 256
FF = 2048


class Buf:
    __slots__ = ("name", "w", "r", "sem", "cnt")

    def __init__(self, name):
        self.name = name
        self.w = None
        self.r = {}
        self.sem = None
        self.cnt = 0


class Sched:
    ENG = ["pe", "act", "dve", "pool", "sp"]

    def __init__(self, nc, es):
        self.nc = nc
        self.es = es
        self.streams = {e: [] for e in self.ENG}
        self.cnt = {e: 0 for e in self.ENG}
        self.esem = {e: es.enter_context(nc.semaphore("c_" + e)) for e in self.ENG}
        self.waited = {e: {} for e in self.ENG}
        self.nsem = len(self.ENG)

    def new_sem(self, name):
        self.nsem += 1
        return self.es.enter_context(self.nc.semaphore("d%d_%s" % (self.nsem, name)))

    def _waits(self, eng, reads, writes, skip_sem=None):
        deps = {}

        def add(s, v):
            if deps.get(s, 0) < v:
                deps[s] = v
        for b in reads:
            if b.w is not None:
                add(*b.w)
        for b in writes:
            if b.w is not None:
                add(*b.w)
            for s, v in b.r.items():
                add(s, v)
        own = self.esem[eng]
        wd = self.waited[eng]
        for s, v in deps.items():
            if s == own and eng == "pe":
                continue
            if skip_sem is not None and s == skip_sem:
                continue
            if wd.get(s, 0) < v:
                wd[s] = v
                self.streams[eng].append(("wait", s, v))

    def _commit(self, ev, reads, writes):
        s, v = ev
        for b in reads:
            if b.r.get(s, 0) < v:
                b.r[s] = v
        for b in writes:
            b.w = ev
            b.r = {}

    def op(self, eng, fn, reads=(), writes=()):
        self._waits(eng, reads, writes)
        self.cnt[eng] += 1
        s = self.esem[eng]
        self.streams[eng].append(("op", fn, s, 1))
        self._commit((s, self.cnt[eng]), reads, writes)

    def pe(self, fns, reads=(), writes=()):
        self._waits("pe", reads, writes)
        self.cnt["pe"] += 1
        s = self.esem["pe"]
        for f in fns[:-1]:
            self.streams["pe"].append(("op", f, None, 0))
        self.streams["pe"].append(("op", fns[-1], s, 1))
        self._commit((s, self.cnt["pe"]), reads, writes)

    def dma(self, eng, fn, dst, reads=(), writes=None):
        if writes is None:
            writes = (dst,)
        if dst.sem is None:
            dst.sem = self.new_sem(dst.name)
        self._waits(eng, reads, writes, skip_sem=dst.sem)
        dst.cnt += 16
        self.streams[eng].append(("op", fn, dst.sem, 16))
        self._commit((dst.sem, dst.cnt), reads, writes)

    def wait_bufs(self, eng, bufs):
        self._waits(eng, bufs, bufs)

    def inherit(self, new, olds):
        for o in olds:
            if o.w is not None:
                s, v = o.w
                if new.r.get(s, 0) < v:
                    new.r[s] = v
            for s, v in o.r.items():
                if new.r.get(s, 0) < v:
                    new.r[s] = v

    def emit(self):
        nc = self.nc
        streams = self.streams

        def run(name, eng):
            for item in streams[name]:
                if item[0] == "wait":
                    eng.wait_ge(item[1], item[2])
                else:
                    ins = item[1](eng)
                    if item[2] is not None:
                        ins.then_inc(item[2], item[3])

        with nc.Block() as block:
            @block.tensor
            def _(e):
                run("pe", e)

            @block.scalar
            def _(e):
                run("act", e)

            @block.vector
            def _(e):
                run("dve", e)

            @block.gpsimd
            def _(e):
                run("pool", e)

            @block.sync
            def _(e):
                run("sp", e)


def build_program(stop_after=None, dumps=()):
    nc = bass.Bass("TRN2", target_bir_lowering=False)

    def din(name, shape, dt=F32):
        return nc.dram_tensor(name, list(shape), dt, kind="ExternalInput").ap()

    x = din("x", [S_, D])
    mem = din("mem", [256, D])
    pos = din("positions", [1, S_], I32)
    w_in = din("w_in", [D, 1440])
    q_norm_g = din("q_norm_g", [256])
    w_uq = din("w_uq", [256, 768])
    kv_norm_g = din("kv_norm_g", [128])
    w_uk = din("w_uk", [128, 512])
    w_uv = din("w_uv", [128, 512])
    conv_w = din("conv_w", [31, 512])
    conv_b = din("conv_b", [512])
    conv_ln_g = din("conv_ln_g", [512])
    conv_ln_b = din("conv_ln_b", [512])
    w_o = din("w_o", [D, D])
    ln_g = [din("ln%d_g" % i, [1, D]) for i in (1, 2, 3)]
    ln_b = [din("ln%d_b" % i, [1, D]) for i in (1, 2, 3)]
    xa_w = {k: din("xa_w_" + k, [D, D]) for k in "qkvo"}
    w_router = din("w_router", [D, NE])
    w_gate = din("w_gate", [NE, D, FF])
    w_up = din("w_up", [NE, D, FF])
    w_down = din("w_down", [NE, FF, D])
    c_ident = din("c_ident", [128, 128])
    c_iota = din("c_iota", [128, 128])
    c_tokid = din("c_tokid", [128, NT])
    c_esel = din("c_esel", [16, 16 * 128])
    c_rope = din("c_rope", [128, 2])
    out = nc.dram_tensor("out", [S_, D], F32, kind="ExternalOutput").ap()
    dump_d = {}
    for (nm, shape, dt) in dumps:
        dump_d[nm] = nc.dram_tensor("dbg_" + nm, list(shape), dt, kind="ExternalOutput").ap()

    es = ExitStack()
    with es:
        S = Sched(nc, es)
        cnt = [0]

        def T(shape, dt, off, name=None):
            cnt[0] += 1
            return nc.alloc_sbuf_tensor_at("%s_%d" % (name or "t", cnt[0]), list(shape), dt, offset=off)

        b_dbg = Buf("dbg")

        def dump(nm, ap, bufs):
            if nm in dump_d:
                S.dma("sp", lambda e: e.dma_start(out=dump_d[nm], in_=ap), b_dbg, reads=bufs, writes=[b_dbg])

        R0 = 16640
        R1 = R0 + 66048
        R2 = R1 + 32768
        R3 = R2 + 26624
        R4 = R3 + 24576
        R5 = R4 + 8192
        R6 = R5 + 16384
        R7 = R6 + 8192
        R8 = R7 + 8192
        R9 = R8 + 8192
        ident = T([128, 128], F32, R9); b_ident = Buf("ident")
        identb = T([128, 128], BF16, R9 + 512); b_identb = Buf("identb")
        onesb = T([128, 128], BF16, R9 + 768); b_onesb = Buf("onesb")
        onesf = T([128, 128], F32, R9 + 1024); b_onesf = Buf("onesf")
        ropec = T([128, 2], F32, R9 + 1536); b_ropec = Buf("ropec")
        gsm = T([128, 16], F32, R9 + 1600); b_gsm = Buf("gsm")
        cwT = T([128, 4, 32], F32, R9 + 1664); b_cwT = Buf("cwT")
        iota = T([128, 128], F32, R9 + 2176); b_iota = Buf("iota")
        tokid = T([128, NT], F32, R9 + 2688); b_tokid = Buf("tokid")
        stat = T([128, 16], F32, R9 + 2752); b_stat = Buf("stat")
        cst = T([128, 8], F32, R9 + 2816); b_cst = Buf('cst')
        CEND = R9 + 2848
        assert CEND <= 229376, CEND

        PP = [nc.alloc_psum_tensor("pp%d" % i, [128, 1024], F32) for i in range(4)]
        PB = [Buf("bank%d" % i) for i in range(8)]

        def bank(k):
            return PP[k // 2][:, (k % 2) * 512:(k % 2) * 512 + 512]

        S.dma("sp", lambda e: e.dma_start(out=ident[:], in_=c_ident), b_ident)
        S.dma("sp", lambda e: e.dma_start(out=iota[:], in_=c_iota), b_iota)
        S.dma("sp", lambda e: e.dma_start(out=tokid[:], in_=c_tokid), b_tokid)
        S.dma("sp", lambda e: e.dma_start(out=ropec[:], in_=c_rope), b_ropec)
        S.op("dve", lambda e: e.tensor_copy(out=identb[:], in_=ident[:]), reads=[b_ident], writes=[b_identb])
        S.op("dve", lambda e: e.memset(onesb[:], 1.0), writes=[b_onesb])
        S.op("dve", lambda e: e.memset(onesf[:], 1.0), writes=[b_onesf])
        S.op("dve", lambda e: e.memset(cst[:, 0:1], 256.0 * EPS), writes=[b_cst])
        S.op("dve", lambda e: e.memset(cst[:, 1:2], 128.0 * EPS), writes=[b_cst])
        S.op("dve", lambda e: e.memset(cst[:, 2:3], EPS), writes=[b_cst])
        S.op("dve", lambda e: e.memset(cst[:, 3:4], 0.0), writes=[b_cst])
        with nc.allow_non_contiguous_dma(reason="tiny per-channel parameter vectors"):
            S.dma("sp", lambda e: e.dma_start(out=gsm[:, 0:2], in_=q_norm_g.rearrange("(c p) -> p c", p=128), allow_slow_non_contiguous=True), b_gsm)
            S.dma("sp", lambda e: e.dma_start(out=gsm[:, 2:3], in_=kv_norm_g.rearrange("(c p) -> p c", p=128), allow_slow_non_contiguous=True), b_gsm)
            S.dma("sp", lambda e: e.dma_start(out=gsm[:, 4:8], in_=conv_b.rearrange("(c p) -> p c", p=128), allow_slow_non_contiguous=True), b_gsm)
            S.dma("sp", lambda e: e.dma_start(out=gsm[:, 8:12], in_=conv_ln_g.rearrange("(c p) -> p c", p=128), allow_slow_non_contiguous=True), b_gsm)
            S.dma("sp", lambda e: e.dma_start(out=gsm[:, 12:16], in_=conv_ln_b.rearrange("(c p) -> p c", p=128), allow_slow_non_contiguous=True), b_gsm)
        S.op("dve", lambda e: e.tensor_scalar(out=gsm[:, 0:2], in0=gsm[:, 0:2], scalar1=16.0, scalar2=None, op0=ALU.mult),
             reads=[b_gsm], writes=[b_gsm])
        S.op("dve", lambda e: e.tensor_scalar(out=gsm[:, 2:3], in0=gsm[:, 2:3], scalar1=math.sqrt(128.0), scalar2=None, op0=ALU.mult),
             reads=[b_gsm], writes=[b_gsm])

        resid = T([128, NT, D], F32, R0); b_res = [Buf("res%d" % j) for j in range(NT)]
        u = T([128, 4, 2078], F32, R0); b_u = [Buf("u%d" % c) for c in range(4)]
        acc = T([128, 4, 2048], F32, R0 + 33248); b_acc = [Buf("acc%d" % c) for c in range(4)]
        xstage = [T([128, D], F32, R0 + 33248 + i * 4096) for i in range(2)]; b_xst = [Buf("xst%d" % i) for i in range(2)]
        actT = T([128, 8, S_], BF16, R1); b_actT = [Buf("actT%d" % t) for t in range(4)]
        kT = actT; b_kT = Buf("kT")
        w_in_sb = T([128, 8, 1440], BF16, R2); b_w_in = Buf("w_in")
        wkr = T([128, 8, 2, 96], BF16, R2 + 23040); b_wkr = Buf("wkr")
        attnT = T([128, 4, S_], BF16, R2); b_attnT = Buf("attnT")
        qT = [T([128, 512], BF16, R2 + 16384 + i * 1024) for i in range(2)]; b_qT = [Buf("qT%d" % i) for i in range(2)]
        NPT = 4
        pTt = [T([128, 512], BF16, R2 + 18432 + i * 1024) for i in range(NPT)]; b_pT = [Buf("pT%d" % i) for i in range(NPT)]
        Vaug = T([128, NT, 4, 192], BF16, R3); b_V = Buf("V")
        cqnT = T([128, 2, S_], BF16, R4); b_cqn = Buf("cqn")
        convT = T([128, 4, S_], BF16, R5); b_convT = Buf("convT")
        kr = T([128, S_], BF16, R5); b_kr = Buf("kr")
        ckvnT = T([128, S_], BF16, R5 + 4096); b_ckvn = Buf("ckvn")
        COS = T([128, S_], BF16, R6); SIN = T([128, S_], BF16, R6 + 4096); b_cs = Buf("cossin")
        scr = [T([128, 512], F32, R7 + i * 2048) for i in range(4)]; b_scr = [Buf("scr%d" % i) for i in range(4)]
        wuq = T([128, 2, 768], BF16, R8); b_wuq = Buf("wuq")
        wqsw = T([128, 2, 8, 96], BF16, R8 + 3072); b_wqsw = Buf("wqsw")
        wuk = T([128, 512], BF16, R8 + 6144); b_wuk = Buf("wuk")
        wuv = T([128, 512], BF16, R8 + 7168); b_wuv = Buf("wuv")

        w_in_v = w_in.rearrange("(c p) f -> p c f", p=128)
        for c0 in range(0, 8, 4):
            S.dma("pool", lambda e, c0=c0: e.dma_start(out=w_in_sb[:, c0:c0 + 4, :], in_=w_in_v[:, c0:c0 + 4, :]), b_w_in)
        S.op("pool", lambda e: e.memset(wkr[:], 0.0), writes=[b_wkr])
        S.op("pool", lambda e: e.memset(wqsw[:], 0.0), writes=[b_wqsw])
        with nc.allow_non_contiguous_dma(reason="rope column permutation"):
            S.dma("pool", lambda e: e.dma_start(out=wkr[:, :, 0, 64:96], in_=w_in_v[:, :, 384:416], allow_slow_non_contiguous=True), b_wkr)
            S.dma("pool", lambda e: e.dma_start(out=wkr[:, :, 1, 64:80], in_=w_in_v[:, :, 400:416], allow_slow_non_contiguous=True), b_wkr)
            S.dma("pool", lambda e: e.dma_start(out=wkr[:, :, 1, 80:96], in_=w_in_v[:, :, 384:400], allow_slow_non_contiguous=True), b_wkr)
            w_uq_v = w_uq.rearrange("(c p) (h f) -> p c h f", p=128, f=96)
            for c in range(2):
                S.dma("pool", lambda e, c=c: e.dma_start(out=wqsw[:, c, :, 64:80], in_=w_uq_v[:, c, :, 80:96], allow_slow_non_contiguous=True), b_wqsw)
                S.dma("pool", lambda e, c=c: e.dma_start(out=wqsw[:, c, :, 80:96], in_=w_uq_v[:, c, :, 64:80], allow_slow_non_contiguous=True), b_wqsw)
        S.dma("pool", lambda e: e.dma_start(out=wuq[:], in_=w_uq.rearrange("(c p) f -> p c f", p=128)), b_wuq)
        S.dma("pool", lambda e: e.dma_start(out=wuk[:], in_=w_uk), b_wuk)
        S.dma("pool", lambda e: e.dma_start(out=wuv[:], in_=w_uv), b_wuv)

        cw_st = scr[0]
        S.dma("sp", lambda e: e.dma_start(out=cw_st[0:31, 0:512], in_=conv_w), b_scr[0])
        S.pe([lambda e, cc=cc: e.transpose(bank(0)[:, cc * 32:cc * 32 + 31], cw_st[0:31, cc * 128:(cc + 1) * 128], ident[0:31, 0:31])
              for cc in range(4)], reads=[b_scr[0], b_ident], writes=[PB[0]])
        S.op("dve", lambda e: e.tensor_copy(out=cwT[:, :, 0:31], in_=bank(0)[:, 0:128].rearrange("p (c k) -> p c k", k=32)[:, :, 0:31]),
             reads=[PB[0]], writes=[b_cwT])

        posi = T([128, S_], I32, R0)
        posf = T([128, S_], F32, R0 + 8192)
        angb = T([128, S_], F32, R0 + 16384)
        b_pos = Buf("pos")
        S.dma("sp", lambda e: e.dma_start(out=posi[:], in_=pos.broadcast_to([128, S_])), b_pos)
        rp = slice(64, 96)
        S.op("dve", lambda e: e.tensor_copy(out=posf[rp, :], in_=posi[rp, :]), reads=[b_pos], writes=[b_pos])
        kint = T([128, S_], I32, R0 + 24576)
        sinf = T([128, S_], F32, R0 + 24576)

        def trig(phase, dst_fn):
            S.op("dve", lambda e: e.tensor_scalar(out=angb[rp, :], in0=posf[rp, :], scalar1=ropec[rp, 0:1], scalar2=phase,
                                                  op0=ALU.mult, op1=ALU.add), reads=[b_pos, b_ropec], writes=[b_pos])
            S.op("dve", lambda e: e.tensor_copy(out=kint[rp, :], in_=angb[rp, :]), reads=[b_pos], writes=[b_pos])
            S.op("dve", lambda e: e.tensor_copy(out=posi[rp, :].bitcast(F32), in_=kint[rp, :]), reads=[b_pos], writes=[b_pos])
            S.op("dve", lambda e: e.tensor_tensor(out=angb[rp, :], in0=angb[rp, :], in1=posi[rp, :].bitcast(F32), op=ALU.subtract),
                 reads=[b_pos], writes=[b_pos])
            S.op("dve", lambda e: e.scalar_tensor_tensor(out=angb[rp, :], in0=angb[rp, :], scalar=0.5, in1=angb[rp, :],
                                                         op0=ALU.is_gt, op1=ALU.subtract), reads=[b_pos], writes=[b_pos])
            dst_fn()

        def fin_sin():
            S.op("act", lambda e: e.activation(out=sinf[rp, :], in_=angb[rp, :], func=AF.Sin, scale=-2.0 * math.pi), reads=[b_pos], writes=[b_pos])
            S.op("dve", lambda e: e.tensor_scalar(out=SIN[rp, :], in0=sinf[rp, :], scalar1=ropec[rp, 1:2], scalar2=None, op0=ALU.mult),
                 reads=[b_pos, b_ropec], writes=[b_cs])

        def fin_cos():
            S.op("act", lambda e: e.activation(out=COS[rp, :], in_=angb[rp, :], func=AF.Sin, scale=-2.0 * math.pi), reads=[b_pos], writes=[b_cs])
        trig(0.0, fin_sin)
        trig(0.25, fin_cos)
        for c in range(4):
            S.inherit(b_u[c], [b_pos])

        xv = x.rearrange("(j p) d -> j p d", p=128)
        for j in range(NT):
            st = j % 2
            S.dma("sp", lambda e, j=j, st=st: e.dma_start(out=xstage[st][:], in_=xv[j]), b_xst[st])
            pb = [PB[(j % 2) * 2], PB[(j % 2) * 2 + 1]]
            pt = PP[j % 2]
            S.pe([lambda e, c=c, st=st, pt=pt: e.transpose(pt[:, c * 128:(c + 1) * 128], xstage[st][:, c * 128:(c + 1) * 128], ident[:])
                  for c in range(8)], reads=[b_xst[st], b_ident], writes=pb)
            eng = "act" if j % 2 == 0 else "dve"
            if eng == "act":
                S.op("act", lambda e, j=j, pt=pt: e.activation(out=actT[:, :, j * 128:(j + 1) * 128],
                                                             in_=pt[:].rearrange("p (c t) -> p c t", t=128), func=AF.Copy),
                     reads=pb, writes=[b_actT[j // 4]])
            else:
                S.op("dve", lambda e, j=j, pt=pt: e.tensor_copy(out=actT[:, :, j * 128:(j + 1) * 128],
                                                              in_=pt[:].rearrange("p (c t) -> p c t", t=128)),
                     reads=pb, writes=[b_actT[j // 4]])
        dump("xT", actT[:, :, :], b_actT)

        S.op("pool", lambda e: e.memset(u[:, :, 0:15], 0.0), writes=b_u)
        S.op("pool", lambda e: e.memset(u[:, :, 2063:2078], 0.0), writes=b_u)
        for c in range(4):
            S.inherit(b_acc[c], b_xst)

        def inproj_group(bk, lhs_fn, tb, M):
            cols = slice(tb * 512, (tb + 1) * 512)
            S.pe([lambda e, c=c: e.matmul(bank(bk)[0:M, :], lhsT=lhs_fn(c), rhs=actT[:, c, cols], start=(c == 0), stop=(c == 7))
                  for c in range(8)], reads=[b_w_in, b_wkr, b_actT[tb]], writes=[PB[bk]])

        def inproj_tb(tb):
            cols = slice(tb * 512, (tb + 1) * 512)
            for c2 in range(2):
                inproj_group(c2, lambda c, c2=c2: w_in_sb[:, c, c2 * 128:(c2 + 1) * 128], tb, 128)
                S.op("act", lambda e, c2=c2: e.activation(out=scr[c2][:].bitcast(BF16)[:, 0:512], in_=bank(c2), func=AF.Square),
                     reads=[PB[c2]], writes=[b_scr[c2]])
            S.pe([lambda e, c2=c2: e.matmul(bank(2), lhsT=onesb[:], rhs=scr[c2][:].bitcast(BF16)[:, 0:512], start=(c2 == 0), stop=(c2 == 1))
                  for c2 in range(2)], reads=[b_onesb, b_scr[0], b_scr[1]], writes=[PB[2]])
            S.op("act", lambda e: e.activation(out=scr[2][:], in_=bank(2), func=AF.Sqrt, bias=cst[:, 0:1], scale=1.0),
                 reads=[PB[2], b_cst], writes=[b_scr[2]])
            S.op("dve", lambda e: e.reciprocal(out=scr[2][:], in_=scr[2][:]), reads=[b_scr[2]], writes=[b_scr[2]])
            for c2 in range(2):
                S.op("dve", lambda e, c2=c2: e.scalar_tensor_tensor(out=cqnT[:, c2, cols], in0=bank(c2), scalar=gsm[:, c2:c2 + 1], in1=scr[2][:],
                                                                   op0=ALU.mult, op1=ALU.mult),
                     reads=[PB[c2], b_gsm, b_scr[2]], writes=[b_cqn])
            inproj_group(3, lambda c: w_in_sb[:, c, 256:384], tb, 128)
            S.op("act", lambda e: e.activation(out=scr[3][:].bitcast(BF16)[:, 0:512], in_=bank(3), func=AF.Square),
                 reads=[PB[3]], writes=[b_scr[3]])
            S.pe([lambda e: e.matmul(bank(2), lhsT=onesb[:], rhs=scr[3][:].bitcast(BF16)[:, 0:512], start=True, stop=True)],
                 reads=[b_onesb, b_scr[3]], writes=[PB[2]])
            S.op("act", lambda e: e.activation(out=scr[2][:], in_=bank(2), func=AF.Sqrt, bias=cst[:, 1:2], scale=1.0),
                 reads=[PB[2], b_cst], writes=[b_scr[2]])
            S.op("dve", lambda e: e.reciprocal(out=scr[2][:], in_=scr[2][:]), reads=[b_scr[2]], writes=[b_scr[2]])
            S.op("dve", lambda e: e.scalar_tensor_tensor(out=ckvnT[:, cols], in0=bank(3), scalar=gsm[:, 2:3], in1=scr[2][:],
                                                         op0=ALU.mult, op1=ALU.mult),
                 reads=[PB[3], b_gsm, b_scr[2]], writes=[b_ckvn])
            inproj_group(4, lambda c: wkr[:, c, 0, :], tb, 96)
            inproj_group(5, lambda c: wkr[:, c, 1, :], tb, 96)
            S.op("dve", lambda e: e.tensor_tensor(out=scr[0][rp, :], in0=bank(4)[rp, :], in1=COS[rp, cols], op=ALU.mult),
                 reads=[PB[4], b_cs], writes=[b_scr[0]])
            S.op("dve", lambda e: e.tensor_tensor(out=scr[1][rp, :], in0=bank(5)[rp, :], in1=SIN[rp, cols], op=ALU.mult),
                 reads=[PB[5], b_cs], writes=[b_scr[1]])
            S.op("dve", lambda e: e.tensor_tensor(out=kr[rp, cols], in0=scr[0][rp, :], in1=scr[1][rp, :], op=ALU.add),
                 reads=[b_scr[0], b_scr[1]], writes=[b_kr])
            for cc in range(4):
                inproj_group(6, lambda c, cc=cc: w_in_sb[:, c, 416 + cc * 128:416 + (cc + 1) * 128], tb, 128)
                inproj_group(7, lambda c, cc=cc: w_in_sb[:, c, 928 + cc * 128:928 + (cc + 1) * 128], tb, 128)
                S.op("act", lambda e: e.activation(out=scr[3][:], in_=bank(7), func=AF.Sigmoid), reads=[PB[7]], writes=[b_scr[3]])
                S.op("dve", lambda e, cc=cc: e.tensor_tensor(out=u[:, cc, 15 + tb * 512:15 + (tb + 1) * 512], in0=bank(6), in1=scr[3][:], op=ALU.mult),
                     reads=[PB[6], b_scr[3]], writes=[b_u[cc]])
        for tb in range(4):
            inproj_tb(tb)
        dump("cqnT", cqnT[:, :, :], [b_cqn])
        dump("ckvnT", ckvnT[:, :], [b_ckvn])
        dump("kr", kr[64:96, :], [b_kr])
        dump("u", u[:, :, 15:2063], b_u)
        if stop_after == "inproj":
            S.wait_bufs("sp", [b_dbg])
            S.emit()
            return nc


        def mm(o, l, r, st=True, sp=True):
            return lambda e: e.matmul(o, lhsT=l, rhs=r, start=st, stop=sp)

        evc = [0]

        def evac(out_ap, in_ap, reads, writes):
            evc[0] += 1
            if evc[0] % 2 == 0:
                S.op("act", lambda e: e.activation(out=out_ap, in_=in_ap, func=AF.Copy), reads=reads, writes=writes)
            else:
                S.op("dve", lambda e: e.tensor_copy(out=out_ap, in_=in_ap), reads=reads, writes=writes)

        S.inherit(b_kT, b_actT)

        def kup(h, tb):
            cols = slice(tb * 512, (tb + 1) * 512)
            bk = (h * 4 + tb) % 4
            S.pe([mm(bank(bk)[0:64, :], wuk[:, h * 64:(h + 1) * 64], ckvnT[:, cols])], reads=[b_wuk, b_ckvn], writes=[PB[bk]])
            evac(kT[0:64, h, cols], bank(bk)[0:64, :], [PB[bk]], [b_kT])
        for h in range(8):
            for tb in range(4):
                kup(h, tb)

        def krb(tb):
            cols = slice(tb * 512, (tb + 1) * 512)
            S.op("pool", lambda e: e.tensor_copy(out=kT[64:96, :, cols], in_=kr[64:96, cols].unsqueeze(1).broadcast_to([32, 8, 512])),
                 reads=[b_kr], writes=[b_kT])
        for tb in range(4):
            krb(tb)
        S.op("pool", lambda e: e.memset(Vaug[:, :, :, 64:128], 0.0), writes=[b_V])
        S.op("pool", lambda e: e.memset(Vaug[:, :, :, 64:65], 1.0), writes=[b_V])

        def vup(j):
            bk = 4 + (j % 4)
            S.pe([mm(bank(bk), ckvnT[:, j * 128:(j + 1) * 128], wuv[:])], reads=[b_wuv, b_ckvn], writes=[PB[bk]])
            pv = bank(bk).rearrange("p (q t d) -> p q t d", t=2, d=64)
            S.op("act", lambda e: e.activation(out=Vaug[:, j, :, 0:64], in_=pv[:, :, 0, :], func=AF.Copy), reads=[PB[bk]], writes=[b_V])
            S.op("dve", lambda e: e.tensor_copy(out=Vaug[:, j, :, 128:192], in_=pv[:, :, 1, :]), reads=[PB[bk]], writes=[b_V])
        for j in range(NT):
            vup(j)
        dump("kT", kT[0:96, :, :], [b_kT])
        dump("V", Vaug[:, :, :, :], [b_V])

        def conv_chunk(cc, eng):
            S.op(eng, lambda e: e.tensor_scalar(out=acc[:, cc, :], in0=u[:, cc, 0:2048], scalar1=cwT[:, cc, 0:1], scalar2=gsm[:, 4 + cc:5 + cc],
                                                op0=ALU.mult, op1=ALU.add), reads=[b_u[cc], b_cwT, b_gsm], writes=[b_acc[cc]])
            for k in range(1, 31):
                S.op(eng, lambda e, k=k: e.scalar_tensor_tensor(out=acc[:, cc, :], in0=u[:, cc, k:k + 2048], scalar=cwT[:, cc, k:k + 1],
                                                               in1=acc[:, cc, :], op0=ALU.mult, op1=ALU.add),
                     reads=[b_u[cc], b_acc[cc], b_cwT], writes=[b_acc[cc]])
        for cc in range(4):
            conv_chunk(cc, "dve")
        dump("acc", acc[:, :, :], b_acc)

        S.inherit(b_attnT, [b_w_in, b_wkr])
        for i in range(2):
            S.inherit(b_qT[i], [b_w_in, b_wkr])
        for i in range(NPT):
            S.inherit(b_pT[i], [b_w_in, b_wkr])
        SCALE = 1.0 / math.sqrt(96.0)

        def qup(s):
            qb, h = divmod(s, 8)
            cols = slice(qb * 512, (qb + 1) * 512)
            qt = qT[s % 2]
            bq = b_qT[s % 2]
            S.pe([mm(bank(5)[0:96, :], wuq[:, c, h * 96:(h + 1) * 96], cqnT[:, c, cols], c == 0, c == 1) for c in range(2)],
                 reads=[b_wuq, b_cqn], writes=[PB[5]])
            S.pe([mm(bank(6)[0:96, :], wqsw[:, c, h, :], cqnT[:, c, cols], c == 0, c == 1) for c in range(2)],
                 reads=[b_wqsw, b_cqn], writes=[PB[6]])
            S.op("dve", lambda e: e.tensor_copy(out=qt[0:64, :], in_=bank(5)[0:64, :]), reads=[PB[5]], writes=[bq])
            S.op("dve", lambda e: e.tensor_tensor(out=scr[0][rp, :], in0=bank(5)[rp, :], in1=COS[rp, cols], op=ALU.mult),
                 reads=[PB[5], b_cs], writes=[b_scr[0]])
            S.op("dve", lambda e: e.tensor_tensor(out=scr[1][rp, :], in0=bank(6)[rp, :], in1=SIN[rp, cols], op=ALU.mult),
                 reads=[PB[6], b_cs], writes=[b_scr[1]])
            S.op("dve", lambda e: e.tensor_tensor(out=qt[rp, :], in0=scr[0][rp, :], in1=scr[1][rp, :], op=ALU.add),
                 reads=[b_scr[0], b_scr[1]], writes=[bq])

        pctr = [0]

        def attn_step(s):
            qb, h = divmod(s, 8)
            pair, odd = divmod(h, 2)
            cols = slice(qb * 512, (qb + 1) * 512)
            qt = qT[s % 2]
            bq = b_qT[s % 2]
            ob = 3 + (s % 2)
            M = 128 if odd else 65

            def lhsV(kt):
                return Vaug[:, kt, pair, 64:192] if odd else Vaug[:, kt, pair, 0:65]

            def Smm(kt):
                bk = kt % 3
                S.pe([mm(bank(bk), kT[0:96, h, kt * 128:(kt + 1) * 128], qt[0:96, :])], reads=[b_kT, bq], writes=[PB[bk]])

            def EXPV(kt):
                pi = pctr[0] % NPT
                pctr[0] += 1
                p = pTt[pi]
                S.op("act", lambda e: e.activation(out=p[:], in_=bank(kt % 3), func=AF.Exp, scale=SCALE), reads=[PB[kt % 3]], writes=[b_pT[pi]])
                if kt + 2 < 16:
                    Smm(kt + 2)
                S.pe([mm(bank(ob)[0:M, :], lhsV(kt), p[:], kt == 0, kt == 15)], reads=[b_V, b_pT[pi]], writes=[PB[ob]])
            Smm(0)
            Smm(1)
            for kt in range(16):
                EXPV(kt)
            if odd:
                dr, orow = slice(0, 1), slice(64, 128)
                lo = onesf[0:1, 0:128]
                bo = bank(7)
            else:
                dr, orow = slice(64, 65), slice(0, 64)
                lo = onesf[64:65, 0:64]
                bo = bank(7)[0:64, :]
            S.op("dve", lambda e: e.reciprocal(out=scr[2][dr, :], in_=bank(ob)[dr, :]), reads=[PB[ob]], writes=[b_scr[2]])
            S.pe([mm(bo, lo, scr[2][dr, :])], reads=[b_onesf, b_scr[2]], writes=[PB[7]])
            S.op("dve", lambda e: e.tensor_copy(out=scr[3][orow, :], in_=bank(ob)[orow, :]), reads=[PB[ob]], writes=[b_scr[3]])
            S.op("dve", lambda e: e.tensor_tensor(out=attnT[orow, pair, cols], in0=scr[3][orow, :], in1=bank(7)[orow, :], op=ALU.mult),
                 reads=[b_scr[3], PB[7]], writes=[b_attnT])

        NSTEP = 32
        qup(0)
        for s in range(NSTEP):
            if s + 1 < NSTEP:
                qup(s + 1)
            attn_step(s)
        dump("attnT", attnT[:, :, :], [b_attnT])

        S.inherit(b_convT, [b_kr, b_ckvn])

        def conv_ln(tb):
            cols = slice(tb * 512, (tb + 1) * 512)
            S.pe([mm(bank(0), onesf[:], acc[:, cc, cols], cc == 0, cc == 3) for cc in range(4)], reads=[b_onesf] + b_acc, writes=[PB[0]])
            for cc in range(4):
                S.op("act", lambda e, cc=cc: e.activation(out=scr[3][:], in_=acc[:, cc, cols], func=AF.Square), reads=[b_acc[cc]], writes=[b_scr[3]])
                S.pe([mm(bank(1), onesf[:], scr[3][:], cc == 0, cc == 3)], reads=[b_onesf, b_scr[3]], writes=[PB[1]])
            S.op("dve", lambda e: e.tensor_scalar(out=scr[0][:], in0=bank(0), scalar1=1.0 / 512.0, scalar2=None, op0=ALU.mult),
                 reads=[PB[0]], writes=[b_scr[0]])
            S.op("dve", lambda e: e.tensor_tensor(out=scr[1][:], in0=scr[0][:], in1=scr[0][:], op=ALU.mult), reads=[b_scr[0]], writes=[b_scr[1]])
            S.op("dve", lambda e: e.scalar_tensor_tensor(out=scr[1][:], in0=bank(1), scalar=1.0 / 512.0, in1=scr[1][:], op0=ALU.mult, op1=ALU.subtract),
                 reads=[PB[1], b_scr[1]], writes=[b_scr[1]])
            S.op("act", lambda e: e.activation(out=scr[1][:], in_=scr[1][:], func=AF.Sqrt, bias=cst[:, 2:3], scale=1.0), reads=[b_scr[1], b_cst], writes=[b_scr[1]])
            S.op("dve", lambda e: e.reciprocal(out=scr[1][:], in_=scr[1][:]), reads=[b_scr[1]], writes=[b_scr[1]])
            for cc in range(4):
                S.op("dve", lambda e, cc=cc: e.tensor_tensor(out=scr[2][:], in0=acc[:, cc, cols], in1=scr[0][:], op=ALU.subtract),
                     reads=[b_acc[cc], b_scr[0]], writes=[b_scr[2]])
                S.op("dve", lambda e: e.tensor_tensor(out=scr[2][:], in0=scr[2][:], in1=scr[1][:], op=ALU.mult), reads=[b_scr[2], b_scr[1]], writes=[b_scr[2]])
                S.op("act", lambda e, cc=cc: e.activation(out=convT[:, cc, cols], in_=scr[2][:], func=AF.Silu, scale=gsm[:, 8 + cc:9 + cc], bias=gsm[:, 12 + cc:13 + cc]),
                     reads=[b_scr[2], b_gsm], writes=[b_convT])
        for tb in range(4):
            conv_ln(tb)
        dump("convT", convT[:, :, :], [b_convT])

        lnp = T([128, 2, D], F32, R6); b_lnp = Buf("lnp")
        S.inherit(b_lnp, [b_cs])
        ysb = T([128, D], F32, R7); junk = T([128, D], F32, R7 + 4096)
        b_ysb = Buf("ysb"); b_junk = Buf("junk")
        S.inherit(b_ysb, b_scr); S.inherit(b_junk, b_scr)
        b_st = [Buf("st0"), Buf("st1")]

        def load_lnp(i):
            S.dma("sp", lambda e: e.dma_start(out=lnp[:, 0, :], in_=ln_g[i].broadcast_to([128, D])), b_lnp)
            S.dma("sp", lambda e: e.dma_start(out=lnp[:, 1, :], in_=ln_b[i].broadcast_to([128, D])), b_lnp)

        def ln_tile(j, xin_ap, xin_bufs, pk, tpk, extra=None):
            so = (j % 2) * 8
            bs = b_st[j % 2]
            st_ = lambda a: stat[:, so + a:so + a + 1]
            pbs = [PB[2 * pk], PB[2 * pk + 1]]
            S.op("dve", lambda e: e.scalar_tensor_tensor(out=ysb[:], in0=xin_ap, scalar=ALPHA, in1=PP[pk][:], op0=ALU.mult, op1=ALU.add),
                 reads=pbs + xin_bufs, writes=[b_ysb])
            S.op("dve", lambda e: e.memset(stat[:, so:so + 2], 0.0), writes=[bs])
            S.op("act", lambda e: e.activation(out=junk[:], in_=ysb[:], func=AF.Copy, accum_out=st_(0)), reads=[b_ysb], writes=[b_junk, bs])
            S.op("act", lambda e: e.activation(out=junk[:], in_=ysb[:], func=AF.Square, accum_out=st_(1)), reads=[b_ysb], writes=[b_junk, bs])
            S.op("dve", lambda e: e.tensor_scalar(out=st_(2), in0=st_(0), scalar1=1.0 / D, scalar2=None, op0=ALU.mult), reads=[bs], writes=[bs])
            S.op("dve", lambda e: e.tensor_tensor(out=st_(3), in0=st_(2), in1=st_(2), op=ALU.mult), reads=[bs], writes=[bs])
            S.op("dve", lambda e: e.scalar_tensor_tensor(out=st_(3), in0=st_(1), scalar=1.0 / D, in1=st_(3), op0=ALU.mult, op1=ALU.subtract),
                 reads=[bs], writes=[bs])
            S.op("act", lambda e: e.activation(out=st_(4), in_=st_(3), func=AF.Sqrt, bias=cst[:, 2:3], scale=1.0), reads=[bs, b_cst], writes=[bs])
            S.op("dve", lambda e: e.reciprocal(out=st_(4), in_=st_(4)), reads=[bs], writes=[bs])
            S.op("dve", lambda e: e.scalar_tensor_tensor(out=st_(5), in0=st_(2), scalar=-1.0, in1=st_(4), op0=ALU.mult, op1=ALU.mult), reads=[bs], writes=[bs])
            S.op("act", lambda e: e.activation(out=ysb[:], in_=ysb[:], func=AF.Identity, scale=st_(4), bias=st_(5)), reads=[b_ysb, bs], writes=[b_ysb])
            S.op("pool", lambda e: e.tensor_tensor(out=ysb[:], in0=ysb[:], in1=lnp[:, 0, :], op=ALU.mult), reads=[b_ysb, b_lnp], writes=[b_ysb])
            S.op("pool", lambda e: e.tensor_tensor(out=resid[:, j, :], in0=ysb[:], in1=lnp[:, 1, :], op=ALU.add), reads=[b_ysb, b_lnp], writes=[b_res[j]])
            if extra is not None:
                extra(j)
            tpb = [PB[2 * tpk], PB[2 * tpk + 1]]
            S.pe([lambda e, c=c: e.transpose(PP[tpk][:, c * 128:(c + 1) * 128], resid[:, j, c * 128:(c + 1) * 128], ident[:]) for c in range(8)],
                 reads=[b_res[j], b_ident], writes=tpb)
            evac(actT[:, :, j * 128:(j + 1) * 128], PP[tpk][:].rearrange("p (c t) -> p c t", t=128), tpb, [b_actT[j // 4]])

        wo_sb = T([128, 8, D], BF16, R3); b_wo = Buf("wo")
        S.inherit(b_wo, [b_V])
        w_o_v = w_o.rearrange("(c p) f -> p c f", p=128)
        for c0 in range(0, 8, 4):
            S.dma("pool", lambda e, c0=c0: e.dma_start(out=wo_sb[:, c0:c0 + 4, :], in_=w_o_v[:, c0:c0 + 4, :]), b_wo)
        load_lnp(0)
        xst2 = [T([128, D], F32, R4 + i * 4096) for i in range(2)]; b_xst2 = [Buf("xs2_%d" % i) for i in range(2)]
        for i in range(2):
            S.inherit(b_xst2[i], [b_cqn])
        for j in range(NT):
            S.inherit(b_res[j], b_u + b_acc + b_xst + [b_pos])
        for t in range(4):
            S.inherit(b_actT[t], [b_kT])

        def wo_tile(j):
            st = j % 2
            S.dma("sp", lambda e: e.dma_start(out=xst2[st][:], in_=xv[j]), b_xst2[st])
            tsl = slice(j * 128, (j + 1) * 128)
            for half in range(2):
                hs = slice(half * 512, (half + 1) * 512)
                fns = [mm(bank(half), attnT[:, pr, tsl], wo_sb[:, pr, hs], pr == 0, False) for pr in range(4)]
                fns += [mm(bank(half), convT[:, cc, tsl], wo_sb[:, 4 + cc, hs], False, cc == 3) for cc in range(4)]
                S.pe(fns, reads=[b_attnT, b_convT, b_wo], writes=[PB[half]])
            ln_tile(j, xst2[st][:], [b_xst2[st]], 0, 1)
        for j in range(NT):
            wo_tile(j)
        dump("h1", resid[:, :, :], b_res)
        if stop_after == "mixer":
            S.wait_bufs("sp", [b_dbg])
            S.emit()
            return nc

        phaseA = [b_attnT, b_convT, b_wo, b_cqn, b_kr, b_ckvn, b_w_in, b_wkr, b_V, b_wuq, b_wqsw, b_wuk, b_wuv] + b_qT + b_pT + b_xst2
        B0 = R2
        xw = {}
        bxw = {}
        for i, k in enumerate("qokv"):
            xw[k] = T([128, 8, D], BF16, B0 + i * 16384)
            bxw[k] = Buf("xw" + k)
            S.inherit(bxw[k], phaseA)
        xqT = [T([128, 2, 512], BF16, B0 + 65536 + i * 2048) for i in range(2)]; b_xq = [Buf("xq%d" % i) for i in range(2)]
        xvv = T([128, 2, D], BF16, B0 + 69632); b_xv = Buf("xv")
        pT2 = [T([128, 512], BF16, B0 + 73728 + i * 1024) for i in range(2)]; b_p2 = [Buf("p2_%d" % i) for i in range(2)]
        oT = T([128, 8, 512], BF16, R8); b_oT = Buf("oT")
        TAIL = CEND + 32
        memT = T([128, 8, 256], BF16, TAIL); b_memT = Buf("memT")
        xkT = T([128, 8, 256], BF16, TAIL + 4096); b_xk = Buf("xkT")
        assert TAIL + 8192 <= 229376
        for bb in b_xq + [b_xv] + b_p2 + [b_oT]:
            S.inherit(bb, phaseA)
        for k in "kvqo":
            wv_ = xa_w[k].rearrange("(c p) f -> p c f", p=128)
            for c0 in range(0, 8, 4):
                S.dma("pool", lambda e, k=k, c0=c0, wv_=wv_: e.dma_start(out=xw[k][:, c0:c0 + 4, :], in_=wv_[:, c0:c0 + 4, :]), bxw[k])
        load_lnp(1)
        memv = mem.rearrange("(j p) d -> j p d", p=128)

        def mem_tile(mt):
            S.dma("sp", lambda e: e.dma_start(out=ysb[:], in_=memv[mt]), b_ysb)
            S.pe([lambda e, c=c: e.transpose(PP[1][:, c * 128:(c + 1) * 128], ysb[:, c * 128:(c + 1) * 128], ident[:]) for c in range(8)],
                 reads=[b_ysb, b_ident], writes=[PB[2], PB[3]])
            evac(memT[:, :, mt * 128:(mt + 1) * 128], PP[1][:].rearrange("p (c t) -> p c t", t=128), [PB[2], PB[3]], [b_memT])
        for mt in range(2):
            mem_tile(mt)

        def xk_fc(fc):
            bk = fc % 2
            S.pe([mm(bank(bk)[:, 0:256], xw["k"][:, c, fc * 128:(fc + 1) * 128], memT[:, c, :], c == 0, c == 7) for c in range(8)],
                 reads=[bxw["k"], b_memT], writes=[PB[bk]])
            evac(xkT[:, fc, :], bank(bk)[:, 0:256], [PB[bk]], [b_xk])
        for fc in range(8):
            xk_fc(fc)

        def xv_mh(mt, half):
            bk = 2 + (mt * 2 + half) % 2
            hs = slice(half * 512, (half + 1) * 512)
            S.pe([mm(bank(bk), memT[:, c, mt * 128:(mt + 1) * 128], xw["v"][:, c, hs], c == 0, c == 7) for c in range(8)],
                 reads=[bxw["v"], b_memT], writes=[PB[bk]])
            evac(xvv[:, mt, hs], bank(bk), [PB[bk]], [b_xv])
        for mt in range(2):
            for half in range(2):
                xv_mh(mt, half)

        h2tm = T([128, NT, D], BF16, B0 + 32768); b_h2tm = Buf("h2tm")
        S.inherit(b_h2tm, [bxw["k"], bxw["v"]])

        def xa_head(tb, hh):
            cols = slice(tb * 512, (tb + 1) * 512)
            xq = xqT[hh % 2]
            bq = b_xq[hh % 2]
            for i in range(2):
                fc = 2 * hh + i
                S.pe([mm(bank(i), xw["q"][:, c, fc * 128:(fc + 1) * 128], actT[:, c, cols], c == 0, c == 7) for c in range(8)],
                     reads=[bxw["q"], b_actT[tb]], writes=[PB[i]])
                evac(xq[:, i, :], bank(i), [PB[i]], [bq])
            for mt in range(2):
                S.pe([mm(bank(2 + mt), xkT[:, 2 * hh + i, mt * 128:(mt + 1) * 128], xq[:, i, :], i == 0, i == 1) for i in range(2)],
                     reads=[b_xk, bq], writes=[PB[2 + mt]])
                S.op("act", lambda e, mt=mt: e.activation(out=pT2[mt][:], in_=bank(2 + mt), func=AF.Exp, scale=1.0 / 16.0),
                     reads=[PB[2 + mt]], writes=[b_p2[mt]])
            S.pe([mm(bank(4), onesb[:], pT2[mt][:], mt == 0, mt == 1) for mt in range(2)], reads=[b_onesb] + b_p2, writes=[PB[4]])
            S.op("dve", lambda e: e.reciprocal(out=junk[:, 0:512], in_=bank(4)), reads=[PB[4]], writes=[b_junk])
            for dc in range(2):
                S.pe([mm(bank(5 + dc), xvv[:, mt, hh * 256 + dc * 128:hh * 256 + (dc + 1) * 128], pT2[mt][:], mt == 0, mt == 1) for mt in range(2)],
                     reads=[b_xv] + b_p2, writes=[PB[5 + dc]])
                S.op("dve", lambda e, dc=dc: e.tensor_tensor(out=oT[:, hh * 2 + dc, :], in0=bank(5 + dc), in1=junk[:, 0:512], op=ALU.mult),
                     reads=[PB[5 + dc], b_junk], writes=[b_oT])

        def cast_h2(j):
            S.op("pool", lambda e: e.tensor_copy(out=h2tm[:, j, :], in_=resid[:, j, :]), reads=[b_res[j]], writes=[b_h2tm])

        def xa_out(tb, jj):
            j = tb * 4 + jj
            tsl = slice(jj * 128, (jj + 1) * 128)
            for half in range(2):
                hs = slice(half * 512, (half + 1) * 512)
                S.pe([mm(bank(6 + half), oT[:, fc, tsl], xw["o"][:, fc, hs], fc == 0, fc == 7) for fc in range(8)],
                     reads=[b_oT, bxw["o"]], writes=[PB[6 + half]])
            ln_tile(j, resid[:, j, :], [b_res[j]], 3, 0, extra=cast_h2)
        for tb in range(4):
            for hh in range(4):
                xa_head(tb, hh)
            for jj in range(4):
                xa_out(tb, jj)
        dump("h2", resid[:, :, :], b_res)
        if stop_after == "xattn":
            S.wait_bufs("sp", [b_dbg])
            S.emit()
            return nc

        phaseB = [bxw["q"], bxw["o"], b_xv, b_oT, b_memT, b_xk] + b_xq + b_p2
        wr_sb = T([128, 8, NE], BF16, TAIL); b_wr = Buf("wr")
        S.inherit(b_wr, [b_memT])
        with_nc = w_router.rearrange("(c p) e -> p c e", p=128)
        S.dma("pool", lambda e: e.dma_start(out=wr_sb[:], in_=with_nc), b_wr)
        esel = T([16, 16 * 128], F32, TAIL + 256); b_esel = Buf("esel")
        S.inherit(b_esel, [b_memT, b_xk])
        S.dma("sp", lambda e: e.dma_start(out=esel[:], in_=c_esel), b_esel)
        aff = T([16, S_], F32, B0); affw = [T([16, S_], F32, B0 + 8192), T([16, S_], F32, B0 + 16384)]
        b_aff = Buf("aff"); b_affw = [Buf("affw0"), Buf("affw1")]
        gate = T([16, CAP], F32, B0 + 24576); idxu = T([16, CAP], U32, B0 + 25600); idxf = T([16, CAP], F32, B0 + 26624)
        b_gate = Buf("gate"); b_idxu = Buf("idxu"); b_idxf = Buf("idxf")
        idxT = T([128, 2, NE], F32, B0 + 27648); gateT = T([128, 2, NE], F32, B0 + 27776); b_igT = Buf("igT")
        idxs = T([128, 32], F32, B0 + 27904); b_idxs = Buf("idxs")
        for bb in [b_aff, b_gate, b_idxu, b_idxf, b_igT, b_idxs] + b_affw:
            S.inherit(bb, phaseB)
        for tb in range(4):
            def router_tb(tb=tb):
                cols = slice(tb * 512, (tb + 1) * 512)
                S.pe([mm(bank(tb)[0:16, :], wr_sb[:, c, :], actT[:, c, cols], c == 0, c == 7) for c in range(8)],
                     reads=[b_wr, b_actT[tb]], writes=[PB[tb]])
                S.op("act", lambda e: e.activation(out=affw[0][:, cols], in_=bank(tb)[0:16, :], func=AF.Exp), reads=[PB[tb]], writes=[b_affw[0]])
                S.pe([mm(bank(4 + tb)[0:16, :], onesf[0:16, 0:16], affw[0][:, cols])], reads=[b_onesf, b_affw[0]], writes=[PB[4 + tb]])
                S.op("dve", lambda e: e.reciprocal(out=affw[1][:, cols], in_=bank(4 + tb)[0:16, :]), reads=[PB[4 + tb]], writes=[b_affw[1]])
                S.op("dve", lambda e: e.tensor_tensor(out=aff[:, cols], in0=affw[0][:, cols], in1=affw[1][:, cols], op=ALU.mult),
                     reads=b_affw, writes=[b_aff])
            router_tb()
        dump("aff", aff[:, :], [b_aff])
        for r in range(CAP // 8):
            def topk_round(r=r):
                src = aff if r == 0 else affw[(r - 1) % 2]
                bsrc = b_aff if r == 0 else b_affw[(r - 1) % 2]
                dst = affw[r % 2]
                sl = slice(r * 8, r * 8 + 8)
                S.op("dve", lambda e: e.max(out=gate[:, sl], in_=src[:]), reads=[bsrc], writes=[b_gate])
                S.op("dve", lambda e: e.max_index(out=idxu[:, sl], in_max=gate[:, sl], in_values=src[:]), reads=[bsrc, b_gate], writes=[b_idxu])
                if r + 1 < CAP // 8:
                    S.op("dve", lambda e: e.match_replace(out=dst[:], in_to_replace=gate[:, sl], in_values=src[:], imm_value=-1.0),
                         reads=[bsrc, b_gate], writes=[b_affw[r % 2]])
            topk_round()
        S.op("dve", lambda e: e.tensor_copy(out=idxf[:], in_=idxu[:]), reads=[b_idxu], writes=[b_idxf])
        dump("gate", gate[:, :], [b_gate])
        dump("idxf", idxf[:, :], [b_idxf])
        S.pe([lambda e, ch=ch: e.transpose(bank(0)[:, ch * 16:(ch + 1) * 16], idxf[0:16, ch * 128:(ch + 1) * 128], ident[0:16, 0:16]) for ch in range(2)]
             + [lambda e, ch=ch: e.transpose(bank(0)[:, 32 + ch * 16:32 + (ch + 1) * 16], gate[0:16, ch * 128:(ch + 1) * 128], ident[0:16, 0:16]) for ch in range(2)],
             reads=[b_idxf, b_gate, b_ident], writes=[PB[0]])
        S.op("dve", lambda e: e.tensor_copy(out=idxT[:].rearrange("p a b -> p (a b)"), in_=bank(0)[:, 0:32]), reads=[PB[0]], writes=[b_igT])
        S.op("dve", lambda e: e.tensor_copy(out=gateT[:].rearrange("p a b -> p (a b)"), in_=bank(0)[:, 32:64]), reads=[PB[0]], writes=[b_igT])

        for j in range(NT):
            S.op("pool", lambda e, j=j: e.tensor_scalar(out=resid[:, j, :], in0=resid[:, j, :], scalar1=ALPHA, scalar2=None, op0=ALU.mult),
                 reads=[b_res[j]], writes=[b_res[j]])

        C1 = R1
        SelT = [T([128, NT, CAP], BF16, C1 + i * 8192) for i in range(2)]; b_sel = [Buf("sel%d" % i) for i in range(2)]
        GRP = 4
        ysc = T([128, GRP * 2, D], BF16, C1 + 16384); b_ysc = Buf("ysc")
        for bb in b_sel + [b_ysc]:
            S.inherit(bb, b_actT)
        xsT = [T([128, 8, CAP], BF16, B0 + 65536 + i * 4096) for i in range(2)]; b_xs = [Buf("xs%d" % i) for i in range(2)]
        hidT = [T([128, 2, CAP], BF16, B0 + 73728 + i * 1024) for i in range(2)]; b_hid = [Buf("hid%d" % i) for i in range(2)]
        for bb in b_xs + b_hid:
            S.inherit(bb, phaseB)
        selsc = [T([128, GRP * 2, 128], BF16, R8 + i * 2048) for i in range(2)]; b_selsc = [Buf("selsc%d" % i) for i in range(2)]
        silu_t = T([128, 512], F32, R8 + 4096); b_silu = Buf("silu")
        idxs = [T([128, 8], F32, R8 + 6144 + i * 32) for i in range(2)]; b_idxs2 = [Buf("idxs%d" % i) for i in range(2)]
        for bb in b_selsc + [b_silu] + b_idxs2:
            S.inherit(bb, [b_oT])
        NRING = 3
        ring_off = [B0, B0 + 12288, R6]
        ringG = [T([128, 8, 256], BF16, ring_off[i]) for i in range(NRING)]
        ringU = [T([128, 8, 256], BF16, ring_off[i] + 4096) for i in range(NRING)]
        ringD = [T([128, 2, D], BF16, ring_off[i] + 8192) for i in range(NRING)]
        b_ring = [Buf("ring%d" % i) for i in range(NRING)]
        for bb in b_ring[0:2]:
            S.inherit(bb, [b_aff] + b_affw + phaseB)
        S.inherit(b_ring[2], [b_lnp, b_ysb, b_junk])

        def load_unit(n):
            e, fb = divmod(n, 8)
            sl = n % NRING
            fs = slice(fb * 256, (fb + 1) * 256)
            S.dma("pool", lambda e_: e_.dma_start(out=ringG[sl][:], in_=w_gate[e].rearrange("(c p) f -> p c f", p=128)[:, :, fs]), b_ring[sl])
            S.dma("pool", lambda e_: e_.dma_start(out=ringU[sl][:], in_=w_up[e].rearrange("(c p) f -> p c f", p=128)[:, :, fs]), b_ring[sl])
            S.dma("pool", lambda e_: e_.dma_start(out=ringD[sl][:], in_=w_down[e][fb * 256:(fb + 1) * 256, :].rearrange("(c p) d -> p c d", p=128)), b_ring[sl])

        def gather_expert(e):
            st = SelT[e % 2]
            bs_ = b_sel[e % 2]
            xs = xsT[e % 2]
            S.pe([mm(bank(3)[:, 0:CAP], esel[0:16, e * 128:(e + 1) * 128], idxf[0:16, :])], reads=[b_esel, b_idxf], writes=[PB[3]])
            S.op("dve", lambda e_: e_.tensor_tensor(out=st[:], in0=bank(3)[:, 0:CAP].unsqueeze(1).broadcast_to([128, NT, CAP]),
                                                    in1=tokid[:, :].unsqueeze(2).broadcast_to([128, NT, CAP]), op=ALU.is_equal),
                 reads=[PB[3], b_tokid], writes=[bs_])
            for ps_ in range(2):
                for dcl in range(4):
                    dc = ps_ * 4 + dcl
                    bk = dcl // 2
                    co = (dcl % 2) * 256
                    S.pe([mm(bank(bk)[:, co:co + 256], h2tm[:, j, dc * 128:(dc + 1) * 128], st[:, j, :], j == 0, j == NT - 1) for j in range(NT)],
                         reads=[b_h2tm, bs_], writes=[PB[bk]])
                evac(xs[:, ps_ * 4:(ps_ + 1) * 4, :], PP[0][:].rearrange("p (d c) -> p d c", c=CAP), [PB[0], PB[1]], [b_xs[e % 2]])

        def ffn_unit(n):
            e, fb = divmod(n, 8)
            sl = n % NRING
            xs = xsT[e % 2]
            hd = hidT[n % 2]
            for (bk, W) in ((2, ringG[sl]), (3, ringU[sl])):
                fns = []
                for fl in range(2):
                    fns += [mm(bank(bk)[:, fl * 256:(fl + 1) * 256], W[:, dc, fl * 128:(fl + 1) * 128], xs[:, dc, :], dc == 0, dc == 7) for dc in range(8)]
                S.pe(fns, reads=[b_ring[sl], b_xs[e % 2]], writes=[PB[bk]])
            S.op("act", lambda e_: e_.activation(out=silu_t[:], in_=bank(2), func=AF.Silu), reads=[PB[2]], writes=[b_silu])
            S.op("dve", lambda e_: e_.tensor_tensor(out=hd[:].rearrange("p a b -> p (a b)"), in0=silu_t[:], in1=bank(3), op=ALU.mult),
                 reads=[b_silu, PB[3]], writes=[b_hid[n % 2]])
            for cc2 in range(2):
                for half in range(2):
                    bk = 4 + cc2 * 2 + half
                    S.pe([mm(bank(bk), hd[:, fl, cc2 * 128:(cc2 + 1) * 128], ringD[sl][:, fl, half * 512:(half + 1) * 512],
                             fb == 0 and fl == 0, fb == 7 and fl == 1) for fl in range(2)],
                         reads=[b_hid[n % 2], b_ring[sl]], writes=[PB[bk]])

        def finish_expert(e):
            for cc2 in range(2):
                S.op("act", lambda e_, cc2=cc2: e_.activation(out=ysc[:, (e % GRP) * 2 + cc2, :], in_=PP[2 + cc2][:], func=AF.Copy,
                                                              scale=gateT[:, cc2, e:e + 1]),
                     reads=[PB[4 + 2 * cc2], PB[5 + 2 * cc2], b_igT], writes=[b_ysc])

        def scatter_group(g):
            def sc_tile(j):
                ix = idxs[j % 2]
                bi = b_idxs2[j % 2]
                sc = selsc[j % 2]
                bsc = b_selsc[j % 2]
                S.op("dve", lambda e_: e_.tensor_scalar(out=ix[:, 0:8].rearrange("p (e c) -> p e c", c=2),
                                                        in0=idxT[:, :, g * GRP:(g + 1) * GRP].rearrange("p c e -> p e c"),
                                                        scalar1=-128.0 * j, scalar2=None, op0=ALU.add), reads=[b_igT], writes=[bi])
                S.op("dve", lambda e_: e_.tensor_tensor(out=sc[:], in0=ix[:, 0:8].unsqueeze(2).broadcast_to([128, 8, 128]),
                                                        in1=iota[:, :].unsqueeze(1).broadcast_to([128, 8, 128]), op=ALU.is_equal),
                     reads=[bi, b_iota], writes=[bsc])
                for half in range(2):
                    S.pe([mm(bank(half), sc[:, k, :], ysc[:, k, half * 512:(half + 1) * 512], k == 0, k == 7) for k in range(8)],
                         reads=[bsc, b_ysc], writes=[PB[half]])
                S.op("dve", lambda e_: e_.tensor_tensor(out=resid[:, j, :], in0=resid[:, j, :], in1=PP[0][:], op=ALU.add),
                     reads=[b_res[j], PB[0], PB[1]], writes=[b_res[j]])
            for j in range(NT):
                sc_tile(j)

        NEXP = NE
        for n0 in range(NRING):
            load_unit(n0)
        for e in range(NEXP):
            gather_expert(e)
            for fb in range(8):
                n = e * 8 + fb
                ffn_unit(n)
                if n + NRING < NEXP * 8:
                    load_unit(n + NRING)
            finish_expert(e)
            if e % GRP == GRP - 1:
                scatter_group(e // GRP)

        for bb in (b_lnp, b_ysb, b_junk):
            S.inherit(bb, [b_ring[2]])
        load_lnp(2)
        outv = out.rearrange("(j p) d -> j p d", p=128)
        b_out = Buf("out")

        def ln3_tile(j):
            so = (j % 2) * 8
            bs = b_st[j % 2]
            st_ = lambda a: stat[:, so + a:so + a + 1]
            S.op("dve", lambda e: e.tensor_copy(out=ysb[:], in_=resid[:, j, :]), reads=[b_res[j]], writes=[b_ysb])
            S.op("dve", lambda e: e.memset(stat[:, so:so + 2], 0.0), writes=[bs])
            S.op("act", lambda e: e.activation(out=junk[:], in_=ysb[:], func=AF.Copy, accum_out=st_(0)), reads=[b_ysb], writes=[b_junk, bs])
            S.op("act", lambda e: e.activation(out=junk[:], in_=ysb[:], func=AF.Square, accum_out=st_(1)), reads=[b_ysb], writes=[b_junk, bs])
            S.op("dve", lambda e: e.tensor_scalar(out=st_(2), in0=st_(0), scalar1=1.0 / D, scalar2=None, op0=ALU.mult), reads=[bs], writes=[bs])
            S.op("dve", lambda e: e.tensor_tensor(out=st_(3), in0=st_(2), in1=st_(2), op=ALU.mult), reads=[bs], writes=[bs])
            S.op("dve", lambda e: e.scalar_tensor_tensor(out=st_(3), in0=st_(1), scalar=1.0 / D, in1=st_(3), op0=ALU.mult, op1=ALU.subtract),
                 reads=[bs], writes=[bs])
            S.op("act", lambda e: e.activation(out=st_(4), in_=st_(3), func=AF.Sqrt, bias=cst[:, 2:3], scale=1.0), reads=[bs, b_cst], writes=[bs])
            S.op("dve", lambda e: e.reciprocal(out=st_(4), in_=st_(4)), reads=[bs], writes=[bs])
            S.op("dve", lambda e: e.scalar_tensor_tensor(out=st_(5), in0=st_(2), scalar=-1.0, in1=st_(4), op0=ALU.mult, op1=ALU.mult), reads=[bs], writes=[bs])
            S.op("act", lambda e: e.activation(out=ysb[:], in_=ysb[:], func=AF.Identity, scale=st_(4), bias=st_(5)), reads=[b_ysb, bs], writes=[b_ysb])
            S.op("pool", lambda e: e.tensor_tensor(out=ysb[:], in0=ysb[:], in1=lnp[:, 0, :], op=ALU.mult), reads=[b_ysb, b_lnp], writes=[b_ysb])
            S.op("pool", lambda e: e.tensor_tensor(out=resid[:, j, :], in0=ysb[:], in1=lnp[:, 1, :], op=ALU.add), reads=[b_ysb, b_lnp], writes=[b_res[j]])
            S.dma("sp", lambda e: e.dma_start(out=outv[j], in_=resid[:, j, :]), b_out, reads=[b_res[j]], writes=[b_out])
        for j in range(NT):
            ln3_tile(j)
        S.wait_bufs("sp", [b_out, b_dbg])
        S.emit()
    return nc


def make_consts():
    c = {}
    c["c_ident"] = np.eye(128, dtype=np.float32)
    c["c_iota"] = np.tile(np.arange(128, dtype=np.float32)[None, :], (128, 1))
    c["c_tokid"] = (np.arange(NT, dtype=np.float32)[None, :] * 128 + np.arange(128, dtype=np.float32)[:, None]).astype(np.float32)
    es = np.zeros((16, 16, 128), np.float32)
    for e in range(16):
        es[e, e, :] = 1.0
    c["c_esel"] = es.reshape(16, 16 * 128)
    rope = np.zeros((128, 2), np.float32)
    inv_freq = (10000.0 ** (-np.arange(0, 32, 2, dtype=np.float32) / 32.0)).astype(np.float32)
    for p in range(64, 96):
        rope[p, 0] = np.float32(np.float64(inv_freq[(p - 64) % 16]) / (2.0 * np.pi))
        rope[p, 1] = -1.0 if p < 80 else 1.0
    c["c_rope"] = rope
    return c


def make_in_maps(inputs, n_cores=8):
    consts = make_consts()
    maps = []
    f = lambda a: np.ascontiguousarray(np.asarray(a))
    for b in range(n_cores):
        m = dict(consts)
        m["x"] = f(inputs["x"][b])
        m["mem"] = f(inputs["mem"][b])
        m["positions"] = f(np.asarray(inputs["positions"])[b:b + 1]).astype(np.int32)
        for k in ["w_in", "q_norm_g", "w_uq", "kv_norm_g", "w_uk", "w_uv", "conv_w", "conv_b", "conv_ln_g", "conv_ln_b",
                  "w_o", "xa_w_q", "xa_w_k", "xa_w_v", "xa_w_o", "w_router", "w_gate", "w_up", "w_down"]:
            m[k] = f(np.asarray(inputs[k])[0])
        for k in ["ln1_g", "ln1_b", "ln2_g", "ln2_b", "ln3_g", "ln3_b"]:
            m[k] = f(np.asarray(inputs[k])[0:1])
        maps.append(m)
    return maps


def kernel(**inputs):
    nc = build_program()
    maps = make_in_maps(inputs, 8)
    res = run_bass_kernel_spmd(nc, maps, core_ids=list(range(8)))
    return np.stack([r["out"] for r in res.results], axis=0).astype(np.float32)
```

```python
import math
import os
from contextlib import ExitStack

import numpy as np
import concourse.bass as bass
import concourse.mybir as mybir
from concourse.bass_utils import run_bass_kernel_spmd

F32 = mybir.dt.float32
BF16 = mybir.dt.bfloat16
I32 = mybir.dt.int32
U32 = mybir.dt.uint32
ALU = mybir.AluOpType
AF = mybir.ActivationFunctionType

S_ = 2048
D = 1024
NT = 16
EPS = 1e-5
ALPHA = 2.0 ** 0.25
NE = 16
CAP = 256
FF = 2048


class Buf:
    __slots__ = ("name", "w", "r", "sem", "cnt")

    def __init__(self, name):
        self.name = name
        self.w = None
        self.r = {}
        self.sem = None
        self.cnt = 0


class Sched:
    ENG = ["pe", "act", "dve", "pool", "sp"]

    def __init__(self, nc, es):
        self.nc = nc
        self.es = es
        self.streams = {e: [] for e in self.ENG}
        self.cnt = {e: 0 for e in self.ENG}
        self.esem = {e: es.enter_context(nc.semaphore("c_" + e)) for e in self.ENG}
        self.waited = {e: {} for e in self.ENG}
        self.nsem = len(self.ENG)

    def new_sem(self, name):
        self.nsem += 1
        return self.es.enter_context(self.nc.semaphore("d%d_%s" % (self.nsem, name)))

    def _waits(self, eng, reads, writes, skip_sem=None):
        deps = {}

        def add(s, v):
            if deps.get(s, 0) < v:
                deps[s] = v
        for b in reads:
            if b.w is not None:
                add(*b.w)
        for b in writes:
            if b.w is not None:
                add(*b.w)
            for s, v in b.r.items():
                add(s, v)
        own = self.esem[eng]
        wd = self.waited[eng]
        for s, v in deps.items():
            if s == own and eng == "pe":
                continue
            if skip_sem is not None and s == skip_sem:
                continue
            if wd.get(s, 0) < v:
                wd[s] = v
                self.streams[eng].append(("wait", s, v))

    def _commit(self, ev, reads, writes):
        s, v = ev
        for b in reads:
            if b.r.get(s, 0) < v:
                b.r[s] = v
        for b in writes:
            b.w = ev
            b.r = {}

    def op(self, eng, fn, reads=(), writes=()):
        self._waits(eng, reads, writes)
        self.cnt[eng] += 1
        s = self.esem[eng]
        self.streams[eng].append(("op", fn, s, 1))
        self._commit((s, self.cnt[eng]), reads, writes)

    def pe(self, fns, reads=(), writes=()):
        self._waits("pe", reads, writes)
        self.cnt["pe"] += 1
        s = self.esem["pe"]
        for f in fns[:-1]:
            self.streams["pe"].append(("op", f, None, 0))
        self.streams["pe"].append(("op", fns[-1], s, 1))
        self._commit((s, self.cnt["pe"]), reads, writes)

    def dma(self, eng, fn, dst, reads=(), writes=None):
        if writes is None:
            writes = (dst,)
        if dst.sem is None:
            dst.sem = self.new_sem(dst.name)
        self._waits(eng, reads, writes, skip_sem=dst.sem)
        dst.cnt += 16
        self.streams[eng].append(("op", fn, dst.sem, 16))
        self._commit((dst.sem, dst.cnt), reads, writes)

    def wait_bufs(self, eng, bufs):
        self._waits(eng, bufs, bufs)

    def inherit(self, new, olds):
        for o in olds:
            if o.w is not None:
                s, v = o.w
                if new.r.get(s, 0) < v:
                    new.r[s] = v
            for s, v in o.r.items():
                if new.r.get(s, 0) < v:
                    new.r[s] = v

    def emit(self):
        nc = self.nc
        streams = self.streams

        def run(name, eng):
            for item in streams[name]:
                if item[0] == "wait":
                    eng.wait_ge(item[1], item[2])
                else:
                    ins = item[1](eng)
                    if item[2] is not None:
                        ins.then_inc(item[2], item[3])

        with nc.Block() as block:
            @block.tensor
            def _(e):
                run("pe", e)

            @block.scalar
            def _(e):
                run("act", e)

            @block.vector
            def _(e):
                run("dve", e)

            @block.gpsimd
            def _(e):
                run("pool", e)

            @block.sync
            def _(e):
                run("sp", e)


def build_program(stop_after=None, dumps=()):
    nc = bass.Bass("TRN2", target_bir_lowering=False)

    def din(name, shape, dt=F32):
        return nc.dram_tensor(name, list(shape), dt, kind="ExternalInput").ap()

    x = din("x", [S_, D])
    mem = din("mem", [256, D])
    pos = din("positions", [1, S_], I32)
    w_in = din("w_in", [D, 1440])
    q_norm_g = din("q_norm_g", [256])
    w_uq = din("w_uq", [256, 768])
    kv_norm_g = din("kv_norm_g", [128])
    w_uk = din("w_uk", [128, 512])
    w_uv = din("w_uv", [128, 512])
    conv_w = din("conv_w", [31, 512])
    conv_b = din("conv_b", [512])
    conv_ln_g = din("conv_ln_g", [512])
    conv_ln_b = din("conv_ln_b", [512])
    w_o = din("w_o", [D, D])
    ln_g = [din("ln%d_g" % i, [1, D]) for i in (1, 2, 3)]
    ln_b = [din("ln%d_b" % i, [1, D]) for i in (1, 2, 3)]
    xa_w = {k: din("xa_w_" + k, [D, D]) for k in "qkvo"}
    w_router = din("w_router", [D, NE])
    w_gate = din("w_gate", [NE, D, FF])
    w_up = din("w_up", [NE, D, FF])
    w_down = din("w_down", [NE, FF, D])
    c_ident = din("c_ident", [128, 128])
    c_iota = din("c_iota", [128, 128])
    c_tokid = din("c_tokid", [128, NT])
    c_esel = din("c_esel", [16, 16 * 128])
    c_rope = din("c_rope", [128, 2])
    out = nc.dram_tensor("out", [S_, D], F32, kind="ExternalOutput").ap()
    dump_d = {}
    for (nm, shape, dt) in dumps:
        dump_d[nm] = nc.dram_tensor("dbg_" + nm, list(shape), dt, kind="ExternalOutput").ap()

    es = ExitStack()
    with es:
        S = Sched(nc, es)
        cnt = [0]

        def T(shape, dt, off, name=None):
            cnt[0] += 1
            return nc.alloc_sbuf_tensor_at("%s_%d" % (name or "t", cnt[0]), list(shape), dt, offset=off)

        b_dbg = Buf("dbg")
        conv_todo = []
        drain = {"rate": 0.0, "credit": 0.0, "busy": False}
        _raw_op = S.op

        def op_hook(eng, fn, reads=(), writes=()):
            _raw_op(eng, fn, reads=reads, writes=writes)
            if eng == "dve" and drain["rate"] > 0.0 and not drain["busy"] and conv_todo:
                drain["credit"] += drain["rate"]
                drain["busy"] = True
                while drain["credit"] >= 1.0 and conv_todo:
                    conv_todo.pop(0)()
                    drain["credit"] -= 1.0
                drain["busy"] = False
        S.op = op_hook

        def rsqrt_act(dst, src, bias_ap, rd, wr):
            S.op("act", lambda e: e.activation(out=dst, in_=src, func=AF.Ln, bias=bias_ap, scale=1.0), reads=rd + [b_cst], writes=wr)
            S.op("act", lambda e: e.activation(out=dst, in_=dst, func=AF.Exp, scale=-0.5), reads=wr, writes=wr)

        def recip_act(dst, src, rd, wr):
            S.op("act", lambda e: e.activation(out=dst, in_=src, func=AF.Ln), reads=rd, writes=wr)
            S.op("act", lambda e: e.activation(out=dst, in_=dst, func=AF.Exp, scale=-1.0), reads=wr, writes=wr)

        def dump(nm, ap, bufs):
            if nm in dump_d:
                S.dma("sp", lambda e: e.dma_start(out=dump_d[nm], in_=ap), b_dbg, reads=bufs, writes=[b_dbg])

        R0 = 16640
        R1 = R0 + 66048
        R2 = R1 + 32768
        R3 = R2 + 26624
        R4 = R3 + 24576
        R5 = R4 + 8192
        R6 = R5 + 16384
        R7 = R6 + 8192
        R8 = R7 + 8192
        R9 = R8 + 8192
        ident = T([128, 128], F32, R9); b_ident = Buf("ident")
        identb = T([128, 128], BF16, R9 + 512); b_identb = Buf("identb")
        onesb = T([128, 128], BF16, R9 + 768); b_onesb = Buf("onesb")
        onesf = T([128, 128], F32, R9 + 1024); b_onesf = Buf("onesf")
        ropec = T([128, 2], F32, R9 + 1536); b_ropec = Buf("ropec")
        gsm = T([128, 16], F32, R9 + 1600); b_gsm = Buf("gsm")
        cwT = T([128, 4, 32], F32, R9 + 1664); b_cwT = Buf("cwT")
        iota = T([128, 128], F32, R9 + 2176); b_iota = Buf("iota")
        tokid = T([128, NT], F32, R9 + 2688); b_tokid = Buf("tokid")
        stat = T([128, 16], F32, R9 + 2752); b_stat = Buf("stat")
        cst = T([128, 8], F32, R9 + 2816); b_cst = Buf('cst')
        stat2 = T([128, 32], F32, R9 + 2848); b_st = [Buf("st0"), Buf("st1")]
        CEND = R9 + 2976
        assert CEND <= 229376, CEND

        PP = [nc.alloc_psum_tensor("pp%d" % i, [128, 1024], F32) for i in range(4)]
        PB = [Buf("bank%d" % i) for i in range(8)]

        def bank(k):
            return PP[k // 2][:, (k % 2) * 512:(k % 2) * 512 + 512]

        S.dma("sp", lambda e: e.dma_start(out=ident[:], in_=c_ident), b_ident)
        S.dma("sp", lambda e: e.dma_start(out=iota[:], in_=c_iota), b_iota)
        S.dma("sp", lambda e: e.dma_start(out=tokid[:], in_=c_tokid), b_tokid)
        S.dma("sp", lambda e: e.dma_start(out=ropec[:], in_=c_rope), b_ropec)
        S.op("dve", lambda e: e.tensor_copy(out=identb[:], in_=ident[:]), reads=[b_ident], writes=[b_identb])
        S.op("dve", lambda e: e.memset(onesb[:], 1.0), writes=[b_onesb])
        S.op("dve", lambda e: e.memset(onesf[:], 1.0), writes=[b_onesf])
        S.op("dve", lambda e: e.memset(cst[:, 0:1], 256.0 * EPS), writes=[b_cst])
        S.op("dve", lambda e: e.memset(cst[:, 1:2], 128.0 * EPS), writes=[b_cst])
        S.op("dve", lambda e: e.memset(cst[:, 2:3], EPS), writes=[b_cst])
        S.op("dve", lambda e: e.memset(cst[:, 3:4], 0.0), writes=[b_cst])
        with nc.allow_non_contiguous_dma(reason="tiny per-channel parameter vectors"):
            S.dma("sp", lambda e: e.dma_start(out=gsm[:, 0:2], in_=q_norm_g.rearrange("(c p) -> p c", p=128), allow_slow_non_contiguous=True), b_gsm)
            S.dma("sp", lambda e: e.dma_start(out=gsm[:, 2:3], in_=kv_norm_g.rearrange("(c p) -> p c", p=128), allow_slow_non_contiguous=True), b_gsm)
            S.dma("sp", lambda e: e.dma_start(out=gsm[:, 4:8], in_=conv_b.rearrange("(c p) -> p c", p=128), allow_slow_non_contiguous=True), b_gsm)
            S.dma("sp", lambda e: e.dma_start(out=gsm[:, 8:12], in_=conv_ln_g.rearrange("(c p) -> p c", p=128), allow_slow_non_contiguous=True), b_gsm)
            S.dma("sp", lambda e: e.dma_start(out=gsm[:, 12:16], in_=conv_ln_b.rearrange("(c p) -> p c", p=128), allow_slow_non_contiguous=True), b_gsm)
        S.op("dve", lambda e: e.tensor_scalar(out=gsm[:, 0:2], in0=gsm[:, 0:2], scalar1=16.0, scalar2=None, op0=ALU.mult),
             reads=[b_gsm], writes=[b_gsm])
        S.op("dve", lambda e: e.tensor_scalar(out=gsm[:, 2:3], in0=gsm[:, 2:3], scalar1=math.sqrt(128.0), scalar2=None, op0=ALU.mult),
             reads=[b_gsm], writes=[b_gsm])

        resid = T([128, NT, D], F32, R0); b_res = [Buf("res%d" % j) for j in range(NT)]
        u = T([128, 4, 2078], F32, R0); b_u = [Buf("u%d" % c) for c in range(4)]
        acc = T([128, 4, 2048], F32, R0 + 33248); b_acc = [Buf("acc%d" % c) for c in range(4)]
        xstage = [T([128, D], F32, R0 + 33248 + i * 4096) for i in range(2)]; b_xst = [Buf("xst%d" % i) for i in range(2)]
        actT = T([128, 8, S_], BF16, R1); b_actT = [Buf("actT%d" % t) for t in range(4)]
        kT = actT; b_kT = Buf("kT")
        w_in_sb = T([128, 8, 1440], BF16, R2); b_w_in = Buf("w_in")
        wkr = T([128, 8, 2, 96], BF16, R2 + 23040); b_wkr = Buf("wkr")
        attnT = T([128, 4, S_], BF16, R2); b_attnT = Buf("attnT")
        qT = [T([128, 512], BF16, R2 + 16384 + i * 1024) for i in range(2)]; b_qT = [Buf("qT%d" % i) for i in range(2)]
        NPT = 4
        pTt = [T([128, 512], BF16, R2 + 18432 + i * 1024) for i in range(NPT)]; b_pT = [Buf("pT%d" % i) for i in range(NPT)]
        Vaug = T([128, NT, 4, 192], BF16, R3); b_V = Buf("V")
        cqnT = T([128, 2, S_], BF16, R4); b_cqn = Buf("cqn")
        convT = T([128, 4, S_], BF16, R5); b_convT = Buf("convT")
        kr = T([128, S_], BF16, R5); b_kr = Buf("kr")
        ckvnT = T([128, S_], BF16, R5 + 4096); b_ckvn = Buf("ckvn")
        COS = T([128, S_], BF16, R6); SIN = T([128, S_], BF16, R6 + 4096); b_cs = Buf("cossin")
        scr = [T([128, 512], F32, R7 + i * 2048) for i in range(4)]; b_scr = [Buf("scr%d" % i) for i in range(4)]
        wuq = T([128, 2, 768], BF16, R8); b_wuq = Buf("wuq")
        wqsw = T([128, 2, 8, 96], BF16, R8 + 3072); b_wqsw = Buf("wqsw")
        wuk = T([128, 512], BF16, R8 + 6144); b_wuk = Buf("wuk")
        wuv = T([128, 512], BF16, R8 + 7168); b_wuv = Buf("wuv")

        w_in_v = w_in.rearrange("(c p) f -> p c f", p=128)
        for c0 in range(0, 8, 4):
            S.dma("pool", lambda e, c0=c0: e.dma_start(out=w_in_sb[:, c0:c0 + 4, :], in_=w_in_v[:, c0:c0 + 4, :]), b_w_in)
        S.op("pool", lambda e: e.memset(wkr[:], 0.0), writes=[b_wkr])
        S.op("pool", lambda e: e.memset(wqsw[:], 0.0), writes=[b_wqsw])
        with nc.allow_non_contiguous_dma(reason="rope column permutation"):
            S.dma("pool", lambda e: e.dma_start(out=wkr[:, :, 0, 64:96], in_=w_in_v[:, :, 384:416], allow_slow_non_contiguous=True), b_wkr)
            S.dma("pool", lambda e: e.dma_start(out=wkr[:, :, 1, 64:80], in_=w_in_v[:, :, 400:416], allow_slow_non_contiguous=True), b_wkr)
            S.dma("pool", lambda e: e.dma_start(out=wkr[:, :, 1, 80:96], in_=w_in_v[:, :, 384:400], allow_slow_non_contiguous=True), b_wkr)
            w_uq_v = w_uq.rearrange("(c p) (h f) -> p c h f", p=128, f=96)
            for c in range(2):
                S.dma("pool", lambda e, c=c: e.dma_start(out=wqsw[:, c, :, 64:80], in_=w_uq_v[:, c, :, 80:96], allow_slow_non_contiguous=True), b_wqsw)
                S.dma("pool", lambda e, c=c: e.dma_start(out=wqsw[:, c, :, 80:96], in_=w_uq_v[:, c, :, 64:80], allow_slow_non_contiguous=True), b_wqsw)
        S.dma("pool", lambda e: e.dma_start(out=wuq[:], in_=w_uq.rearrange("(c p) f -> p c f", p=128)), b_wuq)
        S.dma("pool", lambda e: e.dma_start(out=wuk[:], in_=w_uk), b_wuk)
        S.dma("pool", lambda e: e.dma_start(out=wuv[:], in_=w_uv), b_wuv)

        cw_st = scr[0]
        S.dma("sp", lambda e: e.dma_start(out=cw_st[0:31, 0:512], in_=conv_w), b_scr[0])
        S.pe([lambda e, cc=cc: e.transpose(bank(0)[:, cc * 32:cc * 32 + 31], cw_st[0:31, cc * 128:(cc + 1) * 128], ident[0:31, 0:31])
              for cc in range(4)], reads=[b_scr[0], b_ident], writes=[PB[0]])
        S.op("dve", lambda e: e.tensor_copy(out=cwT[:, :, 0:31], in_=bank(0)[:, 0:128].rearrange("p (c k) -> p c k", k=32)[:, :, 0:31]),
             reads=[PB[0]], writes=[b_cwT])

        posi = T([128, S_], I32, R0)
        posf = T([128, S_], F32, R0 + 8192)
        angb = T([128, S_], F32, R0 + 16384)
        b_pos = Buf("pos")
        S.dma("sp", lambda e: e.dma_start(out=posi[:], in_=pos.broadcast_to([128, S_])), b_pos)
        rp = slice(64, 96)
        S.op("dve", lambda e: e.tensor_copy(out=posf[rp, :], in_=posi[rp, :]), reads=[b_pos], writes=[b_pos])
        kint = T([128, S_], I32, R0 + 24576)
        sinf = T([128, S_], F32, R0 + 24576)

        def trig(phase, dst_fn):
            S.op("dve", lambda e: e.tensor_scalar(out=angb[rp, :], in0=posf[rp, :], scalar1=ropec[rp, 0:1], scalar2=phase,
                                                  op0=ALU.mult, op1=ALU.add), reads=[b_pos, b_ropec], writes=[b_pos])
            S.op("dve", lambda e: e.tensor_copy(out=kint[rp, :], in_=angb[rp, :]), reads=[b_pos], writes=[b_pos])
            S.op("dve", lambda e: e.tensor_copy(out=posi[rp, :].bitcast(F32), in_=kint[rp, :]), reads=[b_pos], writes=[b_pos])
            S.op("dve", lambda e: e.tensor_tensor(out=angb[rp, :], in0=angb[rp, :], in1=posi[rp, :].bitcast(F32), op=ALU.subtract),
                 reads=[b_pos], writes=[b_pos])
            S.op("dve", lambda e: e.scalar_tensor_tensor(out=angb[rp, :], in0=angb[rp, :], scalar=0.5, in1=angb[rp, :],
                                                         op0=ALU.is_gt, op1=ALU.subtract), reads=[b_pos], writes=[b_pos])
            dst_fn()

        def fin_sin():
            S.op("act", lambda e: e.activation(out=sinf[rp, :], in_=angb[rp, :], func=AF.Sin, scale=-2.0 * math.pi), reads=[b_pos], writes=[b_pos])
            S.op("dve", lambda e: e.tensor_scalar(out=SIN[rp, :], in0=sinf[rp, :], scalar1=ropec[rp, 1:2], scalar2=None, op0=ALU.mult),
                 reads=[b_pos, b_ropec], writes=[b_cs])

        def fin_cos():
            S.op("act", lambda e: e.activation(out=COS[rp, :], in_=angb[rp, :], func=AF.Sin, scale=-2.0 * math.pi), reads=[b_pos], writes=[b_cs])
        trig(0.0, fin_sin)
        trig(0.25, fin_cos)
        for c in range(4):
            S.inherit(b_u[c], [b_pos])

        xv = x.rearrange("(j p) d -> j p d", p=128)
        for j in range(NT):
            st = j % 2
            S.dma("sp", lambda e, j=j, st=st: e.dma_start(out=xstage[st][:], in_=xv[j]), b_xst[st])
            pb = [PB[(j % 2) * 2], PB[(j % 2) * 2 + 1]]
            pt = PP[j % 2]
            S.pe([lambda e, c=c, st=st, pt=pt: e.transpose(pt[:, c * 128:(c + 1) * 128], xstage[st][:, c * 128:(c + 1) * 128], ident[:])
                  for c in range(8)], reads=[b_xst[st], b_ident], writes=pb)
            eng = "act" if j % 2 == 0 else "dve"
            if eng == "act":
                S.op("act", lambda e, j=j, pt=pt: e.activation(out=actT[:, :, j * 128:(j + 1) * 128],
                                                             in_=pt[:].rearrange("p (c t) -> p c t", t=128), func=AF.Copy),
                     reads=pb, writes=[b_actT[j // 4]])
            else:
                S.op("dve", lambda e, j=j, pt=pt: e.tensor_copy(out=actT[:, :, j * 128:(j + 1) * 128],
                                                              in_=pt[:].rearrange("p (c t) -> p c t", t=128)),
                     reads=pb, writes=[b_actT[j // 4]])
        dump("xT", actT[:, :, :], b_actT)

        S.op("pool", lambda e: e.memset(u[:, :, 0:15], 0.0), writes=b_u)
        S.op("pool", lambda e: e.memset(u[:, :, 2063:2078], 0.0), writes=b_u)
        for c in range(4):
            S.inherit(b_acc[c], b_xst)

        def inproj_group(bk, lhs_fn, tb, M):
            cols = slice(tb * 512, (tb + 1) * 512)
            S.pe([lambda e, c=c: e.matmul(bank(bk)[0:M, :], lhsT=lhs_fn(c), rhs=actT[:, c, cols], start=(c == 0), stop=(c == 7))
                  for c in range(8)], reads=[b_w_in, b_wkr, b_actT[tb]], writes=[PB[bk]])

        def inproj_tb(tb):
            cols = slice(tb * 512, (tb + 1) * 512)
            for c2 in range(2):
                inproj_group(c2, lambda c, c2=c2: w_in_sb[:, c, c2 * 128:(c2 + 1) * 128], tb, 128)
                S.op("act", lambda e, c2=c2: e.activation(out=scr[c2][:].bitcast(BF16)[:, 0:512], in_=bank(c2), func=AF.Square),
                     reads=[PB[c2]], writes=[b_scr[c2]])
            S.pe([lambda e, c2=c2: e.matmul(bank(2), lhsT=onesb[:], rhs=scr[c2][:].bitcast(BF16)[:, 0:512], start=(c2 == 0), stop=(c2 == 1))
                  for c2 in range(2)], reads=[b_onesb, b_scr[0], b_scr[1]], writes=[PB[2]])
            rsqrt_act(scr[2][:], bank(2), cst[:, 0:1], [PB[2]], [b_scr[2]])
            for c2 in range(2):
                S.op("dve", lambda e, c2=c2: e.scalar_tensor_tensor(out=cqnT[:, c2, cols], in0=bank(c2), scalar=gsm[:, c2:c2 + 1], in1=scr[2][:],
                                                                   op0=ALU.mult, op1=ALU.mult),
                     reads=[PB[c2], b_gsm, b_scr[2]], writes=[b_cqn])
            inproj_group(3, lambda c: w_in_sb[:, c, 256:384], tb, 128)
            S.op("act", lambda e: e.activation(out=scr[3][:].bitcast(BF16)[:, 0:512], in_=bank(3), func=AF.Square),
                 reads=[PB[3]], writes=[b_scr[3]])
            S.pe([lambda e: e.matmul(bank(2), lhsT=onesb[:], rhs=scr[3][:].bitcast(BF16)[:, 0:512], start=True, stop=True)],
                 reads=[b_onesb, b_scr[3]], writes=[PB[2]])
            rsqrt_act(scr[2][:], bank(2), cst[:, 1:2], [PB[2]], [b_scr[2]])
            S.op("dve", lambda e: e.scalar_tensor_tensor(out=ckvnT[:, cols], in0=bank(3), scalar=gsm[:, 2:3], in1=scr[2][:],
                                                         op0=ALU.mult, op1=ALU.mult),
                 reads=[PB[3], b_gsm, b_scr[2]], writes=[b_ckvn])
            inproj_group(4, lambda c: wkr[:, c, 0, :], tb, 96)
            inproj_group(5, lambda c: wkr[:, c, 1, :], tb, 96)
            S.op("dve", lambda e: e.tensor_tensor(out=scr[0][rp, :], in0=bank(4)[rp, :], in1=COS[rp, cols], op=ALU.mult),
                 reads=[PB[4], b_cs], writes=[b_scr[0]])
            S.op("dve", lambda e: e.tensor_tensor(out=scr[1][rp, :], in0=bank(5)[rp, :], in1=SIN[rp, cols], op=ALU.mult),
                 reads=[PB[5], b_cs], writes=[b_scr[1]])
            S.op("dve", lambda e: e.tensor_tensor(out=kr[rp, cols], in0=scr[0][rp, :], in1=scr[1][rp, :], op=ALU.add),
                 reads=[b_scr[0], b_scr[1]], writes=[b_kr])
        def inproj_glu(tb):
            cols = slice(tb * 512, (tb + 1) * 512)
            for cc in range(4):
                ba = 6 - 2 * (cc % 2)
                bg = 7 - 2 * (cc % 2)
                sg = scr[2 + cc % 2]
                bsg = b_scr[2 + cc % 2]
                inproj_group(ba, lambda c, cc=cc: w_in_sb[:, c, 416 + cc * 128:416 + (cc + 1) * 128], tb, 128)
                inproj_group(bg, lambda c, cc=cc: w_in_sb[:, c, 928 + cc * 128:928 + (cc + 1) * 128], tb, 128)
                S.op("act", lambda e, sg=sg, bg=bg: e.activation(out=sg[:], in_=bank(bg), func=AF.Sigmoid), reads=[PB[bg]], writes=[bsg])
                S.op("dve", lambda e, cc=cc, sg=sg, ba=ba: e.tensor_tensor(out=u[:, cc, 15 + tb * 512:15 + (tb + 1) * 512], in0=bank(ba), in1=sg[:], op=ALU.mult),
                     reads=[PB[ba], bsg], writes=[b_u[cc]])
        for tb in range(4):
            inproj_glu(tb)
        def conv_chunk(cc, eng):
            conv_todo.append(lambda: _raw_op(eng, lambda e: e.tensor_scalar(out=acc[:, cc, :], in0=u[:, cc, 0:2048], scalar1=cwT[:, cc, 0:1],
                                                                            scalar2=gsm[:, 4 + cc:5 + cc], op0=ALU.mult, op1=ALU.add),
                                             reads=[b_u[cc], b_cwT, b_gsm], writes=[b_acc[cc]]))
            for k in range(1, 31):
                conv_todo.append(lambda k=k: _raw_op(eng, lambda e: e.scalar_tensor_tensor(out=acc[:, cc, :], in0=u[:, cc, k:k + 2048],
                                                                                           scalar=cwT[:, cc, k:k + 1], in1=acc[:, cc, :],
                                                                                           op0=ALU.mult, op1=ALU.add),
                                                     reads=[b_u[cc], b_acc[cc], b_cwT], writes=[b_acc[cc]]))
        for cc in range(4):
            conv_chunk(cc, "dve")
        drain["rate"] = 0.5
        for tb in range(4):
            inproj_tb(tb)
        dump("cqnT", cqnT[:, :, :], [b_cqn])
        dump("ckvnT", ckvnT[:, :], [b_ckvn])
        dump("kr", kr[64:96, :], [b_kr])
        dump("u", u[:, :, 15:2063], b_u)
        if stop_after == "inproj":
            S.wait_bufs("sp", [b_dbg])
            S.emit()
            return nc


        def mm(o, l, r, st=True, sp=True):
            return lambda e: e.matmul(o, lhsT=l, rhs=r, start=st, stop=sp)

        evc = [0]

        def evac(out_ap, in_ap, reads, writes):
            evc[0] += 1
            if evc[0] % 2 == 0:
                S.op("act", lambda e: e.activation(out=out_ap, in_=in_ap, func=AF.Copy), reads=reads, writes=writes)
            else:
                S.op("dve", lambda e: e.tensor_copy(out=out_ap, in_=in_ap), reads=reads, writes=writes)

        S.inherit(b_kT, b_actT)

        def kup(h, tb):
            cols = slice(tb * 512, (tb + 1) * 512)
            bk = (h * 4 + tb) % 4
            S.pe([mm(bank(bk)[0:64, :], wuk[:, h * 64:(h + 1) * 64], ckvnT[:, cols])], reads=[b_wuk, b_ckvn], writes=[PB[bk]])
            evac(kT[0:64, h, cols], bank(bk)[0:64, :], [PB[bk]], [b_kT])
        for h in range(8):
            for tb in range(4):
                kup(h, tb)

        def krb(tb):
            cols = slice(tb * 512, (tb + 1) * 512)
            S.op("pool", lambda e: e.tensor_copy(out=kT[64:96, :, cols], in_=kr[64:96, cols].unsqueeze(1).broadcast_to([32, 8, 512])),
                 reads=[b_kr], writes=[b_kT])
        for tb in range(4):
            krb(tb)
        S.op("pool", lambda e: e.memset(Vaug[:, :, :, 64:128], 0.0), writes=[b_V])
        S.op("pool", lambda e: e.memset(Vaug[:, :, :, 64:65], 1.0), writes=[b_V])

        def vup(j):
            bk = 4 + (j % 4)
            S.pe([mm(bank(bk), ckvnT[:, j * 128:(j + 1) * 128], wuv[:])], reads=[b_wuv, b_ckvn], writes=[PB[bk]])
            pv = bank(bk).rearrange("p (q t d) -> p q t d", t=2, d=64)
            S.op("act", lambda e: e.activation(out=Vaug[:, j, :, 0:64], in_=pv[:, :, 0, :], func=AF.Copy), reads=[PB[bk]], writes=[b_V])
            S.op("dve", lambda e: e.tensor_copy(out=Vaug[:, j, :, 128:192], in_=pv[:, :, 1, :]), reads=[PB[bk]], writes=[b_V])
        for j in range(NT):
            vup(j)
        dump("kT", kT[0:96, :, :], [b_kT])
        dump("V", Vaug[:, :, :, :], [b_V])

        S.inherit(b_attnT, [b_w_in, b_wkr])
        for i in range(2):
            S.inherit(b_qT[i], [b_w_in, b_wkr])
        for i in range(NPT):
            S.inherit(b_pT[i], [b_w_in, b_wkr])
        SCALE = 1.0 / math.sqrt(96.0)

        def qup(s):
            qb, h = divmod(s, 8)
            cols = slice(qb * 512, (qb + 1) * 512)
            qt = qT[s % 2]
            bq = b_qT[s % 2]
            S.pe([mm(bank(5)[0:96, :], wuq[:, c, h * 96:(h + 1) * 96], cqnT[:, c, cols], c == 0, c == 1) for c in range(2)],
                 reads=[b_wuq, b_cqn], writes=[PB[5]])
            S.pe([mm(bank(6)[0:96, :], wqsw[:, c, h, :], cqnT[:, c, cols], c == 0, c == 1) for c in range(2)],
                 reads=[b_wqsw, b_cqn], writes=[PB[6]])
            S.op("dve", lambda e: e.tensor_copy(out=qt[0:64, :], in_=bank(5)[0:64, :]), reads=[PB[5]], writes=[bq])
            S.op("dve", lambda e: e.tensor_tensor(out=scr[0][rp, :], in0=bank(5)[rp, :], in1=COS[rp, cols], op=ALU.mult),
                 reads=[PB[5], b_cs], writes=[b_scr[0]])
            S.op("dve", lambda e: e.tensor_tensor(out=scr[1][rp, :], in0=bank(6)[rp, :], in1=SIN[rp, cols], op=ALU.mult),
                 reads=[PB[6], b_cs], writes=[b_scr[1]])
            S.op("dve", lambda e: e.tensor_tensor(out=qt[rp, :], in0=scr[0][rp, :], in1=scr[1][rp, :], op=ALU.add),
                 reads=[b_scr[0], b_scr[1]], writes=[bq])

        pctr = [0]

        def attn_step(s):
            qb, h = divmod(s, 8)
            pair, odd = divmod(h, 2)
            cols = slice(qb * 512, (qb + 1) * 512)
            qt = qT[s % 2]
            bq = b_qT[s % 2]
            ob = 3 + (s % 2)
            M = 128 if odd else 65

            def lhsV(kt):
                return Vaug[:, kt, pair, 64:192] if odd else Vaug[:, kt, pair, 0:65]

            def Smm(kt):
                bk = kt % 3
                S.pe([mm(bank(bk), kT[0:96, h, kt * 128:(kt + 1) * 128], qt[0:96, :])], reads=[b_kT, bq], writes=[PB[bk]])

            def EXPV(kt):
                pi = pctr[0] % NPT
                pctr[0] += 1
                p = pTt[pi]
                S.op("act", lambda e: e.activation(out=p[:], in_=bank(kt % 3), func=AF.Exp, scale=SCALE), reads=[PB[kt % 3]], writes=[b_pT[pi]])
                if kt + 2 < 16:
                    Smm(kt + 2)
                S.pe([mm(bank(ob)[0:M, :], lhsV(kt), p[:], kt == 0, kt == 15)], reads=[b_V, b_pT[pi]], writes=[PB[ob]])
            Smm(0)
            Smm(1)
            for kt in range(16):
                EXPV(kt)
                if kt == 2 and s > 0:
                    attn_norm(s - 1)

        def attn_norm(s):
            qb, h = divmod(s, 8)
            pair, odd = divmod(h, 2)
            cols = slice(qb * 512, (qb + 1) * 512)
            ob = 3 + (s % 2)
            if odd:
                dr, orow = slice(0, 1), slice(64, 128)
                lo = onesf[0:1, 0:128]
                bo = bank(7)
            else:
                dr, orow = slice(64, 65), slice(0, 64)
                lo = onesf[64:65, 0:64]
                bo = bank(7)[0:64, :]
            recip_act(scr[2][dr, :], bank(ob)[dr, :], [PB[ob]], [b_scr[2]])
            S.pe([mm(bo, lo, scr[2][dr, :])], reads=[b_onesf, b_scr[2]], writes=[PB[7]])
            S.op("dve", lambda e: e.tensor_copy(out=scr[3][orow, :], in_=bank(ob)[orow, :]), reads=[PB[ob]], writes=[b_scr[3]])
            S.op("dve", lambda e: e.tensor_tensor(out=attnT[orow, pair, cols], in0=scr[3][orow, :], in1=bank(7)[orow, :], op=ALU.mult),
                 reads=[b_scr[3], PB[7]], writes=[b_attnT])

        NSTEP = 32
        qup(0)
        for s in range(NSTEP):
            if s + 1 < NSTEP:
                qup(s + 1)
            attn_step(s)
        attn_norm(NSTEP - 1)
        drain["rate"] = 0.0
        while conv_todo:
            conv_todo.pop(0)()
        dump("acc", acc[:, :, :], b_acc)
        dump("attnT", attnT[:, :, :], [b_attnT])

        S.inherit(b_convT, [b_kr, b_ckvn])

        def conv_ln(tb):
            cols = slice(tb * 512, (tb + 1) * 512)
            S.pe([mm(bank(0), onesf[:], acc[:, cc, cols], cc == 0, cc == 3) for cc in range(4)], reads=[b_onesf] + b_acc, writes=[PB[0]])
            for cc in range(4):
                S.op("act", lambda e, cc=cc: e.activation(out=scr[3][:], in_=acc[:, cc, cols], func=AF.Square), reads=[b_acc[cc]], writes=[b_scr[3]])
                S.pe([mm(bank(1), onesf[:], scr[3][:], cc == 0, cc == 3)], reads=[b_onesf, b_scr[3]], writes=[PB[1]])
            S.op("dve", lambda e: e.tensor_scalar(out=scr[0][:], in0=bank(0), scalar1=1.0 / 512.0, scalar2=None, op0=ALU.mult),
                 reads=[PB[0]], writes=[b_scr[0]])
            S.op("dve", lambda e: e.tensor_tensor(out=scr[1][:], in0=scr[0][:], in1=scr[0][:], op=ALU.mult), reads=[b_scr[0]], writes=[b_scr[1]])
            S.op("dve", lambda e: e.scalar_tensor_tensor(out=scr[1][:], in0=bank(1), scalar=1.0 / 512.0, in1=scr[1][:], op0=ALU.mult, op1=ALU.subtract),
                 reads=[PB[1], b_scr[1]], writes=[b_scr[1]])
            rsqrt_act(scr[1][:], scr[1][:], cst[:, 2:3], [b_scr[1]], [b_scr[1]])
            for cc in range(4):
                S.op("dve", lambda e, cc=cc: e.tensor_tensor(out=scr[2][:], in0=acc[:, cc, cols], in1=scr[0][:], op=ALU.subtract),
                     reads=[b_acc[cc], b_scr[0]], writes=[b_scr[2]])
                S.op("dve", lambda e: e.tensor_tensor(out=scr[2][:], in0=scr[2][:], in1=scr[1][:], op=ALU.mult), reads=[b_scr[2], b_scr[1]], writes=[b_scr[2]])
                S.op("act", lambda e, cc=cc: e.activation(out=convT[:, cc, cols], in_=scr[2][:], func=AF.Silu, scale=gsm[:, 8 + cc:9 + cc], bias=gsm[:, 12 + cc:13 + cc]),
                     reads=[b_scr[2], b_gsm], writes=[b_convT])
        for tb in range(4):
            conv_ln(tb)
        dump("convT", convT[:, :, :], [b_convT])

        lnp = T([128, 2, D], F32, R6); b_lnp = Buf("lnp")
        S.inherit(b_lnp, [b_cs])
        ysbs = [T([128, D], F32, R7 + i * 4096) for i in range(2)]
        b_ysbs = [Buf("ysb0"), Buf("ysb1")]
        for bb in b_ysbs:
            S.inherit(bb, b_scr)
        ysb = ysbs[0]; b_ysb = b_ysbs[0]

        def load_lnp(i):
            S.dma("sp", lambda e: e.dma_start(out=lnp[:, 0, :], in_=ln_g[i].broadcast_to([128, D])), b_lnp)
            S.dma("sp", lambda e: e.dma_start(out=lnp[:, 1, :], in_=ln_b[i].broadcast_to([128, D])), b_lnp)

        def ln_tile(j, xin_ap, xin_bufs, pk, tpk, extra=None, final_out=None):
            yb = ysbs[j % 2]
            byb = b_ysbs[j % 2]
            so = (j % 2) * 16
            bs = b_st[j % 2]
            st_ = lambda a, n=1: stat2[:, so + a:so + a + n]
            if pk is not None:
                pbs = [PB[2 * pk], PB[2 * pk + 1]]
                S.op("dve", lambda e: e.scalar_tensor_tensor(out=yb[:], in0=xin_ap, scalar=ALPHA, in1=PP[pk][:], op0=ALU.mult, op1=ALU.add),
                     reads=pbs + xin_bufs, writes=[byb])
                ysrc, ysrc_b = yb[:], [byb]
            else:
                ysrc, ysrc_b = resid[:, j, :], [b_res[j]]
            S.op("dve", lambda e: e.bn_stats(out=st_(0, 6), in_=ysrc[:, 0:512]), reads=ysrc_b, writes=[bs])
            S.op("dve", lambda e: e.bn_stats(out=st_(6, 6), in_=ysrc[:, 512:1024]), reads=ysrc_b, writes=[bs])
            S.op("dve", lambda e: e.bn_aggr(out=st_(12, 2), in_=st_(0, 12)), reads=[bs], writes=[bs])
            rsqrt_act(st_(14), st_(13), cst[:, 2:3], [bs], [bs])
            S.op("dve", lambda e: e.scalar_tensor_tensor(out=st_(15), in0=st_(12), scalar=-1.0, in1=st_(14), op0=ALU.mult, op1=ALU.mult), reads=[bs], writes=[bs])
            S.op("act", lambda e: e.activation(out=yb[:], in_=ysrc, func=AF.Identity, scale=st_(14), bias=st_(15)), reads=ysrc_b + [bs], writes=[byb])
            S.op("dve", lambda e: e.tensor_tensor(out=yb[:], in0=yb[:], in1=lnp[:, 0, :], op=ALU.mult), reads=[byb, b_lnp], writes=[byb])
            S.op("pool", lambda e: e.tensor_tensor(out=resid[:, j, :], in0=yb[:], in1=lnp[:, 1, :], op=ALU.add), reads=[byb, b_lnp], writes=[b_res[j]])
            if final_out is not None:
                final_out(j)
                return
            if extra is not None:
                extra(j)
            tpb = [PB[2 * tpk], PB[2 * tpk + 1]]
            S.pe([lambda e, c=c: e.transpose(PP[tpk][:, c * 128:(c + 1) * 128], resid[:, j, c * 128:(c + 1) * 128], ident[:]) for c in range(8)],
                 reads=[b_res[j], b_ident], writes=tpb)
            evac(actT[:, :, j * 128:(j + 1) * 128], PP[tpk][:].rearrange("p (c t) -> p c t", t=128), tpb, [b_actT[j // 4]])

        wo_sb = T([128, 8, D], BF16, R3); b_wo = Buf("wo")
        S.inherit(b_wo, [b_V])
        w_o_v = w_o.rearrange("(c p) f -> p c f", p=128)
        for c0 in range(0, 8, 4):
            S.dma("pool", lambda e, c0=c0: e.dma_start(out=wo_sb[:, c0:c0 + 4, :], in_=w_o_v[:, c0:c0 + 4, :]), b_wo)
        load_lnp(0)
        xst2 = [T([128, D], F32, R4 + i * 4096) for i in range(2)]; b_xst2 = [Buf("xs2_%d" % i) for i in range(2)]
        for i in range(2):
            S.inherit(b_xst2[i], [b_cqn])
        for j in range(NT):
            S.inherit(b_res[j], b_u + b_acc + b_xst + [b_pos])
        for t in range(4):
            S.inherit(b_actT[t], [b_kT])

        def wo_tile(j):
            st = j % 2
            S.dma("sp", lambda e: e.dma_start(out=xst2[st][:], in_=xv[j]), b_xst2[st])
            tsl = slice(j * 128, (j + 1) * 128)
            pk = (j % 2) * 2
            for half in range(2):
                hs = slice(half * 512, (half + 1) * 512)
                bk = 2 * pk + half
                fns = [mm(bank(bk), attnT[:, pr, tsl], wo_sb[:, pr, hs], pr == 0, False) for pr in range(4)]
                fns += [mm(bank(bk), convT[:, cc, tsl], wo_sb[:, 4 + cc, hs], False, cc == 3) for cc in range(4)]
                S.pe(fns, reads=[b_attnT, b_convT, b_wo], writes=[PB[bk]])
            ln_tile(j, xst2[st][:], [b_xst2[st]], pk, pk + 1)
        for j in range(NT):
            wo_tile(j)
        dump("h1", resid[:, :, :], b_res)
        if stop_after == "mixer":
            S.wait_bufs("sp", [b_dbg])
            S.emit()
            return nc

        phaseA = [b_attnT, b_convT, b_wo, b_cqn, b_kr, b_ckvn, b_w_in, b_wkr, b_V, b_wuq, b_wqsw, b_wuk, b_wuv] + b_qT + b_pT + b_xst2
        B0 = R2
        xw = {}
        bxw = {}
        for i, k in enumerate("qokv"):
            xw[k] = T([128, 8, D], BF16, B0 + i * 16384)
            bxw[k] = Buf("xw" + k)
            S.inherit(bxw[k], phaseA)
        xqT = [T([128, 2, 512], BF16, B0 + 65536 + i * 2048) for i in range(2)]; b_xq = [Buf("xq%d" % i) for i in range(2)]
        xvv = T([128, 2, D], BF16, B0 + 69632); b_xv = Buf("xv")
        pT2 = [T([128, 512], BF16, B0 + 73728 + i * 1024) for i in range(2)]; b_p2 = [Buf("p2_%d" % i) for i in range(2)]
        oT = T([128, 8, 512], BF16, R8); b_oT = Buf("oT")
        TAIL = CEND + 32
        memT = T([128, 8, 256], BF16, TAIL); b_memT = Buf("memT")
        xkT = T([128, 8, 256], BF16, TAIL + 4096); b_xk = Buf("xkT")
        rscr = T([128, 512], F32, TAIL + 8448); b_rscr = Buf("rscr")
        assert TAIL + 8448 + 2048 <= 229376, TAIL
        for bb in b_xq + [b_xv] + b_p2 + [b_oT]:
            S.inherit(bb, phaseA)
        for k in "kvqo":
            wv_ = xa_w[k].rearrange("(c p) f -> p c f", p=128)
            for c0 in range(0, 8, 4):
                S.dma("pool", lambda e, k=k, c0=c0, wv_=wv_: e.dma_start(out=xw[k][:, c0:c0 + 4, :], in_=wv_[:, c0:c0 + 4, :]), bxw[k])
        load_lnp(1)
        memv = mem.rearrange("(j p) d -> j p d", p=128)

        def mem_tile(mt):
            S.dma("sp", lambda e: e.dma_start(out=ysbs[mt][:], in_=memv[mt]), b_ysbs[mt])
            S.pe([lambda e, c=c: e.transpose(PP[1][:, c * 128:(c + 1) * 128], ysbs[mt][:, c * 128:(c + 1) * 128], ident[:]) for c in range(8)],
                 reads=[b_ysbs[mt], b_ident], writes=[PB[2], PB[3]])
            evac(memT[:, :, mt * 128:(mt + 1) * 128], PP[1][:].rearrange("p (c t) -> p c t", t=128), [PB[2], PB[3]], [b_memT])
        for mt in range(2):
            mem_tile(mt)

        def xk_fc(fc):
            bk = fc % 2
            S.pe([mm(bank(bk)[:, 0:256], xw["k"][:, c, fc * 128:(fc + 1) * 128], memT[:, c, :], c == 0, c == 7) for c in range(8)],
                 reads=[bxw["k"], b_memT], writes=[PB[bk]])
            evac(xkT[:, fc, :], bank(bk)[:, 0:256], [PB[bk]], [b_xk])
        for fc in range(8):
            xk_fc(fc)

        def xv_mh(mt, half):
            bk = 2 + (mt * 2 + half) % 2
            hs = slice(half * 512, (half + 1) * 512)
            S.pe([mm(bank(bk), memT[:, c, mt * 128:(mt + 1) * 128], xw["v"][:, c, hs], c == 0, c == 7) for c in range(8)],
                 reads=[bxw["v"], b_memT], writes=[PB[bk]])
            evac(xvv[:, mt, hs], bank(bk), [PB[bk]], [b_xv])
        for mt in range(2):
            for half in range(2):
                xv_mh(mt, half)

        h2tm = T([128, NT, D], BF16, B0 + 32768); b_h2tm = Buf("h2tm")
        S.inherit(b_h2tm, [bxw["k"], bxw["v"]])

        def xa_head(tb, hh):
            cols = slice(tb * 512, (tb + 1) * 512)
            xq = xqT[hh % 2]
            bq = b_xq[hh % 2]
            for i in range(2):
                fc = 2 * hh + i
                S.pe([mm(bank(i), xw["q"][:, c, fc * 128:(fc + 1) * 128], actT[:, c, cols], c == 0, c == 7) for c in range(8)],
                     reads=[bxw["q"], b_actT[tb]], writes=[PB[i]])
                evac(xq[:, i, :], bank(i), [PB[i]], [bq])
            for mt in range(2):
                S.pe([mm(bank(2 + mt), xkT[:, 2 * hh + i, mt * 128:(mt + 1) * 128], xq[:, i, :], i == 0, i == 1) for i in range(2)],
                     reads=[b_xk, bq], writes=[PB[2 + mt]])
                S.op("act", lambda e, mt=mt: e.activation(out=pT2[mt][:], in_=bank(2 + mt), func=AF.Exp, scale=1.0 / 16.0),
                     reads=[PB[2 + mt]], writes=[b_p2[mt]])
            S.pe([mm(bank(4), onesb[:], pT2[mt][:], mt == 0, mt == 1) for mt in range(2)], reads=[b_onesb] + b_p2, writes=[PB[4]])
            recip_act(rscr[:], bank(4), [PB[4]], [b_rscr])
            for dc in range(2):
                S.pe([mm(bank(5 + dc), xvv[:, mt, hh * 256 + dc * 128:hh * 256 + (dc + 1) * 128], pT2[mt][:], mt == 0, mt == 1) for mt in range(2)],
                     reads=[b_xv] + b_p2, writes=[PB[5 + dc]])
                S.op("dve", lambda e, dc=dc: e.tensor_tensor(out=oT[:, hh * 2 + dc, :], in0=bank(5 + dc), in1=rscr[:], op=ALU.mult),
                     reads=[PB[5 + dc], b_rscr], writes=[b_oT])

        def cast_h2(j):
            S.op("pool", lambda e: e.tensor_copy(out=h2tm[:, j, :], in_=resid[:, j, :]), reads=[b_res[j]], writes=[b_h2tm])

        def xa_out(tb, jj):
            j = tb * 4 + jj
            tsl = slice(jj * 128, (jj + 1) * 128)
            for half in range(2):
                hs = slice(half * 512, (half + 1) * 512)
                S.pe([mm(bank(6 + half), oT[:, fc, tsl], xw["o"][:, fc, hs], fc == 0, fc == 7) for fc in range(8)],
                     reads=[b_oT, bxw["o"]], writes=[PB[6 + half]])
            ln_tile(j, resid[:, j, :], [b_res[j]], 3, 0, extra=cast_h2)
        for tb in range(4):
            for hh in range(4):
                xa_head(tb, hh)
            for jj in range(4):
                xa_out(tb, jj)
        dump("h2", resid[:, :, :], b_res)
        if stop_after == "xattn":
            S.wait_bufs("sp", [b_dbg])
            S.emit()
            return nc

        phaseB = [bxw["q"], bxw["o"], b_xv, b_oT, b_memT, b_xk] + b_xq + b_p2
        wr_sb = T([128, 8, NE], BF16, TAIL); b_wr = Buf("wr")
        S.inherit(b_wr, [b_memT])
        with_nc = w_router.rearrange("(c p) e -> p c e", p=128)
        S.dma("pool", lambda e: e.dma_start(out=wr_sb[:], in_=with_nc), b_wr)
        esel = T([16, 16 * 128], F32, TAIL + 256); b_esel = Buf("esel")
        S.inherit(b_esel, [b_memT, b_xk])
        S.dma("sp", lambda e: e.dma_start(out=esel[:], in_=c_esel), b_esel)
        aff = T([16, S_], F32, B0); affw = [T([16, S_], F32, B0 + 8192), T([16, S_], F32, B0 + 16384)]
        b_aff = Buf("aff"); b_affw = [Buf("affw0"), Buf("affw1")]
        gate = T([16, CAP], F32, B0 + 24576); idxu = T([16, CAP], U32, B0 + 25600); idxf = T([16, CAP], F32, B0 + 26624)
        b_gate = Buf("gate"); b_idxu = Buf("idxu"); b_idxf = Buf("idxf")
        idxT = T([128, 2, NE], F32, B0 + 27648); gateT = T([128, 2, NE], F32, B0 + 27776); b_igT = Buf("igT")
        idxs = T([128, 32], F32, B0 + 27904); b_idxs = Buf("idxs")
        for bb in [b_aff, b_gate, b_idxu, b_idxf, b_igT, b_idxs] + b_affw:
            S.inherit(bb, phaseB)
        for tb in range(4):
            def router_tb(tb=tb):
                cols = slice(tb * 512, (tb + 1) * 512)
                S.pe([mm(bank(tb)[0:16, :], wr_sb[:, c, :], actT[:, c, cols], c == 0, c == 7) for c in range(8)],
                     reads=[b_wr, b_actT[tb]], writes=[PB[tb]])
                S.op("act", lambda e: e.activation(out=affw[0][:, cols], in_=bank(tb)[0:16, :], func=AF.Exp), reads=[PB[tb]], writes=[b_affw[0]])
                S.pe([mm(bank(4 + tb)[0:16, :], onesf[0:16, 0:16], affw[0][:, cols])], reads=[b_onesf, b_affw[0]], writes=[PB[4 + tb]])
                S.op("dve", lambda e: e.reciprocal(out=affw[1][:, cols], in_=bank(4 + tb)[0:16, :]), reads=[PB[4 + tb]], writes=[b_affw[1]])
                S.op("dve", lambda e: e.tensor_tensor(out=aff[:, cols], in0=affw[0][:, cols], in1=affw[1][:, cols], op=ALU.mult),
                     reads=b_affw, writes=[b_aff])
            router_tb()
        dump("aff", aff[:, :], [b_aff])
        for r in range(CAP // 8):
            def topk_round(r=r):
                src = aff if r == 0 else affw[(r - 1) % 2]
                bsrc = b_aff if r == 0 else b_affw[(r - 1) % 2]
                dst = affw[r % 2]
                sl = slice(r * 8, r * 8 + 8)
                S.op("dve", lambda e: e.max(out=gate[:, sl], in_=src[:]), reads=[bsrc], writes=[b_gate])
                S.op("dve", lambda e: e.max_index(out=idxu[:, sl], in_max=gate[:, sl], in_values=src[:]), reads=[bsrc, b_gate], writes=[b_idxu])
                if r + 1 < CAP // 8:
                    S.op("dve", lambda e: e.match_replace(out=dst[:], in_to_replace=gate[:, sl], in_values=src[:], imm_value=-1.0),
                         reads=[bsrc, b_gate], writes=[b_affw[r % 2]])
            topk_round()
        S.op("dve", lambda e: e.tensor_copy(out=idxf[:], in_=idxu[:]), reads=[b_idxu], writes=[b_idxf])
        dump("gate", gate[:, :], [b_gate])
        dump("idxf", idxf[:, :], [b_idxf])
        S.pe([lambda e, ch=ch: e.transpose(bank(0)[:, ch * 16:(ch + 1) * 16], idxf[0:16, ch * 128:(ch + 1) * 128], ident[0:16, 0:16]) for ch in range(2)]
             + [lambda e, ch=ch: e.transpose(bank(0)[:, 32 + ch * 16:32 + (ch + 1) * 16], gate[0:16, ch * 128:(ch + 1) * 128], ident[0:16, 0:16]) for ch in range(2)],
             reads=[b_idxf, b_gate, b_ident], writes=[PB[0]])
        S.op("dve", lambda e: e.tensor_copy(out=idxT[:].rearrange("p a b -> p (a b)"), in_=bank(0)[:, 0:32]), reads=[PB[0]], writes=[b_igT])
        S.op("dve", lambda e: e.tensor_copy(out=gateT[:].rearrange("p a b -> p (a b)"), in_=bank(0)[:, 32:64]), reads=[PB[0]], writes=[b_igT])

        for j in range(NT):
            S.op("pool", lambda e, j=j: e.tensor_scalar(out=resid[:, j, :], in0=resid[:, j, :], scalar1=ALPHA, scalar2=None, op0=ALU.mult),
                 reads=[b_res[j]], writes=[b_res[j]])

        C1 = R1
        SelT = [T([128, NT, CAP], BF16, C1 + i * 8192) for i in range(2)]; b_sel = [Buf("sel%d" % i) for i in range(2)]
        GRP = 2
        ysc = [T([128, GRP * 2, D], BF16, C1 + 16384 + i * 8192) for i in range(2)]; b_ysc = [Buf("ysc%d" % i) for i in range(2)]
        for bb in b_sel + b_ysc:
            S.inherit(bb, b_actT)
        xsT = [T([128, 8, CAP], BF16, B0 + 65536 + i * 4096) for i in range(2)]; b_xs = [Buf("xs%d" % i) for i in range(2)]
        hidT = [T([128, 2, CAP], BF16, B0 + 73728 + i * 1024) for i in range(2)]; b_hid = [Buf("hid%d" % i) for i in range(2)]
        for bb in b_xs + b_hid:
            S.inherit(bb, phaseB)
        selsc = [T([128, GRP * 2, 128], BF16, R8 + i * 1024) for i in range(2)]; b_selsc = [Buf("selsc%d" % i) for i in range(2)]
        silu_t = [T([128, 512], F32, R8 + 2048 + i * 2048) for i in range(2)]; b_silu = [Buf("silu%d" % i) for i in range(2)]
        idxs = [T([128, 4], F32, R8 + 6144 + i * 32) for i in range(2)]; b_idxs2 = [Buf("idxs%d" % i) for i in range(2)]
        for bb in b_selsc + b_silu + b_idxs2:
            S.inherit(bb, [b_oT])
        NRING = 3
        ring_off = [B0, B0 + 12288, R6]
        ringG = [T([128, 8, 256], BF16, ring_off[i]) for i in range(NRING)]
        ringU = [T([128, 8, 256], BF16, ring_off[i] + 4096) for i in range(NRING)]
        ringD = [T([128, 2, D], BF16, ring_off[i] + 8192) for i in range(NRING)]
        b_ring = [Buf("ring%d" % i) for i in range(NRING)]
        for bb in b_ring[0:2]:
            S.inherit(bb, [b_aff] + b_affw + phaseB)
        S.inherit(b_ring[2], [b_lnp] + b_ysbs)

        def load_unit(n):
            e, fb = divmod(n, 8)
            sl = n % NRING
            fs = slice(fb * 256, (fb + 1) * 256)
            S.dma("pool", lambda e_: e_.dma_start(out=ringG[sl][:], in_=w_gate[e].rearrange("(c p) f -> p c f", p=128)[:, :, fs]), b_ring[sl])
            S.dma("pool", lambda e_: e_.dma_start(out=ringU[sl][:], in_=w_up[e].rearrange("(c p) f -> p c f", p=128)[:, :, fs]), b_ring[sl])
            S.dma("pool", lambda e_: e_.dma_start(out=ringD[sl][:], in_=w_down[e][fb * 256:(fb + 1) * 256, :].rearrange("(c p) d -> p c d", p=128)), b_ring[sl])

        def gather_expert(e):
            st = SelT[e % 2]
            bs_ = b_sel[e % 2]
            xs = xsT[e % 2]
            S.pe([mm(bank(1)[:, 0:CAP], esel[0:16, e * 128:(e + 1) * 128], idxf[0:16, :])], reads=[b_esel, b_idxf], writes=[PB[1]])
            S.op("dve", lambda e_: e_.tensor_tensor(out=st[:], in0=bank(1)[:, 0:CAP].unsqueeze(1).broadcast_to([128, NT, CAP]),
                                                    in1=tokid[:, :].unsqueeze(2).broadcast_to([128, NT, CAP]), op=ALU.is_equal),
                 reads=[PB[1], b_tokid], writes=[bs_])
            for ps_ in range(2):
                for dcl in range(4):
                    dc = ps_ * 4 + dcl
                    bk = dcl // 2
                    co = (dcl % 2) * 256
                    S.pe([mm(bank(bk)[:, co:co + 256], h2tm[:, j, dc * 128:(dc + 1) * 128], st[:, j, :], j == 0, j == NT - 1) for j in range(NT)],
                         reads=[b_h2tm, bs_], writes=[PB[bk]])
                evac(xs[:, ps_ * 4:(ps_ + 1) * 4, :], PP[0][:].rearrange("p (d c) -> p d c", c=CAP), [PB[0], PB[1]], [b_xs[e % 2]])

        def unit_banks(n):
            return (2, 3) if n % 2 == 0 else (0, 1)

        def ffn_gu(n):
            e, fb = divmod(n, 8)
            sl = n % NRING
            xs = xsT[e % 2]
            hd = hidT[n % 2]
            bg, bu = unit_banks(n)
            for (bk, W) in ((bg, ringG[sl]), (bu, ringU[sl])):
                fns = []
                for fl in range(2):
                    fns += [mm(bank(bk)[:, fl * 256:(fl + 1) * 256], W[:, dc, fl * 128:(fl + 1) * 128], xs[:, dc, :], dc == 0, dc == 7) for dc in range(8)]
                S.pe(fns, reads=[b_ring[sl], b_xs[e % 2]], writes=[PB[bk]])
            sv = silu_t[n % 2]
            S.op("act", lambda e_: e_.activation(out=sv[:], in_=bank(bg), func=AF.Silu), reads=[PB[bg]], writes=[b_silu[n % 2]])
            S.op("dve", lambda e_: e_.tensor_tensor(out=hd[:].rearrange("p a b -> p (a b)"), in0=sv[:], in1=bank(bu), op=ALU.mult),
                 reads=[b_silu[n % 2], PB[bu]], writes=[b_hid[n % 2]])

        def ffn_down(n):
            e, fb = divmod(n, 8)
            sl = n % NRING
            hd = hidT[n % 2]
            for cc2 in range(2):
                for half in range(2):
                    bk = 4 + cc2 * 2 + half
                    S.pe([mm(bank(bk), hd[:, fl, cc2 * 128:(cc2 + 1) * 128], ringD[sl][:, fl, half * 512:(half + 1) * 512],
                             fb == 0 and fl == 0, fb == 7 and fl == 1) for fl in range(2)],
                         reads=[b_hid[n % 2], b_ring[sl]], writes=[PB[bk]])

        def finish_expert(e):
            g = e // GRP
            for cc2 in range(2):
                S.op("act", lambda e_, cc2=cc2: e_.activation(out=ysc[g % 2][:, (e % GRP) * 2 + cc2, :], in_=PP[2 + cc2][:], func=AF.Copy,
                                                              scale=gateT[:, cc2, e:e + 1]),
                     reads=[PB[4 + 2 * cc2], PB[5 + 2 * cc2], b_igT], writes=[b_ysc[g % 2]])

        sc_todo = []

        def scatter_group(g):
            def sc_tile(j, pp):
                ix = idxs[j % 2]
                bi = b_idxs2[j % 2]
                sc = selsc[j % 2]
                bsc = b_selsc[j % 2]
                S.op("dve", lambda e_: e_.tensor_scalar(out=ix[:, 0:4].rearrange("p (e c) -> p e c", c=2),
                                                        in0=idxT[:, :, g * GRP:(g + 1) * GRP].rearrange("p c e -> p e c"),
                                                        scalar1=-128.0 * j, scalar2=None, op0=ALU.add), reads=[b_igT], writes=[bi])
                S.op("dve", lambda e_: e_.tensor_tensor(out=sc[:], in0=ix[:, 0:4].unsqueeze(2).broadcast_to([128, 4, 128]),
                                                        in1=iota[:, :].unsqueeze(1).broadcast_to([128, 4, 128]), op=ALU.is_equal),
                     reads=[bi, b_iota], writes=[bsc])
                pbs = [PB[2 * pp], PB[2 * pp + 1]]
                S.pe([mm(PP[pp][:, half * 512:(half + 1) * 512], sc[:, k, :], ysc[g % 2][:, k, half * 512:(half + 1) * 512], k == 0, k == 3)
                      for half in range(2) for k in range(4)], reads=[bsc, b_ysc[g % 2]], writes=pbs)
                S.op("dve", lambda e_: e_.tensor_tensor(out=resid[:, j, :], in0=resid[:, j, :], in1=PP[pp][:], op=ALU.add),
                     reads=[b_res[j]] + pbs, writes=[b_res[j]])
            for j in range(NT):
                sc_todo.append(lambda pp, j=j: sc_tile(j, pp))

        NEXP = NE
        NU = NEXP * 8
        for n0 in range(NRING):
            load_unit(n0)
        gather_expert(0)
        ffn_gu(0)
        for n in range(NU):
            e, fb = divmod(n, 8)
            if n + 1 < NU:
                if (n + 1) % 8 == 0:
                    gather_expert((n + 1) // 8)
                ffn_gu(n + 1)
            ffn_down(n)
            if n + NRING < NU:
                load_unit(n + NRING)
            if sc_todo:
                sc_todo.pop(0)(1 if n % 2 == 0 else 0)
            if fb == 7:
                finish_expert(e)
                if e % GRP == GRP - 1:
                    scatter_group(e // GRP)
        k_ = 0
        while sc_todo:
            sc_todo.pop(0)(k_ % 2)
            k_ += 1

        for bb in [b_lnp] + b_ysbs:
            S.inherit(bb, [b_ring[2]])
        load_lnp(2)
        outv = out.rearrange("(j p) d -> j p d", p=128)
        b_out = Buf("out")

        def out_tile(j):
            S.dma("sp", lambda e: e.dma_start(out=outv[j], in_=resid[:, j, :]), b_out, reads=[b_res[j]], writes=[b_out])
        for j in range(NT):
            ln_tile(j, None, [], None, None, final_out=out_tile)
        S.wait_bufs("sp", [b_out, b_dbg])
        S.emit()
    return nc


def make_consts():
    c = {}
    c["c_ident"] = np.eye(128, dtype=np.float32)
    c["c_iota"] = np.tile(np.arange(128, dtype=np.float32)[None, :], (128, 1))
    c["c_tokid"] = (np.arange(NT, dtype=np.float32)[None, :] * 128 + np.arange(128, dtype=np.float32)[:, None]).astype(np.float32)
    es = np.zeros((16, 16, 128), np.float32)
    for e in range(16):
        es[e, e, :] = 1.0
    c["c_esel"] = es.reshape(16, 16 * 128)
    rope = np.zeros((128, 2), np.float32)
    inv_freq = (10000.0 ** (-np.arange(0, 32, 2, dtype=np.float32) / 32.0)).astype(np.float32)
    for p in range(64, 96):
        rope[p, 0] = np.float32(np.float64(inv_freq[(p - 64) % 16]) / (2.0 * np.pi))
        rope[p, 1] = -1.0 if p < 80 else 1.0
    c["c_rope"] = rope
    return c


def make_in_maps(inputs, n_cores=8):
    consts = make_consts()
    maps = []
    f = lambda a: np.ascontiguousarray(np.asarray(a))
    for b in range(n_cores):
        m = dict(consts)
        m["x"] = f(inputs["x"][b])
        m["mem"] = f(inputs["mem"][b])
        m["positions"] = f(np.asarray(inputs["positions"])[b:b + 1]).astype(np.int32)
        for k in ["w_in", "q_norm_g", "w_uq", "kv_norm_g", "w_uk", "w_uv", "conv_w", "conv_b", "conv_ln_g", "conv_ln_b",
                  "w_o", "xa_w_q", "xa_w_k", "xa_w_v", "xa_w_o", "w_router", "w_gate", "w_up", "w_down"]:
            m[k] = f(np.asarray(inputs[k])[0])
        for k in ["ln1_g", "ln1_b", "ln2_g", "ln2_b", "ln3_g", "ln3_b"]:
            m[k] = f(np.asarray(inputs[k])[0:1])
        maps.append(m)
    return maps


def kernel(**inputs):
    nc = build_program()
    maps = make_in_maps(inputs, 8)
    res = run_bass_kernel_spmd(nc, maps, core_ids=list(range(8)))
    return np.stack([r["out"] for r in res.results], axis=0).astype(np.float32)
```

```python
import math
import os
from contextlib import ExitStack

import numpy as np
import concourse.bass as bass
import concourse.mybir as mybir
from concourse.bass_utils import run_bass_kernel_spmd

F32 = mybir.dt.float32
BF16 = mybir.dt.bfloat16
I32 = mybir.dt.int32
U32 = mybir.dt.uint32
ALU = mybir.AluOpType
AF = mybir.ActivationFunctionType

S_ = 2048
D = 1024
NT = 16
EPS = 1e-5
ALPHA = 2.0 ** 0.25
NE = 16
CAP = 256
FF = 2048


class Buf:
    __slots__ = ("name", "w", "r", "sem", "cnt")

    def __init__(self, name):
        self.name = name
        self.w = None
        self.r = {}
        self.sem = None
        self.cnt = 0


class Sched:
    ENG = ["pe", "act", "dve", "pool", "sp"]

    def __init__(self, nc, es):
        self.nc = nc
        self.es = es
        self.streams = {e: [] for e in self.ENG}
        self.cnt = {e: 0 for e in self.ENG}
        self.esem = {e: es.enter_context(nc.semaphore("c_" + e)) for e in self.ENG}
        self.waited = {e: {} for e in self.ENG}
        self.nsem = len(self.ENG)

    def new_sem(self, name):
        self.nsem += 1
        return self.es.enter_context(self.nc.semaphore("d%d_%s" % (self.nsem, name)))

    def _waits(self, eng, reads, writes, skip_sem=None):
        deps = {}

        def add(s, v):
            if deps.get(s, 0) < v:
                deps[s] = v
        for b in reads:
            if b.w is not None:
                add(*b.w)
        for b in writes:
            if b.w is not None:
                add(*b.w)
            for s, v in b.r.items():
                add(s, v)
        own = self.esem[eng]
        wd = self.waited[eng]
        for s, v in deps.items():
            if s == own and eng == "pe":
                continue
            if skip_sem is not None and s == skip_sem:
                continue
            if wd.get(s, 0) < v:
                wd[s] = v
                self.streams[eng].append(("wait", s, v))

    def _commit(self, ev, reads, writes):
        s, v = ev
        for b in reads:
            if b.r.get(s, 0) < v:
                b.r[s] = v
        for b in writes:
            b.w = ev
            b.r = {}

    def op(self, eng, fn, reads=(), writes=()):
        self._waits(eng, reads, writes)
        self.cnt[eng] += 1
        s = self.esem[eng]
        self.streams[eng].append(("op", fn, s, 1))
        self._commit((s, self.cnt[eng]), reads, writes)

    def pe(self, fns, reads=(), writes=()):
        self._waits("pe", reads, writes)
        self.cnt["pe"] += 1
        s = self.esem["pe"]
        for f in fns[:-1]:
            self.streams["pe"].append(("op", f, None, 0))
        self.streams["pe"].append(("op", fns[-1], s, 1))
        self._commit((s, self.cnt["pe"]), reads, writes)

    def dma(self, eng, fn, dst, reads=(), writes=None):
        if writes is None:
            writes = (dst,)
        if dst.sem is None:
            dst.sem = self.new_sem(dst.name)
        self._waits(eng, reads, writes, skip_sem=dst.sem)
        dst.cnt += 16
        self.streams[eng].append(("op", fn, dst.sem, 16))
        self._commit((dst.sem, dst.cnt), reads, writes)

    def wait_bufs(self, eng, bufs):
        self._waits(eng, bufs, bufs)

    def inherit(self, new, olds):
        for o in olds:
            if o.w is not None:
                s, v = o.w
                if new.r.get(s, 0) < v:
                    new.r[s] = v
            for s, v in o.r.items():
                if new.r.get(s, 0) < v:
                    new.r[s] = v

    def emit(self):
        nc = self.nc
        streams = self.streams

        def run(name, eng):
            for item in streams[name]:
                if item[0] == "wait":
                    eng.wait_ge(item[1], item[2])
                else:
                    ins = item[1](eng)
                    if item[2] is not None:
                        ins.then_inc(item[2], item[3])

        with nc.Block() as block:
            @block.tensor
            def _(e):
                run("pe", e)

            @block.scalar
            def _(e):
                run("act", e)

            @block.vector
            def _(e):
                run("dve", e)

            @block.gpsimd
            def _(e):
                run("pool", e)

            @block.sync
            def _(e):
                run("sp", e)


def build_program(stop_after=None, dumps=()):
    nc = bass.Bass("TRN2", target_bir_lowering=False)

    def din(name, shape, dt=F32):
        return nc.dram_tensor(name, list(shape), dt, kind="ExternalInput").ap()

    x = din("x", [S_, D])
    mem = din("mem", [256, D])
    pos = din("positions", [1, S_], I32)
    w_in = din("w_in", [D, 1440])
    q_norm_g = din("q_norm_g", [256])
    w_uq = din("w_uq", [256, 768])
    kv_norm_g = din("kv_norm_g", [128])
    w_uk = din("w_uk", [128, 512])
    w_uv = din("w_uv", [128, 512])
    conv_w = din("conv_w", [31, 512])
    conv_b = din("conv_b", [512])
    conv_ln_g = din("conv_ln_g", [512])
    conv_ln_b = din("conv_ln_b", [512])
    w_o = din("w_o", [D, D])
    ln_g = [din("ln%d_g" % i, [1, D]) for i in (1, 2, 3)]
    ln_b = [din("ln%d_b" % i, [1, D]) for i in (1, 2, 3)]
    xa_w = {k: din("xa_w_" + k, [D, D]) for k in "qkvo"}
    w_router = din("w_router", [D, NE])
    w_gate = din("w_gate", [NE, D, FF])
    w_up = din("w_up", [NE, D, FF])
    w_down = din("w_down", [NE, FF, D])
    c_ident = din("c_ident", [128, 128])
    c_iota = din("c_iota", [128, 128])
    c_tokid = din("c_tokid", [128, NT])
    c_esel = din("c_esel", [16, 16 * 128])
    c_rope = din("c_rope", [128, 2])
    out = nc.dram_tensor("out", [S_, D], F32, kind="ExternalOutput").ap()
    dump_d = {}
    for (nm, shape, dt) in dumps:
        dump_d[nm] = nc.dram_tensor("dbg_" + nm, list(shape), dt, kind="ExternalOutput").ap()

    es = ExitStack()
    with es:
        S = Sched(nc, es)
        cnt = [0]

        def T(shape, dt, off, name=None):
            cnt[0] += 1
            return nc.alloc_sbuf_tensor_at("%s_%d" % (name or "t", cnt[0]), list(shape), dt, offset=off)

        b_dbg = Buf("dbg")
        conv_todo = []
        drain = {"rate": 0.0, "credit": 0.0, "busy": False}
        _raw_op = S.op

        def op_hook(eng, fn, reads=(), writes=()):
            _raw_op(eng, fn, reads=reads, writes=writes)
            if eng == "dve" and drain["rate"] > 0.0 and not drain["busy"] and conv_todo:
                drain["credit"] += drain["rate"]
                drain["busy"] = True
                while drain["credit"] >= 1.0 and conv_todo:
                    conv_todo.pop(0)()
                    drain["credit"] -= 1.0
                drain["busy"] = False
        S.op = op_hook

        def rsqrt_act(dst, src, bias_ap, rd, wr):
            S.op("act", lambda e: e.activation(out=dst, in_=src, func=AF.Ln, bias=bias_ap, scale=1.0), reads=rd + [b_cst], writes=wr)
            S.op("act", lambda e: e.activation(out=dst, in_=dst, func=AF.Exp, scale=-0.5), reads=wr, writes=wr)

        def recip_act(dst, src, rd, wr):
            S.op("act", lambda e: e.activation(out=dst, in_=src, func=AF.Ln), reads=rd, writes=wr)
            S.op("act", lambda e: e.activation(out=dst, in_=dst, func=AF.Exp, scale=-1.0), reads=wr, writes=wr)

        def dump(nm, ap, bufs):
            if nm in dump_d:
                S.dma("sp", lambda e: e.dma_start(out=dump_d[nm], in_=ap), b_dbg, reads=bufs, writes=[b_dbg])

        R0 = 16640
        R1 = R0 + 66048
        R2 = R1 + 32768
        R3 = R2 + 26624
        R4 = R3 + 24576
        R5 = R4 + 8192
        R6 = R5 + 16384
        R7 = R6 + 8192
        R8 = R7 + 8192
        R9 = R8 + 8192
        ident = T([128, 128], F32, R9); b_ident = Buf("ident")
        identb = T([128, 128], BF16, R9 + 512); b_identb = Buf("identb")
        onesb = T([128, 128], BF16, R9 + 768); b_onesb = Buf("onesb")
        onesf = T([128, 128], F32, R9 + 1024); b_onesf = Buf("onesf")
        ropec = T([128, 2], F32, R9 + 1536); b_ropec = Buf("ropec")
        gsm = T([128, 16], F32, R9 + 1600); b_gsm = Buf("gsm")
        cwT = T([128, 4, 32], F32, R9 + 1664); b_cwT = Buf("cwT")
        iota = T([128, 128], F32, R9 + 2176); b_iota = Buf("iota")
        tokid = T([128, NT], F32, R9 + 2688); b_tokid = Buf("tokid")
        stat = T([128, 16], F32, R9 + 2752); b_stat = Buf("stat")
        cst = T([128, 8], F32, R9 + 2816); b_cst = Buf('cst')
        stat2 = T([128, 32], F32, R9 + 2848); b_st = [Buf("st0"), Buf("st1")]
        CEND = R9 + 2976
        assert CEND <= 229376, CEND

        PP = [nc.alloc_psum_tensor("pp%d" % i, [128, 1024], F32) for i in range(4)]
        PB = [Buf("bank%d" % i) for i in range(8)]

        def bank(k):
            return PP[k // 2][:, (k % 2) * 512:(k % 2) * 512 + 512]

        S.dma("sp", lambda e: e.dma_start(out=ident[:], in_=c_ident), b_ident)
        S.dma("sp", lambda e: e.dma_start(out=iota[:], in_=c_iota), b_iota)
        S.dma("sp", lambda e: e.dma_start(out=tokid[:], in_=c_tokid), b_tokid)
        S.dma("sp", lambda e: e.dma_start(out=ropec[:], in_=c_rope), b_ropec)
        S.op("dve", lambda e: e.tensor_copy(out=identb[:], in_=ident[:]), reads=[b_ident], writes=[b_identb])
        S.op("dve", lambda e: e.memset(onesb[:], 1.0), writes=[b_onesb])
        S.op("dve", lambda e: e.memset(onesf[:], 1.0), writes=[b_onesf])
        S.op("dve", lambda e: e.memset(cst[:, 0:1], 256.0 * EPS), writes=[b_cst])
        S.op("dve", lambda e: e.memset(cst[:, 1:2], 128.0 * EPS), writes=[b_cst])
        S.op("dve", lambda e: e.memset(cst[:, 2:3], EPS), writes=[b_cst])
        S.op("dve", lambda e: e.memset(cst[:, 3:4], 0.0), writes=[b_cst])
        with nc.allow_non_contiguous_dma(reason="tiny per-channel parameter vectors"):
            S.dma("sp", lambda e: e.dma_start(out=gsm[:, 0:2], in_=q_norm_g.rearrange("(c p) -> p c", p=128), allow_slow_non_contiguous=True), b_gsm)
            S.dma("sp", lambda e: e.dma_start(out=gsm[:, 2:3], in_=kv_norm_g.rearrange("(c p) -> p c", p=128), allow_slow_non_contiguous=True), b_gsm)
            S.dma("sp", lambda e: e.dma_start(out=gsm[:, 4:8], in_=conv_b.rearrange("(c p) -> p c", p=128), allow_slow_non_contiguous=True), b_gsm)
            S.dma("sp", lambda e: e.dma_start(out=gsm[:, 8:12], in_=conv_ln_g.rearrange("(c p) -> p c", p=128), allow_slow_non_contiguous=True), b_gsm)
            S.dma("sp", lambda e: e.dma_start(out=gsm[:, 12:16], in_=conv_ln_b.rearrange("(c p) -> p c", p=128), allow_slow_non_contiguous=True), b_gsm)
        S.op("dve", lambda e: e.tensor_scalar(out=gsm[:, 0:2], in0=gsm[:, 0:2], scalar1=16.0, scalar2=None, op0=ALU.mult),
             reads=[b_gsm], writes=[b_gsm])
        S.op("dve", lambda e: e.tensor_scalar(out=gsm[:, 2:3], in0=gsm[:, 2:3], scalar1=math.sqrt(128.0), scalar2=None, op0=ALU.mult),
             reads=[b_gsm], writes=[b_gsm])

        resid = T([128, NT, D], F32, R0); b_res = [Buf("res%d" % j) for j in range(NT)]
        u = T([128, 4, 2078], F32, R0); b_u = [Buf("u%d" % c) for c in range(4)]
        acc = T([128, 4, 2048], F32, R0 + 33248); b_acc = [Buf("acc%d" % c) for c in range(4)]
        xstage = [T([128, D], F32, R0 + 33248 + i * 4096) for i in range(2)]; b_xst = [Buf("xst%d" % i) for i in range(2)]
        actT = T([128, 8, S_], BF16, R1); b_actT = [Buf("actT%d" % t) for t in range(4)]
        kT = actT; b_kT = Buf("kT")
        w_in_sb = T([128, 8, 1440], BF16, R2); b_w_in = Buf("w_in")
        wkr = T([128, 8, 2, 96], BF16, R2 + 23040); b_wkr = Buf("wkr")
        attnT = T([128, 4, S_], BF16, R2); b_attnT = Buf("attnT")
        qT = [T([128, 512], BF16, R2 + 16384 + i * 1024) for i in range(2)]; b_qT = [Buf("qT%d" % i) for i in range(2)]
        NPT = 4
        pTt = [T([128, 512], BF16, R2 + 18432 + i * 1024) for i in range(NPT)]; b_pT = [Buf("pT%d" % i) for i in range(NPT)]
        Vaug = T([128, NT, 4, 192], BF16, R3); b_V = Buf("V")
        cqnT = T([128, 2, S_], BF16, R4); b_cqn = Buf("cqn")
        convT = T([128, 4, S_], BF16, R5); b_convT = Buf("convT")
        kr = T([128, S_], BF16, R5); b_kr = Buf("kr")
        ckvnT = T([128, S_], BF16, R5 + 4096); b_ckvn = Buf("ckvn")
        COS = T([128, S_], BF16, R6); SIN = T([128, S_], BF16, R6 + 4096); b_cs = Buf("cossin")
        scr = [T([128, 512], F32, R7 + i * 2048) for i in range(4)]; b_scr = [Buf("scr%d" % i) for i in range(4)]
        wuq = T([128, 2, 768], BF16, R8); b_wuq = Buf("wuq")
        wqsw = T([128, 2, 8, 96], BF16, R8 + 3072); b_wqsw = Buf("wqsw")
        wuk = T([128, 512], BF16, R8 + 6144); b_wuk = Buf("wuk")
        wuv = T([128, 512], BF16, R8 + 7168); b_wuv = Buf("wuv")

        w_in_v = w_in.rearrange("(c p) f -> p c f", p=128)
        for c0 in range(0, 8, 4):
            S.dma("pool", lambda e, c0=c0: e.dma_start(out=w_in_sb[:, c0:c0 + 4, :], in_=w_in_v[:, c0:c0 + 4, :]), b_w_in)
        S.op("pool", lambda e: e.memset(wkr[:], 0.0), writes=[b_wkr])
        S.op("pool", lambda e: e.memset(wqsw[:], 0.0), writes=[b_wqsw])
        with nc.allow_non_contiguous_dma(reason="rope column permutation"):
            S.dma("pool", lambda e: e.dma_start(out=wkr[:, :, 0, 64:96], in_=w_in_v[:, :, 384:416], allow_slow_non_contiguous=True), b_wkr)
            S.dma("pool", lambda e: e.dma_start(out=wkr[:, :, 1, 64:80], in_=w_in_v[:, :, 400:416], allow_slow_non_contiguous=True), b_wkr)
            S.dma("pool", lambda e: e.dma_start(out=wkr[:, :, 1, 80:96], in_=w_in_v[:, :, 384:400], allow_slow_non_contiguous=True), b_wkr)
            w_uq_v = w_uq.rearrange("(c p) (h f) -> p c h f", p=128, f=96)
            for c in range(2):
                S.dma("pool", lambda e, c=c: e.dma_start(out=wqsw[:, c, :, 64:80], in_=w_uq_v[:, c, :, 80:96], allow_slow_non_contiguous=True), b_wqsw)
                S.dma("pool", lambda e, c=c: e.dma_start(out=wqsw[:, c, :, 80:96], in_=w_uq_v[:, c, :, 64:80], allow_slow_non_contiguous=True), b_wqsw)
        S.dma("pool", lambda e: e.dma_start(out=wuq[:], in_=w_uq.rearrange("(c p) f -> p c f", p=128)), b_wuq)
        S.dma("pool", lambda e: e.dma_start(out=wuk[:], in_=w_uk), b_wuk)
        S.dma("pool", lambda e: e.dma_start(out=wuv[:], in_=w_uv), b_wuv)

        cw_st = scr[0]
        S.dma("sp", lambda e: e.dma_start(out=cw_st[0:31, 0:512], in_=conv_w), b_scr[0])
        S.pe([lambda e, cc=cc: e.transpose(bank(0)[:, cc * 32:cc * 32 + 31], cw_st[0:31, cc * 128:(cc + 1) * 128], ident[0:31, 0:31])
              for cc in range(4)], reads=[b_scr[0], b_ident], writes=[PB[0]])
        S.op("dve", lambda e: e.tensor_copy(out=cwT[:, :, 0:31], in_=bank(0)[:, 0:128].rearrange("p (c k) -> p c k", k=32)[:, :, 0:31]),
             reads=[PB[0]], writes=[b_cwT])

        posi = T([128, S_], I32, R0)
        posf = T([128, S_], F32, R0 + 8192)
        angb = T([128, S_], F32, R0 + 16384)
        b_pos = Buf("pos")
        S.dma("sp", lambda e: e.dma_start(out=posi[:], in_=pos.broadcast_to([128, S_])), b_pos)
        rp = slice(64, 96)
        S.op("dve", lambda e: e.tensor_copy(out=posf[rp, :], in_=posi[rp, :]), reads=[b_pos], writes=[b_pos])
        kint = T([128, S_], I32, R0 + 24576)
        sinf = T([128, S_], F32, R0 + 24576)

        def trig(phase, dst_fn):
            S.op("dve", lambda e: e.tensor_scalar(out=angb[rp, :], in0=posf[rp, :], scalar1=ropec[rp, 0:1], scalar2=phase,
                                                  op0=ALU.mult, op1=ALU.add), reads=[b_pos, b_ropec], writes=[b_pos])
            S.op("dve", lambda e: e.tensor_copy(out=kint[rp, :], in_=angb[rp, :]), reads=[b_pos], writes=[b_pos])
            S.op("dve", lambda e: e.tensor_copy(out=posi[rp, :].bitcast(F32), in_=kint[rp, :]), reads=[b_pos], writes=[b_pos])
            S.op("dve", lambda e: e.tensor_tensor(out=angb[rp, :], in0=angb[rp, :], in1=posi[rp, :].bitcast(F32), op=ALU.subtract),
                 reads=[b_pos], writes=[b_pos])
            S.op("dve", lambda e: e.scalar_tensor_tensor(out=angb[rp, :], in0=angb[rp, :], scalar=0.5, in1=angb[rp, :],
                                                         op0=ALU.is_gt, op1=ALU.subtract), reads=[b_pos], writes=[b_pos])
            dst_fn()

        def fin_sin():
            S.op("act", lambda e: e.activation(out=sinf[rp, :], in_=angb[rp, :], func=AF.Sin, scale=-2.0 * math.pi), reads=[b_pos], writes=[b_pos])
            S.op("dve", lambda e: e.tensor_scalar(out=SIN[rp, :], in0=sinf[rp, :], scalar1=ropec[rp, 1:2], scalar2=None, op0=ALU.mult),
                 reads=[b_pos, b_ropec], writes=[b_cs])

        def fin_cos():
            S.op("act", lambda e: e.activation(out=COS[rp, :], in_=angb[rp, :], func=AF.Sin, scale=-2.0 * math.pi), reads=[b_pos], writes=[b_cs])
        trig(0.0, fin_sin)
        trig(0.25, fin_cos)
        for c in range(4):
            S.inherit(b_u[c], [b_pos])

        xv = x.rearrange("(j p) d -> j p d", p=128)
        for j in range(NT):
            st = j % 2
            S.dma("sp", lambda e, j=j, st=st: e.dma_start(out=xstage[st][:], in_=xv[j]), b_xst[st])
            pb = [PB[(j % 2) * 2], PB[(j % 2) * 2 + 1]]
            pt = PP[j % 2]
            S.pe([lambda e, c=c, st=st, pt=pt: e.transpose(pt[:, c * 128:(c + 1) * 128], xstage[st][:, c * 128:(c + 1) * 128], ident[:])
                  for c in range(8)], reads=[b_xst[st], b_ident], writes=pb)
            eng = "act" if j % 2 == 0 else "dve"
            if eng == "act":
                S.op("act", lambda e, j=j, pt=pt: e.activation(out=actT[:, :, j * 128:(j + 1) * 128],
                                                             in_=pt[:].rearrange("p (c t) -> p c t", t=128), func=AF.Copy),
                     reads=pb, writes=[b_actT[j // 4]])
            else:
                S.op("dve", lambda e, j=j, pt=pt: e.tensor_copy(out=actT[:, :, j * 128:(j + 1) * 128],
                                                              in_=pt[:].rearrange("p (c t) -> p c t", t=128)),
                     reads=pb, writes=[b_actT[j // 4]])
        dump("xT", actT[:, :, :], b_actT)

        S.op("pool", lambda e: e.memset(u[:, :, 0:15], 0.0), writes=b_u)
        S.op("pool", lambda e: e.memset(u[:, :, 2063:2078], 0.0), writes=b_u)
        for c in range(4):
            S.inherit(b_acc[c], b_xst)

        def inproj_group(bk, lhs_fn, tb, M):
            cols = slice(tb * 512, (tb + 1) * 512)
            S.pe([lambda e, c=c: e.matmul(bank(bk)[0:M, :], lhsT=lhs_fn(c), rhs=actT[:, c, cols], start=(c == 0), stop=(c == 7))
                  for c in range(8)], reads=[b_w_in, b_wkr, b_actT[tb]], writes=[PB[bk]])

        def inproj_tb(tb):
            cols = slice(tb * 512, (tb + 1) * 512)
            for c2 in range(2):
                inproj_group(c2, lambda c, c2=c2: w_in_sb[:, c, c2 * 128:(c2 + 1) * 128], tb, 128)
                S.op("act", lambda e, c2=c2: e.activation(out=scr[c2][:].bitcast(BF16)[:, 0:512], in_=bank(c2), func=AF.Square),
                     reads=[PB[c2]], writes=[b_scr[c2]])
            S.pe([lambda e, c2=c2: e.matmul(bank(2), lhsT=onesb[:], rhs=scr[c2][:].bitcast(BF16)[:, 0:512], start=(c2 == 0), stop=(c2 == 1))
                  for c2 in range(2)], reads=[b_onesb, b_scr[0], b_scr[1]], writes=[PB[2]])
            rsqrt_act(scr[2][:], bank(2), cst[:, 0:1], [PB[2]], [b_scr[2]])
            for c2 in range(2):
                S.op("dve", lambda e, c2=c2: e.scalar_tensor_tensor(out=cqnT[:, c2, cols], in0=bank(c2), scalar=gsm[:, c2:c2 + 1], in1=scr[2][:],
                                                                   op0=ALU.mult, op1=ALU.mult),
                     reads=[PB[c2], b_gsm, b_scr[2]], writes=[b_cqn])
            inproj_group(3, lambda c: w_in_sb[:, c, 256:384], tb, 128)
            S.op("act", lambda e: e.activation(out=scr[3][:].bitcast(BF16)[:, 0:512], in_=bank(3), func=AF.Square),
                 reads=[PB[3]], writes=[b_scr[3]])
            S.pe([lambda e: e.matmul(bank(2), lhsT=onesb[:], rhs=scr[3][:].bitcast(BF16)[:, 0:512], start=True, stop=True)],
                 reads=[b_onesb, b_scr[3]], writes=[PB[2]])
            rsqrt_act(scr[2][:], bank(2), cst[:, 1:2], [PB[2]], [b_scr[2]])
            S.op("dve", lambda e: e.scalar_tensor_tensor(out=ckvnT[:, cols], in0=bank(3), scalar=gsm[:, 2:3], in1=scr[2][:],
                                                         op0=ALU.mult, op1=ALU.mult),
                 reads=[PB[3], b_gsm, b_scr[2]], writes=[b_ckvn])
            inproj_group(4, lambda c: wkr[:, c, 0, :], tb, 96)
            inproj_group(5, lambda c: wkr[:, c, 1, :], tb, 96)
            S.op("dve", lambda e: e.tensor_tensor(out=scr[0][rp, :], in0=bank(4)[rp, :], in1=COS[rp, cols], op=ALU.mult),
                 reads=[PB[4], b_cs], writes=[b_scr[0]])
            S.op("dve", lambda e: e.tensor_tensor(out=scr[1][rp, :], in0=bank(5)[rp, :], in1=SIN[rp, cols], op=ALU.mult),
                 reads=[PB[5], b_cs], writes=[b_scr[1]])
            S.op("dve", lambda e: e.tensor_tensor(out=kr[rp, cols], in0=scr[0][rp, :], in1=scr[1][rp, :], op=ALU.add),
                 reads=[b_scr[0], b_scr[1]], writes=[b_kr])
        def inproj_glu(tb):
            cols = slice(tb * 512, (tb + 1) * 512)
            for cc in range(4):
                ba = 6 - 2 * (cc % 2)
                bg = 7 - 2 * (cc % 2)
                sg = scr[2 + cc % 2]
                bsg = b_scr[2 + cc % 2]
                inproj_group(ba, lambda c, cc=cc: w_in_sb[:, c, 416 + cc * 128:416 + (cc + 1) * 128], tb, 128)
                inproj_group(bg, lambda c, cc=cc: w_in_sb[:, c, 928 + cc * 128:928 + (cc + 1) * 128], tb, 128)
                S.op("act", lambda e, sg=sg, bg=bg: e.activation(out=sg[:], in_=bank(bg), func=AF.Sigmoid), reads=[PB[bg]], writes=[bsg])
                S.op("dve", lambda e, cc=cc, sg=sg, ba=ba: e.tensor_tensor(out=u[:, cc, 15 + tb * 512:15 + (tb + 1) * 512], in0=bank(ba), in1=sg[:], op=ALU.mult),
                     reads=[PB[ba], bsg], writes=[b_u[cc]])
        for tb in range(4):
            inproj_glu(tb)
        def conv_chunk(cc, eng):
            conv_todo.append(lambda: _raw_op(eng, lambda e: e.tensor_scalar(out=acc[:, cc, :], in0=u[:, cc, 0:2048], scalar1=cwT[:, cc, 0:1],
                                                                            scalar2=gsm[:, 4 + cc:5 + cc], op0=ALU.mult, op1=ALU.add),
                                             reads=[b_u[cc], b_cwT, b_gsm], writes=[b_acc[cc]]))
            for k in range(1, 31):
                conv_todo.append(lambda k=k: _raw_op(eng, lambda e: e.scalar_tensor_tensor(out=acc[:, cc, :], in0=u[:, cc, k:k + 2048],
                                                                                           scalar=cwT[:, cc, k:k + 1], in1=acc[:, cc, :],
                                                                                           op0=ALU.mult, op1=ALU.add),
                                                     reads=[b_u[cc], b_acc[cc], b_cwT], writes=[b_acc[cc]]))
        for cc in range(4):
            conv_chunk(cc, "dve")
        drain["rate"] = 0.5
        for tb in range(4):
            inproj_tb(tb)
        dump("cqnT", cqnT[:, :, :], [b_cqn])
        dump("ckvnT", ckvnT[:, :], [b_ckvn])
        dump("kr", kr[64:96, :], [b_kr])
        dump("u", u[:, :, 15:2063], b_u)
        if stop_after == "inproj":
            S.wait_bufs("sp", [b_dbg])
            S.emit()
            return nc


        def mm(o, l, r, st=True, sp=True):
            return lambda e: e.matmul(o, lhsT=l, rhs=r, start=st, stop=sp)

        evc = [0]

        def evac(out_ap, in_ap, reads, writes):
            evc[0] += 1
            if evc[0] % 2 == 0:
                S.op("act", lambda e: e.activation(out=out_ap, in_=in_ap, func=AF.Copy), reads=reads, writes=writes)
            else:
                S.op("dve", lambda e: e.tensor_copy(out=out_ap, in_=in_ap), reads=reads, writes=writes)

        S.inherit(b_kT, b_actT)

        def kup(h, tb):
            cols = slice(tb * 512, (tb + 1) * 512)
            bk = (h * 4 + tb) % 4
            S.pe([mm(bank(bk)[0:64, :], wuk[:, h * 64:(h + 1) * 64], ckvnT[:, cols])], reads=[b_wuk, b_ckvn], writes=[PB[bk]])
            evac(kT[0:64, h, cols], bank(bk)[0:64, :], [PB[bk]], [b_kT])
        for h in range(8):
            for tb in range(4):
                kup(h, tb)

        def krb(tb):
            cols = slice(tb * 512, (tb + 1) * 512)
            S.op("pool", lambda e: e.tensor_copy(out=kT[64:96, :, cols], in_=kr[64:96, cols].unsqueeze(1).broadcast_to([32, 8, 512])),
                 reads=[b_kr], writes=[b_kT])
        for tb in range(4):
            krb(tb)
        S.op("pool", lambda e: e.memset(Vaug[:, :, :, 64:128], 0.0), writes=[b_V])
        S.op("pool", lambda e: e.memset(Vaug[:, :, :, 64:65], 1.0), writes=[b_V])

        def vup(j):
            bk = 4 + (j % 4)
            S.pe([mm(bank(bk), ckvnT[:, j * 128:(j + 1) * 128], wuv[:])], reads=[b_wuv, b_ckvn], writes=[PB[bk]])
            pv = bank(bk).rearrange("p (q t d) -> p q t d", t=2, d=64)
            S.op("act", lambda e: e.activation(out=Vaug[:, j, :, 0:64], in_=pv[:, :, 0, :], func=AF.Copy), reads=[PB[bk]], writes=[b_V])
            S.op("dve", lambda e: e.tensor_copy(out=Vaug[:, j, :, 128:192], in_=pv[:, :, 1, :]), reads=[PB[bk]], writes=[b_V])
        for j in range(NT):
            vup(j)
        dump("kT", kT[0:96, :, :], [b_kT])
        dump("V", Vaug[:, :, :, :], [b_V])

        S.inherit(b_attnT, [b_w_in, b_wkr])
        for i in range(2):
            S.inherit(b_qT[i], [b_w_in, b_wkr])
        for i in range(NPT):
            S.inherit(b_pT[i], [b_w_in, b_wkr])
        SCALE = 1.0 / math.sqrt(96.0)

        def qup(s):
            qb, h = divmod(s, 8)
            cols = slice(qb * 512, (qb + 1) * 512)
            qt = qT[s % 2]
            bq = b_qT[s % 2]
            S.pe([mm(bank(5)[0:96, :], wuq[:, c, h * 96:(h + 1) * 96], cqnT[:, c, cols], c == 0, c == 1) for c in range(2)],
                 reads=[b_wuq, b_cqn], writes=[PB[5]])
            S.pe([mm(bank(6)[0:96, :], wqsw[:, c, h, :], cqnT[:, c, cols], c == 0, c == 1) for c in range(2)],
                 reads=[b_wqsw, b_cqn], writes=[PB[6]])
            S.op("dve", lambda e: e.tensor_copy(out=qt[0:64, :], in_=bank(5)[0:64, :]), reads=[PB[5]], writes=[bq])
            S.op("dve", lambda e: e.tensor_tensor(out=scr[0][rp, :], in0=bank(5)[rp, :], in1=COS[rp, cols], op=ALU.mult),
                 reads=[PB[5], b_cs], writes=[b_scr[0]])
            S.op("dve", lambda e: e.tensor_tensor(out=scr[1][rp, :], in0=bank(6)[rp, :], in1=SIN[rp, cols], op=ALU.mult),
                 reads=[PB[6], b_cs], writes=[b_scr[1]])
            S.op("dve", lambda e: e.tensor_tensor(out=qt[rp, :], in0=scr[0][rp, :], in1=scr[1][rp, :], op=ALU.add),
                 reads=[b_scr[0], b_scr[1]], writes=[bq])

        pctr = [0]

        def attn_step(s):
            qb, h = divmod(s, 8)
            pair, odd = divmod(h, 2)
            cols = slice(qb * 512, (qb + 1) * 512)
            qt = qT[s % 2]
            bq = b_qT[s % 2]
            ob = 3 + (s % 2)
            M = 128 if odd else 65

            def lhsV(kt):
                return Vaug[:, kt, pair, 64:192] if odd else Vaug[:, kt, pair, 0:65]

            def Smm(kt):
                bk = kt % 3
                S.pe([mm(bank(bk), kT[0:96, h, kt * 128:(kt + 1) * 128], qt[0:96, :])], reads=[b_kT, bq], writes=[PB[bk]])

            def EXPV(kt):
                pi = pctr[0] % NPT
                pctr[0] += 1
                p = pTt[pi]
                S.op("act", lambda e: e.activation(out=p[:], in_=bank(kt % 3), func=AF.Exp, scale=SCALE), reads=[PB[kt % 3]], writes=[b_pT[pi]])
                if kt + 2 < 16:
                    Smm(kt + 2)
                S.pe([mm(bank(ob)[0:M, :], lhsV(kt), p[:], kt == 0, kt == 15)], reads=[b_V, b_pT[pi]], writes=[PB[ob]])
            Smm(0)
            Smm(1)
            for kt in range(16):
                EXPV(kt)
                if kt == 2 and s > 0:
                    attn_norm(s - 1)

        def attn_norm(s):
            qb, h = divmod(s, 8)
            pair, odd = divmod(h, 2)
            cols = slice(qb * 512, (qb + 1) * 512)
            ob = 3 + (s % 2)
            if odd:
                dr, orow = slice(0, 1), slice(64, 128)
                lo = onesf[0:1, 0:128]
                bo = bank(7)
            else:
                dr, orow = slice(64, 65), slice(0, 64)
                lo = onesf[64:65, 0:64]
                bo = bank(7)[0:64, :]
            recip_act(scr[2][dr, :], bank(ob)[dr, :], [PB[ob]], [b_scr[2]])
            S.pe([mm(bo, lo, scr[2][dr, :])], reads=[b_onesf, b_scr[2]], writes=[PB[7]])
            S.op("dve", lambda e: e.tensor_copy(out=scr[3][orow, :], in_=bank(ob)[orow, :]), reads=[PB[ob]], writes=[b_scr[3]])
            S.op("dve", lambda e: e.tensor_tensor(out=attnT[orow, pair, cols], in0=scr[3][orow, :], in1=bank(7)[orow, :], op=ALU.mult),
                 reads=[b_scr[3], PB[7]], writes=[b_attnT])

        NSTEP = 32
        qup(0)
        for s in range(NSTEP):
            if s + 1 < NSTEP:
                qup(s + 1)
            attn_step(s)
        attn_norm(NSTEP - 1)
        drain["rate"] = 0.0
        while conv_todo:
            conv_todo.pop(0)()
        dump("acc", acc[:, :, :], b_acc)
        dump("attnT", attnT[:, :, :], [b_attnT])

        S.inherit(b_convT, [b_kr, b_ckvn])

        def conv_ln(tb):
            cols = slice(tb * 512, (tb + 1) * 512)
            S.pe([mm(bank(0), onesf[:], acc[:, cc, cols], cc == 0, cc == 3) for cc in range(4)], reads=[b_onesf] + b_acc, writes=[PB[0]])
            for cc in range(4):
                S.op("act", lambda e, cc=cc: e.activation(out=scr[3][:], in_=acc[:, cc, cols], func=AF.Square), reads=[b_acc[cc]], writes=[b_scr[3]])
                S.pe([mm(bank(1), onesf[:], scr[3][:], cc == 0, cc == 3)], reads=[b_onesf, b_scr[3]], writes=[PB[1]])
            S.op("dve", lambda e: e.tensor_scalar(out=scr[0][:], in0=bank(0), scalar1=1.0 / 512.0, scalar2=None, op0=ALU.mult),
                 reads=[PB[0]], writes=[b_scr[0]])
            S.op("dve", lambda e: e.tensor_tensor(out=scr[1][:], in0=scr[0][:], in1=scr[0][:], op=ALU.mult), reads=[b_scr[0]], writes=[b_scr[1]])
            S.op("dve", lambda e: e.scalar_tensor_tensor(out=scr[1][:], in0=bank(1), scalar=1.0 / 512.0, in1=scr[1][:], op0=ALU.mult, op1=ALU.subtract),
                 reads=[PB[1], b_scr[1]], writes=[b_scr[1]])
            rsqrt_act(scr[1][:], scr[1][:], cst[:, 2:3], [b_scr[1]], [b_scr[1]])
            for cc in range(4):
                S.op("dve", lambda e, cc=cc: e.tensor_tensor(out=scr[2][:], in0=acc[:, cc, cols], in1=scr[0][:], op=ALU.subtract),
                     reads=[b_acc[cc], b_scr[0]], writes=[b_scr[2]])
                S.op("dve", lambda e: e.tensor_tensor(out=scr[2][:], in0=scr[2][:], in1=scr[1][:], op=ALU.mult), reads=[b_scr[2], b_scr[1]], writes=[b_scr[2]])
                S.op("act", lambda e, cc=cc: e.activation(out=convT[:, cc, cols], in_=scr[2][:], func=AF.Silu, scale=gsm[:, 8 + cc:9 + cc], bias=gsm[:, 12 + cc:13 + cc]),
                     reads=[b_scr[2], b_gsm], writes=[b_convT])
        for tb in range(4):
            conv_ln(tb)
        dump("convT", convT[:, :, :], [b_convT])

        lnp = T([128, 2, D], F32, R6); b_lnp = Buf("lnp")
        S.inherit(b_lnp, [b_cs])
        ysbs = [T([128, D], F32, R7 + i * 4096) for i in range(2)]
        b_ysbs = [Buf("ysb0"), Buf("ysb1")]
        for bb in b_ysbs:
            S.inherit(bb, b_scr)
        ysb = ysbs[0]; b_ysb = b_ysbs[0]

        def load_lnp(i):
            S.dma("sp", lambda e: e.dma_start(out=lnp[:, 0, :], in_=ln_g[i].broadcast_to([128, D])), b_lnp)
            S.dma("sp", lambda e: e.dma_start(out=lnp[:, 1, :], in_=ln_b[i].broadcast_to([128, D])), b_lnp)

        def ln_A(j, xin_ap, xin_bufs, pk):
            yb = ysbs[j % 2]
            byb = b_ysbs[j % 2]
            so = (j % 2) * 16
            bs = b_st[j % 2]
            st_ = lambda a, n=1: stat2[:, so + a:so + a + n]
            if pk is not None:
                pbs = [PB[2 * pk], PB[2 * pk + 1]]
                S.op("dve", lambda e: e.scalar_tensor_tensor(out=yb[:], in0=xin_ap, scalar=ALPHA, in1=PP[pk][:], op0=ALU.mult, op1=ALU.add),
                     reads=pbs + xin_bufs, writes=[byb])
                ysrc, ysrc_b = yb[:], [byb]
            else:
                ysrc, ysrc_b = resid[:, j, :], [b_res[j]]
            S.op("dve", lambda e: e.bn_stats(out=st_(0, 6), in_=ysrc[:, 0:512]), reads=ysrc_b, writes=[bs])
            S.op("dve", lambda e: e.bn_stats(out=st_(6, 6), in_=ysrc[:, 512:1024]), reads=ysrc_b, writes=[bs])
            S.op("dve", lambda e: e.bn_aggr(out=st_(12, 2), in_=st_(0, 12)), reads=[bs], writes=[bs])
            rsqrt_act(st_(14), st_(13), cst[:, 2:3], [bs], [bs])
            S.op("dve", lambda e: e.scalar_tensor_tensor(out=st_(15), in0=st_(12), scalar=-1.0, in1=st_(14), op0=ALU.mult, op1=ALU.mult), reads=[bs], writes=[bs])

        def ln_B(j, from_resid=False, extra=None, final_out=None):
            yb = ysbs[j % 2]
            byb = b_ysbs[j % 2]
            so = (j % 2) * 16
            bs = b_st[j % 2]
            st_ = lambda a, n=1: stat2[:, so + a:so + a + n]
            if from_resid:
                ysrc, ysrc_b = resid[:, j, :], [b_res[j]]
            else:
                ysrc, ysrc_b = yb[:], [byb]
            S.op("act", lambda e: e.activation(out=yb[:], in_=ysrc, func=AF.Identity, scale=st_(14), bias=st_(15)), reads=ysrc_b + [bs], writes=[byb])
            S.op("dve", lambda e: e.tensor_tensor(out=yb[:], in0=yb[:], in1=lnp[:, 0, :], op=ALU.mult), reads=[byb, b_lnp], writes=[byb])
            S.op("pool", lambda e: e.tensor_tensor(out=resid[:, j, :], in0=yb[:], in1=lnp[:, 1, :], op=ALU.add), reads=[byb, b_lnp], writes=[b_res[j]])
            if final_out is not None:
                final_out(j)
            if extra is not None:
                extra(j)

        def ln_T(j, tpk):
            tpb = [PB[2 * tpk], PB[2 * tpk + 1]]
            S.pe([lambda e, c=c: e.transpose(PP[tpk][:, c * 128:(c + 1) * 128], resid[:, j, c * 128:(c + 1) * 128], ident[:]) for c in range(8)],
                 reads=[b_res[j], b_ident], writes=tpb)
            evac(actT[:, :, j * 128:(j + 1) * 128], PP[tpk][:].rearrange("p (c t) -> p c t", t=128), tpb, [b_actT[j // 4]])

        wo_sb = T([128, 8, D], BF16, R3); b_wo = Buf("wo")
        S.inherit(b_wo, [b_V])
        w_o_v = w_o.rearrange("(c p) f -> p c f", p=128)
        for c0 in range(0, 8, 4):
            S.dma("pool", lambda e, c0=c0: e.dma_start(out=wo_sb[:, c0:c0 + 4, :], in_=w_o_v[:, c0:c0 + 4, :]), b_wo)
        load_lnp(0)
        xst2 = [T([128, D], F32, R4 + i * 4096) for i in range(2)]; b_xst2 = [Buf("xs2_%d" % i) for i in range(2)]
        for i in range(2):
            S.inherit(b_xst2[i], [b_cqn])
        for j in range(NT):
            S.inherit(b_res[j], b_u + b_acc + b_xst + [b_pos])
        for t in range(4):
            S.inherit(b_actT[t], [b_kT])

        def wo_mm(j):
            st = j % 2
            S.dma("sp", lambda e: e.dma_start(out=xst2[st][:], in_=xv[j]), b_xst2[st])
            tsl = slice(j * 128, (j + 1) * 128)
            pk = (j % 2) * 2
            for half in range(2):
                hs = slice(half * 512, (half + 1) * 512)
                bk = 2 * pk + half
                fns = [mm(bank(bk), attnT[:, pr, tsl], wo_sb[:, pr, hs], pr == 0, False) for pr in range(4)]
                fns += [mm(bank(bk), convT[:, cc, tsl], wo_sb[:, 4 + cc, hs], False, cc == 3) for cc in range(4)]
                S.pe(fns, reads=[b_attnT, b_convT, b_wo], writes=[PB[bk]])

        def ln1_A(j):
            ln_A(j, xst2[j % 2][:], [b_xst2[j % 2]], (j % 2) * 2)
        wo_mm(0)
        wo_mm(1)
        ln1_A(0)
        for j in range(NT):
            if j + 1 < NT:
                ln1_A(j + 1)
            ln_B(j)
            if j + 2 < NT:
                wo_mm(j + 2)
            ln_T(j, (j % 2) * 2 + 1)
        dump("h1", resid[:, :, :], b_res)
        if stop_after == "mixer":
            S.wait_bufs("sp", [b_dbg])
            S.emit()
            return nc

        phaseA = [b_attnT, b_convT, b_wo, b_cqn, b_kr, b_ckvn, b_w_in, b_wkr, b_V, b_wuq, b_wqsw, b_wuk, b_wuv] + b_qT + b_pT + b_xst2
        B0 = R2
        xw = {}
        bxw = {}
        for i, k in enumerate("qokv"):
            xw[k] = T([128, 8, D], BF16, B0 + i * 16384)
            bxw[k] = Buf("xw" + k)
            S.inherit(bxw[k], phaseA)
        xqT = [T([128, 2, 512], BF16, B0 + 65536 + i * 2048) for i in range(2)]; b_xq = [Buf("xq%d" % i) for i in range(2)]
        xvv = T([128, 2, D], BF16, B0 + 69632); b_xv = Buf("xv")
        pT2 = [T([128, 512], BF16, B0 + 73728 + i * 1024) for i in range(2)]; b_p2 = [Buf("p2_%d" % i) for i in range(2)]
        oT = T([128, 8, 512], BF16, R8); b_oT = Buf("oT")
        TAIL = CEND + 32
        memT = T([128, 8, 256], BF16, TAIL); b_memT = Buf("memT")
        xkT = T([128, 8, 256], BF16, TAIL + 4096); b_xk = Buf("xkT")
        rscr = T([128, 512], F32, TAIL + 8448); b_rscr = Buf("rscr")
        assert TAIL + 8448 + 2048 <= 229376, TAIL
        for bb in b_xq + [b_xv] + b_p2 + [b_oT]:
            S.inherit(bb, phaseA)
        for k in "kvqo":
            wv_ = xa_w[k].rearrange("(c p) f -> p c f", p=128)
            for c0 in range(0, 8, 4):
                S.dma("pool", lambda e, k=k, c0=c0, wv_=wv_: e.dma_start(out=xw[k][:, c0:c0 + 4, :], in_=wv_[:, c0:c0 + 4, :]), bxw[k])
        load_lnp(1)
        memv = mem.rearrange("(j p) d -> j p d", p=128)

        def mem_tile(mt):
            S.dma("sp", lambda e: e.dma_start(out=ysbs[mt][:], in_=memv[mt]), b_ysbs[mt])
            S.pe([lambda e, c=c: e.transpose(PP[1][:, c * 128:(c + 1) * 128], ysbs[mt][:, c * 128:(c + 1) * 128], ident[:]) for c in range(8)],
                 reads=[b_ysbs[mt], b_ident], writes=[PB[2], PB[3]])
            evac(memT[:, :, mt * 128:(mt + 1) * 128], PP[1][:].rearrange("p (c t) -> p c t", t=128), [PB[2], PB[3]], [b_memT])
        for mt in range(2):
            mem_tile(mt)

        def xk_fc(fc):
            bk = fc % 2
            S.pe([mm(bank(bk)[:, 0:256], xw["k"][:, c, fc * 128:(fc + 1) * 128], memT[:, c, :], c == 0, c == 7) for c in range(8)],
                 reads=[bxw["k"], b_memT], writes=[PB[bk]])
            evac(xkT[:, fc, :], bank(bk)[:, 0:256], [PB[bk]], [b_xk])
        for fc in range(8):
            xk_fc(fc)

        def xv_mh(mt, half):
            bk = 2 + (mt * 2 + half) % 2
            hs = slice(half * 512, (half + 1) * 512)
            S.pe([mm(bank(bk), memT[:, c, mt * 128:(mt + 1) * 128], xw["v"][:, c, hs], c == 0, c == 7) for c in range(8)],
                 reads=[bxw["v"], b_memT], writes=[PB[bk]])
            evac(xvv[:, mt, hs], bank(bk), [PB[bk]], [b_xv])
        for mt in range(2):
            for half in range(2):
                xv_mh(mt, half)

        h2tm = T([128, NT, D], BF16, B0 + 32768); b_h2tm = Buf("h2tm")
        S.inherit(b_h2tm, [bxw["k"], bxw["v"]])

        def xa_head(tb, hh):
            cols = slice(tb * 512, (tb + 1) * 512)
            xq = xqT[hh % 2]
            bq = b_xq[hh % 2]
            for i in range(2):
                fc = 2 * hh + i
                S.pe([mm(bank(i), xw["q"][:, c, fc * 128:(fc + 1) * 128], actT[:, c, cols], c == 0, c == 7) for c in range(8)],
                     reads=[bxw["q"], b_actT[tb]], writes=[PB[i]])
                evac(xq[:, i, :], bank(i), [PB[i]], [bq])
            for mt in range(2):
                S.pe([mm(bank(2 + mt), xkT[:, 2 * hh + i, mt * 128:(mt + 1) * 128], xq[:, i, :], i == 0, i == 1) for i in range(2)],
                     reads=[b_xk, bq], writes=[PB[2 + mt]])
                S.op("act", lambda e, mt=mt: e.activation(out=pT2[mt][:], in_=bank(2 + mt), func=AF.Exp, scale=1.0 / 16.0),
                     reads=[PB[2 + mt]], writes=[b_p2[mt]])
            S.pe([mm(bank(4), onesb[:], pT2[mt][:], mt == 0, mt == 1) for mt in range(2)], reads=[b_onesb] + b_p2, writes=[PB[4]])
            recip_act(rscr[:], bank(4), [PB[4]], [b_rscr])
            for dc in range(2):
                S.pe([mm(bank(5 + dc), xvv[:, mt, hh * 256 + dc * 128:hh * 256 + (dc + 1) * 128], pT2[mt][:], mt == 0, mt == 1) for mt in range(2)],
                     reads=[b_xv] + b_p2, writes=[PB[5 + dc]])
                S.op("dve", lambda e, dc=dc: e.tensor_tensor(out=oT[:, hh * 2 + dc, :], in0=bank(5 + dc), in1=rscr[:], op=ALU.mult),
                     reads=[PB[5 + dc], b_rscr], writes=[b_oT])

        def cast_h2(j):
            S.op("act", lambda e: e.activation(out=h2tm[:, j, :], in_=resid[:, j, :], func=AF.Copy), reads=[b_res[j]], writes=[b_h2tm])

        def xa_out_mm(tb, jj):
            tsl = slice(jj * 128, (jj + 1) * 128)
            pk = 3 - (jj % 2)
            for half in range(2):
                hs = slice(half * 512, (half + 1) * 512)
                S.pe([mm(bank(2 * pk + half), oT[:, fc, tsl], xw["o"][:, fc, hs], fc == 0, fc == 7) for fc in range(8)],
                     reads=[b_oT, bxw["o"]], writes=[PB[2 * pk + half]])

        def ln2_A(tb, jj):
            j = tb * 4 + jj
            ln_A(j, resid[:, j, :], [b_res[j]], 3 - (jj % 2))

        def ln2_B(tb, jj):
            ln_B(tb * 4 + jj, extra=cast_h2)
        for hh in range(4):
            xa_head(0, hh)
        for tb in range(4):
            xa_out_mm(tb, 0)
            xa_out_mm(tb, 1)
            ln2_A(tb, 0)
            xa_out_mm(tb, 2)
            ln2_A(tb, 1)
            ln2_B(tb, 0)
            xa_out_mm(tb, 3)
            ln2_A(tb, 2)
            ln2_B(tb, 1)
            ln2_A(tb, 3)
            ln2_B(tb, 2)
            ln2_B(tb, 3)
            if tb + 1 < 4:
                for hh in range(4):
                    xa_head(tb + 1, hh)
            for jj in range(4):
                ln_T(tb * 4 + jj, jj % 2)
        dump("h2", resid[:, :, :], b_res)
        if stop_after == "xattn":
            S.wait_bufs("sp", [b_dbg])
            S.emit()
            return nc

        phaseB = [bxw["q"], bxw["o"], b_xv, b_oT, b_memT, b_xk] + b_xq + b_p2
        wr_sb = T([128, 8, NE], BF16, TAIL); b_wr = Buf("wr")
        S.inherit(b_wr, [b_memT])
        with_nc = w_router.rearrange("(c p) e -> p c e", p=128)
        S.dma("pool", lambda e: e.dma_start(out=wr_sb[:], in_=with_nc), b_wr)
        esel = T([16, 16 * 128], F32, TAIL + 256); b_esel = Buf("esel")
        S.inherit(b_esel, [b_memT, b_xk])
        S.dma("sp", lambda e: e.dma_start(out=esel[:], in_=c_esel), b_esel)
        aff = T([16, S_], F32, B0); affw = [T([16, S_], F32, B0 + 8192), T([16, S_], F32, B0 + 16384)]
        b_aff = Buf("aff"); b_affw = [Buf("affw0"), Buf("affw1")]
        gate = T([16, CAP], F32, B0 + 24576); idxu = T([16, CAP], U32, B0 + 25600); idxf = T([16, CAP], F32, B0 + 26624)
        b_gate = Buf("gate"); b_idxu = Buf("idxu"); b_idxf = Buf("idxf")
        idxT = T([128, 2, NE], F32, B0 + 27648); gateT = T([128, 2, NE], F32, B0 + 27776); b_igT = Buf("igT")
        idxs = T([128, 32], F32, B0 + 27904); b_idxs = Buf("idxs")
        for bb in [b_aff, b_gate, b_idxu, b_idxf, b_igT, b_idxs] + b_affw:
            S.inherit(bb, phaseB)
        for tb in range(4):
            def router_tb(tb=tb):
                cols = slice(tb * 512, (tb + 1) * 512)
                S.pe([mm(bank(tb)[0:16, :], wr_sb[:, c, :], actT[:, c, cols], c == 0, c == 7) for c in range(8)],
                     reads=[b_wr, b_actT[tb]], writes=[PB[tb]])
                S.op("act", lambda e: e.activation(out=affw[0][:, cols], in_=bank(tb)[0:16, :], func=AF.Exp), reads=[PB[tb]], writes=[b_affw[0]])
                S.pe([mm(bank(4 + tb)[0:16, :], onesf[0:16, 0:16], affw[0][:, cols])], reads=[b_onesf, b_affw[0]], writes=[PB[4 + tb]])
                S.op("dve", lambda e: e.reciprocal(out=affw[1][:, cols], in_=bank(4 + tb)[0:16, :]), reads=[PB[4 + tb]], writes=[b_affw[1]])
                S.op("dve", lambda e: e.tensor_tensor(out=aff[:, cols], in0=affw[0][:, cols], in1=affw[1][:, cols], op=ALU.mult),
                     reads=b_affw, writes=[b_aff])
            router_tb()
        dump("aff", aff[:, :], [b_aff])
        for r in range(CAP // 8):
            def topk_round(r=r):
                src = aff if r == 0 else affw[(r - 1) % 2]
                bsrc = b_aff if r == 0 else b_affw[(r - 1) % 2]
                dst = affw[r % 2]
                sl = slice(r * 8, r * 8 + 8)
                S.op("dve", lambda e: e.max(out=gate[:, sl], in_=src[:]), reads=[bsrc], writes=[b_gate])
                S.op("dve", lambda e: e.max_index(out=idxu[:, sl], in_max=gate[:, sl], in_values=src[:]), reads=[bsrc, b_gate], writes=[b_idxu])
                if r + 1 < CAP // 8:
                    S.op("dve", lambda e: e.match_replace(out=dst[:], in_to_replace=gate[:, sl], in_values=src[:], imm_value=-1.0),
                         reads=[bsrc, b_gate], writes=[b_affw[r % 2]])
            topk_round()
        S.op("dve", lambda e: e.tensor_copy(out=idxf[:], in_=idxu[:]), reads=[b_idxu], writes=[b_idxf])
        dump("gate", gate[:, :], [b_gate])
        dump("idxf", idxf[:, :], [b_idxf])
        S.pe([lambda e, ch=ch: e.transpose(bank(0)[:, ch * 16:(ch + 1) * 16], idxf[0:16, ch * 128:(ch + 1) * 128], ident[0:16, 0:16]) for ch in range(2)]
             + [lambda e, ch=ch: e.transpose(bank(0)[:, 32 + ch * 16:32 + (ch + 1) * 16], gate[0:16, ch * 128:(ch + 1) * 128], ident[0:16, 0:16]) for ch in range(2)],
             reads=[b_idxf, b_gate, b_ident], writes=[PB[0]])
        S.op("dve", lambda e: e.tensor_copy(out=idxT[:].rearrange("p a b -> p (a b)"), in_=bank(0)[:, 0:32]), reads=[PB[0]], writes=[b_igT])
        S.op("dve", lambda e: e.tensor_copy(out=gateT[:].rearrange("p a b -> p (a b)"), in_=bank(0)[:, 32:64]), reads=[PB[0]], writes=[b_igT])

        for j in range(NT):
            S.op("pool", lambda e, j=j: e.tensor_scalar(out=resid[:, j, :], in0=resid[:, j, :], scalar1=ALPHA, scalar2=None, op0=ALU.mult),
                 reads=[b_res[j]], writes=[b_res[j]])

        C1 = R1
        SelT = [T([128, NT, CAP], BF16, C1 + i * 8192) for i in range(2)]; b_sel = [Buf("sel%d" % i) for i in range(2)]
        GRP = 2
        ysc = [T([128, GRP * 2, D], BF16, C1 + 16384 + i * 8192) for i in range(2)]; b_ysc = [Buf("ysc%d" % i) for i in range(2)]
        for bb in b_sel + b_ysc:
            S.inherit(bb, b_actT)
        xsT = [T([128, 8, CAP], BF16, B0 + 65536 + i * 4096) for i in range(2)]; b_xs = [Buf("xs%d" % i) for i in range(2)]
        hidT = [T([128, 2, CAP], BF16, B0 + 73728 + i * 1024) for i in range(2)]; b_hid = [Buf("hid%d" % i) for i in range(2)]
        for bb in b_xs + b_hid:
            S.inherit(bb, phaseB)
        selsc = [T([128, GRP * 2, 128], BF16, R8 + i * 1024) for i in range(2)]; b_selsc = [Buf("selsc%d" % i) for i in range(2)]
        silu_t = [T([128, 512], F32, R8 + 2048 + i * 2048) for i in range(2)]; b_silu = [Buf("silu%d" % i) for i in range(2)]
        idxs = [T([128, 4], F32, R8 + 6144 + i * 32) for i in range(2)]; b_idxs2 = [Buf("idxs%d" % i) for i in range(2)]
        for bb in b_selsc + b_silu + b_idxs2:
            S.inherit(bb, [b_oT])
        NRING = 3
        ring_off = [B0, B0 + 12288, R6]
        ringG = [T([128, 8, 256], BF16, ring_off[i]) for i in range(NRING)]
        ringU = [T([128, 8, 256], BF16, ring_off[i] + 4096) for i in range(NRING)]
        ringD = [T([128, 2, D], BF16, ring_off[i] + 8192) for i in range(NRING)]
        b_ring = [Buf("ring%d" % i) for i in range(NRING)]
        for bb in b_ring[0:2]:
            S.inherit(bb, [b_aff] + b_affw + phaseB)
        S.inherit(b_ring[2], [b_lnp] + b_ysbs)

        def load_unit(n):
            e, fb = divmod(n, 8)
            sl = n % NRING
            fs = slice(fb * 256, (fb + 1) * 256)
            S.dma("pool", lambda e_: e_.dma_start(out=ringG[sl][:], in_=w_gate[e].rearrange("(c p) f -> p c f", p=128)[:, :, fs]), b_ring[sl])
            S.dma("pool", lambda e_: e_.dma_start(out=ringU[sl][:], in_=w_up[e].rearrange("(c p) f -> p c f", p=128)[:, :, fs]), b_ring[sl])
            S.dma("pool", lambda e_: e_.dma_start(out=ringD[sl][:], in_=w_down[e][fb * 256:(fb + 1) * 256, :].rearrange("(c p) d -> p c d", p=128)), b_ring[sl])

        def gather_prep(e):
            st = SelT[e % 2]
            bs_ = b_sel[e % 2]
            S.pe([mm(bank(1)[:, 0:CAP], esel[0:16, e * 128:(e + 1) * 128], idxf[0:16, :])], reads=[b_esel, b_idxf], writes=[PB[1]])
            S.op("dve", lambda e_: e_.tensor_tensor(out=st[:], in0=bank(1)[:, 0:CAP].unsqueeze(1).broadcast_to([128, NT, CAP]),
                                                    in1=tokid[:, :].unsqueeze(2).broadcast_to([128, NT, CAP]), op=ALU.is_equal),
                 reads=[PB[1], b_tokid], writes=[bs_])

        def gather_expert(e):
            st = SelT[e % 2]
            bs_ = b_sel[e % 2]
            xs = xsT[e % 2]
            for ps_ in range(2):
                for dcl in range(4):
                    dc = ps_ * 4 + dcl
                    bk = 2 * ps_ + dcl // 2
                    co = (dcl % 2) * 256
                    S.pe([mm(bank(bk)[:, co:co + 256], h2tm[:, j, dc * 128:(dc + 1) * 128], st[:, j, :], j == 0, j == NT - 1) for j in range(NT)],
                         reads=[b_h2tm, bs_], writes=[PB[bk]])
                evac(xs[:, ps_ * 4:(ps_ + 1) * 4, :], PP[ps_][:].rearrange("p (d c) -> p d c", c=CAP), [PB[2 * ps_], PB[2 * ps_ + 1]], [b_xs[e % 2]])

        def unit_banks(n):
            return (2, 3) if n % 2 == 0 else (0, 1)

        def ffn_gu(n):
            e, fb = divmod(n, 8)
            sl = n % NRING
            xs = xsT[e % 2]
            hd = hidT[n % 2]
            bg, bu = unit_banks(n)
            for (bk, W) in ((bg, ringG[sl]), (bu, ringU[sl])):
                fns = []
                for fl in range(2):
                    fns += [mm(bank(bk)[:, fl * 256:(fl + 1) * 256], W[:, dc, fl * 128:(fl + 1) * 128], xs[:, dc, :], dc == 0, dc == 7) for dc in range(8)]
                S.pe(fns, reads=[b_ring[sl], b_xs[e % 2]], writes=[PB[bk]])
            sv = silu_t[n % 2]
            S.op("act", lambda e_: e_.activation(out=sv[:], in_=bank(bg), func=AF.Silu), reads=[PB[bg]], writes=[b_silu[n % 2]])
            S.op("dve", lambda e_: e_.tensor_tensor(out=hd[:].rearrange("p a b -> p (a b)"), in0=sv[:], in1=bank(bu), op=ALU.mult),
                 reads=[b_silu[n % 2], PB[bu]], writes=[b_hid[n % 2]])

        def ffn_down(n):
            e, fb = divmod(n, 8)
            sl = n % NRING
            hd = hidT[n % 2]
            for cc2 in range(2):
                for half in range(2):
                    bk = 4 + cc2 * 2 + half
                    S.pe([mm(bank(bk), hd[:, fl, cc2 * 128:(cc2 + 1) * 128], ringD[sl][:, fl, half * 512:(half + 1) * 512],
                             fb == 0 and fl == 0, fb == 7 and fl == 1) for fl in range(2)],
                         reads=[b_hid[n % 2], b_ring[sl]], writes=[PB[bk]])

        def finish_expert(e):
            g = e // GRP
            for cc2 in range(2):
                S.op("act", lambda e_, cc2=cc2: e_.activation(out=ysc[g % 2][:, (e % GRP) * 2 + cc2, :], in_=PP[2 + cc2][:], func=AF.Copy,
                                                              scale=gateT[:, cc2, e:e + 1]),
                     reads=[PB[4 + 2 * cc2], PB[5 + 2 * cc2], b_igT], writes=[b_ysc[g % 2]])

        sc_todo = []

        def scatter_group(g):
            def sc_tile(j, pp):
                ix = idxs[j % 2]
                bi = b_idxs2[j % 2]
                sc = selsc[j % 2]
                bsc = b_selsc[j % 2]
                S.op("dve", lambda e_: e_.tensor_scalar(out=ix[:, 0:4].rearrange("p (e c) -> p e c", c=2),
                                                        in0=idxT[:, :, g * GRP:(g + 1) * GRP].rearrange("p c e -> p e c"),
                                                        scalar1=-128.0 * j, scalar2=None, op0=ALU.add), reads=[b_igT], writes=[bi])
                S.op("dve", lambda e_: e_.tensor_tensor(out=sc[:], in0=ix[:, 0:4].unsqueeze(2).broadcast_to([128, 4, 128]),
                                                        in1=iota[:, :].unsqueeze(1).broadcast_to([128, 4, 128]), op=ALU.is_equal),
                     reads=[bi, b_iota], writes=[bsc])
                pbs = [PB[2 * pp], PB[2 * pp + 1]]
                S.pe([mm(PP[pp][:, half * 512:(half + 1) * 512], sc[:, k, :], ysc[g % 2][:, k, half * 512:(half + 1) * 512], k == 0, k == 3)
                      for half in range(2) for k in range(4)], reads=[bsc, b_ysc[g % 2]], writes=pbs)
                S.op("dve", lambda e_: e_.tensor_tensor(out=resid[:, j, :], in0=resid[:, j, :], in1=PP[pp][:], op=ALU.add),
                     reads=[b_res[j]] + pbs, writes=[b_res[j]])
            for j in range(NT):
                sc_todo.append(lambda pp, j=j: sc_tile(j, pp))

        NEXP = NE
        NU = NEXP * 8
        for n0 in range(NRING):
            load_unit(n0)
        gather_prep(0)
        gather_expert(0)
        ffn_gu(0)
        for n in range(NU):
            e, fb = divmod(n, 8)
            if n + 1 < NU:
                if (n + 1) % 8 == 0:
                    gather_expert((n + 1) // 8)
                ffn_gu(n + 1)
            ffn_down(n)
            if n + NRING < NU:
                load_unit(n + NRING)
            if fb == 3 and e + 1 < NEXP:
                gather_prep(e + 1)
            if sc_todo:
                sc_todo.pop(0)(1 if n % 2 == 0 else 0)
            if fb == 7:
                finish_expert(e)
                if e % GRP == GRP - 1:
                    scatter_group(e // GRP)
        k_ = 0
        while sc_todo:
            sc_todo.pop(0)(k_ % 2)
            k_ += 1

        for bb in [b_lnp] + b_ysbs:
            S.inherit(bb, [b_ring[2]])
        load_lnp(2)
        outv = out.rearrange("(j p) d -> j p d", p=128)
        b_out = Buf("out")

        def out_tile(j):
            S.dma("sp", lambda e: e.dma_start(out=outv[j], in_=resid[:, j, :]), b_out, reads=[b_res[j]], writes=[b_out])
        ln_A(0, None, [], None)
        for j in range(NT):
            if j + 1 < NT:
                ln_A(j + 1, None, [], None)
            ln_B(j, from_resid=True, final_out=out_tile)
        S.wait_bufs("sp", [b_out, b_dbg])
        S.emit()
    return nc


def make_consts():
    c = {}
    c["c_ident"] = np.eye(128, dtype=np.float32)
    c["c_iota"] = np.tile(np.arange(128, dtype=np.float32)[None, :], (128, 1))
    c["c_tokid"] = (np.arange(NT, dtype=np.float32)[None, :] * 128 + np.arange(128, dtype=np.float32)[:, None]).astype(np.float32)
    es = np.zeros((16, 16, 128), np.float32)
    for e in range(16):
        es[e, e, :] = 1.0
    c["c_esel"] = es.reshape(16, 16 * 128)
    rope = np.zeros((128, 2), np.float32)
    inv_freq = (10000.0 ** (-np.arange(0, 32, 2, dtype=np.float32) / 32.0)).astype(np.float32)
    for p in range(64, 96):
        rope[p, 0] = np.float32(np.float64(inv_freq[(p - 64) % 16]) / (2.0 * np.pi))
        rope[p, 1] = -1.0 if p < 80 else 1.0
    c["c_rope"] = rope
    return c


def make_in_maps(inputs, n_cores=8):
    consts = make_consts()
    maps = []
    f = lambda a: np.ascontiguousarray(np.asarray(a))
    for b in range(n_cores):
        m = dict(consts)
        m["x"] = f(inputs["x"][b])
        m["mem"] = f(inputs["mem"][b])
        m["positions"] = f(np.asarray(inputs["positions"])[b:b + 1]).astype(np.int32)
        for k in ["w_in", "q_norm_g", "w_uq", "kv_norm_g", "w_uk", "w_uv", "conv_w", "conv_b", "conv_ln_g", "conv_ln_b",
                  "w_o", "xa_w_q", "xa_w_k", "xa_w_v", "xa_w_o", "w_router", "w_gate", "w_up", "w_down"]:
            m[k] = f(np.asarray(inputs[k])[0])
        for k in ["ln1_g", "ln1_b", "ln2_g", "ln2_b", "ln3_g", "ln3_b"]:
            m[k] = f(np.asarray(inputs[k])[0:1])
        maps.append(m)
    return maps


def kernel(**inputs):
    nc = build_program()
    maps = make_in_maps(inputs, 8)
    res = run_bass_kernel_spmd(nc, maps, core_ids=list(range(8)))
    return np.stack([r["out"] for r in res.results], axis=0).astype(np.float32)
```

```python
import math
import os
from contextlib import ExitStack

import numpy as np
import concourse.bass as bass
import concourse.mybir as mybir
from concourse.bass_utils import run_bass_kernel_spmd

F32 = mybir.dt.float32
BF16 = mybir.dt.bfloat16
I32 = mybir.dt.int32
U32 = mybir.dt.uint32
ALU = mybir.AluOpType
AF = mybir.ActivationFunctionType

S_ = 2048
D = 1024
NT = 16
EPS = 1e-5
ALPHA = 2.0 ** 0.25
NE = 16
CAP = 256
FF = 2048


class Buf:
    __slots__ = ("name", "w", "r", "sem", "cnt")

    def __init__(self, name):
        self.name = name
        self.w = None
        self.r = {}
        self.sem = None
        self.cnt = 0


class Sched:
    ENG = ["pe", "act", "dve", "pool", "sp"]

    def __init__(self, nc, es):
        self.nc = nc
        self.es = es
        self.streams = {e: [] for e in self.ENG}
        self.cnt = {e: 0 for e in self.ENG}
        self.esem = {e: es.enter_context(nc.semaphore("c_" + e)) for e in self.ENG}
        self.waited = {e: {} for e in self.ENG}
        self.nsem = len(self.ENG)

    def new_sem(self, name):
        self.nsem += 1
        return self.es.enter_context(self.nc.semaphore("d%d_%s" % (self.nsem, name)))

    def _waits(self, eng, reads, writes, skip_sem=None):
        deps = {}

        def add(s, v):
            if deps.get(s, 0) < v:
                deps[s] = v
        for b in reads:
            if b.w is not None:
                add(*b.w)
        for b in writes:
            if b.w is not None:
                add(*b.w)
            for s, v in b.r.items():
                add(s, v)
        own = self.esem[eng]
        wd = self.waited[eng]
        for s, v in deps.items():
            if s == own and eng == "pe":
                continue
            if skip_sem is not None and s == skip_sem:
                continue
            if wd.get(s, 0) < v:
                wd[s] = v
                self.streams[eng].append(("wait", s, v))

    def _commit(self, ev, reads, writes):
        s, v = ev
        for b in reads:
            if b.r.get(s, 0) < v:
                b.r[s] = v
        for b in writes:
            b.w = ev
            b.r = {}

    def op(self, eng, fn, reads=(), writes=()):
        self._waits(eng, reads, writes)
        self.cnt[eng] += 1
        s = self.esem[eng]
        self.streams[eng].append(("op", fn, s, 1))
        self._commit((s, self.cnt[eng]), reads, writes)

    def pe(self, fns, reads=(), writes=()):
        self._waits("pe", reads, writes)
        self.cnt["pe"] += 1
        s = self.esem["pe"]
        for f in fns[:-1]:
            self.streams["pe"].append(("op", f, None, 0))
        self.streams["pe"].append(("op", fns[-1], s, 1))
        self._commit((s, self.cnt["pe"]), reads, writes)

    def dma(self, eng, fn, dst, reads=(), writes=None):
        if writes is None:
            writes = (dst,)
        if dst.sem is None:
            dst.sem = self.new_sem(dst.name)
        self._waits(eng, reads, writes, skip_sem=dst.sem)
        dst.cnt += 16
        self.streams[eng].append(("op", fn, dst.sem, 16))
        self._commit((dst.sem, dst.cnt), reads, writes)

    def wait_bufs(self, eng, bufs):
        self._waits(eng, bufs, bufs)

    def inherit(self, new, olds):
        for o in olds:
            if o.w is not None:
                s, v = o.w
                if new.r.get(s, 0) < v:
                    new.r[s] = v
            for s, v in o.r.items():
                if new.r.get(s, 0) < v:
                    new.r[s] = v

    def emit(self):
        nc = self.nc
        streams = self.streams

        def run(name, eng):
            for item in streams[name]:
                if item[0] == "wait":
                    eng.wait_ge(item[1], item[2])
                else:
                    ins = item[1](eng)
                    if item[2] is not None:
                        ins.then_inc(item[2], item[3])

        with nc.Block() as block:
            @block.tensor
            def _(e):
                run("pe", e)

            @block.scalar
            def _(e):
                run("act", e)

            @block.vector
            def _(e):
                run("dve", e)

            @block.gpsimd
            def _(e):
                run("pool", e)

            @block.sync
            def _(e):
                run("sp", e)


def build_program(stop_after=None, dumps=()):
    nc = bass.Bass("TRN2", target_bir_lowering=False)

    def din(name, shape, dt=F32):
        return nc.dram_tensor(name, list(shape), dt, kind="ExternalInput").ap()

    x = din("x", [S_, D])
    mem = din("mem", [256, D])
    pos = din("positions", [1, S_], I32)
    w_in = din("w_in", [D, 1440])
    q_norm_g = din("q_norm_g", [256])
    w_uq = din("w_uq", [256, 768])
    kv_norm_g = din("kv_norm_g", [128])
    w_uk = din("w_uk", [128, 512])
    w_uv = din("w_uv", [128, 512])
    conv_w = din("conv_w", [31, 512])
    conv_b = din("conv_b", [512])
    conv_ln_g = din("conv_ln_g", [512])
    conv_ln_b = din("conv_ln_b", [512])
    w_o = din("w_o", [D, D])
    ln_g = [din("ln%d_g" % i, [1, D]) for i in (1, 2, 3)]
    ln_b = [din("ln%d_b" % i, [1, D]) for i in (1, 2, 3)]
    xa_w = {k: din("xa_w_" + k, [D, D]) for k in "qkvo"}
    w_router = din("w_router", [D, NE])
    w_gate = din("w_gate", [NE, D, FF])
    w_up = din("w_up", [NE, D, FF])
    w_down = din("w_down", [NE, FF, D])
    c_ident = din("c_ident", [128, 128])
    c_iota = din("c_iota", [128, 128])
    c_tokid = din("c_tokid", [128, NT])
    c_esel = din("c_esel", [16, 16 * 128])
    c_rope = din("c_rope", [128, 2])
    out = nc.dram_tensor("out", [S_, D], F32, kind="ExternalOutput").ap()
    h2d = nc.dram_tensor("h2_scratch", [S_, D], BF16).ap()
    dump_d = {}
    for (nm, shape, dt) in dumps:
        dump_d[nm] = nc.dram_tensor("dbg_" + nm, list(shape), dt, kind="ExternalOutput").ap()

    es = ExitStack()
    with es:
        S = Sched(nc, es)
        cnt = [0]

        def T(shape, dt, off, name=None):
            cnt[0] += 1
            return nc.alloc_sbuf_tensor_at("%s_%d" % (name or "t", cnt[0]), list(shape), dt, offset=off)

        b_dbg = Buf("dbg")
        conv_todo = []
        drain = {"rate": 0.0, "credit": 0.0, "busy": False}
        _raw_op = S.op

        def op_hook(eng, fn, reads=(), writes=()):
            _raw_op(eng, fn, reads=reads, writes=writes)
            if eng == "dve" and drain["rate"] > 0.0 and not drain["busy"] and conv_todo:
                drain["credit"] += drain["rate"]
                drain["busy"] = True
                while drain["credit"] >= 1.0 and conv_todo:
                    conv_todo.pop(0)()
                    drain["credit"] -= 1.0
                drain["busy"] = False
        S.op = op_hook

        def rsqrt_act(dst, src, bias_ap, rd, wr):
            S.op("act", lambda e: e.activation(out=dst, in_=src, func=AF.Ln, bias=bias_ap, scale=1.0), reads=rd + [b_cst], writes=wr)
            S.op("act", lambda e: e.activation(out=dst, in_=dst, func=AF.Exp, scale=-0.5), reads=wr, writes=wr)

        def recip_act(dst, src, rd, wr):
            S.op("act", lambda e: e.activation(out=dst, in_=src, func=AF.Ln), reads=rd, writes=wr)
            S.op("act", lambda e: e.activation(out=dst, in_=dst, func=AF.Exp, scale=-1.0), reads=wr, writes=wr)

        def dump(nm, ap, bufs):
            if nm in dump_d:
                S.dma("sp", lambda e: e.dma_start(out=dump_d[nm], in_=ap), b_dbg, reads=bufs, writes=[b_dbg])

        R0 = 16640
        R1 = R0 + 66048
        R2 = R1 + 32768
        R3 = R2 + 26624
        R4 = R3 + 24576
        R5 = R4 + 8192
        R6 = R5 + 16384
        R7 = R6 + 8192
        R8 = R7 + 8192
        R9 = R8 + 8192
        ident = T([128, 128], F32, R9); b_ident = Buf("ident")
        identb = T([128, 128], BF16, R9 + 512); b_identb = Buf("identb")
        onesb = T([128, 128], BF16, R9 + 768); b_onesb = Buf("onesb")
        onesf = T([128, 128], F32, R9 + 1024); b_onesf = Buf("onesf")
        ropec = T([128, 2], F32, R9 + 1536); b_ropec = Buf("ropec")
        gsm = T([128, 16], F32, R9 + 1600); b_gsm = Buf("gsm")
        cwT = T([128, 4, 32], F32, R9 + 1664); b_cwT = Buf("cwT")
        iota = T([128, 128], F32, R9 + 2176); b_iota = Buf("iota")
        tokid = T([128, NT], F32, R9 + 2688); b_tokid = Buf("tokid")
        stat = T([128, 16], F32, R9 + 2752); b_stat = Buf("stat")
        cst = T([128, 8], F32, R9 + 2816); b_cst = Buf('cst')
        stat2 = T([128, 32], F32, R9 + 2848); b_st = [Buf("st0"), Buf("st1")]
        CEND = R9 + 2976
        assert CEND <= 229376, CEND

        PP = [nc.alloc_psum_tensor("pp%d" % i, [128, 1024], F32) for i in range(4)]
        PB = [Buf("bank%d" % i) for i in range(8)]

        def bank(k):
            return PP[k // 2][:, (k % 2) * 512:(k % 2) * 512 + 512]

        S.dma("sp", lambda e: e.dma_start(out=ident[:], in_=c_ident), b_ident)
        S.dma("sp", lambda e: e.dma_start(out=iota[:], in_=c_iota), b_iota)
        S.dma("sp", lambda e: e.dma_start(out=tokid[:], in_=c_tokid), b_tokid)
        S.dma("sp", lambda e: e.dma_start(out=ropec[:], in_=c_rope), b_ropec)
        S.op("dve", lambda e: e.tensor_copy(out=identb[:], in_=ident[:]), reads=[b_ident], writes=[b_identb])
        S.op("dve", lambda e: e.memset(onesb[:], 1.0), writes=[b_onesb])
        S.op("dve", lambda e: e.memset(onesf[:], 1.0), writes=[b_onesf])
        S.op("dve", lambda e: e.memset(cst[:, 0:1], 256.0 * EPS), writes=[b_cst])
        S.op("dve", lambda e: e.memset(cst[:, 1:2], 128.0 * EPS), writes=[b_cst])
        S.op("dve", lambda e: e.memset(cst[:, 2:3], EPS), writes=[b_cst])
        S.op("dve", lambda e: e.memset(cst[:, 3:4], 0.0), writes=[b_cst])
        S.op("dve", lambda e: e.memset(cst[:, 4:5], EPS / (ALPHA * ALPHA)), writes=[b_cst])
        with nc.allow_non_contiguous_dma(reason="tiny per-channel parameter vectors"):
            S.dma("sp", lambda e: e.dma_start(out=gsm[:, 0:2], in_=q_norm_g.rearrange("(c p) -> p c", p=128), allow_slow_non_contiguous=True), b_gsm)
            S.dma("sp", lambda e: e.dma_start(out=gsm[:, 2:3], in_=kv_norm_g.rearrange("(c p) -> p c", p=128), allow_slow_non_contiguous=True), b_gsm)
            S.dma("sp", lambda e: e.dma_start(out=gsm[:, 4:8], in_=conv_b.rearrange("(c p) -> p c", p=128), allow_slow_non_contiguous=True), b_gsm)
            S.dma("sp", lambda e: e.dma_start(out=gsm[:, 8:12], in_=conv_ln_g.rearrange("(c p) -> p c", p=128), allow_slow_non_contiguous=True), b_gsm)
            S.dma("sp", lambda e: e.dma_start(out=gsm[:, 12:16], in_=conv_ln_b.rearrange("(c p) -> p c", p=128), allow_slow_non_contiguous=True), b_gsm)
        S.op("dve", lambda e: e.tensor_scalar(out=gsm[:, 0:2], in0=gsm[:, 0:2], scalar1=16.0, scalar2=None, op0=ALU.mult),
             reads=[b_gsm], writes=[b_gsm])
        S.op("dve", lambda e: e.tensor_scalar(out=gsm[:, 2:3], in0=gsm[:, 2:3], scalar1=math.sqrt(128.0), scalar2=None, op0=ALU.mult),
             reads=[b_gsm], writes=[b_gsm])

        resid = T([128, NT, D], F32, R0); b_res = [Buf("res%d" % j) for j in range(NT)]
        u = T([128, 4, 2078], F32, R0); b_u = [Buf("u%d" % c) for c in range(4)]
        acc = T([128, 4, 2048], F32, R0 + 33248); b_acc = [Buf("acc%d" % c) for c in range(4)]
        xstage = [T([128, D], F32, R0 + 33248 + i * 4096) for i in range(2)]; b_xst = [Buf("xst%d" % i) for i in range(2)]
        actT = T([128, 8, S_], BF16, R1); b_actT = [Buf("actT%d" % t) for t in range(4)]
        kT = actT; b_kT = Buf("kT")
        w_in_sb = T([128, 8, 1440], BF16, R2); b_w_in = Buf("w_in")
        wkr = T([128, 8, 2, 96], BF16, R2 + 23040); b_wkr = Buf("wkr")
        attnT = T([128, 4, S_], BF16, R2); b_attnT = Buf("attnT")
        qT = [T([128, 512], BF16, R2 + 16384 + i * 1024) for i in range(2)]; b_qT = [Buf("qT%d" % i) for i in range(2)]
        NPT = 4
        pTt = [T([128, 512], BF16, R2 + 18432 + i * 1024) for i in range(NPT)]; b_pT = [Buf("pT%d" % i) for i in range(NPT)]
        Vaug = T([128, NT, 4, 192], BF16, R3); b_V = Buf("V")
        cqnT = T([128, 2, S_], BF16, R4); b_cqn = Buf("cqn")
        convT = T([128, 4, S_], BF16, R5); b_convT = Buf("convT")
        kr = T([128, S_], BF16, R5); b_kr = Buf("kr")
        ckvnT = T([128, S_], BF16, R5 + 4096); b_ckvn = Buf("ckvn")
        COS = T([128, S_], BF16, R6); SIN = T([128, S_], BF16, R6 + 4096); b_cs = Buf("cossin")
        scr = [T([128, 512], F32, R7 + i * 2048) for i in range(4)]; b_scr = [Buf("scr%d" % i) for i in range(4)]
        wuq = T([128, 2, 768], BF16, R8); b_wuq = Buf("wuq")
        wqsw = T([128, 2, 8, 96], BF16, R8 + 3072); b_wqsw = Buf("wqsw")
        wuk = T([128, 512], BF16, R8 + 6144); b_wuk = Buf("wuk")
        wuv = T([128, 512], BF16, R8 + 7168); b_wuv = Buf("wuv")

        w_in_v = w_in.rearrange("(c p) f -> p c f", p=128)
        for c0 in range(0, 8, 4):
            S.dma("pool", lambda e, c0=c0: e.dma_start(out=w_in_sb[:, c0:c0 + 4, :], in_=w_in_v[:, c0:c0 + 4, :]), b_w_in)
        S.op("pool", lambda e: e.memset(wkr[:], 0.0), writes=[b_wkr])
        S.op("pool", lambda e: e.memset(wqsw[:], 0.0), writes=[b_wqsw])
        with nc.allow_non_contiguous_dma(reason="rope column permutation"):
            S.dma("pool", lambda e: e.dma_start(out=wkr[:, :, 0, 64:96], in_=w_in_v[:, :, 384:416], allow_slow_non_contiguous=True), b_wkr)
            S.dma("pool", lambda e: e.dma_start(out=wkr[:, :, 1, 64:80], in_=w_in_v[:, :, 400:416], allow_slow_non_contiguous=True), b_wkr)
            S.dma("pool", lambda e: e.dma_start(out=wkr[:, :, 1, 80:96], in_=w_in_v[:, :, 384:400], allow_slow_non_contiguous=True), b_wkr)
            w_uq_v = w_uq.rearrange("(c p) (h f) -> p c h f", p=128, f=96)
            for c in range(2):
                S.dma("pool", lambda e, c=c: e.dma_start(out=wqsw[:, c, :, 64:80], in_=w_uq_v[:, c, :, 80:96], allow_slow_non_contiguous=True), b_wqsw)
                S.dma("pool", lambda e, c=c: e.dma_start(out=wqsw[:, c, :, 80:96], in_=w_uq_v[:, c, :, 64:80], allow_slow_non_contiguous=True), b_wqsw)
        S.dma("pool", lambda e: e.dma_start(out=wuq[:], in_=w_uq.rearrange("(c p) f -> p c f", p=128)), b_wuq)
        S.dma("pool", lambda e: e.dma_start(out=wuk[:], in_=w_uk), b_wuk)
        S.dma("pool", lambda e: e.dma_start(out=wuv[:], in_=w_uv), b_wuv)

        cw_st = scr[0]
        S.dma("sp", lambda e: e.dma_start(out=cw_st[0:31, 0:512], in_=conv_w), b_scr[0])
        S.pe([lambda e, cc=cc: e.transpose(bank(0)[:, cc * 32:cc * 32 + 31], cw_st[0:31, cc * 128:(cc + 1) * 128], ident[0:31, 0:31])
              for cc in range(4)], reads=[b_scr[0], b_ident], writes=[PB[0]])
        S.op("dve", lambda e: e.tensor_copy(out=cwT[:, :, 0:31], in_=bank(0)[:, 0:128].rearrange("p (c k) -> p c k", k=32)[:, :, 0:31]),
             reads=[PB[0]], writes=[b_cwT])

        posi = T([128, S_], I32, R0)
        posf = T([128, S_], F32, R0 + 8192)
        angb = T([128, S_], F32, R0 + 16384)
        b_pos = Buf("pos")
        S.dma("sp", lambda e: e.dma_start(out=posi[:], in_=pos.broadcast_to([128, S_])), b_pos)
        rp = slice(64, 96)
        S.op("dve", lambda e: e.tensor_copy(out=posf[rp, :], in_=posi[rp, :]), reads=[b_pos], writes=[b_pos])
        kint = T([128, S_], I32, R0 + 24576)
        sinf = T([128, S_], F32, R0 + 24576)

        def trig(phase, dst_fn):
            S.op("dve", lambda e: e.tensor_scalar(out=angb[rp, :], in0=posf[rp, :], scalar1=ropec[rp, 0:1], scalar2=phase,
                                                  op0=ALU.mult, op1=ALU.add), reads=[b_pos, b_ropec], writes=[b_pos])
            S.op("dve", lambda e: e.tensor_copy(out=kint[rp, :], in_=angb[rp, :]), reads=[b_pos], writes=[b_pos])
            S.op("dve", lambda e: e.tensor_copy(out=posi[rp, :].bitcast(F32), in_=kint[rp, :]), reads=[b_pos], writes=[b_pos])
            S.op("dve", lambda e: e.tensor_tensor(out=angb[rp, :], in0=angb[rp, :], in1=posi[rp, :].bitcast(F32), op=ALU.subtract),
                 reads=[b_pos], writes=[b_pos])
            S.op("dve", lambda e: e.scalar_tensor_tensor(out=angb[rp, :], in0=angb[rp, :], scalar=0.5, in1=angb[rp, :],
                                                         op0=ALU.is_gt, op1=ALU.subtract), reads=[b_pos], writes=[b_pos])
            dst_fn()

        def fin_sin():
            S.op("act", lambda e: e.activation(out=sinf[rp, :], in_=angb[rp, :], func=AF.Sin, scale=-2.0 * math.pi), reads=[b_pos], writes=[b_pos])
            S.op("dve", lambda e: e.tensor_scalar(out=SIN[rp, :], in0=sinf[rp, :], scalar1=ropec[rp, 1:2], scalar2=None, op0=ALU.mult),
                 reads=[b_pos, b_ropec], writes=[b_cs])

        def fin_cos():
            S.op("act", lambda e: e.activation(out=COS[rp, :], in_=angb[rp, :], func=AF.Sin, scale=-2.0 * math.pi), reads=[b_pos], writes=[b_cs])
        trig(0.0, fin_sin)
        trig(0.25, fin_cos)
        for c in range(4):
            S.inherit(b_u[c], [b_pos])

        xv = x.rearrange("(j p) d -> j p d", p=128)
        for j in range(NT):
            st = j % 2
            S.dma("sp", lambda e, j=j, st=st: e.dma_start(out=xstage[st][:], in_=xv[j]), b_xst[st])
            pb = [PB[(j % 2) * 2], PB[(j % 2) * 2 + 1]]
            pt = PP[j % 2]
            S.pe([lambda e, c=c, st=st, pt=pt: e.transpose(pt[:, c * 128:(c + 1) * 128], xstage[st][:, c * 128:(c + 1) * 128], ident[:])
                  for c in range(8)], reads=[b_xst[st], b_ident], writes=pb)
            eng = "act" if j % 2 == 0 else "dve"
            if eng == "act":
                S.op("act", lambda e, j=j, pt=pt: e.activation(out=actT[:, :, j * 128:(j + 1) * 128],
                                                             in_=pt[:].rearrange("p (c t) -> p c t", t=128), func=AF.Copy),
                     reads=pb, writes=[b_actT[j // 4]])
            else:
                S.op("dve", lambda e, j=j, pt=pt: e.tensor_copy(out=actT[:, :, j * 128:(j + 1) * 128],
                                                              in_=pt[:].rearrange("p (c t) -> p c t", t=128)),
                     reads=pb, writes=[b_actT[j // 4]])
        dump("xT", actT[:, :, :], b_actT)

        S.op("pool", lambda e: e.memset(u[:, :, 0:15], 0.0), writes=b_u)
        S.op("pool", lambda e: e.memset(u[:, :, 2063:2078], 0.0), writes=b_u)
        for c in range(4):
            S.inherit(b_acc[c], b_xst)

        def inproj_group(bk, lhs_fn, tb, M):
            cols = slice(tb * 512, (tb + 1) * 512)
            S.pe([lambda e, c=c: e.matmul(bank(bk)[0:M, :], lhsT=lhs_fn(c), rhs=actT[:, c, cols], start=(c == 0), stop=(c == 7))
                  for c in range(8)], reads=[b_w_in, b_wkr, b_actT[tb]], writes=[PB[bk]])

        def inproj_tb(tb):
            cols = slice(tb * 512, (tb + 1) * 512)
            for c2 in range(2):
                inproj_group(c2, lambda c, c2=c2: w_in_sb[:, c, c2 * 128:(c2 + 1) * 128], tb, 128)
                S.op("act", lambda e, c2=c2: e.activation(out=scr[c2][:].bitcast(BF16)[:, 0:512], in_=bank(c2), func=AF.Square),
                     reads=[PB[c2]], writes=[b_scr[c2]])
            S.pe([lambda e, c2=c2: e.matmul(bank(2), lhsT=onesb[:], rhs=scr[c2][:].bitcast(BF16)[:, 0:512], start=(c2 == 0), stop=(c2 == 1))
                  for c2 in range(2)], reads=[b_onesb, b_scr[0], b_scr[1]], writes=[PB[2]])
            rsqrt_act(scr[2][:], bank(2), cst[:, 0:1], [PB[2]], [b_scr[2]])
            for c2 in range(2):
                S.op("dve", lambda e, c2=c2: e.scalar_tensor_tensor(out=cqnT[:, c2, cols], in0=bank(c2), scalar=gsm[:, c2:c2 + 1], in1=scr[2][:],
                                                                   op0=ALU.mult, op1=ALU.mult),
                     reads=[PB[c2], b_gsm, b_scr[2]], writes=[b_cqn])
            inproj_group(3, lambda c: w_in_sb[:, c, 256:384], tb, 128)
            S.op("act", lambda e: e.activation(out=scr[3][:].bitcast(BF16)[:, 0:512], in_=bank(3), func=AF.Square),
                 reads=[PB[3]], writes=[b_scr[3]])
            S.pe([lambda e: e.matmul(bank(2), lhsT=onesb[:], rhs=scr[3][:].bitcast(BF16)[:, 0:512], start=True, stop=True)],
                 reads=[b_onesb, b_scr[3]], writes=[PB[2]])
            rsqrt_act(scr[2][:], bank(2), cst[:, 1:2], [PB[2]], [b_scr[2]])
            S.op("dve", lambda e: e.scalar_tensor_tensor(out=ckvnT[:, cols], in0=bank(3), scalar=gsm[:, 2:3], in1=scr[2][:],
                                                         op0=ALU.mult, op1=ALU.mult),
                 reads=[PB[3], b_gsm, b_scr[2]], writes=[b_ckvn])
            inproj_group(4, lambda c: wkr[:, c, 0, :], tb, 96)
            inproj_group(5, lambda c: wkr[:, c, 1, :], tb, 96)
            S.op("dve", lambda e: e.tensor_tensor(out=scr[0][rp, :], in0=bank(4)[rp, :], in1=COS[rp, cols], op=ALU.mult),
                 reads=[PB[4], b_cs], writes=[b_scr[0]])
            S.op("dve", lambda e: e.tensor_tensor(out=scr[1][rp, :], in0=bank(5)[rp, :], in1=SIN[rp, cols], op=ALU.mult),
                 reads=[PB[5], b_cs], writes=[b_scr[1]])
            S.op("dve", lambda e: e.tensor_tensor(out=kr[rp, cols], in0=scr[0][rp, :], in1=scr[1][rp, :], op=ALU.add),
                 reads=[b_scr[0], b_scr[1]], writes=[b_kr])
        def inproj_glu(tb):
            cols = slice(tb * 512, (tb + 1) * 512)
            for cc in range(4):
                ba = 6 - 2 * (cc % 2)
                bg = 7 - 2 * (cc % 2)
                sg = scr[2 + cc % 2]
                bsg = b_scr[2 + cc % 2]
                inproj_group(ba, lambda c, cc=cc: w_in_sb[:, c, 416 + cc * 128:416 + (cc + 1) * 128], tb, 128)
                inproj_group(bg, lambda c, cc=cc: w_in_sb[:, c, 928 + cc * 128:928 + (cc + 1) * 128], tb, 128)
                S.op("act", lambda e, sg=sg, bg=bg: e.activation(out=sg[:], in_=bank(bg), func=AF.Sigmoid), reads=[PB[bg]], writes=[bsg])
                S.op("dve", lambda e, cc=cc, sg=sg, ba=ba: e.tensor_tensor(out=u[:, cc, 15 + tb * 512:15 + (tb + 1) * 512], in0=bank(ba), in1=sg[:], op=ALU.mult),
                     reads=[PB[ba], bsg], writes=[b_u[cc]])
        for tb in range(4):
            inproj_glu(tb)
        def conv_chunk(cc, eng):
            conv_todo.append(lambda: _raw_op(eng, lambda e: e.tensor_scalar(out=acc[:, cc, :], in0=u[:, cc, 0:2048], scalar1=cwT[:, cc, 0:1],
                                                                            scalar2=gsm[:, 4 + cc:5 + cc], op0=ALU.mult, op1=ALU.add),
                                             reads=[b_u[cc], b_cwT, b_gsm], writes=[b_acc[cc]]))
            for k in range(1, 31):
                conv_todo.append(lambda k=k: _raw_op(eng, lambda e: e.scalar_tensor_tensor(out=acc[:, cc, :], in0=u[:, cc, k:k + 2048],
                                                                                           scalar=cwT[:, cc, k:k + 1], in1=acc[:, cc, :],
                                                                                           op0=ALU.mult, op1=ALU.add),
                                                     reads=[b_u[cc], b_acc[cc], b_cwT], writes=[b_acc[cc]]))
        for cc in range(4):
            conv_chunk(cc, "dve")
        drain["rate"] = 0.5
        for tb in range(4):
            inproj_tb(tb)
        dump("cqnT", cqnT[:, :, :], [b_cqn])
        dump("ckvnT", ckvnT[:, :], [b_ckvn])
        dump("kr", kr[64:96, :], [b_kr])
        dump("u", u[:, :, 15:2063], b_u)
        if stop_after == "inproj":
            S.wait_bufs("sp", [b_dbg])
            S.emit()
            return nc


        def mm(o, l, r, st=True, sp=True):
            return lambda e: e.matmul(o, lhsT=l, rhs=r, start=st, stop=sp)

        evc = [0]

        def evac(out_ap, in_ap, reads, writes):
            evc[0] += 1
            if evc[0] % 2 == 0:
                S.op("act", lambda e: e.activation(out=out_ap, in_=in_ap, func=AF.Copy), reads=reads, writes=writes)
            else:
                S.op("dve", lambda e: e.tensor_copy(out=out_ap, in_=in_ap), reads=reads, writes=writes)

        S.inherit(b_kT, b_actT)

        def kup(h, tb):
            cols = slice(tb * 512, (tb + 1) * 512)
            bk = (h * 4 + tb) % 4
            S.pe([mm(bank(bk)[0:64, :], wuk[:, h * 64:(h + 1) * 64], ckvnT[:, cols])], reads=[b_wuk, b_ckvn], writes=[PB[bk]])
            evac(kT[0:64, h, cols], bank(bk)[0:64, :], [PB[bk]], [b_kT])
        for h in range(8):
            for tb in range(4):
                kup(h, tb)

        def krb(tb):
            cols = slice(tb * 512, (tb + 1) * 512)
            S.op("act", lambda e: e.activation(out=kT[64:96, :, cols], in_=kr[64:96, cols].unsqueeze(1).broadcast_to([32, 8, 512]), func=AF.Copy),
                 reads=[b_kr], writes=[b_kT])
        for tb in range(4):
            krb(tb)
        S.op("pool", lambda e: e.memset(Vaug[:, :, :, 64:128], 0.0), writes=[b_V])
        S.op("pool", lambda e: e.memset(Vaug[:, :, :, 64:65], 1.0), writes=[b_V])

        def vup(j):
            bk = 4 + (j % 4)
            S.pe([mm(bank(bk), ckvnT[:, j * 128:(j + 1) * 128], wuv[:])], reads=[b_wuv, b_ckvn], writes=[PB[bk]])
            pv = bank(bk).rearrange("p (q t d) -> p q t d", t=2, d=64)
            S.op("act", lambda e: e.activation(out=Vaug[:, j, :, 0:64], in_=pv[:, :, 0, :], func=AF.Copy), reads=[PB[bk]], writes=[b_V])
            S.op("dve", lambda e: e.tensor_copy(out=Vaug[:, j, :, 128:192], in_=pv[:, :, 1, :]), reads=[PB[bk]], writes=[b_V])
        for j in range(NT):
            vup(j)
        dump("kT", kT[0:96, :, :], [b_kT])
        dump("V", Vaug[:, :, :, :], [b_V])

        S.inherit(b_attnT, [b_w_in, b_wkr])
        for i in range(2):
            S.inherit(b_qT[i], [b_w_in, b_wkr])
        for i in range(NPT):
            S.inherit(b_pT[i], [b_w_in, b_wkr])
        SCALE = 1.0 / math.sqrt(96.0)

        def qup(s):
            qb, h = divmod(s, 8)
            cols = slice(qb * 512, (qb + 1) * 512)
            qt = qT[s % 2]
            bq = b_qT[s % 2]
            S.pe([mm(bank(5)[0:96, :], wuq[:, c, h * 96:(h + 1) * 96], cqnT[:, c, cols], c == 0, c == 1) for c in range(2)],
                 reads=[b_wuq, b_cqn], writes=[PB[5]])
            S.pe([mm(bank(6)[0:96, :], wqsw[:, c, h, :], cqnT[:, c, cols], c == 0, c == 1) for c in range(2)],
                 reads=[b_wqsw, b_cqn], writes=[PB[6]])
            S.op("dve", lambda e: e.tensor_copy(out=qt[0:64, :], in_=bank(5)[0:64, :]), reads=[PB[5]], writes=[bq])
            S.op("dve", lambda e: e.tensor_tensor(out=scr[0][rp, :], in0=bank(5)[rp, :], in1=COS[rp, cols], op=ALU.mult),
                 reads=[PB[5], b_cs], writes=[b_scr[0]])
            S.op("dve", lambda e: e.tensor_tensor(out=scr[1][rp, :], in0=bank(6)[rp, :], in1=SIN[rp, cols], op=ALU.mult),
                 reads=[PB[6], b_cs], writes=[b_scr[1]])
            S.op("dve", lambda e: e.tensor_tensor(out=qt[rp, :], in0=scr[0][rp, :], in1=scr[1][rp, :], op=ALU.add),
                 reads=[b_scr[0], b_scr[1]], writes=[bq])

        pctr = [0]

        def attn_step(s):
            qb, h = divmod(s, 8)
            pair, odd = divmod(h, 2)
            cols = slice(qb * 512, (qb + 1) * 512)
            qt = qT[s % 2]
            bq = b_qT[s % 2]
            ob = 3 + (s % 2)
            M = 128 if odd else 65

            def lhsV(kt):
                return Vaug[:, kt, pair, 64:192] if odd else Vaug[:, kt, pair, 0:65]

            def Smm(kt):
                bk = kt % 3
                S.pe([mm(bank(bk), kT[0:96, h, kt * 128:(kt + 1) * 128], qt[0:96, :])], reads=[b_kT, bq], writes=[PB[bk]])

            def EXPV(kt):
                pi = pctr[0] % NPT
                pctr[0] += 1
                p = pTt[pi]
                S.op("act", lambda e: e.activation(out=p[:], in_=bank(kt % 3), func=AF.Exp, scale=SCALE), reads=[PB[kt % 3]], writes=[b_pT[pi]])
                if kt + 2 < 16:
                    Smm(kt + 2)
                S.pe([mm(bank(ob)[0:M, :], lhsV(kt), p[:], kt == 0, kt == 15)], reads=[b_V, b_pT[pi]], writes=[PB[ob]])
            Smm(0)
            Smm(1)
            for kt in range(16):
                EXPV(kt)
                if kt == 2 and s > 0:
                    attn_norm(s - 1)

        def attn_norm(s):
            qb, h = divmod(s, 8)
            pair, odd = divmod(h, 2)
            cols = slice(qb * 512, (qb + 1) * 512)
            ob = 3 + (s % 2)
            if odd:
                dr, orow = slice(0, 1), slice(64, 128)
                lo = onesf[0:1, 0:128]
                bo = bank(7)
            else:
                dr, orow = slice(64, 65), slice(0, 64)
                lo = onesf[64:65, 0:64]
                bo = bank(7)[0:64, :]
            recip_act(scr[2][dr, :], bank(ob)[dr, :], [PB[ob]], [b_scr[2]])
            S.pe([mm(bo, lo, scr[2][dr, :])], reads=[b_onesf, b_scr[2]], writes=[PB[7]])
            S.op("dve", lambda e: e.tensor_copy(out=scr[3][orow, :], in_=bank(ob)[orow, :]), reads=[PB[ob]], writes=[b_scr[3]])
            S.op("dve", lambda e: e.tensor_tensor(out=attnT[orow, pair, cols], in0=scr[3][orow, :], in1=bank(7)[orow, :], op=ALU.mult),
                 reads=[b_scr[3], PB[7]], writes=[b_attnT])

        NSTEP = 32
        qup(0)
        for s in range(NSTEP):
            if s + 1 < NSTEP:
                qup(s + 1)
            attn_step(s)
        attn_norm(NSTEP - 1)
        drain["rate"] = 0.0
        while conv_todo:
            conv_todo.pop(0)()
        dump("acc", acc[:, :, :], b_acc)
        dump("attnT", attnT[:, :, :], [b_attnT])

        S.inherit(b_convT, [b_kr, b_ckvn])

        def conv_ln(tb):
            cols = slice(tb * 512, (tb + 1) * 512)
            S.pe([mm(bank(0), onesf[:], acc[:, cc, cols], cc == 0, cc == 3) for cc in range(4)], reads=[b_onesf] + b_acc, writes=[PB[0]])
            for cc in range(4):
                S.op("act", lambda e, cc=cc: e.activation(out=scr[3][:], in_=acc[:, cc, cols], func=AF.Square), reads=[b_acc[cc]], writes=[b_scr[3]])
                S.pe([mm(bank(1), onesf[:], scr[3][:], cc == 0, cc == 3)], reads=[b_onesf, b_scr[3]], writes=[PB[1]])
            S.op("dve", lambda e: e.tensor_scalar(out=scr[0][:], in0=bank(0), scalar1=1.0 / 512.0, scalar2=None, op0=ALU.mult),
                 reads=[PB[0]], writes=[b_scr[0]])
            S.op("dve", lambda e: e.tensor_tensor(out=scr[1][:], in0=scr[0][:], in1=scr[0][:], op=ALU.mult), reads=[b_scr[0]], writes=[b_scr[1]])
            S.op("dve", lambda e: e.scalar_tensor_tensor(out=scr[1][:], in0=bank(1), scalar=1.0 / 512.0, in1=scr[1][:], op0=ALU.mult, op1=ALU.subtract),
                 reads=[PB[1], b_scr[1]], writes=[b_scr[1]])
            rsqrt_act(scr[1][:], scr[1][:], cst[:, 2:3], [b_scr[1]], [b_scr[1]])
            for cc in range(4):
                S.op("dve", lambda e, cc=cc: e.tensor_tensor(out=scr[2][:], in0=acc[:, cc, cols], in1=scr[0][:], op=ALU.subtract),
                     reads=[b_acc[cc], b_scr[0]], writes=[b_scr[2]])
                S.op("dve", lambda e: e.tensor_tensor(out=scr[2][:], in0=scr[2][:], in1=scr[1][:], op=ALU.mult), reads=[b_scr[2], b_scr[1]], writes=[b_scr[2]])
                S.op("act", lambda e, cc=cc: e.activation(out=convT[:, cc, cols], in_=scr[2][:], func=AF.Silu, scale=gsm[:, 8 + cc:9 + cc], bias=gsm[:, 12 + cc:13 + cc]),
                     reads=[b_scr[2], b_gsm], writes=[b_convT])
        for tb in range(4):
            conv_ln(tb)
        dump("convT", convT[:, :, :], [b_convT])

        lnp = T([128, 2, D], F32, R6); b_lnp = Buf("lnp")
        S.inherit(b_lnp, [b_cs])
        ysbs = [T([128, D], F32, R7 + i * 4096) for i in range(2)]
        b_ysbs = [Buf("ysb0"), Buf("ysb1")]
        for bb in b_ysbs:
            S.inherit(bb, b_scr)
        ysb = ysbs[0]; b_ysb = b_ysbs[0]

        def load_lnp(i):
            S.dma("sp", lambda e: e.dma_start(out=lnp[:, 0, :], in_=ln_g[i].broadcast_to([128, D])), b_lnp)
            S.dma("sp", lambda e: e.dma_start(out=lnp[:, 1, :], in_=ln_b[i].broadcast_to([128, D])), b_lnp)

        def ln_A(j, xin_ap, xin_bufs, pk, eps_col=2):
            yb = ysbs[j % 2]
            byb = b_ysbs[j % 2]
            so = (j % 2) * 16
            bs = b_st[j % 2]
            st_ = lambda a, n=1: stat2[:, so + a:so + a + n]
            if pk is not None:
                pbs = [PB[2 * pk], PB[2 * pk + 1]]
                S.op("dve", lambda e: e.scalar_tensor_tensor(out=yb[:], in0=xin_ap, scalar=ALPHA, in1=PP[pk][:], op0=ALU.mult, op1=ALU.add),
                     reads=pbs + xin_bufs, writes=[byb])
                ysrc, ysrc_b = yb[:], [byb]
            else:
                ysrc, ysrc_b = resid[:, j, :], [b_res[j]]
            S.op("dve", lambda e: e.bn_stats(out=st_(0, 6), in_=ysrc[:, 0:512]), reads=ysrc_b, writes=[bs])
            S.op("dve", lambda e: e.bn_stats(out=st_(6, 6), in_=ysrc[:, 512:1024]), reads=ysrc_b, writes=[bs])
            S.op("dve", lambda e: e.bn_aggr(out=st_(12, 2), in_=st_(0, 12)), reads=[bs], writes=[bs])
            rsqrt_act(st_(14), st_(13), cst[:, eps_col:eps_col + 1], [bs], [bs])
            S.op("dve", lambda e: e.scalar_tensor_tensor(out=st_(15), in0=st_(12), scalar=-1.0, in1=st_(14), op0=ALU.mult, op1=ALU.mult), reads=[bs], writes=[bs])

        def ln_B(j, from_resid=False, extra=None, final_out=None):
            yb = ysbs[j % 2]
            byb = b_ysbs[j % 2]
            so = (j % 2) * 16
            bs = b_st[j % 2]
            st_ = lambda a, n=1: stat2[:, so + a:so + a + n]
            if from_resid:
                ysrc, ysrc_b = resid[:, j, :], [b_res[j]]
            else:
                ysrc, ysrc_b = yb[:], [byb]
            S.op("act", lambda e: e.activation(out=yb[:], in_=ysrc, func=AF.Identity, scale=st_(14), bias=st_(15)), reads=ysrc_b + [bs], writes=[byb])
            S.op("dve", lambda e: e.tensor_tensor(out=yb[:], in0=yb[:], in1=lnp[:, 0, :], op=ALU.mult), reads=[byb, b_lnp], writes=[byb])
            S.op("pool", lambda e: e.tensor_tensor(out=resid[:, j, :], in0=yb[:], in1=lnp[:, 1, :], op=ALU.add), reads=[byb, b_lnp], writes=[b_res[j]])
            if final_out is not None:
                final_out(j)
            if extra is not None:
                extra(j)

        def ln_T(j, tpk):
            tpb = [PB[2 * tpk], PB[2 * tpk + 1]]
            S.pe([lambda e, c=c: e.transpose(PP[tpk][:, c * 128:(c + 1) * 128], resid[:, j, c * 128:(c + 1) * 128], ident[:]) for c in range(8)],
                 reads=[b_res[j], b_ident], writes=tpb)
            evac(actT[:, :, j * 128:(j + 1) * 128], PP[tpk][:].rearrange("p (c t) -> p c t", t=128), tpb, [b_actT[j // 4]])

        wo_sb = T([128, 8, D], BF16, R3); b_wo = Buf("wo")
        S.inherit(b_wo, [b_V])
        w_o_v = w_o.rearrange("(c p) f -> p c f", p=128)
        for c0 in range(0, 8, 4):
            S.dma("pool", lambda e, c0=c0: e.dma_start(out=wo_sb[:, c0:c0 + 4, :], in_=w_o_v[:, c0:c0 + 4, :]), b_wo)
        load_lnp(0)
        xst2 = [T([128, D], F32, R4 + i * 4096) for i in range(2)]; b_xst2 = [Buf("xs2_%d" % i) for i in range(2)]
        for i in range(2):
            S.inherit(b_xst2[i], [b_cqn])
        for j in range(NT):
            S.inherit(b_res[j], b_u + b_acc + b_xst + [b_pos])
        for t in range(4):
            S.inherit(b_actT[t], [b_kT])

        def wo_mm(j):
            st = j % 2
            S.dma("sp", lambda e: e.dma_start(out=xst2[st][:], in_=xv[j]), b_xst2[st])
            tsl = slice(j * 128, (j + 1) * 128)
            pk = (j % 2) * 2
            for half in range(2):
                hs = slice(half * 512, (half + 1) * 512)
                bk = 2 * pk + half
                fns = [mm(bank(bk), attnT[:, pr, tsl], wo_sb[:, pr, hs], pr == 0, False) for pr in range(4)]
                fns += [mm(bank(bk), convT[:, cc, tsl], wo_sb[:, 4 + cc, hs], False, cc == 3) for cc in range(4)]
                S.pe(fns, reads=[b_attnT, b_convT, b_wo], writes=[PB[bk]])

        def ln1_A(j):
            ln_A(j, xst2[j % 2][:], [b_xst2[j % 2]], (j % 2) * 2)
        wo_mm(0)
        wo_mm(1)
        ln1_A(0)
        for j in range(NT):
            if j + 1 < NT:
                ln1_A(j + 1)
            ln_B(j)
            if j + 2 < NT:
                wo_mm(j + 2)
            ln_T(j, (j % 2) * 2 + 1)
        dump("h1", resid[:, :, :], b_res)
        if stop_after == "mixer":
            S.wait_bufs("sp", [b_dbg])
            S.emit()
            return nc

        phaseA = [b_attnT, b_convT, b_wo, b_cqn, b_kr, b_ckvn, b_w_in, b_wkr, b_V, b_wuq, b_wqsw, b_wuk, b_wuv] + b_qT + b_pT + b_xst2
        B0 = R2
        xw = {}
        bxw = {}
        for i, k in enumerate("qokv"):
            xw[k] = T([128, 8, D], BF16, B0 + i * 16384)
            bxw[k] = Buf("xw" + k)
            S.inherit(bxw[k], phaseA)
        xqT = [T([128, 2, 512], BF16, B0 + 65536 + i * 2048) for i in range(2)]; b_xq = [Buf("xq%d" % i) for i in range(2)]
        xvv = T([128, 2, D], BF16, B0 + 69632); b_xv = Buf("xv")
        pT2 = [T([128, 512], BF16, B0 + 73728 + i * 1024) for i in range(2)]; b_p2 = [Buf("p2_%d" % i) for i in range(2)]
        oT = T([128, 8, 512], BF16, R8); b_oT = Buf("oT")
        TAIL = CEND + 32
        memT = T([128, 8, 256], BF16, TAIL); b_memT = Buf("memT")
        xkT = T([128, 8, 256], BF16, TAIL + 4096); b_xk = Buf("xkT")
        rscr = T([128, 512], F32, TAIL + 8448); b_rscr = Buf("rscr")
        assert TAIL + 8448 + 2048 <= 229376, TAIL
        for bb in b_xq + [b_xv] + b_p2 + [b_oT]:
            S.inherit(bb, phaseA)
        for k in "kvqo":
            wv_ = xa_w[k].rearrange("(c p) f -> p c f", p=128)
            for c0 in range(0, 8, 4):
                S.dma("pool", lambda e, k=k, c0=c0, wv_=wv_: e.dma_start(out=xw[k][:, c0:c0 + 4, :], in_=wv_[:, c0:c0 + 4, :]), bxw[k])
        load_lnp(1)
        memv = mem.rearrange("(j p) d -> j p d", p=128)

        def mem_tile(mt):
            S.dma("sp", lambda e: e.dma_start(out=ysbs[mt][:], in_=memv[mt]), b_ysbs[mt])
            S.pe([lambda e, c=c: e.transpose(PP[1][:, c * 128:(c + 1) * 128], ysbs[mt][:, c * 128:(c + 1) * 128], ident[:]) for c in range(8)],
                 reads=[b_ysbs[mt], b_ident], writes=[PB[2], PB[3]])
            evac(memT[:, :, mt * 128:(mt + 1) * 128], PP[1][:].rearrange("p (c t) -> p c t", t=128), [PB[2], PB[3]], [b_memT])
        for mt in range(2):
            mem_tile(mt)

        def xk_fc(fc):
            bk = fc % 2
            S.pe([mm(bank(bk)[:, 0:256], xw["k"][:, c, fc * 128:(fc + 1) * 128], memT[:, c, :], c == 0, c == 7) for c in range(8)],
                 reads=[bxw["k"], b_memT], writes=[PB[bk]])
            evac(xkT[:, fc, :], bank(bk)[:, 0:256], [PB[bk]], [b_xk])
        for fc in range(8):
            xk_fc(fc)

        def xv_mh(mt, half):
            bk = 2 + (mt * 2 + half) % 2
            hs = slice(half * 512, (half + 1) * 512)
            S.pe([mm(bank(bk), memT[:, c, mt * 128:(mt + 1) * 128], xw["v"][:, c, hs], c == 0, c == 7) for c in range(8)],
                 reads=[bxw["v"], b_memT], writes=[PB[bk]])
            evac(xvv[:, mt, hs], bank(bk), [PB[bk]], [b_xv])
        for mt in range(2):
            for half in range(2):
                xv_mh(mt, half)

        h2st = [T([128, D], BF16, B0 + 32768 + i * 2048) for i in range(2)]; b_h2st = [Buf("h2st%d" % i) for i in range(2)]
        for bb in b_h2st:
            S.inherit(bb, [bxw["k"], bxw["v"]])
        h2dv = h2d.rearrange("(j p) d -> j p d", p=128)
        b_h2d = Buf("h2d")

        def xa_head(tb, hh):
            cols = slice(tb * 512, (tb + 1) * 512)
            xq = xqT[hh % 2]
            bq = b_xq[hh % 2]
            for i in range(2):
                fc = 2 * hh + i
                S.pe([mm(bank(i), xw["q"][:, c, fc * 128:(fc + 1) * 128], actT[:, c, cols], c == 0, c == 7) for c in range(8)],
                     reads=[bxw["q"], b_actT[tb]], writes=[PB[i]])
                evac(xq[:, i, :], bank(i), [PB[i]], [bq])
            for mt in range(2):
                S.pe([mm(bank(2 + mt), xkT[:, 2 * hh + i, mt * 128:(mt + 1) * 128], xq[:, i, :], i == 0, i == 1) for i in range(2)],
                     reads=[b_xk, bq], writes=[PB[2 + mt]])
                S.op("act", lambda e, mt=mt: e.activation(out=pT2[mt][:], in_=bank(2 + mt), func=AF.Exp, scale=1.0 / 16.0),
                     reads=[PB[2 + mt]], writes=[b_p2[mt]])
            S.pe([mm(bank(4), onesb[:], pT2[mt][:], mt == 0, mt == 1) for mt in range(2)], reads=[b_onesb] + b_p2, writes=[PB[4]])
            recip_act(rscr[:], bank(4), [PB[4]], [b_rscr])
            for dc in range(2):
                S.pe([mm(bank(5 + dc), xvv[:, mt, hh * 256 + dc * 128:hh * 256 + (dc + 1) * 128], pT2[mt][:], mt == 0, mt == 1) for mt in range(2)],
                     reads=[b_xv] + b_p2, writes=[PB[5 + dc]])
                S.op("dve", lambda e, dc=dc: e.tensor_tensor(out=oT[:, hh * 2 + dc, :], in0=bank(5 + dc), in1=rscr[:], op=ALU.mult),
                     reads=[PB[5 + dc], b_rscr], writes=[b_oT])

        def cast_h2(j):
            S.op("act", lambda e: e.activation(out=h2st[j % 2][:], in_=resid[:, j, :], func=AF.Copy), reads=[b_res[j]], writes=[b_h2st[j % 2]])
            S.dma("sp", lambda e: e.dma_start(out=h2dv[j], in_=h2st[j % 2][:]), b_h2d, reads=[b_h2st[j % 2]], writes=[b_h2d])

        def xa_out_mm(tb, jj):
            tsl = slice(jj * 128, (jj + 1) * 128)
            pk = 3 - (jj % 2)
            for half in range(2):
                hs = slice(half * 512, (half + 1) * 512)
                S.pe([mm(bank(2 * pk + half), oT[:, fc, tsl], xw["o"][:, fc, hs], fc == 0, fc == 7) for fc in range(8)],
                     reads=[b_oT, bxw["o"]], writes=[PB[2 * pk + half]])

        def ln2_A(tb, jj):
            j = tb * 4 + jj
            ln_A(j, resid[:, j, :], [b_res[j]], 3 - (jj % 2))

        def ln2_B(tb, jj):
            ln_B(tb * 4 + jj, extra=cast_h2)
        for hh in range(4):
            xa_head(0, hh)
        for tb in range(4):
            xa_out_mm(tb, 0)
            xa_out_mm(tb, 1)
            ln2_A(tb, 0)
            xa_out_mm(tb, 2)
            ln2_A(tb, 1)
            ln2_B(tb, 0)
            xa_out_mm(tb, 3)
            ln2_A(tb, 2)
            ln2_B(tb, 1)
            ln2_A(tb, 3)
            ln2_B(tb, 2)
            ln2_B(tb, 3)
            if tb + 1 < 4:
                for hh in range(4):
                    xa_head(tb + 1, hh)
            for jj in range(4):
                ln_T(tb * 4 + jj, jj % 2)
        dump("h2", resid[:, :, :], b_res)
        if stop_after == "xattn":
            S.wait_bufs("sp", [b_dbg])
            S.emit()
            return nc

        phaseB = [bxw["q"], bxw["o"], b_xv, b_oT, b_memT, b_xk] + b_xq + b_p2
        wr_sb = T([128, 8, NE], BF16, TAIL); b_wr = Buf("wr")
        S.inherit(b_wr, [b_memT])
        with_nc = w_router.rearrange("(c p) e -> p c e", p=128)
        S.dma("pool", lambda e: e.dma_start(out=wr_sb[:], in_=with_nc), b_wr)
        aff = T([16, S_], F32, B0); affw = [T([16, S_], F32, B0 + 8192), T([16, S_], F32, B0 + 16384)]
        b_aff = Buf("aff"); b_affw = [Buf("affw0"), Buf("affw1")]
        gate = T([16, CAP], F32, B0 + 24576); idxu = T([16, CAP], U32, B0 + 25600); idxf = T([16, CAP], F32, B0 + 26624)
        b_gate = Buf("gate"); b_idxu = Buf("idxu"); b_idxf = Buf("idxf")
        idxT = T([128, 2, NE], F32, B0 + 27648); gateT = T([128, 2, NE], F32, B0 + 27776); b_igT = Buf("igT")
        idxs = T([128, 32], F32, B0 + 27904); b_idxs = Buf("idxs")
        for bb in [b_aff, b_gate, b_idxu, b_idxf, b_igT, b_idxs] + b_affw:
            S.inherit(bb, phaseB)
        NRING = 5
        ring_off = [R6, B0 + 45056, B0 + 57344, B0, B0 + 12288]
        ringG = [T([128, 8, 256], BF16, ring_off[i]) for i in range(NRING)]
        ringU = [T([128, 8, 256], BF16, ring_off[i] + 4096) for i in range(NRING)]
        ringD = [T([128, 2, D], BF16, ring_off[i] + 8192) for i in range(NRING)]
        b_ring = [Buf("ring%d" % i) for i in range(NRING)]
        phaseB2 = phaseB + [bxw["k"], bxw["v"]] + b_h2st
        S.inherit(b_ring[0], [b_lnp] + b_ysbs)
        for bb in b_ring[1:3]:
            S.inherit(bb, phaseB2)
        for bb in b_ring[3:5]:
            S.inherit(bb, [b_aff] + b_affw + phaseB)

        def load_unit(n):
            e, fb = divmod(n, 8)
            sl = n % NRING
            fs = slice(fb * 256, (fb + 1) * 256)
            S.dma("pool", lambda e_: e_.dma_start(out=ringG[sl][:], in_=w_gate[e].rearrange("(c p) f -> p c f", p=128)[:, :, fs]), b_ring[sl])
            S.dma("pool", lambda e_: e_.dma_start(out=ringU[sl][:], in_=w_up[e].rearrange("(c p) f -> p c f", p=128)[:, :, fs]), b_ring[sl])
            S.dma("pool", lambda e_: e_.dma_start(out=ringD[sl][:], in_=w_down[e][fb * 256:(fb + 1) * 256, :].rearrange("(c p) d -> p c d", p=128)), b_ring[sl])

        for n0 in range(3):
            load_unit(n0)
        for tb in range(4):
            def router_tb(tb=tb):
                cols = slice(tb * 512, (tb + 1) * 512)
                S.pe([mm(bank(tb)[0:16, :], wr_sb[:, c, :], actT[:, c, cols], c == 0, c == 7) for c in range(8)],
                     reads=[b_wr, b_actT[tb]], writes=[PB[tb]])
                S.op("act", lambda e: e.activation(out=affw[0][:, cols], in_=bank(tb)[0:16, :], func=AF.Exp), reads=[PB[tb]], writes=[b_affw[0]])
                S.pe([mm(bank(4 + tb)[0:16, :], onesf[0:16, 0:16], affw[0][:, cols])], reads=[b_onesf, b_affw[0]], writes=[PB[4 + tb]])
                recip_act(affw[1][:, cols], bank(4 + tb)[0:16, :], [PB[4 + tb]], [b_affw[1]])
                S.op("dve", lambda e: e.tensor_tensor(out=aff[:, cols], in0=affw[0][:, cols], in1=affw[1][:, cols], op=ALU.mult),
                     reads=b_affw, writes=[b_aff])
            router_tb()
        dump("aff", aff[:, :], [b_aff])
        for r in range(CAP // 8):
            def topk_round(r=r):
                src = aff if r == 0 else affw[(r - 1) % 2]
                bsrc = b_aff if r == 0 else b_affw[(r - 1) % 2]
                dst = affw[r % 2]
                sl = slice(r * 8, r * 8 + 8)
                S.op("dve", lambda e: e.max(out=gate[:, sl], in_=src[:]), reads=[bsrc], writes=[b_gate])
                S.op("dve", lambda e: e.max_index(out=idxu[:, sl], in_max=gate[:, sl], in_values=src[:]), reads=[bsrc, b_gate], writes=[b_idxu])
                if r + 1 < CAP // 8:
                    S.op("dve", lambda e: e.match_replace(out=dst[:], in_to_replace=gate[:, sl], in_values=src[:], imm_value=-1.0),
                         reads=[bsrc, b_gate], writes=[b_affw[r % 2]])
            topk_round()
        S.op("dve", lambda e: e.tensor_copy(out=idxf[:], in_=idxu[:]), reads=[b_idxu], writes=[b_idxf])
        dump("gate", gate[:, :], [b_gate])
        dump("idxf", idxf[:, :], [b_idxf])
        S.pe([lambda e, ch=ch: e.transpose(bank(0)[:, ch * 16:(ch + 1) * 16], idxf[0:16, ch * 128:(ch + 1) * 128], ident[0:16, 0:16]) for ch in range(2)]
             + [lambda e, ch=ch: e.transpose(bank(0)[:, 32 + ch * 16:32 + (ch + 1) * 16], gate[0:16, ch * 128:(ch + 1) * 128], ident[0:16, 0:16]) for ch in range(2)],
             reads=[b_idxf, b_gate, b_ident], writes=[PB[0]])
        S.op("dve", lambda e: e.tensor_copy(out=idxT[:].rearrange("p a b -> p (a b)"), in_=bank(0)[:, 0:32]), reads=[PB[0]], writes=[b_igT])
        S.op("dve", lambda e: e.tensor_copy(out=gateT[:].rearrange("p a b -> p (a b)"), in_=bank(0)[:, 32:64]), reads=[PB[0]], writes=[b_igT])

        S.op("dve", lambda e: e.tensor_scalar(out=gateT[:].rearrange("p a b -> p (a b)"), in0=gateT[:].rearrange("p a b -> p (a b)"),
                                              scalar1=1.0 / ALPHA, scalar2=None, op0=ALU.mult), reads=[b_igT], writes=[b_igT])
        idxTi = T([128, 2, NE], I32, B0 + 28032); b_idxTi = Buf("idxTi")
        S.inherit(b_idxTi, phaseB)
        S.op("dve", lambda e: e.tensor_copy(out=idxTi[:].rearrange("p a b -> p (a b)"), in_=idxT[:].rearrange("p a b -> p (a b)")),
             reads=[b_igT], writes=[b_idxTi])

        C1 = R1
        GRP = 2
        ysc = [T([128, GRP * 2, D], BF16, C1 + 16384 + i * 8192) for i in range(2)]; b_ysc = [Buf("ysc%d" % i) for i in range(2)]
        xsT = [T([128, 8, CAP], BF16, C1 + i * 4096) for i in range(2)]; b_xs = [Buf("xs%d" % i) for i in range(2)]
        for bb in b_ysc + b_xs:
            S.inherit(bb, b_actT)
        hidT = [T([128, 2, CAP], BF16, B0 + 73728 + i * 1024) for i in range(2)]; b_hid = [Buf("hid%d" % i) for i in range(2)]
        NXG = 3
        xg = [T([128, 2, D], BF16, B0 + 32768 + i * 4096) for i in range(NXG)]; b_xg = [Buf("xg%d" % i) for i in range(NXG)]
        for bb in b_hid + b_xg:
            S.inherit(bb, phaseB2)
        selsc = [T([128, GRP * 2, 128], BF16, R8 + i * 1024) for i in range(2)]; b_selsc = [Buf("selsc%d" % i) for i in range(2)]
        silu_t = [T([128, 512], F32, R8 + 2048 + i * 2048) for i in range(2)]; b_silu = [Buf("silu%d" % i) for i in range(2)]
        idxs = [T([128, 4], F32, R8 + 6144 + i * 32) for i in range(2)]; b_idxs2 = [Buf("idxs%d" % i) for i in range(2)]
        for bb in b_selsc + b_silu + b_idxs2:
            S.inherit(bb, [b_oT])
        def gather_issue(e):
            for cc2 in range(2):
                S.dma("pool", lambda e_, cc2=cc2: e_.indirect_dma_start(out=xg[e % NXG][:, cc2, :], out_offset=None, in_=h2d[:, :],
                                                                        in_offset=bass.IndirectOffsetOnAxis(ap=idxTi[:, cc2, e:e + 1], axis=0),
                                                                        bounds_check=S_ - 1, oob_is_err=False),
                      b_xg[e % NXG], reads=[b_h2d, b_idxTi])

        def gather_expert(e):
            xs = xsT[e % 2]
            ppb = PP[0][:].bitcast(BF16)
            S.pe([lambda e_, dc=dc, c=c: e_.transpose(ppb[:, dc * 256 + c * 128:dc * 256 + (c + 1) * 128], xg[e % NXG][:, c, dc * 128:(dc + 1) * 128], identb[:])
                  for dc in range(8) for c in range(2)], reads=[b_xg[e % NXG], b_identb], writes=[PB[0], PB[1]])
            evac(xs[:].rearrange("p a b -> p (a b)"), ppb, [PB[0], PB[1]], [b_xs[e % 2]])

        def unit_banks(n):
            return (2, 3) if n % 2 == 0 else (0, 1)

        def ffn_gu(n):
            e, fb = divmod(n, 8)
            sl = n % NRING
            xs = xsT[e % 2]
            hd = hidT[n % 2]
            bg, bu = unit_banks(n)
            for (bk, W) in ((bg, ringG[sl]), (bu, ringU[sl])):
                fns = []
                for fl in range(2):
                    fns += [mm(bank(bk)[:, fl * 256:(fl + 1) * 256], W[:, dc, fl * 128:(fl + 1) * 128], xs[:, dc, :], dc == 0, dc == 7) for dc in range(8)]
                S.pe(fns, reads=[b_ring[sl], b_xs[e % 2]], writes=[PB[bk]])
            sv = silu_t[n % 2]
            S.op("act", lambda e_: e_.activation(out=sv[:], in_=bank(bg), func=AF.Silu), reads=[PB[bg]], writes=[b_silu[n % 2]])
            S.op("dve", lambda e_: e_.tensor_tensor(out=hd[:].rearrange("p a b -> p (a b)"), in0=sv[:], in1=bank(bu), op=ALU.mult),
                 reads=[b_silu[n % 2], PB[bu]], writes=[b_hid[n % 2]])

        def ffn_down(n):
            e, fb = divmod(n, 8)
            sl = n % NRING
            hd = hidT[n % 2]
            for cc2 in range(2):
                for half in range(2):
                    bk = 4 + cc2 * 2 + half
                    S.pe([mm(bank(bk), hd[:, fl, cc2 * 128:(cc2 + 1) * 128], ringD[sl][:, fl, half * 512:(half + 1) * 512],
                             fb == 0 and fl == 0, fb == 7 and fl == 1) for fl in range(2)],
                         reads=[b_hid[n % 2], b_ring[sl]], writes=[PB[bk]])

        def finish_expert(e):
            g = e // GRP
            for cc2 in range(2):
                S.op("act", lambda e_, cc2=cc2: e_.activation(out=ysc[g % 2][:, (e % GRP) * 2 + cc2, :], in_=PP[2 + cc2][:], func=AF.Copy,
                                                              scale=gateT[:, cc2, e:e + 1]),
                     reads=[PB[4 + 2 * cc2], PB[5 + 2 * cc2], b_igT], writes=[b_ysc[g % 2]])

        sc_todo = []

        def scatter_group(g):
            def sc_tile(j, pp):
                ix = idxs[j % 2]
                bi = b_idxs2[j % 2]
                sc = selsc[j % 2]
                bsc = b_selsc[j % 2]
                S.op("dve", lambda e_: e_.tensor_scalar(out=ix[:, 0:4].rearrange("p (e c) -> p e c", c=2),
                                                        in0=idxT[:, :, g * GRP:(g + 1) * GRP].rearrange("p c e -> p e c"),
                                                        scalar1=-128.0 * j, scalar2=None, op0=ALU.add), reads=[b_igT], writes=[bi])
                S.op("dve", lambda e_: e_.tensor_tensor(out=sc[:], in0=ix[:, 0:4].unsqueeze(2).broadcast_to([128, 4, 128]),
                                                        in1=iota[:, :].unsqueeze(1).broadcast_to([128, 4, 128]), op=ALU.is_equal),
                     reads=[bi, b_iota], writes=[bsc])
                pbs = [PB[2 * pp], PB[2 * pp + 1]]
                S.pe([mm(PP[pp][:, half * 512:(half + 1) * 512], sc[:, k, :], ysc[g % 2][:, k, half * 512:(half + 1) * 512], k == 0, k == 3)
                      for half in range(2) for k in range(4)], reads=[bsc, b_ysc[g % 2]], writes=pbs)
                S.op("dve", lambda e_: e_.tensor_tensor(out=resid[:, j, :], in0=resid[:, j, :], in1=PP[pp][:], op=ALU.add),
                     reads=[b_res[j]] + pbs, writes=[b_res[j]])
            for j in range(NT):
                sc_todo.append(lambda pp, j=j: sc_tile(j, pp))

        NEXP = NE
        NU = NEXP * 8
        gather_issue(0)
        gather_issue(1)
        for n0 in range(3, NRING):
            load_unit(n0)
        gather_expert(0)
        ffn_gu(0)
        for n in range(NU):
            e, fb = divmod(n, 8)
            if n + 1 < NU:
                if (n + 1) % 8 == 0:
                    gather_expert((n + 1) // 8)
                ffn_gu(n + 1)
            ffn_down(n)
            if n + NRING < NU:
                load_unit(n + NRING)
            if fb == 1 and e + 2 < NEXP:
                gather_issue(e + 2)
            if sc_todo:
                sc_todo.pop(0)(1 if n % 2 == 0 else 0)
            if fb == 7:
                finish_expert(e)
                if e % GRP == GRP - 1:
                    scatter_group(e // GRP)
        k_ = 0
        while sc_todo:
            sc_todo.pop(0)(k_ % 2)
            k_ += 1

        for bb in [b_lnp] + b_ysbs:
            S.inherit(bb, [b_ring[0]])
        load_lnp(2)
        outv = out.rearrange("(j p) d -> j p d", p=128)
        b_out = Buf("out")

        def out_tile(j):
            S.dma("sp", lambda e: e.dma_start(out=outv[j], in_=resid[:, j, :]), b_out, reads=[b_res[j]], writes=[b_out])
        ln_A(0, None, [], None, eps_col=4)
        for j in range(NT):
            if j + 1 < NT:
                ln_A(j + 1, None, [], None, eps_col=4)
            ln_B(j, from_resid=True, final_out=out_tile)
        S.wait_bufs("sp", [b_out, b_dbg])
        S.emit()
    return nc


def make_consts():
    c = {}
    c["c_ident"] = np.eye(128, dtype=np.float32)
    c["c_iota"] = np.tile(np.arange(128, dtype=np.float32)[None, :], (128, 1))
    c["c_tokid"] = (np.arange(NT, dtype=np.float32)[None, :] * 128 + np.arange(128, dtype=np.float32)[:, None]).astype(np.float32)
    es = np.zeros((16, 16, 128), np.float32)
    for e in range(16):
        es[e, e, :] = 1.0
    c["c_esel"] = es.reshape(16, 16 * 128)
    rope = np.zeros((128, 2), np.float32)
    inv_freq = (10000.0 ** (-np.arange(0, 32, 2, dtype=np.float32) / 32.0)).astype(np.float32)
    for p in range(64, 96):
        rope[p, 0] = np.float32(np.float64(inv_freq[(p - 64) % 16]) / (2.0 * np.pi))
        rope[p, 1] = -1.0 if p < 80 else 1.0
    c["c_rope"] = rope
    return c


def make_in_maps(inputs, n_cores=8):
    consts = make_consts()
    maps = []
    f = lambda a: np.ascontiguousarray(np.asarray(a))
    for b in range(n_cores):
        m = dict(consts)
        m["x"] = f(inputs["x"][b])
        m["mem"] = f(inputs["mem"][b])
        m["positions"] = f(np.asarray(inputs["positions"])[b:b + 1]).astype(np.int32)
        for k in ["w_in", "q_norm_g", "w_uq", "kv_norm_g", "w_uk", "w_uv", "conv_w", "conv_b", "conv_ln_g", "conv_ln_b",
                  "w_o", "xa_w_q", "xa_w_k", "xa_w_v", "xa_w_o", "w_router", "w_gate", "w_up", "w_down"]:
            m[k] = f(np.asarray(inputs[k])[0])
        for k in ["ln1_g", "ln1_b", "ln2_g", "ln2_b", "ln3_g", "ln3_b"]:
            m[k] = f(np.asarray(inputs[k])[0:1])
        maps.append(m)
    return maps


def kernel(**inputs):
    nc = build_program()
    maps = make_in_maps(inputs, 8)
    res = run_bass_kernel_spmd(nc, maps, core_ids=list(range(8)))
    return np.stack([r["out"] for r in res.results], axis=0).astype(np.float32)
```

```python
import math
import os
from contextlib import ExitStack

import numpy as np
import concourse.bass as bass
import concourse.mybir as mybir
from concourse.bass_utils import run_bass_kernel_spmd

F32 = mybir.dt.float32
BF16 = mybir.dt.bfloat16
I32 = mybir.dt.int32
U32 = mybir.dt.uint32
ALU = mybir.AluOpType
AF = mybir.ActivationFunctionType

S_ = 2048
D = 1024
NT = 16
EPS = 1e-5
ALPHA = 2.0 ** 0.25
NE = 16
CAP = 256
FF = 2048


class Buf:
    __slots__ = ("name", "w", "r", "sem", "cnt")

    def __init__(self, name):
        self.name = name
        self.w = None
        self.r = {}
        self.sem = None
        self.cnt = 0


class Sched:
    ENG = ["pe", "act", "dve", "pool", "sp"]

    def __init__(self, nc, es):
        self.nc = nc
        self.es = es
        self.streams = {e: [] for e in self.ENG}
        self.cnt = {e: 0 for e in self.ENG}
        self.esem = {e: es.enter_context(nc.semaphore("c_" + e)) for e in self.ENG}
        self.waited = {e: {} for e in self.ENG}
        self.nsem = len(self.ENG)

    def new_sem(self, name):
        self.nsem += 1
        return self.es.enter_context(self.nc.semaphore("d%d_%s" % (self.nsem, name)))

    def _waits(self, eng, reads, writes, skip_sem=None):
        deps = {}

        def add(s, v):
            if deps.get(s, 0) < v:
                deps[s] = v
        for b in reads:
            if b.w is not None:
                add(*b.w)
        for b in writes:
            if b.w is not None:
                add(*b.w)
            for s, v in b.r.items():
                add(s, v)
        own = self.esem[eng]
        wd = self.waited[eng]
        for s, v in deps.items():
            if s == own and eng == "pe":
                continue
            if skip_sem is not None and s == skip_sem:
                continue
            if wd.get(s, 0) < v:
                wd[s] = v
                self.streams[eng].append(("wait", s, v))

    def _commit(self, ev, reads, writes):
        s, v = ev
        for b in reads:
            if b.r.get(s, 0) < v:
                b.r[s] = v
        for b in writes:
            b.w = ev
            b.r = {}

    def op(self, eng, fn, reads=(), writes=()):
        self._waits(eng, reads, writes)
        self.cnt[eng] += 1
        s = self.esem[eng]
        self.streams[eng].append(("op", fn, s, 1))
        self._commit((s, self.cnt[eng]), reads, writes)

    def pe(self, fns, reads=(), writes=()):
        self._waits("pe", reads, writes)
        self.cnt["pe"] += 1
        s = self.esem["pe"]
        for f in fns[:-1]:
            self.streams["pe"].append(("op", f, None, 0))
        self.streams["pe"].append(("op", fns[-1], s, 1))
        self._commit((s, self.cnt["pe"]), reads, writes)

    def dma(self, eng, fn, dst, reads=(), writes=None):
        if writes is None:
            writes = (dst,)
        if dst.sem is None:
            dst.sem = self.new_sem(dst.name)
        self._waits(eng, reads, writes, skip_sem=dst.sem)
        dst.cnt += 16
        self.streams[eng].append(("op", fn, dst.sem, 16))
        self._commit((dst.sem, dst.cnt), reads, writes)

    def wait_bufs(self, eng, bufs):
        self._waits(eng, bufs, bufs)

    def inherit(self, new, olds):
        for o in olds:
            if o.w is not None:
                s, v = o.w
                if new.r.get(s, 0) < v:
                    new.r[s] = v
            for s, v in o.r.items():
                if new.r.get(s, 0) < v:
                    new.r[s] = v

    def emit(self):
        nc = self.nc
        streams = self.streams

        def run(name, eng):
            for item in streams[name]:
                if item[0] == "wait":
                    eng.wait_ge(item[1], item[2])
                else:
                    ins = item[1](eng)
                    if item[2] is not None:
                        ins.then_inc(item[2], item[3])

        with nc.Block() as block:
            @block.tensor
            def _(e):
                run("pe", e)

            @block.scalar
            def _(e):
                run("act", e)

            @block.vector
            def _(e):
                run("dve", e)

            @block.gpsimd
            def _(e):
                run("pool", e)

            @block.sync
            def _(e):
                run("sp", e)


def build_program(stop_after=None, dumps=()):
    nc = bass.Bass("TRN2", target_bir_lowering=False)

    def din(name, shape, dt=F32):
        return nc.dram_tensor(name, list(shape), dt, kind="ExternalInput").ap()

    x = din("x", [S_, D])
    mem = din("mem", [256, D])
    pos = din("positions", [1, S_], I32)
    w_in = din("w_in", [D, 1440])
    q_norm_g = din("q_norm_g", [256])
    w_uq = din("w_uq", [256, 768])
    kv_norm_g = din("kv_norm_g", [128])
    w_uk = din("w_uk", [128, 512])
    w_uv = din("w_uv", [128, 512])
    conv_w = din("conv_w", [31, 512])
    conv_b = din("conv_b", [512])
    conv_ln_g = din("conv_ln_g", [512])
    conv_ln_b = din("conv_ln_b", [512])
    w_o = din("w_o", [D, D])
    ln_g = [din("ln%d_g" % i, [1, D]) for i in (1, 2, 3)]
    ln_b = [din("ln%d_b" % i, [1, D]) for i in (1, 2, 3)]
    xa_w = {k: din("xa_w_" + k, [D, D]) for k in "qkvo"}
    w_router = din("w_router", [D, NE])
    w_gate = din("w_gate", [NE, D, FF])
    w_up = din("w_up", [NE, D, FF])
    w_down = din("w_down", [NE, FF, D])
    c_ident = din("c_ident", [128, 128])
    c_iota = din("c_iota", [128, 128])
    c_tokid = din("c_tokid", [128, NT])
    c_esel = din("c_esel", [16, 16 * 128])
    c_rope = din("c_rope", [128, 2])
    out = nc.dram_tensor("out", [S_, D], F32, kind="ExternalOutput").ap()
    h2d = nc.dram_tensor("h2_scratch", [S_, D], BF16).ap()
    dump_d = {}
    for (nm, shape, dt) in dumps:
        dump_d[nm] = nc.dram_tensor("dbg_" + nm, list(shape), dt, kind="ExternalOutput").ap()

    es = ExitStack()
    with es:
        S = Sched(nc, es)
        cnt = [0]

        def T(shape, dt, off, name=None):
            cnt[0] += 1
            return nc.alloc_sbuf_tensor_at("%s_%d" % (name or "t", cnt[0]), list(shape), dt, offset=off)

        b_dbg = Buf("dbg")
        conv_todo = []
        drain = {"rate": 0.0, "credit": 0.0, "busy": False}
        _raw_op = S.op

        def op_hook(eng, fn, reads=(), writes=()):
            _raw_op(eng, fn, reads=reads, writes=writes)
            if eng == "dve" and drain["rate"] > 0.0 and not drain["busy"] and conv_todo:
                drain["credit"] += drain["rate"]
                drain["busy"] = True
                while drain["credit"] >= 1.0 and conv_todo:
                    conv_todo.pop(0)()
                    drain["credit"] -= 1.0
                drain["busy"] = False
        S.op = op_hook

        def rsqrt_act(dst, src, bias_ap, rd, wr):
            S.op("act", lambda e: e.activation(out=dst, in_=src, func=AF.Ln, bias=bias_ap, scale=1.0), reads=rd + [b_cst], writes=wr)
            S.op("act", lambda e: e.activation(out=dst, in_=dst, func=AF.Exp, scale=-0.5), reads=wr, writes=wr)

        def recip_act(dst, src, rd, wr):
            S.op("act", lambda e: e.activation(out=dst, in_=src, func=AF.Ln), reads=rd, writes=wr)
            S.op("act", lambda e: e.activation(out=dst, in_=dst, func=AF.Exp, scale=-1.0), reads=wr, writes=wr)

        def dump(nm, ap, bufs):
            if nm in dump_d:
                S.dma("sp", lambda e: e.dma_start(out=dump_d[nm], in_=ap), b_dbg, reads=bufs, writes=[b_dbg])

        R0 = 16640
        R1 = R0 + 66048
        R2 = R1 + 32768
        R3 = R2 + 26624
        R4 = R3 + 24576
        R5 = R4 + 8192
        R6 = R5 + 16384
        R7 = R6 + 8192
        R8 = R7 + 8192
        R9 = R8 + 8192
        ident = T([128, 128], F32, R9); b_ident = Buf("ident")
        identb = T([128, 128], BF16, R9 + 512); b_identb = Buf("identb")
        onesb = T([128, 128], BF16, R9 + 768); b_onesb = Buf("onesb")
        onesf = T([128, 128], F32, R9 + 1024); b_onesf = Buf("onesf")
        ropec = T([128, 2], F32, R9 + 1536); b_ropec = Buf("ropec")
        gsm = T([128, 16], F32, R9 + 1600); b_gsm = Buf("gsm")
        cwT = T([128, 4, 32], F32, R9 + 1664); b_cwT = Buf("cwT")
        iota = T([128, 128], F32, R9 + 2176); b_iota = Buf("iota")
        tokid = T([128, NT], F32, R9 + 2688); b_tokid = Buf("tokid")
        stat = T([128, 16], F32, R9 + 2752); b_stat = Buf("stat")
        cst = T([128, 8], F32, R9 + 2816); b_cst = Buf('cst')
        stat2 = T([128, 32], F32, R9 + 2848); b_st = [Buf("st0"), Buf("st1")]
        CEND = R9 + 2976
        assert CEND <= 229376, CEND

        PP = [nc.alloc_psum_tensor("pp%d" % i, [128, 1024], F32) for i in range(4)]
        PB = [Buf("bank%d" % i) for i in range(8)]

        def bank(k):
            return PP[k // 2][:, (k % 2) * 512:(k % 2) * 512 + 512]

        S.dma("sp", lambda e: e.dma_start(out=ident[:], in_=c_ident), b_ident)
        S.dma("sp", lambda e: e.dma_start(out=iota[:], in_=c_iota), b_iota)
        S.dma("sp", lambda e: e.dma_start(out=tokid[:], in_=c_tokid), b_tokid)
        S.dma("sp", lambda e: e.dma_start(out=ropec[:], in_=c_rope), b_ropec)
        S.op("dve", lambda e: e.tensor_copy(out=identb[:], in_=ident[:]), reads=[b_ident], writes=[b_identb])
        S.op("dve", lambda e: e.memset(onesb[:], 1.0), writes=[b_onesb])
        S.op("dve", lambda e: e.memset(onesf[:], 1.0), writes=[b_onesf])
        S.op("dve", lambda e: e.memset(cst[:, 0:1], 256.0 * EPS), writes=[b_cst])
        S.op("dve", lambda e: e.memset(cst[:, 1:2], 128.0 * EPS), writes=[b_cst])
        S.op("dve", lambda e: e.memset(cst[:, 2:3], EPS), writes=[b_cst])
        S.op("dve", lambda e: e.memset(cst[:, 3:4], 0.0), writes=[b_cst])
        S.op("dve", lambda e: e.memset(cst[:, 4:5], EPS / (ALPHA * ALPHA)), writes=[b_cst])
        with nc.allow_non_contiguous_dma(reason="tiny per-channel parameter vectors"):
            S.dma("sp", lambda e: e.dma_start(out=gsm[:, 0:2], in_=q_norm_g.rearrange("(c p) -> p c", p=128), allow_slow_non_contiguous=True), b_gsm)
            S.dma("sp", lambda e: e.dma_start(out=gsm[:, 2:3], in_=kv_norm_g.rearrange("(c p) -> p c", p=128), allow_slow_non_contiguous=True), b_gsm)
            S.dma("sp", lambda e: e.dma_start(out=gsm[:, 4:8], in_=conv_b.rearrange("(c p) -> p c", p=128), allow_slow_non_contiguous=True), b_gsm)
            S.dma("sp", lambda e: e.dma_start(out=gsm[:, 8:12], in_=conv_ln_g.rearrange("(c p) -> p c", p=128), allow_slow_non_contiguous=True), b_gsm)
            S.dma("sp", lambda e: e.dma_start(out=gsm[:, 12:16], in_=conv_ln_b.rearrange("(c p) -> p c", p=128), allow_slow_non_contiguous=True), b_gsm)
        S.op("dve", lambda e: e.tensor_scalar(out=gsm[:, 0:2], in0=gsm[:, 0:2], scalar1=16.0, scalar2=None, op0=ALU.mult),
             reads=[b_gsm], writes=[b_gsm])
        S.op("dve", lambda e: e.tensor_scalar(out=gsm[:, 2:3], in0=gsm[:, 2:3], scalar1=math.sqrt(128.0), scalar2=None, op0=ALU.mult),
             reads=[b_gsm], writes=[b_gsm])

        resid = T([128, NT, D], F32, R0); b_res = [Buf("res%d" % j) for j in range(NT)]
        u = T([128, 4, 2078], F32, R0); b_u = [Buf("u%d" % c) for c in range(4)]
        acc = T([128, 4, 2048], F32, R0 + 33248); b_acc = [Buf("acc%d" % c) for c in range(4)]
        xstage = [T([128, D], F32, R0 + 33248 + i * 4096) for i in range(2)]; b_xst = [Buf("xst%d" % i) for i in range(2)]
        actT = T([128, 8, S_], BF16, R1); b_actT = [Buf("actT%d" % t) for t in range(4)]
        kT = actT; b_kT = Buf("kT")
        w_in_sb = T([128, 8, 1440], BF16, R2); b_w_in = Buf("w_in")
        wkr = T([128, 8, 2, 96], BF16, R2 + 23040); b_wkr = Buf("wkr")
        attnT = T([128, 4, S_], BF16, R2); b_attnT = Buf("attnT")
        qT = [T([128, 512], BF16, R2 + 16384 + i * 1024) for i in range(2)]; b_qT = [Buf("qT%d" % i) for i in range(2)]
        NPT = 4
        pTt = [T([128, 512], BF16, R2 + 18432 + i * 1024) for i in range(NPT)]; b_pT = [Buf("pT%d" % i) for i in range(NPT)]
        Vaug = T([128, NT, 4, 192], BF16, R3); b_V = Buf("V")
        cqnT = T([128, 2, S_], BF16, R4); b_cqn = Buf("cqn")
        convT = T([128, 4, S_], BF16, R5); b_convT = Buf("convT")
        kr = T([128, S_], BF16, R5); b_kr = Buf("kr")
        ckvnT = T([128, S_], BF16, R5 + 4096); b_ckvn = Buf("ckvn")
        COS = T([128, S_], BF16, R6); SIN = T([128, S_], BF16, R6 + 4096); b_cs = Buf("cossin")
        scr = [T([128, 512], F32, R7 + i * 2048) for i in range(4)]; b_scr = [Buf("scr%d" % i) for i in range(4)]
        wuq = T([128, 2, 768], BF16, R8); b_wuq = Buf("wuq")
        wqsw = T([128, 2, 8, 96], BF16, R8 + 3072); b_wqsw = Buf("wqsw")
        wuk = T([128, 512], BF16, R8 + 6144); b_wuk = Buf("wuk")
        wuv = T([128, 512], BF16, R8 + 7168); b_wuv = Buf("wuv")

        w_in_v = w_in.rearrange("(c p) f -> p c f", p=128)
        for c0 in range(0, 8, 4):
            S.dma("pool", lambda e, c0=c0: e.dma_start(out=w_in_sb[:, c0:c0 + 4, :], in_=w_in_v[:, c0:c0 + 4, :]), b_w_in)
        S.op("pool", lambda e: e.memset(wkr[:], 0.0), writes=[b_wkr])
        S.op("pool", lambda e: e.memset(wqsw[:], 0.0), writes=[b_wqsw])
        with nc.allow_non_contiguous_dma(reason="rope column permutation"):
            S.dma("pool", lambda e: e.dma_start(out=wkr[:, :, 0, 64:96], in_=w_in_v[:, :, 384:416], allow_slow_non_contiguous=True), b_wkr)
            S.dma("pool", lambda e: e.dma_start(out=wkr[:, :, 1, 64:80], in_=w_in_v[:, :, 400:416], allow_slow_non_contiguous=True), b_wkr)
            S.dma("pool", lambda e: e.dma_start(out=wkr[:, :, 1, 80:96], in_=w_in_v[:, :, 384:400], allow_slow_non_contiguous=True), b_wkr)
            w_uq_v = w_uq.rearrange("(c p) (h f) -> p c h f", p=128, f=96)
            for c in range(2):
                S.dma("pool", lambda e, c=c: e.dma_start(out=wqsw[:, c, :, 64:80], in_=w_uq_v[:, c, :, 80:96], allow_slow_non_contiguous=True), b_wqsw)
                S.dma("pool", lambda e, c=c: e.dma_start(out=wqsw[:, c, :, 80:96], in_=w_uq_v[:, c, :, 64:80], allow_slow_non_contiguous=True), b_wqsw)
        S.dma("pool", lambda e: e.dma_start(out=wuq[:], in_=w_uq.rearrange("(c p) f -> p c f", p=128)), b_wuq)
        S.dma("pool", lambda e: e.dma_start(out=wuk[:], in_=w_uk), b_wuk)
        S.dma("pool", lambda e: e.dma_start(out=wuv[:], in_=w_uv), b_wuv)

        cw_st = scr[0]
        S.dma("sp", lambda e: e.dma_start(out=cw_st[0:31, 0:512], in_=conv_w), b_scr[0])
        S.pe([lambda e, cc=cc: e.transpose(bank(0)[:, cc * 32:cc * 32 + 31], cw_st[0:31, cc * 128:(cc + 1) * 128], ident[0:31, 0:31])
              for cc in range(4)], reads=[b_scr[0], b_ident], writes=[PB[0]])
        S.op("dve", lambda e: e.tensor_copy(out=cwT[:, :, 0:31], in_=bank(0)[:, 0:128].rearrange("p (c k) -> p c k", k=32)[:, :, 0:31]),
             reads=[PB[0]], writes=[b_cwT])

        posi = T([128, S_], I32, R0)
        posf = T([128, S_], F32, R0 + 8192)
        angb = T([128, S_], F32, R0 + 16384)
        b_pos = Buf("pos")
        S.dma("sp", lambda e: e.dma_start(out=posi[:], in_=pos.broadcast_to([128, S_])), b_pos)
        rp = slice(64, 96)
        S.op("dve", lambda e: e.tensor_copy(out=posf[rp, :], in_=posi[rp, :]), reads=[b_pos], writes=[b_pos])
        kint = T([128, S_], I32, R0 + 24576)
        sinf = T([128, S_], F32, R0 + 24576)

        def trig(phase, dst_fn):
            S.op("dve", lambda e: e.tensor_scalar(out=angb[rp, :], in0=posf[rp, :], scalar1=ropec[rp, 0:1], scalar2=phase,
                                                  op0=ALU.mult, op1=ALU.add), reads=[b_pos, b_ropec], writes=[b_pos])
            S.op("dve", lambda e: e.tensor_copy(out=kint[rp, :], in_=angb[rp, :]), reads=[b_pos], writes=[b_pos])
            S.op("dve", lambda e: e.tensor_copy(out=posi[rp, :].bitcast(F32), in_=kint[rp, :]), reads=[b_pos], writes=[b_pos])
            S.op("dve", lambda e: e.tensor_tensor(out=angb[rp, :], in0=angb[rp, :], in1=posi[rp, :].bitcast(F32), op=ALU.subtract),
                 reads=[b_pos], writes=[b_pos])
            S.op("dve", lambda e: e.scalar_tensor_tensor(out=angb[rp, :], in0=angb[rp, :], scalar=0.5, in1=angb[rp, :],
                                                         op0=ALU.is_gt, op1=ALU.subtract), reads=[b_pos], writes=[b_pos])
            dst_fn()

        def fin_sin():
            S.op("act", lambda e: e.activation(out=sinf[rp, :], in_=angb[rp, :], func=AF.Sin, scale=-2.0 * math.pi), reads=[b_pos], writes=[b_pos])
            S.op("dve", lambda e: e.tensor_scalar(out=SIN[rp, :], in0=sinf[rp, :], scalar1=ropec[rp, 1:2], scalar2=None, op0=ALU.mult),
                 reads=[b_pos, b_ropec], writes=[b_cs])

        def fin_cos():
            S.op("act", lambda e: e.activation(out=COS[rp, :], in_=angb[rp, :], func=AF.Sin, scale=-2.0 * math.pi), reads=[b_pos], writes=[b_cs])
        trig(0.0, fin_sin)
        trig(0.25, fin_cos)
        for c in range(4):
            S.inherit(b_u[c], [b_pos])

        xv = x.rearrange("(j p) d -> j p d", p=128)
        for j in range(NT):
            st = j % 2
            S.dma("sp", lambda e, j=j, st=st: e.dma_start(out=xstage[st][:], in_=xv[j]), b_xst[st])
            pb = [PB[(j % 2) * 2], PB[(j % 2) * 2 + 1]]
            pt = PP[j % 2]
            S.pe([lambda e, c=c, st=st, pt=pt: e.transpose(pt[:, c * 128:(c + 1) * 128], xstage[st][:, c * 128:(c + 1) * 128], ident[:])
                  for c in range(8)], reads=[b_xst[st], b_ident], writes=pb)
            eng = "act" if j % 2 == 0 else "dve"
            if eng == "act":
                S.op("act", lambda e, j=j, pt=pt: e.activation(out=actT[:, :, j * 128:(j + 1) * 128],
                                                             in_=pt[:].rearrange("p (c t) -> p c t", t=128), func=AF.Copy),
                     reads=pb, writes=[b_actT[j // 4]])
            else:
                S.op("dve", lambda e, j=j, pt=pt: e.tensor_copy(out=actT[:, :, j * 128:(j + 1) * 128],
                                                              in_=pt[:].rearrange("p (c t) -> p c t", t=128)),
                     reads=pb, writes=[b_actT[j // 4]])
        dump("xT", actT[:, :, :], b_actT)

        S.op("pool", lambda e: e.memset(u[:, :, 0:15], 0.0), writes=b_u)
        S.op("pool", lambda e: e.memset(u[:, :, 2063:2078], 0.0), writes=b_u)
        for c in range(4):
            S.inherit(b_acc[c], b_xst)

        def inproj_group(bk, lhs_fn, tb, M):
            cols = slice(tb * 512, (tb + 1) * 512)
            S.pe([lambda e, c=c: e.matmul(bank(bk)[0:M, :], lhsT=lhs_fn(c), rhs=actT[:, c, cols], start=(c == 0), stop=(c == 7))
                  for c in range(8)], reads=[b_w_in, b_wkr, b_actT[tb]], writes=[PB[bk]])

        def inproj_tb(tb):
            cols = slice(tb * 512, (tb + 1) * 512)
            for c2 in range(2):
                inproj_group(c2, lambda c, c2=c2: w_in_sb[:, c, c2 * 128:(c2 + 1) * 128], tb, 128)
                S.op("act", lambda e, c2=c2: e.activation(out=scr[c2][:].bitcast(BF16)[:, 0:512], in_=bank(c2), func=AF.Square),
                     reads=[PB[c2]], writes=[b_scr[c2]])
            S.pe([lambda e, c2=c2: e.matmul(bank(2), lhsT=onesb[:], rhs=scr[c2][:].bitcast(BF16)[:, 0:512], start=(c2 == 0), stop=(c2 == 1))
                  for c2 in range(2)], reads=[b_onesb, b_scr[0], b_scr[1]], writes=[PB[2]])
            rsqrt_act(scr[2][:], bank(2), cst[:, 0:1], [PB[2]], [b_scr[2]])
            for c2 in range(2):
                S.op("dve", lambda e, c2=c2: e.scalar_tensor_tensor(out=cqnT[:, c2, cols], in0=bank(c2), scalar=gsm[:, c2:c2 + 1], in1=scr[2][:],
                                                                   op0=ALU.mult, op1=ALU.mult),
                     reads=[PB[c2], b_gsm, b_scr[2]], writes=[b_cqn])
            inproj_group(3, lambda c: w_in_sb[:, c, 256:384], tb, 128)
            S.op("act", lambda e: e.activation(out=scr[3][:].bitcast(BF16)[:, 0:512], in_=bank(3), func=AF.Square),
                 reads=[PB[3]], writes=[b_scr[3]])
            S.pe([lambda e: e.matmul(bank(2), lhsT=onesb[:], rhs=scr[3][:].bitcast(BF16)[:, 0:512], start=True, stop=True)],
                 reads=[b_onesb, b_scr[3]], writes=[PB[2]])
            rsqrt_act(scr[2][:], bank(2), cst[:, 1:2], [PB[2]], [b_scr[2]])
            S.op("dve", lambda e: e.scalar_tensor_tensor(out=ckvnT[:, cols], in0=bank(3), scalar=gsm[:, 2:3], in1=scr[2][:],
                                                         op0=ALU.mult, op1=ALU.mult),
                 reads=[PB[3], b_gsm, b_scr[2]], writes=[b_ckvn])
            inproj_group(4, lambda c: wkr[:, c, 0, :], tb, 96)
            inproj_group(5, lambda c: wkr[:, c, 1, :], tb, 96)
            S.op("dve", lambda e: e.tensor_tensor(out=scr[0][rp, :], in0=bank(4)[rp, :], in1=COS[rp, cols], op=ALU.mult),
                 reads=[PB[4], b_cs], writes=[b_scr[0]])
            S.op("dve", lambda e: e.tensor_tensor(out=scr[1][rp, :], in0=bank(5)[rp, :], in1=SIN[rp, cols], op=ALU.mult),
                 reads=[PB[5], b_cs], writes=[b_scr[1]])
            S.op("dve", lambda e: e.tensor_tensor(out=kr[rp, cols], in0=scr[0][rp, :], in1=scr[1][rp, :], op=ALU.add),
                 reads=[b_scr[0], b_scr[1]], writes=[b_kr])
        def inproj_glu(tb):
            cols = slice(tb * 512, (tb + 1) * 512)
            for cc in range(4):
                ba = 6 - 2 * (cc % 2)
                bg = 7 - 2 * (cc % 2)
                sg = scr[2 + cc % 2]
                bsg = b_scr[2 + cc % 2]
                inproj_group(ba, lambda c, cc=cc: w_in_sb[:, c, 416 + cc * 128:416 + (cc + 1) * 128], tb, 128)
                inproj_group(bg, lambda c, cc=cc: w_in_sb[:, c, 928 + cc * 128:928 + (cc + 1) * 128], tb, 128)
                S.op("act", lambda e, sg=sg, bg=bg: e.activation(out=sg[:], in_=bank(bg), func=AF.Sigmoid), reads=[PB[bg]], writes=[bsg])
                S.op("dve", lambda e, cc=cc, sg=sg, ba=ba: e.tensor_tensor(out=u[:, cc, 15 + tb * 512:15 + (tb + 1) * 512], in0=bank(ba), in1=sg[:], op=ALU.mult),
                     reads=[PB[ba], bsg], writes=[b_u[cc]])
        for tb in range(4):
            inproj_glu(tb)
        def conv_chunk(cc, eng):
            conv_todo.append(lambda: _raw_op(eng, lambda e: e.tensor_scalar(out=acc[:, cc, :], in0=u[:, cc, 0:2048], scalar1=cwT[:, cc, 0:1],
                                                                            scalar2=gsm[:, 4 + cc:5 + cc], op0=ALU.mult, op1=ALU.add),
                                             reads=[b_u[cc], b_cwT, b_gsm], writes=[b_acc[cc]]))
            for k in range(1, 31):
                conv_todo.append(lambda k=k: _raw_op(eng, lambda e: e.scalar_tensor_tensor(out=acc[:, cc, :], in0=u[:, cc, k:k + 2048],
                                                                                           scalar=cwT[:, cc, k:k + 1], in1=acc[:, cc, :],
                                                                                           op0=ALU.mult, op1=ALU.add),
                                                     reads=[b_u[cc], b_acc[cc], b_cwT], writes=[b_acc[cc]]))
        for cc in range(4):
            conv_chunk(cc, "dve")
        drain["rate"] = 0.5
        for tb in range(4):
            inproj_tb(tb)
        dump("cqnT", cqnT[:, :, :], [b_cqn])
        dump("ckvnT", ckvnT[:, :], [b_ckvn])
        dump("kr", kr[64:96, :], [b_kr])
        dump("u", u[:, :, 15:2063], b_u)
        if stop_after == "inproj":
            S.wait_bufs("sp", [b_dbg])
            S.emit()
            return nc


        def mm(o, l, r, st=True, sp=True):
            return lambda e: e.matmul(o, lhsT=l, rhs=r, start=st, stop=sp)

        evc = [0]

        def evac(out_ap, in_ap, reads, writes):
            evc[0] += 1
            if evc[0] % 2 == 0:
                S.op("act", lambda e: e.activation(out=out_ap, in_=in_ap, func=AF.Copy), reads=reads, writes=writes)
            else:
                S.op("dve", lambda e: e.tensor_copy(out=out_ap, in_=in_ap), reads=reads, writes=writes)

        S.inherit(b_kT, b_actT)

        def kup(h, tb):
            cols = slice(tb * 512, (tb + 1) * 512)
            bk = (h * 4 + tb) % 4
            S.pe([mm(bank(bk)[0:64, :], wuk[:, h * 64:(h + 1) * 64], ckvnT[:, cols])], reads=[b_wuk, b_ckvn], writes=[PB[bk]])
            evac(kT[0:64, h, cols], bank(bk)[0:64, :], [PB[bk]], [b_kT])
        for h in range(8):
            for tb in range(4):
                kup(h, tb)

        def krb(tb):
            cols = slice(tb * 512, (tb + 1) * 512)
            S.op("act", lambda e: e.activation(out=kT[64:96, :, cols], in_=kr[64:96, cols].unsqueeze(1).broadcast_to([32, 8, 512]), func=AF.Copy),
                 reads=[b_kr], writes=[b_kT])
        for tb in range(4):
            krb(tb)
        S.op("pool", lambda e: e.memset(Vaug[:, :, :, 64:128], 0.0), writes=[b_V])
        S.op("pool", lambda e: e.memset(Vaug[:, :, :, 64:65], 1.0), writes=[b_V])

        def vup(j):
            bk = 4 + (j % 4)
            S.pe([mm(bank(bk), ckvnT[:, j * 128:(j + 1) * 128], wuv[:])], reads=[b_wuv, b_ckvn], writes=[PB[bk]])
            pv = bank(bk).rearrange("p (q t d) -> p q t d", t=2, d=64)
            S.op("act", lambda e: e.activation(out=Vaug[:, j, :, 0:64], in_=pv[:, :, 0, :], func=AF.Copy), reads=[PB[bk]], writes=[b_V])
            S.op("dve", lambda e: e.tensor_copy(out=Vaug[:, j, :, 128:192], in_=pv[:, :, 1, :]), reads=[PB[bk]], writes=[b_V])
        for j in range(NT):
            vup(j)
        dump("kT", kT[0:96, :, :], [b_kT])
        dump("V", Vaug[:, :, :, :], [b_V])

        S.inherit(b_attnT, [b_w_in, b_wkr])
        for i in range(2):
            S.inherit(b_qT[i], [b_w_in, b_wkr])
        for i in range(NPT):
            S.inherit(b_pT[i], [b_w_in, b_wkr])
        SCALE = 1.0 / math.sqrt(96.0)

        def qup(s):
            qb, h = divmod(s, 8)
            cols = slice(qb * 512, (qb + 1) * 512)
            qt = qT[s % 2]
            bq = b_qT[s % 2]
            S.pe([mm(bank(5)[0:96, :], wuq[:, c, h * 96:(h + 1) * 96], cqnT[:, c, cols], c == 0, c == 1) for c in range(2)],
                 reads=[b_wuq, b_cqn], writes=[PB[5]])
            S.pe([mm(bank(6)[0:96, :], wqsw[:, c, h, :], cqnT[:, c, cols], c == 0, c == 1) for c in range(2)],
                 reads=[b_wqsw, b_cqn], writes=[PB[6]])
            S.op("dve", lambda e: e.tensor_copy(out=qt[0:64, :], in_=bank(5)[0:64, :]), reads=[PB[5]], writes=[bq])
            S.op("dve", lambda e: e.tensor_tensor(out=scr[0][rp, :], in0=bank(5)[rp, :], in1=COS[rp, cols], op=ALU.mult),
                 reads=[PB[5], b_cs], writes=[b_scr[0]])
            S.op("dve", lambda e: e.tensor_tensor(out=scr[1][rp, :], in0=bank(6)[rp, :], in1=SIN[rp, cols], op=ALU.mult),
                 reads=[PB[6], b_cs], writes=[b_scr[1]])
            S.op("dve", lambda e: e.tensor_tensor(out=qt[rp, :], in0=scr[0][rp, :], in1=scr[1][rp, :], op=ALU.add),
                 reads=[b_scr[0], b_scr[1]], writes=[bq])

        pctr = [0]

        def attn_step(s):
            qb, h = divmod(s, 8)
            pair, odd = divmod(h, 2)
            cols = slice(qb * 512, (qb + 1) * 512)
            qt = qT[s % 2]
            bq = b_qT[s % 2]
            ob = 3 + (s % 2)
            M = 128 if odd else 65

            def lhsV(kt):
                return Vaug[:, kt, pair, 64:192] if odd else Vaug[:, kt, pair, 0:65]

            def Smm(kt):
                bk = kt % 3
                S.pe([mm(bank(bk), kT[0:96, h, kt * 128:(kt + 1) * 128], qt[0:96, :])], reads=[b_kT, bq], writes=[PB[bk]])

            def EXPV(kt):
                pi = pctr[0] % NPT
                pctr[0] += 1
                p = pTt[pi]
                S.op("act", lambda e: e.activation(out=p[:], in_=bank(kt % 3), func=AF.Exp, scale=SCALE), reads=[PB[kt % 3]], writes=[b_pT[pi]])
                if kt + 2 < 16:
                    Smm(kt + 2)
                S.pe([mm(bank(ob)[0:M, :], lhsV(kt), p[:], kt == 0, kt == 15)], reads=[b_V, b_pT[pi]], writes=[PB[ob]])
            Smm(0)
            Smm(1)
            for kt in range(16):
                EXPV(kt)
                if kt == 2 and s > 0:
                    attn_norm(s - 1)

        def attn_norm(s):
            qb, h = divmod(s, 8)
            pair, odd = divmod(h, 2)
            cols = slice(qb * 512, (qb + 1) * 512)
            ob = 3 + (s % 2)
            if odd:
                dr, orow = slice(0, 1), slice(64, 128)
                lo = onesf[0:1, 0:128]
                bo = bank(7)
            else:
                dr, orow = slice(64, 65), slice(0, 64)
                lo = onesf[64:65, 0:64]
                bo = bank(7)[0:64, :]
            recip_act(scr[2][dr, :], bank(ob)[dr, :], [PB[ob]], [b_scr[2]])
            S.pe([mm(bo, lo, scr[2][dr, :])], reads=[b_onesf, b_scr[2]], writes=[PB[7]])
            S.op("dve", lambda e: e.tensor_copy(out=scr[3][orow, :], in_=bank(ob)[orow, :]), reads=[PB[ob]], writes=[b_scr[3]])
            S.op("dve", lambda e: e.tensor_tensor(out=attnT[orow, pair, cols], in0=scr[3][orow, :], in1=bank(7)[orow, :], op=ALU.mult),
                 reads=[b_scr[3], PB[7]], writes=[b_attnT])

        NSTEP = 32
        qup(0)
        for s in range(NSTEP):
            if s + 1 < NSTEP:
                qup(s + 1)
            attn_step(s)
        attn_norm(NSTEP - 1)
        drain["rate"] = 0.0
        while conv_todo:
            conv_todo.pop(0)()
        dump("acc", acc[:, :, :], b_acc)
        dump("attnT", attnT[:, :, :], [b_attnT])

        S.inherit(b_convT, [b_kr, b_ckvn])

        def conv_ln(tb):
            cols = slice(tb * 512, (tb + 1) * 512)
            S.pe([mm(bank(0), onesf[:], acc[:, cc, cols], cc == 0, cc == 3) for cc in range(4)], reads=[b_onesf] + b_acc, writes=[PB[0]])
            for cc in range(4):
                S.op("act", lambda e, cc=cc: e.activation(out=scr[3][:], in_=acc[:, cc, cols], func=AF.Square), reads=[b_acc[cc]], writes=[b_scr[3]])
                S.pe([mm(bank(1), onesf[:], scr[3][:], cc == 0, cc == 3)], reads=[b_onesf, b_scr[3]], writes=[PB[1]])
            S.op("dve", lambda e: e.tensor_scalar(out=scr[0][:], in0=bank(0), scalar1=1.0 / 512.0, scalar2=None, op0=ALU.mult),
                 reads=[PB[0]], writes=[b_scr[0]])
            S.op("dve", lambda e: e.tensor_tensor(out=scr[1][:], in0=scr[0][:], in1=scr[0][:], op=ALU.mult), reads=[b_scr[0]], writes=[b_scr[1]])
            S.op("dve", lambda e: e.scalar_tensor_tensor(out=scr[1][:], in0=bank(1), scalar=1.0 / 512.0, in1=scr[1][:], op0=ALU.mult, op1=ALU.subtract),
                 reads=[PB[1], b_scr[1]], writes=[b_scr[1]])
            rsqrt_act(scr[1][:], scr[1][:], cst[:, 2:3], [b_scr[1]], [b_scr[1]])
            for cc in range(4):
                S.op("dve", lambda e, cc=cc: e.tensor_tensor(out=scr[2][:], in0=acc[:, cc, cols], in1=scr[0][:], op=ALU.subtract),
                     reads=[b_acc[cc], b_scr[0]], writes=[b_scr[2]])
                S.op("dve", lambda e: e.tensor_tensor(out=scr[2][:], in0=scr[2][:], in1=scr[1][:], op=ALU.mult), reads=[b_scr[2], b_scr[1]], writes=[b_scr[2]])
                S.op("act", lambda e, cc=cc: e.activation(out=convT[:, cc, cols], in_=scr[2][:], func=AF.Silu, scale=gsm[:, 8 + cc:9 + cc], bias=gsm[:, 12 + cc:13 + cc]),
                     reads=[b_scr[2], b_gsm], writes=[b_convT])
        for tb in range(4):
            conv_ln(tb)
        dump("convT", convT[:, :, :], [b_convT])

        lnp = T([128, 2, D], F32, R6); b_lnp = Buf("lnp")
        S.inherit(b_lnp, [b_cs])
        ysbs = [T([128, D], F32, R7 + i * 4096) for i in range(2)]
        b_ysbs = [Buf("ysb0"), Buf("ysb1")]
        for bb in b_ysbs:
            S.inherit(bb, b_scr)
        ysb = ysbs[0]; b_ysb = b_ysbs[0]

        def load_lnp(i):
            S.dma("sp", lambda e: e.dma_start(out=lnp[:, 0, :], in_=ln_g[i].broadcast_to([128, D])), b_lnp)
            S.dma("sp", lambda e: e.dma_start(out=lnp[:, 1, :], in_=ln_b[i].broadcast_to([128, D])), b_lnp)

        def ln_A(j, xin_ap, xin_bufs, pk, eps_col=2):
            yb = ysbs[j % 2]
            byb = b_ysbs[j % 2]
            so = (j % 2) * 16
            bs = b_st[j % 2]
            st_ = lambda a, n=1: stat2[:, so + a:so + a + n]
            if pk is not None:
                pbs = [PB[2 * pk], PB[2 * pk + 1]]
                S.op("dve", lambda e: e.scalar_tensor_tensor(out=yb[:], in0=xin_ap, scalar=ALPHA, in1=PP[pk][:], op0=ALU.mult, op1=ALU.add),
                     reads=pbs + xin_bufs, writes=[byb])
                ysrc, ysrc_b = yb[:], [byb]
            else:
                ysrc, ysrc_b = resid[:, j, :], [b_res[j]]
            S.op("dve", lambda e: e.bn_stats(out=st_(0, 6), in_=ysrc[:, 0:512]), reads=ysrc_b, writes=[bs])
            S.op("dve", lambda e: e.bn_stats(out=st_(6, 6), in_=ysrc[:, 512:1024]), reads=ysrc_b, writes=[bs])
            S.op("dve", lambda e: e.bn_aggr(out=st_(12, 2), in_=st_(0, 12)), reads=[bs], writes=[bs])
            rsqrt_act(st_(14), st_(13), cst[:, eps_col:eps_col + 1], [bs], [bs])
            S.op("dve", lambda e: e.scalar_tensor_tensor(out=st_(15), in0=st_(12), scalar=-1.0, in1=st_(14), op0=ALU.mult, op1=ALU.mult), reads=[bs], writes=[bs])

        def ln_B(j, from_resid=False, extra=None, final_out=None):
            yb = ysbs[j % 2]
            byb = b_ysbs[j % 2]
            so = (j % 2) * 16
            bs = b_st[j % 2]
            st_ = lambda a, n=1: stat2[:, so + a:so + a + n]
            if from_resid:
                ysrc, ysrc_b = resid[:, j, :], [b_res[j]]
            else:
                ysrc, ysrc_b = yb[:], [byb]
            S.op("act", lambda e: e.activation(out=yb[:], in_=ysrc, func=AF.Identity, scale=st_(14), bias=st_(15)), reads=ysrc_b + [bs], writes=[byb])
            S.op("dve", lambda e: e.tensor_tensor(out=yb[:], in0=yb[:], in1=lnp[:, 0, :], op=ALU.mult), reads=[byb, b_lnp], writes=[byb])
            S.op("pool", lambda e: e.tensor_tensor(out=resid[:, j, :], in0=yb[:], in1=lnp[:, 1, :], op=ALU.add), reads=[byb, b_lnp], writes=[b_res[j]])
            if final_out is not None:
                final_out(j)
            if extra is not None:
                extra(j)

        def ln_T(j, tpk):
            tpb = [PB[2 * tpk], PB[2 * tpk + 1]]
            S.pe([lambda e, c=c: e.transpose(PP[tpk][:, c * 128:(c + 1) * 128], resid[:, j, c * 128:(c + 1) * 128], ident[:]) for c in range(8)],
                 reads=[b_res[j], b_ident], writes=tpb)
            evac(actT[:, :, j * 128:(j + 1) * 128], PP[tpk][:].rearrange("p (c t) -> p c t", t=128), tpb, [b_actT[j // 4]])

        wo_sb = T([128, 8, D], BF16, R3); b_wo = Buf("wo")
        S.inherit(b_wo, [b_V])
        w_o_v = w_o.rearrange("(c p) f -> p c f", p=128)
        for c0 in range(0, 8, 4):
            S.dma("pool", lambda e, c0=c0: e.dma_start(out=wo_sb[:, c0:c0 + 4, :], in_=w_o_v[:, c0:c0 + 4, :]), b_wo)
        load_lnp(0)
        xst2 = [T([128, D], F32, R4 + i * 4096) for i in range(2)]; b_xst2 = [Buf("xs2_%d" % i) for i in range(2)]
        for i in range(2):
            S.inherit(b_xst2[i], [b_cqn])
        for j in range(NT):
            S.inherit(b_res[j], b_u + b_acc + b_xst + [b_pos])
        for t in range(4):
            S.inherit(b_actT[t], [b_kT])

        def wo_mm(j):
            st = j % 2
            S.dma("sp", lambda e: e.dma_start(out=xst2[st][:], in_=xv[j]), b_xst2[st])
            tsl = slice(j * 128, (j + 1) * 128)
            pk = (j % 2) * 2
            for half in range(2):
                hs = slice(half * 512, (half + 1) * 512)
                bk = 2 * pk + half
                fns = [mm(bank(bk), attnT[:, pr, tsl], wo_sb[:, pr, hs], pr == 0, False) for pr in range(4)]
                fns += [mm(bank(bk), convT[:, cc, tsl], wo_sb[:, 4 + cc, hs], False, cc == 3) for cc in range(4)]
                S.pe(fns, reads=[b_attnT, b_convT, b_wo], writes=[PB[bk]])

        def ln1_A(j):
            ln_A(j, xst2[j % 2][:], [b_xst2[j % 2]], (j % 2) * 2)
        wo_mm(0)
        wo_mm(1)
        ln1_A(0)
        for j in range(NT):
            if j + 1 < NT:
                ln1_A(j + 1)
            ln_B(j)
            if j + 2 < NT:
                wo_mm(j + 2)
            ln_T(j, (j % 2) * 2 + 1)
        dump("h1", resid[:, :, :], b_res)
        if stop_after == "mixer":
            S.wait_bufs("sp", [b_dbg])
            S.emit()
            return nc

        phaseA = [b_attnT, b_convT, b_wo, b_cqn, b_kr, b_ckvn, b_w_in, b_wkr, b_V, b_wuq, b_wqsw, b_wuk, b_wuv] + b_qT + b_pT + b_xst2
        B0 = R2
        xw = {}
        bxw = {}
        for i, k in enumerate("qokv"):
            xw[k] = T([128, 8, D], BF16, B0 + i * 16384)
            bxw[k] = Buf("xw" + k)
            S.inherit(bxw[k], phaseA)
        xqT = [T([128, 2, 512], BF16, B0 + 65536 + i * 2048) for i in range(2)]; b_xq = [Buf("xq%d" % i) for i in range(2)]
        xvv = T([128, 2, D], BF16, B0 + 69632); b_xv = Buf("xv")
        pT2 = [T([128, 512], BF16, B0 + 73728 + i * 1024) for i in range(2)]; b_p2 = [Buf("p2_%d" % i) for i in range(2)]
        oT = T([128, 8, 512], BF16, R8); b_oT = Buf("oT")
        TAIL = CEND + 32
        memT = T([128, 8, 256], BF16, TAIL); b_memT = Buf("memT")
        xkT = T([128, 8, 256], BF16, TAIL + 4096); b_xk = Buf("xkT")
        rscr = T([128, 512], F32, TAIL + 8448); b_rscr = Buf("rscr")
        assert TAIL + 8448 + 2048 <= 229376, TAIL
        for bb in b_xq + [b_xv] + b_p2 + [b_oT]:
            S.inherit(bb, phaseA)
        for k in "kvqo":
            wv_ = xa_w[k].rearrange("(c p) f -> p c f", p=128)
            for c0 in range(0, 8, 4):
                S.dma("pool", lambda e, k=k, c0=c0, wv_=wv_: e.dma_start(out=xw[k][:, c0:c0 + 4, :], in_=wv_[:, c0:c0 + 4, :]), bxw[k])
        load_lnp(1)
        memv = mem.rearrange("(j p) d -> j p d", p=128)

        def mem_tile(mt):
            S.dma("sp", lambda e: e.dma_start(out=ysbs[mt][:], in_=memv[mt]), b_ysbs[mt])
            S.pe([lambda e, c=c: e.transpose(PP[1][:, c * 128:(c + 1) * 128], ysbs[mt][:, c * 128:(c + 1) * 128], ident[:]) for c in range(8)],
                 reads=[b_ysbs[mt], b_ident], writes=[PB[2], PB[3]])
            evac(memT[:, :, mt * 128:(mt + 1) * 128], PP[1][:].rearrange("p (c t) -> p c t", t=128), [PB[2], PB[3]], [b_memT])
        for mt in range(2):
            mem_tile(mt)

        def xk_fc(fc):
            bk = fc % 2
            S.pe([mm(bank(bk)[:, 0:256], xw["k"][:, c, fc * 128:(fc + 1) * 128], memT[:, c, :], c == 0, c == 7) for c in range(8)],
                 reads=[bxw["k"], b_memT], writes=[PB[bk]])
            evac(xkT[:, fc, :], bank(bk)[:, 0:256], [PB[bk]], [b_xk])
        for fc in range(8):
            xk_fc(fc)

        def xv_mh(mt, half):
            bk = 2 + (mt * 2 + half) % 2
            hs = slice(half * 512, (half + 1) * 512)
            S.pe([mm(bank(bk), memT[:, c, mt * 128:(mt + 1) * 128], xw["v"][:, c, hs], c == 0, c == 7) for c in range(8)],
                 reads=[bxw["v"], b_memT], writes=[PB[bk]])
            evac(xvv[:, mt, hs], bank(bk), [PB[bk]], [b_xv])
        for mt in range(2):
            for half in range(2):
                xv_mh(mt, half)

        h2st = [T([128, D], BF16, B0 + 32768 + i * 2048) for i in range(2)]; b_h2st = [Buf("h2st%d" % i) for i in range(2)]
        for bb in b_h2st:
            S.inherit(bb, [bxw["k"], bxw["v"]])
        h2dv = h2d.rearrange("(j p) d -> j p d", p=128)
        b_h2d = Buf("h2d")

        def xa_head(tb, hh):
            cols = slice(tb * 512, (tb + 1) * 512)
            xq = xqT[hh % 2]
            bq = b_xq[hh % 2]
            for i in range(2):
                fc = 2 * hh + i
                S.pe([mm(bank(i), xw["q"][:, c, fc * 128:(fc + 1) * 128], actT[:, c, cols], c == 0, c == 7) for c in range(8)],
                     reads=[bxw["q"], b_actT[tb]], writes=[PB[i]])
                evac(xq[:, i, :], bank(i), [PB[i]], [bq])
            for mt in range(2):
                S.pe([mm(bank(2 + mt), xkT[:, 2 * hh + i, mt * 128:(mt + 1) * 128], xq[:, i, :], i == 0, i == 1) for i in range(2)],
                     reads=[b_xk, bq], writes=[PB[2 + mt]])
                S.op("act", lambda e, mt=mt: e.activation(out=pT2[mt][:], in_=bank(2 + mt), func=AF.Exp, scale=1.0 / 16.0),
                     reads=[PB[2 + mt]], writes=[b_p2[mt]])
            S.pe([mm(bank(4), onesb[:], pT2[mt][:], mt == 0, mt == 1) for mt in range(2)], reads=[b_onesb] + b_p2, writes=[PB[4]])
            recip_act(rscr[:], bank(4), [PB[4]], [b_rscr])
            for dc in range(2):
                S.pe([mm(bank(5 + dc), xvv[:, mt, hh * 256 + dc * 128:hh * 256 + (dc + 1) * 128], pT2[mt][:], mt == 0, mt == 1) for mt in range(2)],
                     reads=[b_xv] + b_p2, writes=[PB[5 + dc]])
                S.op("dve", lambda e, dc=dc: e.tensor_tensor(out=oT[:, hh * 2 + dc, :], in0=bank(5 + dc), in1=rscr[:], op=ALU.mult),
                     reads=[PB[5 + dc], b_rscr], writes=[b_oT])

        def cast_h2(j):
            S.op("act", lambda e: e.activation(out=h2st[j % 2][:], in_=resid[:, j, :], func=AF.Copy), reads=[b_res[j]], writes=[b_h2st[j % 2]])
            S.dma("sp", lambda e: e.dma_start(out=h2dv[j], in_=h2st[j % 2][:]), b_h2d, reads=[b_h2st[j % 2]], writes=[b_h2d])

        def xa_out_mm(tb, jj):
            tsl = slice(jj * 128, (jj + 1) * 128)
            pk = 3 - (jj % 2)
            for half in range(2):
                hs = slice(half * 512, (half + 1) * 512)
                S.pe([mm(bank(2 * pk + half), oT[:, fc, tsl], xw["o"][:, fc, hs], fc == 0, fc == 7) for fc in range(8)],
                     reads=[b_oT, bxw["o"]], writes=[PB[2 * pk + half]])

        def ln2_A(tb, jj):
            j = tb * 4 + jj
            ln_A(j, resid[:, j, :], [b_res[j]], 3 - (jj % 2))

        def ln2_B(tb, jj):
            ln_B(tb * 4 + jj, extra=cast_h2)
        for hh in range(4):
            xa_head(0, hh)
        for tb in range(4):
            xa_out_mm(tb, 0)
            xa_out_mm(tb, 1)
            ln2_A(tb, 0)
            xa_out_mm(tb, 2)
            ln2_A(tb, 1)
            ln2_B(tb, 0)
            xa_out_mm(tb, 3)
            ln2_A(tb, 2)
            ln2_B(tb, 1)
            ln2_A(tb, 3)
            ln2_B(tb, 2)
            ln2_B(tb, 3)
            if tb + 1 < 4:
                for hh in range(4):
                    xa_head(tb + 1, hh)
            for jj in range(4):
                ln_T(tb * 4 + jj, jj % 2)
        dump("h2", resid[:, :, :], b_res)
        if stop_after == "xattn":
            S.wait_bufs("sp", [b_dbg])
            S.emit()
            return nc

        phaseB = [bxw["q"], bxw["o"], b_xv, b_oT, b_memT, b_xk] + b_xq + b_p2
        wr_sb = T([128, 8, NE], BF16, TAIL); b_wr = Buf("wr")
        S.inherit(b_wr, [b_memT])
        with_nc = w_router.rearrange("(c p) e -> p c e", p=128)
        S.dma("pool", lambda e: e.dma_start(out=wr_sb[:], in_=with_nc), b_wr)
        aff = T([16, S_], F32, B0); affw = [T([16, S_], F32, B0 + 8192), T([16, S_], F32, B0 + 16384)]
        b_aff = Buf("aff"); b_affw = [Buf("affw0"), Buf("affw1")]
        gate = T([16, CAP], F32, B0 + 24576); idxu = T([16, CAP], U32, B0 + 25600); idxf = T([16, CAP], F32, B0 + 26624)
        b_gate = Buf("gate"); b_idxu = Buf("idxu"); b_idxf = Buf("idxf")
        idxT = T([128, 2, NE], F32, B0 + 27648); gateT = T([128, 2, NE], F32, B0 + 27776); b_igT = Buf("igT")
        idxs = T([128, 32], F32, B0 + 27904); b_idxs = Buf("idxs")
        for bb in [b_aff, b_gate, b_idxu, b_idxf, b_igT, b_idxs] + b_affw:
            S.inherit(bb, phaseB)
        NRING = 5
        ring_off = [R6, B0 + 45056, B0 + 57344, B0, B0 + 12288]
        ringG = [T([128, 8, 256], BF16, ring_off[i]) for i in range(NRING)]
        ringU = [T([128, 8, 256], BF16, ring_off[i] + 4096) for i in range(NRING)]
        ringD = [T([128, 2, D], BF16, ring_off[i] + 8192) for i in range(NRING)]
        b_ring = [Buf("ring%d" % i) for i in range(NRING)]
        phaseB2 = phaseB + [bxw["k"], bxw["v"]] + b_h2st
        S.inherit(b_ring[0], [b_lnp] + b_ysbs)
        for bb in b_ring[1:3]:
            S.inherit(bb, phaseB2)
        for bb in b_ring[3:5]:
            S.inherit(bb, [b_aff] + b_affw + phaseB)

        def load_unit(n):
            e, fb = divmod(n, 8)
            sl = n % NRING
            fs = slice(fb * 256, (fb + 1) * 256)
            S.dma("pool", lambda e_: e_.dma_start(out=ringG[sl][:], in_=w_gate[e].rearrange("(c p) f -> p c f", p=128)[:, :, fs]), b_ring[sl])
            S.dma("pool", lambda e_: e_.dma_start(out=ringU[sl][:], in_=w_up[e].rearrange("(c p) f -> p c f", p=128)[:, :, fs]), b_ring[sl])
            S.dma("pool", lambda e_: e_.dma_start(out=ringD[sl][:], in_=w_down[e][fb * 256:(fb + 1) * 256, :].rearrange("(c p) d -> p c d", p=128)), b_ring[sl])

        for n0 in range(3):
            load_unit(n0)
        for tb in range(4):
            def router_tb(tb=tb):
                cols = slice(tb * 512, (tb + 1) * 512)
                S.pe([mm(bank(tb)[0:16, :], wr_sb[:, c, :], actT[:, c, cols], c == 0, c == 7) for c in range(8)],
                     reads=[b_wr, b_actT[tb]], writes=[PB[tb]])
                S.op("act", lambda e: e.activation(out=affw[0][:, cols], in_=bank(tb)[0:16, :], func=AF.Exp), reads=[PB[tb]], writes=[b_affw[0]])
                S.pe([mm(bank(4 + tb)[0:16, :], onesf[0:16, 0:16], affw[0][:, cols])], reads=[b_onesf, b_affw[0]], writes=[PB[4 + tb]])
                recip_act(affw[1][:, cols], bank(4 + tb)[0:16, :], [PB[4 + tb]], [b_affw[1]])
                S.op("dve", lambda e: e.tensor_tensor(out=aff[:, cols], in0=affw[0][:, cols], in1=affw[1][:, cols], op=ALU.mult),
                     reads=b_affw, writes=[b_aff])
            router_tb()
        dump("aff", aff[:, :], [b_aff])
        b_gates = [Buf("gate_r%d" % i) for i in range(4)]
        b_idxus = [Buf("idxu_r%d" % i) for i in range(4)]
        for bb in b_gates + b_idxus:
            S.inherit(bb, phaseB)
        for r in range(CAP // 8):
            def topk_round(r=r):
                src = aff if r == 0 else affw[(r - 1) % 2]
                bsrc = b_aff if r == 0 else b_affw[(r - 1) % 2]
                dst = affw[r % 2]
                sl = slice(r * 8, r * 8 + 8)
                bg = b_gates[r % 4]
                S.op("dve", lambda e: e.max(out=gate[:, sl], in_=src[:]), reads=[bsrc], writes=[bg])
                if r + 1 < CAP // 8:
                    S.op("dve", lambda e: e.match_replace(out=dst[:], in_to_replace=gate[:, sl], in_values=src[:], imm_value=-1.0),
                         reads=[bsrc, bg], writes=[b_affw[r % 2]])
                S.op("dve", lambda e: e.max_index(out=idxu[:, sl], in_max=gate[:, sl], in_values=src[:]), reads=[bsrc, bg], writes=[b_idxus[r % 4]])
            topk_round()
        S.op("dve", lambda e: e.tensor_copy(out=idxf[:], in_=idxu[:]), reads=b_idxus, writes=[b_idxf])
        dump("gate", gate[:, :], b_gates)
        dump("idxf", idxf[:, :], [b_idxf])
        S.pe([lambda e, ch=ch: e.transpose(bank(0)[:, ch * 16:(ch + 1) * 16], idxf[0:16, ch * 128:(ch + 1) * 128], ident[0:16, 0:16]) for ch in range(2)]
             + [lambda e, ch=ch: e.transpose(bank(0)[:, 32 + ch * 16:32 + (ch + 1) * 16], gate[0:16, ch * 128:(ch + 1) * 128], ident[0:16, 0:16]) for ch in range(2)],
             reads=[b_idxf, b_ident] + b_gates, writes=[PB[0]])
        S.op("dve", lambda e: e.tensor_copy(out=idxT[:].rearrange("p a b -> p (a b)"), in_=bank(0)[:, 0:32]), reads=[PB[0]], writes=[b_igT])
        S.op("dve", lambda e: e.tensor_copy(out=gateT[:].rearrange("p a b -> p (a b)"), in_=bank(0)[:, 32:64]), reads=[PB[0]], writes=[b_igT])

        S.op("dve", lambda e: e.tensor_scalar(out=gateT[:].rearrange("p a b -> p (a b)"), in0=gateT[:].rearrange("p a b -> p (a b)"),
                                              scalar1=1.0 / ALPHA, scalar2=None, op0=ALU.mult), reads=[b_igT], writes=[b_igT])
        idxTi = T([128, 2, NE], I32, B0 + 28032); b_idxTi = Buf("idxTi")
        S.inherit(b_idxTi, phaseB)
        S.op("dve", lambda e: e.tensor_copy(out=idxTi[:].rearrange("p a b -> p (a b)"), in_=idxT[:].rearrange("p a b -> p (a b)")),
             reads=[b_igT], writes=[b_idxTi])

        C1 = R1
        GRP = 2
        ysc = [T([128, GRP * 2, D], BF16, C1 + 16384 + i * 8192) for i in range(2)]; b_ysc = [Buf("ysc%d" % i) for i in range(2)]
        xsT = [T([128, 8, CAP], BF16, C1 + i * 4096) for i in range(2)]; b_xs = [Buf("xs%d" % i) for i in range(2)]
        for bb in b_ysc + b_xs:
            S.inherit(bb, b_actT)
        hidT = [T([128, 2, CAP], BF16, B0 + 73728 + i * 1024) for i in range(2)]; b_hid = [Buf("hid%d" % i) for i in range(2)]
        NXG = 3
        xg = [T([128, 2, D], BF16, B0 + 32768 + i * 4096) for i in range(NXG)]; b_xg = [Buf("xg%d" % i) for i in range(NXG)]
        for bb in b_hid + b_xg:
            S.inherit(bb, phaseB2)
        selsc = [T([128, GRP * 2, 128], BF16, R8 + i * 1024) for i in range(2)]; b_selsc = [Buf("selsc%d" % i) for i in range(2)]
        silu_t = [T([128, 512], F32, R8 + 2048 + i * 2048) for i in range(2)]; b_silu = [Buf("silu%d" % i) for i in range(2)]
        idxs = [T([128, 4], F32, R8 + 6144 + i * 32) for i in range(2)]; b_idxs2 = [Buf("idxs%d" % i) for i in range(2)]
        for bb in b_selsc + b_silu + b_idxs2:
            S.inherit(bb, [b_oT])
        def gather_issue(e):
            for cc2 in range(2):
                S.dma("pool", lambda e_, cc2=cc2: e_.indirect_dma_start(out=xg[e % NXG][:, cc2, :], out_offset=None, in_=h2d[:, :],
                                                                        in_offset=bass.IndirectOffsetOnAxis(ap=idxTi[:, cc2, e:e + 1], axis=0),
                                                                        bounds_check=S_ - 1, oob_is_err=False),
                      b_xg[e % NXG], reads=[b_h2d, b_idxTi])

        def gather_expert(e):
            xs = xsT[e % 2]
            ppb = PP[0][:].bitcast(BF16)
            S.pe([lambda e_, dc=dc, c=c: e_.transpose(ppb[:, dc * 256 + c * 128:dc * 256 + (c + 1) * 128], xg[e % NXG][:, c, dc * 128:(dc + 1) * 128], identb[:])
                  for dc in range(8) for c in range(2)], reads=[b_xg[e % NXG], b_identb], writes=[PB[0], PB[1]])
            evac(xs[:].rearrange("p a b -> p (a b)"), ppb, [PB[0], PB[1]], [b_xs[e % 2]])

        def unit_banks(n):
            return (2, 3) if n % 2 == 0 else (0, 1)

        def ffn_gu(n):
            e, fb = divmod(n, 8)
            sl = n % NRING
            xs = xsT[e % 2]
            hd = hidT[n % 2]
            bg, bu = unit_banks(n)
            for (bk, W) in ((bg, ringG[sl]), (bu, ringU[sl])):
                fns = []
                for fl in range(2):
                    fns += [mm(bank(bk)[:, fl * 256:(fl + 1) * 256], W[:, dc, fl * 128:(fl + 1) * 128], xs[:, dc, :], dc == 0, dc == 7) for dc in range(8)]
                S.pe(fns, reads=[b_ring[sl], b_xs[e % 2]], writes=[PB[bk]])
            sv = silu_t[n % 2]
            S.op("act", lambda e_: e_.activation(out=sv[:], in_=bank(bg), func=AF.Silu), reads=[PB[bg]], writes=[b_silu[n % 2]])
            S.op("dve", lambda e_: e_.tensor_tensor(out=hd[:].rearrange("p a b -> p (a b)"), in0=sv[:], in1=bank(bu), op=ALU.mult),
                 reads=[b_silu[n % 2], PB[bu]], writes=[b_hid[n % 2]])

        def ffn_down(n):
            e, fb = divmod(n, 8)
            sl = n % NRING
            hd = hidT[n % 2]
            for cc2 in range(2):
                for half in range(2):
                    bk = 4 + cc2 * 2 + half
                    S.pe([mm(bank(bk), hd[:, fl, cc2 * 128:(cc2 + 1) * 128], ringD[sl][:, fl, half * 512:(half + 1) * 512],
                             fb == 0 and fl == 0, fb == 7 and fl == 1) for fl in range(2)],
                         reads=[b_hid[n % 2], b_ring[sl]], writes=[PB[bk]])

        def finish_expert(e):
            g = e // GRP
            for cc2 in range(2):
                S.op("act", lambda e_, cc2=cc2: e_.activation(out=ysc[g % 2][:, (e % GRP) * 2 + cc2, :], in_=PP[2 + cc2][:], func=AF.Copy,
                                                              scale=gateT[:, cc2, e:e + 1]),
                     reads=[PB[4 + 2 * cc2], PB[5 + 2 * cc2], b_igT], writes=[b_ysc[g % 2]])

        sc_todo = []

        def scatter_group(g):
            def sc_prep(j):
                ix = idxs[j % 2]
                bi = b_idxs2[j % 2]
                sc = selsc[j % 2]
                bsc = b_selsc[j % 2]
                S.op("dve", lambda e_: e_.tensor_scalar(out=ix[:, 0:4].rearrange("p (e c) -> p e c", c=2),
                                                        in0=idxT[:, :, g * GRP:(g + 1) * GRP].rearrange("p c e -> p e c"),
                                                        scalar1=-128.0 * j, scalar2=None, op0=ALU.add), reads=[b_igT], writes=[bi])
                S.op("dve", lambda e_: e_.tensor_tensor(out=sc[:], in0=ix[:, 0:4].unsqueeze(2).broadcast_to([128, 4, 128]),
                                                        in1=iota[:, :].unsqueeze(1).broadcast_to([128, 4, 128]), op=ALU.is_equal),
                     reads=[bi, b_iota], writes=[bsc])

            def sc_main(j, pp):
                sc = selsc[j % 2]
                bsc = b_selsc[j % 2]
                pbs = [PB[2 * pp], PB[2 * pp + 1]]
                S.pe([mm(PP[pp][:, half * 512:(half + 1) * 512], sc[:, k, :], ysc[g % 2][:, k, half * 512:(half + 1) * 512], k == 0, k == 3)
                      for half in range(2) for k in range(4)], reads=[bsc, b_ysc[g % 2]], writes=pbs)
                S.op("dve", lambda e_: e_.tensor_tensor(out=resid[:, j, :], in0=resid[:, j, :], in1=PP[pp][:], op=ALU.add),
                     reads=[b_res[j]] + pbs, writes=[b_res[j]])
            for j in range(NT):
                sc_todo.append((lambda j=j: sc_prep(j), lambda pp, j=j: sc_main(j, pp)))

        NEXP = NE
        NU = NEXP * 8
        gather_issue(0)
        gather_issue(1)
        for n0 in range(3, NRING):
            load_unit(n0)
        gather_expert(0)
        ffn_gu(0)
        for n in range(NU):
            e, fb = divmod(n, 8)
            if n + 1 < NU:
                if (n + 1) % 8 == 0:
                    gather_expert((n + 1) // 8)
                ffn_gu(n + 1)
            ffn_down(n)
            if n + NRING < NU:
                load_unit(n + NRING)
            if fb == 1 and e + 2 < NEXP:
                gather_issue(e + 2)
            if sc_todo:
                pr_, mn_ = sc_todo.pop(0)
                pr_()
                mn_(1 if n % 2 == 0 else 0)
            if fb == 7:
                finish_expert(e)
                if e % GRP == GRP - 1:
                    scatter_group(e // GRP)
        for bb in [b_lnp] + b_ysbs:
            S.inherit(bb, [b_ring[0]])
        load_lnp(2)
        outv = out.rearrange("(j p) d -> j p d", p=128)
        b_out = Buf("out")

        def out_tile(j):
            S.dma("sp", lambda e: e.dma_start(out=outv[j], in_=resid[:, j, :]), b_out, reads=[b_res[j]], writes=[b_out])
        tail = list(sc_todo)
        del sc_todo[:]
        assert len(tail) == NT, len(tail)
        tail[0][0]()
        for step in range(NT + 2):
            if step + 1 < NT:
                tail[step + 1][0]()
            if step < NT:
                tail[step][1](step % 2)
            if 1 <= step <= NT:
                ln_A(step - 1, None, [], None, eps_col=4)
            if step >= 2:
                ln_B(step - 2, from_resid=True, final_out=out_tile)
        S.wait_bufs("sp", [b_out, b_dbg])
        S.emit()
    return nc


def make_consts():
    c = {}
    c["c_ident"] = np.eye(128, dtype=np.float32)
    c["c_iota"] = np.tile(np.arange(128, dtype=np.float32)[None, :], (128, 1))
    c["c_tokid"] = (np.arange(NT, dtype=np.float32)[None, :] * 128 + np.arange(128, dtype=np.float32)[:, None]).astype(np.float32)
    es = np.zeros((16, 16, 128), np.float32)
    for e in range(16):
        es[e, e, :] = 1.0
    c["c_esel"] = es.reshape(16, 16 * 128)
    rope = np.zeros((128, 2), np.float32)
    inv_freq = (10000.0 ** (-np.arange(0, 32, 2, dtype=np.float32) / 32.0)).astype(np.float32)
    for p in range(64, 96):
        rope[p, 0] = np.float32(np.float64(inv_freq[(p - 64) % 16]) / (2.0 * np.pi))
        rope[p, 1] = -1.0 if p < 80 else 1.0
    c["c_rope"] = rope
    return c


def make_in_maps(inputs, n_cores=8):
    consts = make_consts()
    maps = []
    f = lambda a: np.ascontiguousarray(np.asarray(a))
    for b in range(n_cores):
        m = dict(consts)
        m["x"] = f(inputs["x"][b])
        m["mem"] = f(inputs["mem"][b])
        m["positions"] = f(np.asarray(inputs["positions"])[b:b + 1]).astype(np.int32)
        for k in ["w_in", "q_norm_g", "w_uq", "kv_norm_g", "w_uk", "w_uv", "conv_w", "conv_b", "conv_ln_g", "conv_ln_b",
                  "w_o", "xa_w_q", "xa_w_k", "xa_w_v", "xa_w_o", "w_router", "w_gate", "w_up", "w_down"]:
            m[k] = f(np.asarray(inputs[k])[0])
        for k in ["ln1_g", "ln1_b", "ln2_g", "ln2_b", "ln3_g", "ln3_b"]:
            m[k] = f(np.asarray(inputs[k])[0:1])
        maps.append(m)
    return maps


def kernel(**inputs):
    nc = build_program()
    maps = make_in_maps(inputs, 8)
    res = run_bass_kernel_spmd(nc, maps, core_ids=list(range(8)))
    return np.stack([r["out"] for r in res.results], axis=0).astype(np.float32)
```

```python
import math
import os
from contextlib import ExitStack

import numpy as np
import concourse.bass as bass
import concourse.mybir as mybir
from concourse.bass_utils import run_bass_kernel_spmd

F32 = mybir.dt.float32
BF16 = mybir.dt.bfloat16
I32 = mybir.dt.int32
U32 = mybir.dt.uint32
ALU = mybir.AluOpType
AF = mybir.ActivationFunctionType

S_ = 2048
D = 1024
NT = 16
EPS = 1e-5
ALPHA = 2.0 ** 0.25
NE = 16
CAP = 256
FF = 2048


class Buf:
    __slots__ = ("name", "w", "r", "sem", "cnt")

    def __init__(self, name):
        self.name = name
        self.w = None
        self.r = {}
        self.sem = None
        self.cnt = 0


class Sched:
    ENG = ["pe", "act", "dve", "pool", "sp"]

    def __init__(self, nc, es):
        self.nc = nc
        self.es = es
        self.streams = {e: [] for e in self.ENG}
        self.cnt = {e: 0 for e in self.ENG}
        self.esem = {e: es.enter_context(nc.semaphore("c_" + e)) for e in self.ENG}
        self.waited = {e: {} for e in self.ENG}
        self.nsem = len(self.ENG)

    def new_sem(self, name):
        self.nsem += 1
        return self.es.enter_context(self.nc.semaphore("d%d_%s" % (self.nsem, name)))

    def _waits(self, eng, reads, writes, skip_sem=None):
        deps = {}

        def add(s, v):
            if deps.get(s, 0) < v:
                deps[s] = v
        for b in reads:
            if b.w is not None:
                add(*b.w)
        for b in writes:
            if b.w is not None:
                add(*b.w)
            for s, v in b.r.items():
                add(s, v)
        own = self.esem[eng]
        wd = self.waited[eng]
        for s, v in deps.items():
            if s == own and eng == "pe":
                continue
            if skip_sem is not None and s == skip_sem:
                continue
            if wd.get(s, 0) < v:
                wd[s] = v
                self.streams[eng].append(("wait", s, v))

    def _commit(self, ev, reads, writes):
        s, v = ev
        for b in reads:
            if b.r.get(s, 0) < v:
                b.r[s] = v
        for b in writes:
            b.w = ev
            b.r = {}

    def op(self, eng, fn, reads=(), writes=()):
        self._waits(eng, reads, writes)
        self.cnt[eng] += 1
        s = self.esem[eng]
        self.streams[eng].append(("op", fn, s, 1))
        self._commit((s, self.cnt[eng]), reads, writes)

    def pe(self, fns, reads=(), writes=()):
        self._waits("pe", reads, writes)
        self.cnt["pe"] += 1
        s = self.esem["pe"]
        for f in fns[:-1]:
            self.streams["pe"].append(("op", f, None, 0))
        self.streams["pe"].append(("op", fns[-1], s, 1))
        self._commit((s, self.cnt["pe"]), reads, writes)

    def dma(self, eng, fn, dst, reads=(), writes=None):
        if writes is None:
            writes = (dst,)
        if dst.sem is None:
            dst.sem = self.new_sem(dst.name)
        self._waits(eng, reads, writes, skip_sem=dst.sem)
        dst.cnt += 16
        self.streams[eng].append(("op", fn, dst.sem, 16))
        self._commit((dst.sem, dst.cnt), reads, writes)

    def wait_bufs(self, eng, bufs):
        self._waits(eng, bufs, bufs)

    def inherit(self, new, olds):
        for o in olds:
            if o.w is not None:
                s, v = o.w
                if new.r.get(s, 0) < v:
                    new.r[s] = v
            for s, v in o.r.items():
                if new.r.get(s, 0) < v:
                    new.r[s] = v

    def emit(self):
        nc = self.nc
        streams = self.streams

        def run(name, eng):
            for item in streams[name]:
                if item[0] == "wait":
                    eng.wait_ge(item[1], item[2])
                else:
                    ins = item[1](eng)
                    if item[2] is not None:
                        ins.then_inc(item[2], item[3])

        with nc.Block() as block:
            @block.tensor
            def _(e):
                run("pe", e)

            @block.scalar
            def _(e):
                run("act", e)

            @block.vector
            def _(e):
                run("dve", e)

            @block.gpsimd
            def _(e):
                run("pool", e)

            @block.sync
            def _(e):
                run("sp", e)


def build_program(stop_after=None, dumps=()):
    nc = bass.Bass("TRN2", target_bir_lowering=False)

    def din(name, shape, dt=F32):
        return nc.dram_tensor(name, list(shape), dt, kind="ExternalInput").ap()

    x = din("x", [S_, D])
    mem = din("mem", [256, D])
    pos = din("positions", [1, S_], I32)
    w_in = din("w_in", [D, 1440])
    q_norm_g = din("q_norm_g", [256])
    w_uq = din("w_uq", [256, 768])
    kv_norm_g = din("kv_norm_g", [128])
    w_uk = din("w_uk", [128, 512])
    w_uv = din("w_uv", [128, 512])
    conv_w = din("conv_w", [31, 512])
    conv_b = din("conv_b", [512])
    conv_ln_g = din("conv_ln_g", [512])
    conv_ln_b = din("conv_ln_b", [512])
    w_o = din("w_o", [D, D])
    ln_g = [din("ln%d_g" % i, [1, D]) for i in (1, 2, 3)]
    ln_b = [din("ln%d_b" % i, [1, D]) for i in (1, 2, 3)]
    xa_w = {k: din("xa_w_" + k, [D, D]) for k in "qkvo"}
    w_router = din("w_router", [D, NE])
    w_gate = din("w_gate", [NE, D, FF])
    w_up = din("w_up", [NE, D, FF])
    w_down = din("w_down", [NE, FF, D])
    c_ident = din("c_ident", [128, 128])
    c_iota = din("c_iota", [128, 128])
    c_tokid = din("c_tokid", [128, NT])
    c_esel = din("c_esel", [16, 16 * 128])
    c_rope = din("c_rope", [128, 2])
    out = nc.dram_tensor("out", [S_, D], F32, kind="ExternalOutput").ap()
    h2d = nc.dram_tensor("h2_scratch", [S_, D], BF16).ap()
    dump_d = {}
    for (nm, shape, dt) in dumps:
        dump_d[nm] = nc.dram_tensor("dbg_" + nm, list(shape), dt, kind="ExternalOutput").ap()

    es = ExitStack()
    with es:
        S = Sched(nc, es)
        cnt = [0]

        def T(shape, dt, off, name=None):
            cnt[0] += 1
            return nc.alloc_sbuf_tensor_at("%s_%d" % (name or "t", cnt[0]), list(shape), dt, offset=off)

        b_dbg = Buf("dbg")
        conv_todo = []
        drain = {"rate": 0.0, "credit": 0.0, "busy": False}
        _raw_op = S.op

        def op_hook(eng, fn, reads=(), writes=()):
            _raw_op(eng, fn, reads=reads, writes=writes)
            if eng == "dve" and drain["rate"] > 0.0 and not drain["busy"] and conv_todo:
                drain["credit"] += drain["rate"]
                drain["busy"] = True
                while drain["credit"] >= 1.0 and conv_todo:
                    conv_todo.pop(0)()
                    drain["credit"] -= 1.0
                drain["busy"] = False
        S.op = op_hook

        def rsqrt_act(dst, src, bias_ap, rd, wr):
            S.op("act", lambda e: e.activation(out=dst, in_=src, func=AF.Ln, bias=bias_ap, scale=1.0), reads=rd + [b_cst], writes=wr)
            S.op("act", lambda e: e.activation(out=dst, in_=dst, func=AF.Exp, scale=-0.5), reads=wr, writes=wr)

        def recip_act(dst, src, rd, wr):
            S.op("act", lambda e: e.activation(out=dst, in_=src, func=AF.Ln), reads=rd, writes=wr)
            S.op("act", lambda e: e.activation(out=dst, in_=dst, func=AF.Exp, scale=-1.0), reads=wr, writes=wr)

        def dump(nm, ap, bufs):
            if nm in dump_d:
                S.dma("sp", lambda e: e.dma_start(out=dump_d[nm], in_=ap), b_dbg, reads=bufs, writes=[b_dbg])

        R0 = 16640
        R1 = R0 + 66048
        R2 = R1 + 32768
        R3 = R2 + 26624
        R4 = R3 + 24576
        R5 = R4 + 8192
        R6 = R5 + 16384
        R7 = R6 + 8192
        R8 = R7 + 8192
        R9 = R8 + 8192
        ident = T([128, 128], F32, R9); b_ident = Buf("ident")
        identb = T([128, 128], BF16, R9 + 512); b_identb = Buf("identb")
        onesb = T([128, 128], BF16, R9 + 768); b_onesb = Buf("onesb")
        onesf = T([128, 128], F32, R9 + 1024); b_onesf = Buf("onesf")
        ropec = T([128, 2], F32, R9 + 1536); b_ropec = Buf("ropec")
        gsm = T([128, 16], F32, R9 + 1600); b_gsm = Buf("gsm")
        cwT = T([128, 4, 32], F32, R9 + 1664); b_cwT = Buf("cwT")
        iota = T([128, 128], F32, R9 + 2176); b_iota = Buf("iota")
        tokid = T([128, NT], F32, R9 + 2688); b_tokid = Buf("tokid")
        stat = T([128, 16], F32, R9 + 2752); b_stat = Buf("stat")
        cst = T([128, 8], F32, R9 + 2816); b_cst = Buf('cst')
        stat2 = T([128, 32], F32, R9 + 2848); b_st = [Buf("st0"), Buf("st1"), Buf("st2")]
        st_tiles = [stat2[:, 0:16], stat2[:, 16:32], stat[:, 0:16]]
        CEND = R9 + 2976
        assert CEND <= 229376, CEND

        PP = [nc.alloc_psum_tensor("pp%d" % i, [128, 1024], F32) for i in range(4)]
        PB = [Buf("bank%d" % i) for i in range(8)]

        def bank(k):
            return PP[k // 2][:, (k % 2) * 512:(k % 2) * 512 + 512]

        S.dma("sp", lambda e: e.dma_start(out=ident[:], in_=c_ident), b_ident)
        S.dma("sp", lambda e: e.dma_start(out=iota[:], in_=c_iota), b_iota)
        S.dma("sp", lambda e: e.dma_start(out=tokid[:], in_=c_tokid), b_tokid)
        S.dma("sp", lambda e: e.dma_start(out=ropec[:], in_=c_rope), b_ropec)
        S.op("dve", lambda e: e.tensor_copy(out=identb[:], in_=ident[:]), reads=[b_ident], writes=[b_identb])
        S.op("dve", lambda e: e.memset(onesb[:], 1.0), writes=[b_onesb])
        S.op("dve", lambda e: e.memset(onesf[:], 1.0), writes=[b_onesf])
        S.op("dve", lambda e: e.memset(cst[:, 0:1], 256.0 * EPS), writes=[b_cst])
        S.op("dve", lambda e: e.memset(cst[:, 1:2], 128.0 * EPS), writes=[b_cst])
        S.op("dve", lambda e: e.memset(cst[:, 2:3], EPS), writes=[b_cst])
        S.op("dve", lambda e: e.memset(cst[:, 3:4], 0.0), writes=[b_cst])
        S.op("dve", lambda e: e.memset(cst[:, 4:5], EPS / (ALPHA * ALPHA)), writes=[b_cst])
        with nc.allow_non_contiguous_dma(reason="tiny per-channel parameter vectors"):
            S.dma("sp", lambda e: e.dma_start(out=gsm[:, 0:2], in_=q_norm_g.rearrange("(c p) -> p c", p=128), allow_slow_non_contiguous=True), b_gsm)
            S.dma("sp", lambda e: e.dma_start(out=gsm[:, 2:3], in_=kv_norm_g.rearrange("(c p) -> p c", p=128), allow_slow_non_contiguous=True), b_gsm)
            S.dma("sp", lambda e: e.dma_start(out=gsm[:, 4:8], in_=conv_b.rearrange("(c p) -> p c", p=128), allow_slow_non_contiguous=True), b_gsm)
            S.dma("sp", lambda e: e.dma_start(out=gsm[:, 8:12], in_=conv_ln_g.rearrange("(c p) -> p c", p=128), allow_slow_non_contiguous=True), b_gsm)
            S.dma("sp", lambda e: e.dma_start(out=gsm[:, 12:16], in_=conv_ln_b.rearrange("(c p) -> p c", p=128), allow_slow_non_contiguous=True), b_gsm)
        S.op("dve", lambda e: e.tensor_scalar(out=gsm[:, 0:2], in0=gsm[:, 0:2], scalar1=16.0, scalar2=None, op0=ALU.mult),
             reads=[b_gsm], writes=[b_gsm])
        S.op("dve", lambda e: e.tensor_scalar(out=gsm[:, 2:3], in0=gsm[:, 2:3], scalar1=math.sqrt(128.0), scalar2=None, op0=ALU.mult),
             reads=[b_gsm], writes=[b_gsm])

        resid = T([128, NT, D], F32, R0); b_res = [Buf("res%d" % j) for j in range(NT)]
        u = T([128, 4, 2078], F32, R0); b_u = [Buf("u%d" % c) for c in range(4)]
        acc = T([128, 4, 2048], F32, R0 + 33248); b_acc = [Buf("acc%d" % c) for c in range(4)]
        xstage = [T([128, D], F32, R0 + 33248 + i * 4096) for i in range(2)]; b_xst = [Buf("xst%d" % i) for i in range(2)]
        actT = T([128, 8, S_], BF16, R1); b_actT = [Buf("actT%d" % t) for t in range(4)]
        kT = actT; b_kT = Buf("kT")
        w_in_sb = T([128, 8, 1440], BF16, R2); b_w_in = Buf("w_in")
        wkr = T([128, 8, 2, 96], BF16, R2 + 23040); b_wkr = Buf("wkr")
        attnT = T([128, 4, S_], BF16, R2); b_attnT = Buf("attnT")
        qT = [T([128, 512], BF16, R2 + 16384 + i * 1024) for i in range(2)]; b_qT = [Buf("qT%d" % i) for i in range(2)]
        NPT = 4
        pTt = [T([128, 512], BF16, R2 + 18432 + i * 1024) for i in range(NPT)]; b_pT = [Buf("pT%d" % i) for i in range(NPT)]
        Vaug = T([128, NT, 4, 192], BF16, R3); b_V = Buf("V")
        cqnT = T([128, 2, S_], BF16, R4); b_cqn = Buf("cqn")
        convT = T([128, 4, S_], BF16, R5); b_convT = Buf("convT")
        kr = T([128, S_], BF16, R5); b_kr = Buf("kr")
        ckvnT = T([128, S_], BF16, R5 + 4096); b_ckvn = Buf("ckvn")
        COS = T([128, S_], BF16, R6); SIN = T([128, S_], BF16, R6 + 4096); b_cs = Buf("cossin")
        scr = [T([128, 512], F32, R7 + i * 2048) for i in range(4)]; b_scr = [Buf("scr%d" % i) for i in range(4)]
        wuq = T([128, 2, 768], BF16, R8); b_wuq = Buf("wuq")
        wqsw = T([128, 2, 8, 96], BF16, R8 + 3072); b_wqsw = Buf("wqsw")
        wuk = T([128, 512], BF16, R8 + 6144); b_wuk = Buf("wuk")
        wuv = T([128, 512], BF16, R8 + 7168); b_wuv = Buf("wuv")

        w_in_v = w_in.rearrange("(c p) f -> p c f", p=128)
        for c0 in range(0, 8, 4):
            S.dma("pool", lambda e, c0=c0: e.dma_start(out=w_in_sb[:, c0:c0 + 4, :], in_=w_in_v[:, c0:c0 + 4, :]), b_w_in)
        S.op("pool", lambda e: e.memset(wkr[:], 0.0), writes=[b_wkr])
        S.op("pool", lambda e: e.memset(wqsw[:], 0.0), writes=[b_wqsw])
        with nc.allow_non_contiguous_dma(reason="rope column permutation"):
            S.dma("pool", lambda e: e.dma_start(out=wkr[:, :, 0, 64:96], in_=w_in_v[:, :, 384:416], allow_slow_non_contiguous=True), b_wkr)
            S.dma("pool", lambda e: e.dma_start(out=wkr[:, :, 1, 64:80], in_=w_in_v[:, :, 400:416], allow_slow_non_contiguous=True), b_wkr)
            S.dma("pool", lambda e: e.dma_start(out=wkr[:, :, 1, 80:96], in_=w_in_v[:, :, 384:400], allow_slow_non_contiguous=True), b_wkr)
            w_uq_v = w_uq.rearrange("(c p) (h f) -> p c h f", p=128, f=96)
            for c in range(2):
                S.dma("pool", lambda e, c=c: e.dma_start(out=wqsw[:, c, :, 64:80], in_=w_uq_v[:, c, :, 80:96], allow_slow_non_contiguous=True), b_wqsw)
                S.dma("pool", lambda e, c=c: e.dma_start(out=wqsw[:, c, :, 80:96], in_=w_uq_v[:, c, :, 64:80], allow_slow_non_contiguous=True), b_wqsw)
        S.dma("pool", lambda e: e.dma_start(out=wuq[:], in_=w_uq.rearrange("(c p) f -> p c f", p=128)), b_wuq)
        S.dma("pool", lambda e: e.dma_start(out=wuk[:], in_=w_uk), b_wuk)
        S.dma("pool", lambda e: e.dma_start(out=wuv[:], in_=w_uv), b_wuv)

        cw_st = scr[0]
        S.dma("sp", lambda e: e.dma_start(out=cw_st[0:31, 0:512], in_=conv_w), b_scr[0])
        S.pe([lambda e, cc=cc: e.transpose(bank(0)[:, cc * 32:cc * 32 + 31], cw_st[0:31, cc * 128:(cc + 1) * 128], ident[0:31, 0:31])
              for cc in range(4)], reads=[b_scr[0], b_ident], writes=[PB[0]])
        S.op("dve", lambda e: e.tensor_copy(out=cwT[:, :, 0:31], in_=bank(0)[:, 0:128].rearrange("p (c k) -> p c k", k=32)[:, :, 0:31]),
             reads=[PB[0]], writes=[b_cwT])

        posi = T([128, S_], I32, R0)
        posf = T([128, S_], F32, R0 + 8192)
        angb = T([128, S_], F32, R0 + 16384)
        b_pos = Buf("pos")
        S.dma("sp", lambda e: e.dma_start(out=posi[:], in_=pos.broadcast_to([128, S_])), b_pos)
        rp = slice(64, 96)
        S.op("dve", lambda e: e.tensor_copy(out=posf[rp, :], in_=posi[rp, :]), reads=[b_pos], writes=[b_pos])
        kint = T([128, S_], I32, R0 + 24576)
        sinf = T([128, S_], F32, R0 + 24576)

        def trig(phase, dst_fn):
            S.op("dve", lambda e: e.tensor_scalar(out=angb[rp, :], in0=posf[rp, :], scalar1=ropec[rp, 0:1], scalar2=phase,
                                                  op0=ALU.mult, op1=ALU.add), reads=[b_pos, b_ropec], writes=[b_pos])
            S.op("dve", lambda e: e.tensor_copy(out=kint[rp, :], in_=angb[rp, :]), reads=[b_pos], writes=[b_pos])
            S.op("dve", lambda e: e.tensor_copy(out=posi[rp, :].bitcast(F32), in_=kint[rp, :]), reads=[b_pos], writes=[b_pos])
            S.op("dve", lambda e: e.tensor_tensor(out=angb[rp, :], in0=angb[rp, :], in1=posi[rp, :].bitcast(F32), op=ALU.subtract),
                 reads=[b_pos], writes=[b_pos])
            S.op("dve", lambda e: e.scalar_tensor_tensor(out=angb[rp, :], in0=angb[rp, :], scalar=0.5, in1=angb[rp, :],
                                                         op0=ALU.is_gt, op1=ALU.subtract), reads=[b_pos], writes=[b_pos])
            dst_fn()

        def fin_sin():
            S.op("act", lambda e: e.activation(out=sinf[rp, :], in_=angb[rp, :], func=AF.Sin, scale=-2.0 * math.pi), reads=[b_pos], writes=[b_pos])
            S.op("dve", lambda e: e.tensor_scalar(out=SIN[rp, :], in0=sinf[rp, :], scalar1=ropec[rp, 1:2], scalar2=None, op0=ALU.mult),
                 reads=[b_pos, b_ropec], writes=[b_cs])

        def fin_cos():
            S.op("act", lambda e: e.activation(out=COS[rp, :], in_=angb[rp, :], func=AF.Sin, scale=-2.0 * math.pi), reads=[b_pos], writes=[b_cs])
        trig(0.0, fin_sin)
        trig(0.25, fin_cos)
        for c in range(4):
            S.inherit(b_u[c], [b_pos])

        xv = x.rearrange("(j p) d -> j p d", p=128)
        for j in range(NT):
            st = j % 2
            S.dma("sp", lambda e, j=j, st=st: e.dma_start(out=xstage[st][:], in_=xv[j]), b_xst[st])
            pb = [PB[(j % 2) * 2], PB[(j % 2) * 2 + 1]]
            pt = PP[j % 2]
            S.pe([lambda e, c=c, st=st, pt=pt: e.transpose(pt[:, c * 128:(c + 1) * 128], xstage[st][:, c * 128:(c + 1) * 128], ident[:])
                  for c in range(8)], reads=[b_xst[st], b_ident], writes=pb)
            eng = "act" if j % 2 == 0 else "dve"
            if eng == "act":
                S.op("act", lambda e, j=j, pt=pt: e.activation(out=actT[:, :, j * 128:(j + 1) * 128],
                                                             in_=pt[:].rearrange("p (c t) -> p c t", t=128), func=AF.Copy),
                     reads=pb, writes=[b_actT[j // 4]])
            else:
                S.op("dve", lambda e, j=j, pt=pt: e.tensor_copy(out=actT[:, :, j * 128:(j + 1) * 128],
                                                              in_=pt[:].rearrange("p (c t) -> p c t", t=128)),
                     reads=pb, writes=[b_actT[j // 4]])
        dump("xT", actT[:, :, :], b_actT)

        S.op("pool", lambda e: e.memset(u[:, :, 0:15], 0.0), writes=b_u)
        S.op("pool", lambda e: e.memset(u[:, :, 2063:2078], 0.0), writes=b_u)
        for c in range(4):
            S.inherit(b_acc[c], b_xst)

        def inproj_group(bk, lhs_fn, tb, M):
            cols = slice(tb * 512, (tb + 1) * 512)
            S.pe([lambda e, c=c: e.matmul(bank(bk)[0:M, :], lhsT=lhs_fn(c), rhs=actT[:, c, cols], start=(c == 0), stop=(c == 7))
                  for c in range(8)], reads=[b_w_in, b_wkr, b_actT[tb]], writes=[PB[bk]])

        def inproj_tb(tb):
            cols = slice(tb * 512, (tb + 1) * 512)
            for c2 in range(2):
                inproj_group(c2, lambda c, c2=c2: w_in_sb[:, c, c2 * 128:(c2 + 1) * 128], tb, 128)
                S.op("act", lambda e, c2=c2: e.activation(out=scr[c2][:].bitcast(BF16)[:, 0:512], in_=bank(c2), func=AF.Square),
                     reads=[PB[c2]], writes=[b_scr[c2]])
            S.pe([lambda e, c2=c2: e.matmul(bank(2), lhsT=onesb[:], rhs=scr[c2][:].bitcast(BF16)[:, 0:512], start=(c2 == 0), stop=(c2 == 1))
                  for c2 in range(2)], reads=[b_onesb, b_scr[0], b_scr[1]], writes=[PB[2]])
            rsqrt_act(scr[2][:], bank(2), cst[:, 0:1], [PB[2]], [b_scr[2]])
            for c2 in range(2):
                S.op("dve", lambda e, c2=c2: e.scalar_tensor_tensor(out=cqnT[:, c2, cols], in0=bank(c2), scalar=gsm[:, c2:c2 + 1], in1=scr[2][:],
                                                                   op0=ALU.mult, op1=ALU.mult),
                     reads=[PB[c2], b_gsm, b_scr[2]], writes=[b_cqn])
            inproj_group(3, lambda c: w_in_sb[:, c, 256:384], tb, 128)
            S.op("act", lambda e: e.activation(out=scr[3][:].bitcast(BF16)[:, 0:512], in_=bank(3), func=AF.Square),
                 reads=[PB[3]], writes=[b_scr[3]])
            S.pe([lambda e: e.matmul(bank(2), lhsT=onesb[:], rhs=scr[3][:].bitcast(BF16)[:, 0:512], start=True, stop=True)],
                 reads=[b_onesb, b_scr[3]], writes=[PB[2]])
            rsqrt_act(scr[2][:], bank(2), cst[:, 1:2], [PB[2]], [b_scr[2]])
            S.op("dve", lambda e: e.scalar_tensor_tensor(out=ckvnT[:, cols], in0=bank(3), scalar=gsm[:, 2:3], in1=scr[2][:],
                                                         op0=ALU.mult, op1=ALU.mult),
                 reads=[PB[3], b_gsm, b_scr[2]], writes=[b_ckvn])
            inproj_group(4, lambda c: wkr[:, c, 0, :], tb, 96)
            inproj_group(5, lambda c: wkr[:, c, 1, :], tb, 96)
            S.op("dve", lambda e: e.tensor_tensor(out=scr[0][rp, :], in0=bank(4)[rp, :], in1=COS[rp, cols], op=ALU.mult),
                 reads=[PB[4], b_cs], writes=[b_scr[0]])
            S.op("dve", lambda e: e.tensor_tensor(out=scr[1][rp, :], in0=bank(5)[rp, :], in1=SIN[rp, cols], op=ALU.mult),
                 reads=[PB[5], b_cs], writes=[b_scr[1]])
            S.op("dve", lambda e: e.tensor_tensor(out=kr[rp, cols], in0=scr[0][rp, :], in1=scr[1][rp, :], op=ALU.add),
                 reads=[b_scr[0], b_scr[1]], writes=[b_kr])
        def inproj_glu(tb):
            cols = slice(tb * 512, (tb + 1) * 512)
            for cc in range(4):
                ba = 6 - 2 * (cc % 2)
                bg = 7 - 2 * (cc % 2)
                sg = scr[2 + cc % 2]
                bsg = b_scr[2 + cc % 2]
                inproj_group(ba, lambda c, cc=cc: w_in_sb[:, c, 416 + cc * 128:416 + (cc + 1) * 128], tb, 128)
                inproj_group(bg, lambda c, cc=cc: w_in_sb[:, c, 928 + cc * 128:928 + (cc + 1) * 128], tb, 128)
                S.op("act", lambda e, sg=sg, bg=bg: e.activation(out=sg[:], in_=bank(bg), func=AF.Sigmoid), reads=[PB[bg]], writes=[bsg])
                S.op("dve", lambda e, cc=cc, sg=sg, ba=ba: e.tensor_tensor(out=u[:, cc, 15 + tb * 512:15 + (tb + 1) * 512], in0=bank(ba), in1=sg[:], op=ALU.mult),
                     reads=[PB[ba], bsg], writes=[b_u[cc]])
        for tb in range(4):
            inproj_glu(tb)
        def conv_chunk(cc, eng):
            conv_todo.append(lambda: _raw_op(eng, lambda e: e.tensor_scalar(out=acc[:, cc, :], in0=u[:, cc, 0:2048], scalar1=cwT[:, cc, 0:1],
                                                                            scalar2=gsm[:, 4 + cc:5 + cc], op0=ALU.mult, op1=ALU.add),
                                             reads=[b_u[cc], b_cwT, b_gsm], writes=[b_acc[cc]]))
            for k in range(1, 31):
                conv_todo.append(lambda k=k: _raw_op(eng, lambda e: e.scalar_tensor_tensor(out=acc[:, cc, :], in0=u[:, cc, k:k + 2048],
                                                                                           scalar=cwT[:, cc, k:k + 1], in1=acc[:, cc, :],
                                                                                           op0=ALU.mult, op1=ALU.add),
                                                     reads=[b_u[cc], b_acc[cc], b_cwT], writes=[b_acc[cc]]))
        for cc in range(4):
            conv_chunk(cc, "dve")
        drain["rate"] = 0.5
        for tb in range(4):
            inproj_tb(tb)
        dump("cqnT", cqnT[:, :, :], [b_cqn])
        dump("ckvnT", ckvnT[:, :], [b_ckvn])
        dump("kr", kr[64:96, :], [b_kr])
        dump("u", u[:, :, 15:2063], b_u)
        if stop_after == "inproj":
            S.wait_bufs("sp", [b_dbg])
            S.emit()
            return nc


        def mm(o, l, r, st=True, sp=True):
            return lambda e: e.matmul(o, lhsT=l, rhs=r, start=st, stop=sp)

        evc = [0]

        def evac(out_ap, in_ap, reads, writes, force=None):
            evc[0] += 1
            if force == "act" or (force is None and evc[0] % 2 == 0):
                S.op("act", lambda e: e.activation(out=out_ap, in_=in_ap, func=AF.Copy), reads=reads, writes=writes)
            else:
                S.op("dve", lambda e: e.tensor_copy(out=out_ap, in_=in_ap), reads=reads, writes=writes)

        S.inherit(b_kT, b_actT)

        def kup(h, tb):
            cols = slice(tb * 512, (tb + 1) * 512)
            bk = (h * 4 + tb) % 4
            S.pe([mm(bank(bk)[0:64, :], wuk[:, h * 64:(h + 1) * 64], ckvnT[:, cols])], reads=[b_wuk, b_ckvn], writes=[PB[bk]])
            evac(kT[0:64, h, cols], bank(bk)[0:64, :], [PB[bk]], [b_kT], force="act")
        for h in range(8):
            for tb in range(4):
                kup(h, tb)

        def krb(tb):
            cols = slice(tb * 512, (tb + 1) * 512)
            S.op("act", lambda e: e.activation(out=kT[64:96, :, cols], in_=kr[64:96, cols].unsqueeze(1).broadcast_to([32, 8, 512]), func=AF.Copy),
                 reads=[b_kr], writes=[b_kT])
        for tb in range(4):
            krb(tb)
        S.op("pool", lambda e: e.memset(Vaug[:, :, :, 64:128], 0.0), writes=[b_V])
        S.op("pool", lambda e: e.memset(Vaug[:, :, :, 64:65], 1.0), writes=[b_V])

        def vup(j):
            bk = 4 + (j % 4)
            S.pe([mm(bank(bk), ckvnT[:, j * 128:(j + 1) * 128], wuv[:])], reads=[b_wuv, b_ckvn], writes=[PB[bk]])
            pv = bank(bk).rearrange("p (q t d) -> p q t d", t=2, d=64)
            S.op("act", lambda e: e.activation(out=Vaug[:, j, :, 0:64], in_=pv[:, :, 0, :], func=AF.Copy), reads=[PB[bk]], writes=[b_V])
            S.op("act", lambda e: e.activation(out=Vaug[:, j, :, 128:192], in_=pv[:, :, 1, :], func=AF.Copy), reads=[PB[bk]], writes=[b_V])
        for j in range(NT):
            vup(j)
        dump("kT", kT[0:96, :, :], [b_kT])
        dump("V", Vaug[:, :, :, :], [b_V])

        S.inherit(b_attnT, [b_w_in, b_wkr])
        for i in range(2):
            S.inherit(b_qT[i], [b_w_in, b_wkr])
        for i in range(NPT):
            S.inherit(b_pT[i], [b_w_in, b_wkr])
        SCALE = 1.0 / math.sqrt(96.0)

        def qup(s):
            qb, h = divmod(s, 8)
            cols = slice(qb * 512, (qb + 1) * 512)
            qt = qT[s % 2]
            bq = b_qT[s % 2]
            S.pe([mm(bank(5)[0:96, :], wuq[:, c, h * 96:(h + 1) * 96], cqnT[:, c, cols], c == 0, c == 1) for c in range(2)],
                 reads=[b_wuq, b_cqn], writes=[PB[5]])
            S.pe([mm(bank(6)[0:96, :], wqsw[:, c, h, :], cqnT[:, c, cols], c == 0, c == 1) for c in range(2)],
                 reads=[b_wqsw, b_cqn], writes=[PB[6]])
            S.op("dve", lambda e: e.tensor_copy(out=qt[0:64, :], in_=bank(5)[0:64, :]), reads=[PB[5]], writes=[bq])
            S.op("dve", lambda e: e.tensor_tensor(out=scr[0][rp, :], in0=bank(5)[rp, :], in1=COS[rp, cols], op=ALU.mult),
                 reads=[PB[5], b_cs], writes=[b_scr[0]])
            S.op("dve", lambda e: e.tensor_tensor(out=scr[1][rp, :], in0=bank(6)[rp, :], in1=SIN[rp, cols], op=ALU.mult),
                 reads=[PB[6], b_cs], writes=[b_scr[1]])
            S.op("dve", lambda e: e.tensor_tensor(out=qt[rp, :], in0=scr[0][rp, :], in1=scr[1][rp, :], op=ALU.add),
                 reads=[b_scr[0], b_scr[1]], writes=[bq])

        pctr = [0]

        def attn_step(s):
            qb, h = divmod(s, 8)
            pair, odd = divmod(h, 2)
            cols = slice(qb * 512, (qb + 1) * 512)
            qt = qT[s % 2]
            bq = b_qT[s % 2]
            ob = 3 + (s % 2)
            M = 128 if odd else 65

            def lhsV(kt):
                return Vaug[:, kt, pair, 64:192] if odd else Vaug[:, kt, pair, 0:65]

            def Smm(kt):
                bk = kt % 3
                S.pe([mm(bank(bk), kT[0:96, h, kt * 128:(kt + 1) * 128], qt[0:96, :])], reads=[b_kT, bq], writes=[PB[bk]])

            def EXPV(kt):
                pi = pctr[0] % NPT
                pctr[0] += 1
                p = pTt[pi]
                S.op("act", lambda e: e.activation(out=p[:], in_=bank(kt % 3), func=AF.Exp, scale=SCALE), reads=[PB[kt % 3]], writes=[b_pT[pi]])
                if kt + 2 < 16:
                    Smm(kt + 2)
                S.pe([mm(bank(ob)[0:M, :], lhsV(kt), p[:], kt == 0, kt == 15)], reads=[b_V, b_pT[pi]], writes=[PB[ob]])
            Smm(0)
            Smm(1)
            for kt in range(16):
                EXPV(kt)
                if kt == 2 and s > 0:
                    attn_norm(s - 1)

        def attn_norm(s):
            qb, h = divmod(s, 8)
            pair, odd = divmod(h, 2)
            cols = slice(qb * 512, (qb + 1) * 512)
            ob = 3 + (s % 2)
            if odd:
                dr, orow = slice(0, 1), slice(64, 128)
                lo = onesf[0:1, 0:128]
                bo = bank(7)
            else:
                dr, orow = slice(64, 65), slice(0, 64)
                lo = onesf[64:65, 0:64]
                bo = bank(7)[0:64, :]
            recip_act(scr[2][dr, :], bank(ob)[dr, :], [PB[ob]], [b_scr[2]])
            S.pe([mm(bo, lo, scr[2][dr, :])], reads=[b_onesf, b_scr[2]], writes=[PB[7]])
            S.op("dve", lambda e: e.tensor_copy(out=scr[3][orow, :], in_=bank(ob)[orow, :]), reads=[PB[ob]], writes=[b_scr[3]])
            S.op("dve", lambda e: e.tensor_tensor(out=attnT[orow, pair, cols], in0=scr[3][orow, :], in1=bank(7)[orow, :], op=ALU.mult),
                 reads=[b_scr[3], PB[7]], writes=[b_attnT])

        NSTEP = 32
        qup(0)
        for s in range(NSTEP):
            if s + 1 < NSTEP:
                qup(s + 1)
            attn_step(s)
        attn_norm(NSTEP - 1)
        drain["rate"] = 0.0
        while conv_todo:
            conv_todo.pop(0)()
        dump("acc", acc[:, :, :], b_acc)
        dump("attnT", attnT[:, :, :], [b_attnT])

        S.inherit(b_convT, [b_kr, b_ckvn])

        cl_m = [T([128, 512], F32, R4 + tb * 2048) for tb in range(4)]; b_clm = [Buf("clm%d" % tb) for tb in range(4)]
        cl_r = [T([128, 512], F32, R8 + tb * 2048) for tb in range(4)]; b_clr = [Buf("clr%d" % tb) for tb in range(4)]
        for bb in b_clm:
            S.inherit(bb, [b_cqn])
        for bb in b_clr:
            S.inherit(bb, [b_wuq, b_wqsw, b_wuk, b_wuv])

        def conv_ln_stats(tb):
            cols = slice(tb * 512, (tb + 1) * 512)
            b0, b1 = 2 * tb, 2 * tb + 1
            S.pe([mm(bank(b0), onesf[:], acc[:, cc, cols], cc == 0, cc == 3) for cc in range(4)], reads=[b_onesf] + b_acc, writes=[PB[b0]])
            for cc in range(4):
                sq = scr[2 + cc % 2]
                bsq = b_scr[2 + cc % 2]
                S.op("act", lambda e, cc=cc, sq=sq: e.activation(out=sq[:], in_=acc[:, cc, cols], func=AF.Square), reads=[b_acc[cc]], writes=[bsq])
                S.pe([mm(bank(b1), onesf[:], sq[:], cc == 0, cc == 3)], reads=[b_onesf, bsq], writes=[PB[b1]])

        def conv_ln_fin(tb):
            b0, b1 = 2 * tb, 2 * tb + 1
            m, r = cl_m[tb], cl_r[tb]
            S.op("dve", lambda e: e.tensor_scalar(out=m[:], in0=bank(b0), scalar1=1.0 / 512.0, scalar2=None, op0=ALU.mult),
                 reads=[PB[b0]], writes=[b_clm[tb]])
            S.op("dve", lambda e: e.tensor_tensor(out=r[:], in0=m[:], in1=m[:], op=ALU.mult), reads=[b_clm[tb]], writes=[b_clr[tb]])
            S.op("dve", lambda e: e.scalar_tensor_tensor(out=r[:], in0=bank(b1), scalar=1.0 / 512.0, in1=r[:], op0=ALU.mult, op1=ALU.subtract),
                 reads=[PB[b1], b_clr[tb]], writes=[b_clr[tb]])
            rsqrt_act(r[:], r[:], cst[:, 2:3], [b_clr[tb]], [b_clr[tb]])

        def conv_ln_apply(tb):
            cols = slice(tb * 512, (tb + 1) * 512)
            m, r = cl_m[tb], cl_r[tb]
            for cc in range(4):
                t = scr[cc % 2]
                bt = b_scr[cc % 2]
                S.op("dve", lambda e, cc=cc, t=t: e.tensor_tensor(out=t[:], in0=acc[:, cc, cols], in1=m[:], op=ALU.subtract),
                     reads=[b_acc[cc], b_clm[tb]], writes=[bt])
                S.op("dve", lambda e, t=t: e.tensor_tensor(out=t[:], in0=t[:], in1=r[:], op=ALU.mult), reads=[bt, b_clr[tb]], writes=[bt])
                S.op("act", lambda e, cc=cc, t=t: e.activation(out=convT[:, cc, cols], in_=t[:], func=AF.Silu, scale=gsm[:, 8 + cc:9 + cc], bias=gsm[:, 12 + cc:13 + cc]),
                     reads=[bt, b_gsm], writes=[b_convT])
        for tb in range(4):
            conv_ln_stats(tb)
        for tb in range(4):
            conv_ln_fin(tb)
        for tb in range(4):
            conv_ln_apply(tb)
        dump("convT", convT[:, :, :], [b_convT])

        lnp = T([128, 2, D], F32, R6); b_lnp = Buf("lnp")
        S.inherit(b_lnp, [b_cs])
        ysbs = [T([128, D], F32, R7 + i * 4096) for i in range(2)]
        b_ysbs = [Buf("ysb0"), Buf("ysb1")]
        for bb in b_ysbs:
            S.inherit(bb, b_scr)
        ysb = ysbs[0]; b_ysb = b_ysbs[0]

        def load_lnp(i):
            S.dma("sp", lambda e: e.dma_start(out=lnp[:, 0, :], in_=ln_g[i].broadcast_to([128, D])), b_lnp)
            S.dma("sp", lambda e: e.dma_start(out=lnp[:, 1, :], in_=ln_b[i].broadcast_to([128, D])), b_lnp)

        def ln_A(j, xin_ap, xin_bufs, pk, eps_col=2, nb=2):
            yb = ysbs[j % nb]
            byb = b_ysbs[j % nb]
            bs = b_st[j % nb]
            stt = st_tiles[j % nb]
            st_ = lambda a, n=1: stt[:, a:a + n]
            if pk is not None:
                pbs = [PB[2 * pk], PB[2 * pk + 1]]
                S.op("dve", lambda e: e.scalar_tensor_tensor(out=yb[:], in0=xin_ap, scalar=ALPHA, in1=PP[pk][:], op0=ALU.mult, op1=ALU.add),
                     reads=pbs + xin_bufs, writes=[byb])
                ysrc, ysrc_b = yb[:], [byb]
            else:
                ysrc, ysrc_b = resid[:, j, :], [b_res[j]]
            S.op("dve", lambda e: e.bn_stats(out=st_(0, 6), in_=ysrc[:, 0:512]), reads=ysrc_b, writes=[bs])
            S.op("dve", lambda e: e.bn_stats(out=st_(6, 6), in_=ysrc[:, 512:1024]), reads=ysrc_b, writes=[bs])
            S.op("dve", lambda e: e.bn_aggr(out=st_(12, 2), in_=st_(0, 12)), reads=[bs], writes=[bs])
            rsqrt_act(st_(14), st_(13), cst[:, eps_col:eps_col + 1], [bs], [bs])
            S.op("dve", lambda e: e.scalar_tensor_tensor(out=st_(15), in0=st_(12), scalar=-1.0, in1=st_(14), op0=ALU.mult, op1=ALU.mult), reads=[bs], writes=[bs])

        def ln_B(j, from_resid=False, extra=None, final_out=None, nb=2):
            yb = ysbs[j % nb]
            byb = b_ysbs[j % nb]
            bs = b_st[j % nb]
            stt = st_tiles[j % nb]
            st_ = lambda a, n=1: stt[:, a:a + n]
            if from_resid:
                ysrc, ysrc_b = resid[:, j, :], [b_res[j]]
            else:
                ysrc, ysrc_b = yb[:], [byb]
            S.op("act", lambda e: e.activation(out=yb[:], in_=ysrc, func=AF.Identity, scale=st_(14), bias=st_(15)), reads=ysrc_b + [bs], writes=[byb])
            S.op("dve", lambda e: e.tensor_tensor(out=yb[:], in0=yb[:], in1=lnp[:, 0, :], op=ALU.mult), reads=[byb, b_lnp], writes=[byb])
            S.op("pool", lambda e: e.tensor_tensor(out=resid[:, j, :], in0=yb[:], in1=lnp[:, 1, :], op=ALU.add), reads=[byb, b_lnp], writes=[b_res[j]])
            if final_out is not None:
                final_out(j)
            if extra is not None:
                extra(j)

        def ln_T(j, tpk):
            tpb = [PB[2 * tpk], PB[2 * tpk + 1]]
            S.pe([lambda e, c=c: e.transpose(PP[tpk][:, c * 128:(c + 1) * 128], resid[:, j, c * 128:(c + 1) * 128], ident[:]) for c in range(8)],
                 reads=[b_res[j], b_ident], writes=tpb)
            evac(actT[:, :, j * 128:(j + 1) * 128], PP[tpk][:].rearrange("p (c t) -> p c t", t=128), tpb, [b_actT[j // 4]])

        wo_sb = T([128, 8, D], BF16, R3); b_wo = Buf("wo")
        S.inherit(b_wo, [b_V])
        w_o_v = w_o.rearrange("(c p) f -> p c f", p=128)
        for c0 in range(0, 8, 4):
            S.dma("pool", lambda e, c0=c0: e.dma_start(out=wo_sb[:, c0:c0 + 4, :], in_=w_o_v[:, c0:c0 + 4, :]), b_wo)
        load_lnp(0)
        xst2 = [T([128, D], F32, R4 + i * 4096) for i in range(2)]; b_xst2 = [Buf("xs2_%d" % i) for i in range(2)]
        for i in range(2):
            S.inherit(b_xst2[i], [b_cqn] + b_clm)
        for j in range(NT):
            S.inherit(b_res[j], b_u + b_acc + b_xst + [b_pos])
        for t in range(4):
            S.inherit(b_actT[t], [b_kT])

        def wo_mm(j):
            st = j % 2
            S.dma("sp", lambda e: e.dma_start(out=xst2[st][:], in_=xv[j]), b_xst2[st])
            tsl = slice(j * 128, (j + 1) * 128)
            pk = (j % 2) * 2
            for half in range(2):
                hs = slice(half * 512, (half + 1) * 512)
                bk = 2 * pk + half
                fns = [mm(bank(bk), attnT[:, pr, tsl], wo_sb[:, pr, hs], pr == 0, False) for pr in range(4)]
                fns += [mm(bank(bk), convT[:, cc, tsl], wo_sb[:, 4 + cc, hs], False, cc == 3) for cc in range(4)]
                S.pe(fns, reads=[b_attnT, b_convT, b_wo], writes=[PB[bk]])

        def ln1_A(j):
            ln_A(j, xst2[j % 2][:], [b_xst2[j % 2]], (j % 2) * 2)
        wo_mm(0)
        wo_mm(1)
        ln1_A(0)
        for j in range(NT):
            if j + 1 < NT:
                ln1_A(j + 1)
            ln_B(j)
            if j + 2 < NT:
                wo_mm(j + 2)
            ln_T(j, (j % 2) * 2 + 1)
        dump("h1", resid[:, :, :], b_res)
        if stop_after == "mixer":
            S.wait_bufs("sp", [b_dbg])
            S.emit()
            return nc

        phaseA = [b_attnT, b_convT, b_wo, b_cqn, b_kr, b_ckvn, b_w_in, b_wkr, b_V, b_wuq, b_wqsw, b_wuk, b_wuv] + b_qT + b_pT + b_xst2 + b_clm + b_clr
        B0 = R2
        xw = {}
        bxw = {}
        for i, k in enumerate("qokv"):
            xw[k] = T([128, 8, D], BF16, B0 + i * 16384)
            bxw[k] = Buf("xw" + k)
            S.inherit(bxw[k], phaseA)
        xqT = [T([128, 2, 512], BF16, B0 + 65536 + i * 2048) for i in range(2)]; b_xq = [Buf("xq%d" % i) for i in range(2)]
        xvv = T([128, 2, D], BF16, B0 + 69632); b_xv = Buf("xv")
        pT2 = [T([128, 512], BF16, B0 + 73728 + i * 1024) for i in range(2)]; b_p2 = [Buf("p2_%d" % i) for i in range(2)]
        oT = T([128, 8, 512], BF16, R8); b_oT = Buf("oT")
        TAIL = CEND + 32
        memT = T([128, 8, 256], BF16, TAIL); b_memT = Buf("memT")
        xkT = T([128, 8, 256], BF16, TAIL + 4096); b_xk = Buf("xkT")
        rscr = T([128, 512], F32, TAIL + 8448); b_rscr = Buf("rscr")
        ysbs.append(T([128, D], F32, TAIL)); b_ysbs.append(Buf("ysb2"))
        assert TAIL + 8448 + 2048 <= 229376 - 64, TAIL
        for bb in b_xq + [b_xv] + b_p2 + [b_oT]:
            S.inherit(bb, phaseA)
        for k in "kvqo":
            wv_ = xa_w[k].rearrange("(c p) f -> p c f", p=128)
            for c0 in range(0, 8, 4):
                S.dma("pool", lambda e, k=k, c0=c0, wv_=wv_: e.dma_start(out=xw[k][:, c0:c0 + 4, :], in_=wv_[:, c0:c0 + 4, :]), bxw[k])
        load_lnp(1)
        memv = mem.rearrange("(j p) d -> j p d", p=128)

        def mem_tile(mt):
            S.dma("sp", lambda e: e.dma_start(out=ysbs[mt][:], in_=memv[mt]), b_ysbs[mt])
            S.pe([lambda e, c=c: e.transpose(PP[1][:, c * 128:(c + 1) * 128], ysbs[mt][:, c * 128:(c + 1) * 128], ident[:]) for c in range(8)],
                 reads=[b_ysbs[mt], b_ident], writes=[PB[2], PB[3]])
            evac(memT[:, :, mt * 128:(mt + 1) * 128], PP[1][:].rearrange("p (c t) -> p c t", t=128), [PB[2], PB[3]], [b_memT])
        for mt in range(2):
            mem_tile(mt)

        def xk_fc(fc):
            bk = fc % 2
            S.pe([mm(bank(bk)[:, 0:256], xw["k"][:, c, fc * 128:(fc + 1) * 128], memT[:, c, :], c == 0, c == 7) for c in range(8)],
                 reads=[bxw["k"], b_memT], writes=[PB[bk]])
            evac(xkT[:, fc, :], bank(bk)[:, 0:256], [PB[bk]], [b_xk])
        for fc in range(8):
            xk_fc(fc)

        def xv_mh(mt, half):
            bk = 2 + (mt * 2 + half) % 2
            hs = slice(half * 512, (half + 1) * 512)
            S.pe([mm(bank(bk), memT[:, c, mt * 128:(mt + 1) * 128], xw["v"][:, c, hs], c == 0, c == 7) for c in range(8)],
                 reads=[bxw["v"], b_memT], writes=[PB[bk]])
            evac(xvv[:, mt, hs], bank(bk), [PB[bk]], [b_xv])
        for mt in range(2):
            for half in range(2):
                xv_mh(mt, half)

        h2st = [T([128, D], BF16, B0 + 32768 + i * 2048) for i in range(2)]; b_h2st = [Buf("h2st%d" % i) for i in range(2)]
        for bb in b_h2st:
            S.inherit(bb, [bxw["k"], bxw["v"]])
        h2dv = h2d.rearrange("(j p) d -> j p d", p=128)
        b_h2d = Buf("h2d")

        def xa_head(tb, hh):
            cols = slice(tb * 512, (tb + 1) * 512)
            xq = xqT[hh % 2]
            bq = b_xq[hh % 2]
            for i in range(2):
                fc = 2 * hh + i
                S.pe([mm(bank(i), xw["q"][:, c, fc * 128:(fc + 1) * 128], actT[:, c, cols], c == 0, c == 7) for c in range(8)],
                     reads=[bxw["q"], b_actT[tb]], writes=[PB[i]])
                evac(xq[:, i, :], bank(i), [PB[i]], [bq])
            for mt in range(2):
                S.pe([mm(bank(2 + mt), xkT[:, 2 * hh + i, mt * 128:(mt + 1) * 128], xq[:, i, :], i == 0, i == 1) for i in range(2)],
                     reads=[b_xk, bq], writes=[PB[2 + mt]])
                S.op("act", lambda e, mt=mt: e.activation(out=pT2[mt][:], in_=bank(2 + mt), func=AF.Exp, scale=1.0 / 16.0),
                     reads=[PB[2 + mt]], writes=[b_p2[mt]])
            S.pe([mm(bank(4), onesb[:], pT2[mt][:], mt == 0, mt == 1) for mt in range(2)], reads=[b_onesb] + b_p2, writes=[PB[4]])
            recip_act(rscr[:], bank(4), [PB[4]], [b_rscr])
            for dc in range(2):
                S.pe([mm(bank(5 + dc), xvv[:, mt, hh * 256 + dc * 128:hh * 256 + (dc + 1) * 128], pT2[mt][:], mt == 0, mt == 1) for mt in range(2)],
                     reads=[b_xv] + b_p2, writes=[PB[5 + dc]])
                S.op("dve", lambda e, dc=dc: e.tensor_tensor(out=oT[:, hh * 2 + dc, :], in0=bank(5 + dc), in1=rscr[:], op=ALU.mult),
                     reads=[PB[5 + dc], b_rscr], writes=[b_oT])

        def cast_h2(j):
            S.op("act", lambda e: e.activation(out=h2st[j % 2][:], in_=resid[:, j, :], func=AF.Copy), reads=[b_res[j]], writes=[b_h2st[j % 2]])
            S.dma("sp", lambda e: e.dma_start(out=h2dv[j], in_=h2st[j % 2][:]), b_h2d, reads=[b_h2st[j % 2]], writes=[b_h2d])

        def xa_out_mm(tb, jj):
            tsl = slice(jj * 128, (jj + 1) * 128)
            pk = 3 - (jj % 2)
            for half in range(2):
                hs = slice(half * 512, (half + 1) * 512)
                S.pe([mm(bank(2 * pk + half), oT[:, fc, tsl], xw["o"][:, fc, hs], fc == 0, fc == 7) for fc in range(8)],
                     reads=[b_oT, bxw["o"]], writes=[PB[2 * pk + half]])

        S.inherit(b_ysbs[2], [b_memT])

        def ln2_A(tb, jj):
            j = tb * 4 + jj
            ln_A(j, resid[:, j, :], [b_res[j]], 3 - (jj % 2), nb=3)

        def ln2_B(tb, jj):
            ln_B(tb * 4 + jj, extra=cast_h2, nb=3)
        for hh in range(4):
            xa_head(0, hh)
        def nxt_head(tb, hh):
            if tb + 1 < 4:
                xa_head(tb + 1, hh)
        for tb in range(4):
            xa_out_mm(tb, 0)
            xa_out_mm(tb, 1)
            ln2_A(tb, 0)
            xa_out_mm(tb, 2)
            ln2_A(tb, 1)
            xa_out_mm(tb, 3)
            ln2_A(tb, 2)
            ln2_B(tb, 0)
            ln2_A(tb, 3)
            nxt_head(tb, 0)
            ln2_B(tb, 1)
            nxt_head(tb, 1)
            ln2_B(tb, 2)
            nxt_head(tb, 2)
            ln2_B(tb, 3)
            nxt_head(tb, 3)
            for jj in range(4):
                ln_T(tb * 4 + jj, jj % 2)
        dump("h2", resid[:, :, :], b_res)
        if stop_after == "xattn":
            S.wait_bufs("sp", [b_dbg])
            S.emit()
            return nc

        phaseB = [bxw["q"], bxw["o"], b_xv, b_oT, b_memT, b_xk] + b_xq + b_p2
        wr_sb = T([128, 8, NE], BF16, TAIL); b_wr = Buf("wr")
        S.inherit(b_wr, [b_memT, b_ysbs[2]])
        with_nc = w_router.rearrange("(c p) e -> p c e", p=128)
        S.dma("pool", lambda e: e.dma_start(out=wr_sb[:], in_=with_nc), b_wr)
        aff = T([16, S_], F32, B0); affw = [T([16, S_], F32, B0 + 8192), T([16, S_], F32, B0 + 16384)]
        b_aff = Buf("aff"); b_affw = [Buf("affw0"), Buf("affw1")]
        gate = T([16, CAP], F32, B0 + 24576); idxu = T([16, CAP], U32, B0 + 25600); idxf = T([16, CAP], F32, B0 + 26624)
        b_gate = Buf("gate"); b_idxu = Buf("idxu"); b_idxf = Buf("idxf")
        idxT = T([128, 2, NE], F32, B0 + 27648); gateT = T([128, 2, NE], F32, B0 + 27776); b_igT = Buf("igT")
        idxs = T([128, 32], F32, B0 + 27904); b_idxs = Buf("idxs")
        for bb in [b_aff, b_gate, b_idxu, b_idxf, b_igT, b_idxs] + b_affw:
            S.inherit(bb, phaseB)
        NRING = 5
        ring_off = [R6, B0 + 45056, B0 + 57344, B0, B0 + 12288]
        ringG = [T([128, 8, 256], BF16, ring_off[i]) for i in range(NRING)]
        ringU = [T([128, 8, 256], BF16, ring_off[i] + 4096) for i in range(NRING)]
        ringD = [T([128, 2, D], BF16, ring_off[i] + 8192) for i in range(NRING)]
        b_ring = [Buf("ring%d" % i) for i in range(NRING)]
        phaseB2 = phaseB + [bxw["k"], bxw["v"]] + b_h2st
        S.inherit(b_ring[0], [b_lnp] + b_ysbs)
        for bb in b_ring[1:3]:
            S.inherit(bb, phaseB2)
        for bb in b_ring[3:5]:
            S.inherit(bb, [b_aff] + b_affw + phaseB)

        def load_unit(n):
            e, fb = divmod(n, 8)
            sl = n % NRING
            fs = slice(fb * 256, (fb + 1) * 256)
            S.dma("pool", lambda e_: e_.dma_start(out=ringG[sl][:], in_=w_gate[e].rearrange("(c p) f -> p c f", p=128)[:, :, fs]), b_ring[sl])
            S.dma("pool", lambda e_: e_.dma_start(out=ringU[sl][:], in_=w_up[e].rearrange("(c p) f -> p c f", p=128)[:, :, fs]), b_ring[sl])
            S.dma("pool", lambda e_: e_.dma_start(out=ringD[sl][:], in_=w_down[e][fb * 256:(fb + 1) * 256, :].rearrange("(c p) d -> p c d", p=128)), b_ring[sl])

        for n0 in range(3):
            load_unit(n0)
        for tb in range(4):
            def router_tb(tb=tb):
                cols = slice(tb * 512, (tb + 1) * 512)
                S.pe([mm(bank(tb)[0:16, :], wr_sb[:, c, :], actT[:, c, cols], c == 0, c == 7) for c in range(8)],
                     reads=[b_wr, b_actT[tb]], writes=[PB[tb]])
                S.op("act", lambda e: e.activation(out=affw[0][:, cols], in_=bank(tb)[0:16, :], func=AF.Exp), reads=[PB[tb]], writes=[b_affw[0]])
                S.pe([mm(bank(4 + tb)[0:16, :], onesf[0:16, 0:16], affw[0][:, cols])], reads=[b_onesf, b_affw[0]], writes=[PB[4 + tb]])
                recip_act(affw[1][:, cols], bank(4 + tb)[0:16, :], [PB[4 + tb]], [b_affw[1]])
                S.op("dve", lambda e: e.tensor_tensor(out=aff[:, cols], in0=affw[0][:, cols], in1=affw[1][:, cols], op=ALU.mult),
                     reads=b_affw, writes=[b_aff])
            router_tb()
        dump("aff", aff[:, :], [b_aff])
        b_gates = [Buf("gate_r%d" % i) for i in range(4)]
        b_idxus = [Buf("idxu_r%d" % i) for i in range(4)]
        for bb in b_gates + b_idxus:
            S.inherit(bb, phaseB)
        for r in range(CAP // 8):
            def topk_round(r=r):
                src = aff if r == 0 else affw[(r - 1) % 2]
                bsrc = b_aff if r == 0 else b_affw[(r - 1) % 2]
                dst = affw[r % 2]
                sl = slice(r * 8, r * 8 + 8)
                bg = b_gates[r % 4]
                S.op("dve", lambda e: e.max(out=gate[:, sl], in_=src[:]), reads=[bsrc], writes=[bg])
                if r + 1 < CAP // 8:
                    S.op("dve", lambda e: e.match_replace(out=dst[:], in_to_replace=gate[:, sl], in_values=src[:], imm_value=-1.0),
                         reads=[bsrc, bg], writes=[b_affw[r % 2]])
                S.op("dve", lambda e: e.max_index(out=idxu[:, sl], in_max=gate[:, sl], in_values=src[:]), reads=[bsrc, bg], writes=[b_idxus[r % 4]])
            topk_round()
        S.op("dve", lambda e: e.tensor_copy(out=idxf[:], in_=idxu[:]), reads=b_idxus, writes=[b_idxf])
        dump("gate", gate[:, :], b_gates)
        dump("idxf", idxf[:, :], [b_idxf])
        S.pe([lambda e, ch=ch: e.transpose(bank(0)[:, ch * 16:(ch + 1) * 16], idxf[0:16, ch * 128:(ch + 1) * 128], ident[0:16, 0:16]) for ch in range(2)]
             + [lambda e, ch=ch: e.transpose(bank(0)[:, 32 + ch * 16:32 + (ch + 1) * 16], gate[0:16, ch * 128:(ch + 1) * 128], ident[0:16, 0:16]) for ch in range(2)],
             reads=[b_idxf, b_ident] + b_gates, writes=[PB[0]])
        S.op("dve", lambda e: e.tensor_copy(out=idxT[:].rearrange("p a b -> p (a b)"), in_=bank(0)[:, 0:32]), reads=[PB[0]], writes=[b_igT])
        S.op("dve", lambda e: e.tensor_copy(out=gateT[:].rearrange("p a b -> p (a b)"), in_=bank(0)[:, 32:64]), reads=[PB[0]], writes=[b_igT])

        S.op("dve", lambda e: e.tensor_scalar(out=gateT[:].rearrange("p a b -> p (a b)"), in0=gateT[:].rearrange("p a b -> p (a b)"),
                                              scalar1=1.0 / ALPHA, scalar2=None, op0=ALU.mult), reads=[b_igT], writes=[b_igT])
        idxTi = T([128, 2, NE], I32, B0 + 28032); b_idxTi = Buf("idxTi")
        S.inherit(b_idxTi, phaseB)
        S.op("dve", lambda e: e.tensor_copy(out=idxTi[:].rearrange("p a b -> p (a b)"), in_=idxT[:].rearrange("p a b -> p (a b)")),
             reads=[b_igT], writes=[b_idxTi])

        C1 = R1
        GRP = 2
        ysc = [T([128, GRP * 2, D], BF16, C1 + 16384 + i * 8192) for i in range(2)]; b_ysc = [Buf("ysc%d" % i) for i in range(2)]
        xsT = [T([128, 8, CAP], BF16, C1 + i * 4096) for i in range(2)]; b_xs = [Buf("xs%d" % i) for i in range(2)]
        for bb in b_ysc + b_xs:
            S.inherit(bb, b_actT)
        hidT = [T([128, 2, CAP], BF16, B0 + 73728 + i * 1024) for i in range(2)]; b_hid = [Buf("hid%d" % i) for i in range(2)]
        NXG = 3
        xg = [T([128, 2, D], BF16, B0 + 32768 + i * 4096) for i in range(NXG)]; b_xg = [Buf("xg%d" % i) for i in range(NXG)]
        for bb in b_hid + b_xg:
            S.inherit(bb, phaseB2)
        selsc = [T([128, GRP * 2, 128], BF16, R8 + i * 1024) for i in range(2)]; b_selsc = [Buf("selsc%d" % i) for i in range(2)]
        silu_t = [T([128, 512], F32, R8 + 2048 + i * 2048) for i in range(2)]; b_silu = [Buf("silu%d" % i) for i in range(2)]
        idxs = [T([128, 4], F32, R8 + 6144 + i * 32) for i in range(2)]; b_idxs2 = [Buf("idxs%d" % i) for i in range(2)]
        for bb in b_selsc + b_silu + b_idxs2:
            S.inherit(bb, [b_oT])
        def gather_issue(e):
            for cc2 in range(2):
                S.dma("pool", lambda e_, cc2=cc2: e_.indirect_dma_start(out=xg[e % NXG][:, cc2, :], out_offset=None, in_=h2d[:, :],
                                                                        in_offset=bass.IndirectOffsetOnAxis(ap=idxTi[:, cc2, e:e + 1], axis=0),
                                                                        bounds_check=S_ - 1, oob_is_err=False),
                      b_xg[e % NXG], reads=[b_h2d, b_idxTi])

        def gather_expert(e):
            xs = xsT[e % 2]
            ppb = PP[0][:].bitcast(BF16)
            S.pe([lambda e_, dc=dc, c=c: e_.transpose(ppb[:, dc * 256 + c * 128:dc * 256 + (c + 1) * 128], xg[e % NXG][:, c, dc * 128:(dc + 1) * 128], identb[:])
                  for dc in range(8) for c in range(2)], reads=[b_xg[e % NXG], b_identb], writes=[PB[0], PB[1]])
            evac(xs[:].rearrange("p a b -> p (a b)"), ppb, [PB[0], PB[1]], [b_xs[e % 2]])

        def unit_banks(n):
            return (2, 3) if n % 2 == 0 else (0, 1)

        def ffn_gu(n):
            e, fb = divmod(n, 8)
            sl = n % NRING
            xs = xsT[e % 2]
            hd = hidT[n % 2]
            bg, bu = unit_banks(n)
            for (bk, W) in ((bg, ringG[sl]), (bu, ringU[sl])):
                fns = []
                for fl in range(2):
                    fns += [mm(bank(bk)[:, fl * 256:(fl + 1) * 256], W[:, dc, fl * 128:(fl + 1) * 128], xs[:, dc, :], dc == 0, dc == 7) for dc in range(8)]
                S.pe(fns, reads=[b_ring[sl], b_xs[e % 2]], writes=[PB[bk]])
            sv = silu_t[n % 2]
            S.op("act", lambda e_: e_.activation(out=sv[:], in_=bank(bg), func=AF.Silu), reads=[PB[bg]], writes=[b_silu[n % 2]])
            S.op("dve", lambda e_: e_.tensor_tensor(out=hd[:].rearrange("p a b -> p (a b)"), in0=sv[:], in1=bank(bu), op=ALU.mult),
                 reads=[b_silu[n % 2], PB[bu]], writes=[b_hid[n % 2]])

        def ffn_down(n):
            e, fb = divmod(n, 8)
            sl = n % NRING
            hd = hidT[n % 2]
            for cc2 in range(2):
                for half in range(2):
                    bk = 4 + cc2 * 2 + half
                    S.pe([mm(bank(bk), hd[:, fl, cc2 * 128:(cc2 + 1) * 128], ringD[sl][:, fl, half * 512:(half + 1) * 512],
                             fb == 0 and fl == 0, fb == 7 and fl == 1) for fl in range(2)],
                         reads=[b_hid[n % 2], b_ring[sl]], writes=[PB[bk]])

        def finish_expert(e):
            g = e // GRP
            for cc2 in range(2):
                S.op("act", lambda e_, cc2=cc2: e_.activation(out=ysc[g % 2][:, (e % GRP) * 2 + cc2, :], in_=PP[2 + cc2][:], func=AF.Copy,
                                                              scale=gateT[:, cc2, e:e + 1]),
                     reads=[PB[4 + 2 * cc2], PB[5 + 2 * cc2], b_igT], writes=[b_ysc[g % 2]])

        sc_todo = []

        def scatter_group(g):
            def sc_prep(j):
                ix = idxs[j % 2]
                bi = b_idxs2[j % 2]
                sc = selsc[j % 2]
                bsc = b_selsc[j % 2]
                S.op("dve", lambda e_: e_.tensor_scalar(out=ix[:, 0:4].rearrange("p (e c) -> p e c", c=2),
                                                        in0=idxT[:, :, g * GRP:(g + 1) * GRP].rearrange("p c e -> p e c"),
                                                        scalar1=-128.0 * j, scalar2=None, op0=ALU.add), reads=[b_igT], writes=[bi])
                S.op("dve", lambda e_: e_.tensor_tensor(out=sc[:], in0=ix[:, 0:4].unsqueeze(2).broadcast_to([128, 4, 128]),
                                                        in1=iota[:, :].unsqueeze(1).broadcast_to([128, 4, 128]), op=ALU.is_equal),
                     reads=[bi, b_iota], writes=[bsc])

            def sc_main(j, pp):
                sc = selsc[j % 2]
                bsc = b_selsc[j % 2]
                pbs = [PB[2 * pp], PB[2 * pp + 1]]
                S.pe([mm(PP[pp][:, half * 512:(half + 1) * 512], sc[:, k, :], ysc[g % 2][:, k, half * 512:(half + 1) * 512], k == 0, k == 3)
                      for half in range(2) for k in range(4)], reads=[bsc, b_ysc[g % 2]], writes=pbs)
                S.op("dve", lambda e_: e_.tensor_tensor(out=resid[:, j, :], in0=resid[:, j, :], in1=PP[pp][:], op=ALU.add),
                     reads=[b_res[j]] + pbs, writes=[b_res[j]])
            for j in range(NT):
                sc_todo.append((lambda j=j: sc_prep(j), lambda pp, j=j: sc_main(j, pp)))

        NEXP = NE
        NU = NEXP * 8
        gather_issue(0)
        gather_issue(1)
        for n0 in range(3, NRING):
            load_unit(n0)
        gather_expert(0)
        ffn_gu(0)
        for n in range(NU):
            e, fb = divmod(n, 8)
            if n + 1 < NU:
                if (n + 1) % 8 == 0:
                    gather_expert((n + 1) // 8)
                ffn_gu(n + 1)
            ffn_down(n)
            if n + NRING < NU:
                load_unit(n + NRING)
            if fb == 1 and e + 2 < NEXP:
                gather_issue(e + 2)
            if sc_todo:
                pr_, mn_ = sc_todo.pop(0)
                pr_()
                mn_(1 if n % 2 == 0 else 0)
            if fb == 7:
                finish_expert(e)
                if e % GRP == GRP - 1:
                    scatter_group(e // GRP)
        for bb in [b_lnp] + b_ysbs:
            S.inherit(bb, [b_ring[0]])
        load_lnp(2)
        outv = out.rearrange("(j p) d -> j p d", p=128)
        b_out = Buf("out")

        def out_tile(j):
            S.dma("sp", lambda e: e.dma_start(out=outv[j], in_=resid[:, j, :]), b_out, reads=[b_res[j]], writes=[b_out])
        tail = list(sc_todo)
        del sc_todo[:]
        assert len(tail) == NT, len(tail)
        tail[0][0]()
        for step in range(NT + 2):
            if step + 1 < NT:
                tail[step + 1][0]()
            if step < NT:
                tail[step][1](step % 2)
            if 1 <= step <= NT:
                ln_A(step - 1, None, [], None, eps_col=4)
            if step >= 2:
                ln_B(step - 2, from_resid=True, final_out=out_tile)
        S.wait_bufs("sp", [b_out, b_dbg])
        S.emit()
    return nc


def make_consts():
    c = {}
    c["c_ident"] = np.eye(128, dtype=np.float32)
    c["c_iota"] = np.tile(np.arange(128, dtype=np.float32)[None, :], (128, 1))
    c["c_tokid"] = (np.arange(NT, dtype=np.float32)[None, :] * 128 + np.arange(128, dtype=np.float32)[:, None]).astype(np.float32)
    es = np.zeros((16, 16, 128), np.float32)
    for e in range(16):
        es[e, e, :] = 1.0
    c["c_esel"] = es.reshape(16, 16 * 128)
    rope = np.zeros((128, 2), np.float32)
    inv_freq = (10000.0 ** (-np.arange(0, 32, 2, dtype=np.float32) / 32.0)).astype(np.float32)
    for p in range(64, 96):
        rope[p, 0] = np.float32(np.float64(inv_freq[(p - 64) % 16]) / (2.0 * np.pi))
        rope[p, 1] = -1.0 if p < 80 else 1.0
    c["c_rope"] = rope
    return c


def make_in_maps(inputs, n_cores=8):
    consts = make_consts()
    maps = []
    f = lambda a: np.ascontiguousarray(np.asarray(a))
    for b in range(n_cores):
        m = dict(consts)
        m["x"] = f(inputs["x"][b])
        m["mem"] = f(inputs["mem"][b])
        m["positions"] = f(np.asarray(inputs["positions"])[b:b + 1]).astype(np.int32)
        for k in ["w_in", "q_norm_g", "w_uq", "kv_norm_g", "w_uk", "w_uv", "conv_w", "conv_b", "conv_ln_g", "conv_ln_b",
                  "w_o", "xa_w_q", "xa_w_k", "xa_w_v", "xa_w_o", "w_router", "w_gate", "w_up", "w_down"]:
            m[k] = f(np.asarray(inputs[k])[0])
        for k in ["ln1_g", "ln1_b", "ln2_g", "ln2_b", "ln3_g", "ln3_b"]:
            m[k] = f(np.asarray(inputs[k])[0:1])
        maps.append(m)
    return maps


def kernel(**inputs):
    nc = build_program()
    maps = make_in_maps(inputs, 8)
    res = run_bass_kernel_spmd(nc, maps, core_ids=list(range(8)))
    return np.stack([r["out"] for r in res.results], axis=0).astype(np.float32)
```
